# Optimizing a Trainium2 kernel written in Bass

```python
import math
import jax
import jax.numpy as jnp
from jax import lax
import numpy as np

D_MODEL = 1024
BATCH = 2
SEQ = 8192
DEPTH = 2

GRID_W = 64
CTX_LEN = 256
EPS = 1e-6
NEG_INF = -1e30

HEAD_DIM = 64
NA_HEADS = D_MODEL // (2 * HEAD_DIM)
NA_KH = 8
NA_KW = 16
WA_HEADS = D_MODEL // (2 * HEAD_DIM)
WA_KV_HEADS = 2
WA_WINDOW = 128
WA_BLOCK = 128
ROPE_BASE = 10000.0
ATT_SIZES = (NA_HEADS * HEAD_DIM,) * 3 + (WA_HEADS * HEAD_DIM, WA_KV_HEADS * HEAD_DIM, WA_KV_HEADS * HEAD_DIM)
ATT_IN = sum(ATT_SIZES)
ATT_WIDTH = (NA_HEADS + WA_HEADS) * HEAD_DIM

SSD_INNER = D_MODEL
SSD_HEAD_DIM = 64
SSD_HEADS = SSD_INNER // SSD_HEAD_DIM
SSD_GROUPS = 2
SSD_STATE = 128
SSD_CHUNK = 128
CONV_K = 3
SSD_CONV_CH = SSD_INNER + 2 * SSD_GROUPS * SSD_STATE
S5_WIDTH = D_MODEL // 2
S5_GROUP = 16
S5_GROUPS = S5_WIDTH // S5_GROUP
S5_STATE = 64
SSM_SIZES = (SSD_INNER, SSD_CONV_CH, 2 * SSD_HEADS, S5_WIDTH)
SSM_IN = sum(SSM_SIZES)
SSM_WIDTH = SSD_INNER + S5_WIDTH

MOE_GROUPS = 4
MOE_EPG = 8
MOE_EXPERTS = MOE_GROUPS * MOE_EPG
MOE_TOPK = 2
MOE_FF = 512

kernel_name = 'hybrid_diffusion_trunk'


def _points(sizes):
    return tuple(int(s) for s in np.cumsum(sizes)[:-1])


def rmsnorm(x, g):
    xf = x.astype(jnp.float32)
    y = xf * lax.rsqrt(jnp.mean(xf * xf, axis=-1, keepdims=True) + EPS)
    return (y * g.astype(jnp.float32)).astype(x.dtype)


def ada_norm(x, g, shift, scale):
    return rmsnorm(x, g) * (1 + scale) + shift


def rope_2d(x):
    T = x.shape[1]
    t = jnp.arange(T)
    nf = HEAD_DIM // 4
    inv = ROPE_BASE ** (-jnp.arange(nf, dtype=jnp.float32) / nf)

    def rot(xa, pos):
        ang = pos.astype(jnp.float32)[:, None] * inv
        cos = jnp.cos(ang)[:, None, :].astype(x.dtype)
        sin = jnp.sin(ang)[:, None, :].astype(x.dtype)
        x1, x2 = xa[..., :nf], xa[..., nf:]
        return jnp.concatenate([x1 * cos - x2 * sin, x1 * sin + x2 * cos], axis=-1)

    half = HEAD_DIM // 2
    return jnp.concatenate([rot(x[..., :half], t // GRID_W), rot(x[..., half:], t % GRID_W)], axis=-1)


def _sink_column(sink, kvh, grp, lead_shape):
    return jnp.broadcast_to(sink.astype(jnp.float32).reshape(kvh, grp, 1, 1), lead_shape + (1,))


def context_attention(q, k, v, sink):
    Bn, S, H, d = q.shape
    kvh = k.shape[2]
    grp = H // kvh
    qg = q.reshape(Bn, S, kvh, grp, d)
    s = jnp.einsum('bqkgd,bskd->bkgqs', qg, k).astype(jnp.float32)
    if sink is not None:
        s = jnp.concatenate([s, _sink_column(sink, kvh, grp, s.shape[:-1])], axis=-1)
    p = jax.nn.softmax(s, axis=-1)[..., :S].astype(v.dtype)
    o = jnp.einsum('bkgqs,bskd->bqkgd', p, v)
    return o.reshape(Bn, S, H, d)


def neighborhood_attention(q, k, v, kc, vc, rel_bias):
    Bn, T, H, d = q.shape
    rows = T // GRID_W
    kh = min(NA_KH, rows)
    qg = q.reshape(Bn, rows, GRID_W, H, d)
    kg = k.reshape(Bn, rows, GRID_W, H, d)
    vg = v.reshape(Bn, rows, GRID_W, H, d)
    col = jnp.arange(GRID_W)
    col_idx = jnp.clip(col - NA_KW // 2, 0, GRID_W - NA_KW)[:, None] + jnp.arange(NA_KW)
    dcol = col_idx - col[:, None] + NA_KW - 1
    bias_cols = rel_bias.astype(jnp.float32)[:, :, dcol]
    nl = kh * NA_KW

    def row_block(r):
        r0 = jnp.clip(r - kh // 2, 0, rows - kh)
        q_r = lax.dynamic_index_in_dim(qg, r, axis=1, keepdims=False)
        k_r = lax.dynamic_slice_in_dim(kg, r0, kh, axis=1)[:, :, col_idx]
        v_r = lax.dynamic_slice_in_dim(vg, r0, kh, axis=1)[:, :, col_idx]
        drow = r0 + jnp.arange(kh) - r + NA_KH - 1
        bias = jnp.transpose(bias_cols[:, drow], (0, 2, 1, 3))
        s_loc = jnp.einsum('bihd,briwhd->bhirw', q_r, k_r).astype(jnp.float32) + bias
        s_ctx = jnp.einsum('bihd,bchd->bhic', q_r, kc).astype(jnp.float32)
        s = jnp.concatenate([s_loc.reshape(Bn, H, GRID_W, nl), s_ctx], axis=-1)
        p = jax.nn.softmax(s, axis=-1).astype(v.dtype)
        p_loc = p[..., :nl].reshape(Bn, H, GRID_W, kh, NA_KW)
        return (jnp.einsum('bhirw,briwhd->bihd', p_loc, v_r)
                + jnp.einsum('bhic,bchd->bihd', p[..., nl:], vc))

    out = lax.map(row_block, jnp.arange(rows))
    return jnp.moveaxis(out, 0, 1).reshape(Bn, T, H, d)


def window_attention(q, k, v, kc, vc, sink):
    Bn, T, H, d = q.shape
    kvh = k.shape[2]
    grp = H // kvh
    nb = T // WA_BLOCK
    qb = q.reshape(Bn, nb, WA_BLOCK, kvh, grp, d)

    def band(t):
        tp = jnp.pad(t, ((0, 0), (WA_BLOCK, WA_BLOCK), (0, 0), (0, 0))).reshape(Bn, nb + 2, WA_BLOCK, kvh, d)
        return jnp.concatenate([tp[:, :-2], tp[:, 1:-1], tp[:, 2:]], axis=2)

    kb, vb = band(k), band(v)
    qpos = jnp.arange(T).reshape(nb, WA_BLOCK)
    kpos = (jnp.arange(nb)[:, None] - 1) * WA_BLOCK + jnp.arange(3 * WA_BLOCK)
    valid = ((jnp.abs(qpos[:, :, None] - kpos[:, None, :]) <= WA_WINDOW)
             & (kpos >= 0)[:, None, :] & (kpos < T)[:, None, :])
    s_loc = jnp.einsum('bnqkgd,bnskd->bnkgqs', qb, kb).astype(jnp.float32)
    s_loc = jnp.where(valid[None, :, None, None], s_loc, NEG_INF)
    s_ctx = jnp.einsum('bnqkgd,bckd->bnkgqc', qb, kc).astype(jnp.float32)
    s = jnp.concatenate([s_loc, s_ctx, _sink_column(sink, kvh, grp, s_loc.shape[:-1])], axis=-1)
    p = jax.nn.softmax(s, axis=-1)
    nl = 3 * WA_BLOCK
    nc = kc.shape[1]
    p_loc = p[..., :nl].astype(v.dtype)
    p_ctx = p[..., nl:nl + nc].astype(v.dtype)
    o = (jnp.einsum('bnkgqs,bnskd->bnqkgd', p_loc, vb)
         + jnp.einsum('bnkgqc,bckd->bnqkgd', p_ctx, vc))
    return o.reshape(Bn, T, H, d)


def attention_mixer(hl, hc, w_in, w_out, na_qn, na_kn, na_rpb, wa_qn, wa_kn, wa_sink, need_ctx):
    scale = HEAD_DIM ** -0.5

    def project(h, rotary):
        Bn, T, _ = h.shape
        qa, ka, va, qb, kb, vb = jnp.split(h @ w_in, _points(ATT_SIZES), axis=-1)
        heads = lambda t, n: t.reshape(Bn, T, n, HEAD_DIM)
        qa = rmsnorm(heads(qa, NA_HEADS), na_qn) * scale
        ka = rmsnorm(heads(ka, NA_HEADS), na_kn)
        va = heads(va, NA_HEADS)
        qb = rmsnorm(heads(qb, WA_HEADS), wa_qn)
        kb = rmsnorm(heads(kb, WA_KV_HEADS), wa_kn)
        vb = heads(vb, WA_KV_HEADS)
        if rotary:
            qb, kb = rope_2d(qb), rope_2d(kb)
        return qa, ka, va, qb * scale, kb, vb

    def merge(oa, ob):
        Bn, T = oa.shape[:2]
        return jnp.concatenate([oa.reshape(Bn, T, -1), ob.reshape(Bn, T, -1)], axis=-1) @ w_out

    qa_l, ka_l, va_l, qb_l, kb_l, vb_l = project(hl, True)
    qa_c, ka_c, va_c, qb_c, kb_c, vb_c = project(hc, False)
    y_l = merge(neighborhood_attention(qa_l, ka_l, va_l, ka_c, va_c, na_rpb),
                window_attention(qb_l, kb_l, vb_l, kb_c, vb_c, wa_sink))
    y_c = None
    if need_ctx:
        y_c = merge(context_attention(qa_c, ka_c, va_c, None),
                    context_attention(qb_c, kb_c, vb_c, wa_sink))
    return y_l, y_c


def dwconv_centred(x, w, b):
    K = w.shape[0]
    out = lax.conv_general_dilated(x, w[:, None, :], window_strides=(1,), padding=[((K - 1) // 2, K // 2)],
                                   dimension_numbers=('NWC', 'WIO', 'NWC'), feature_group_count=x.shape[-1])
    return out + b


def ssd_scan(x, dt, A, bm, cm, h0, want_y):
    Bn, T, H, P = x.shape
    G, N = bm.shape[2], bm.shape[3]
    hg = H // G
    Q = SSD_CHUNK
    nc = T // Q
    xq = x.reshape(Bn, nc, Q, G, hg, P)
    dtq = dt.reshape(Bn, nc, Q, G, hg)
    bq = bm.reshape(Bn, nc, Q, G, N)
    cq = cm.reshape(Bn, nc, Q, G, N)
    acs = jnp.cumsum(dtq * A.reshape(G, hg), axis=2)
    w_end = jnp.exp(acs[:, :, -1:] - acs) * dtq
    states = jnp.einsum('bcqgn,bcqgh,bcqghp->bcghpn', bq, w_end, xq)
    decay = jnp.exp(acs[:, :, -1])

    def step(h, inp):
        s, dc = inp
        return dc[..., None, None] * h + s, h

    h_fin, h_in = lax.scan(step, h0.reshape(Bn, G, hg, P, N),
                           (jnp.moveaxis(states, 1, 0), jnp.moveaxis(decay, 1, 0)))
    h_fin = h_fin.reshape(Bn, H, P, N)
    if not want_y:
        return None, h_fin
    h_in = jnp.moveaxis(h_in, 0, 1)
    lower = jnp.tril(jnp.ones((Q, Q), dtype=bool))[:, :, None, None]
    seg = acs[:, :, :, None] - acs[:, :, None, :]
    lmat = jnp.exp(jnp.where(lower, seg, NEG_INF))
    cb = jnp.einsum('bcign,bcjgn->bcijg', cq, bq)
    w = cb[..., None] * lmat * dtq[:, :, None]
    y_diag = jnp.einsum('bcijgh,bcjghp->bcighp', w, xq)
    y_off = jnp.einsum('bcign,bcghpn->bcighp', cq, h_in) * jnp.exp(acs)[..., None]
    return (y_diag + y_off).reshape(Bn, T, H, P), h_fin


def s5_discretise(lam_re, lam_im, log_step, b_re, b_im):
    lam = lax.complex(lam_re.astype(jnp.float32), lam_im.astype(jnp.float32))
    step = jnp.exp(log_step.astype(jnp.float32))[:, None]
    a_bar = jnp.exp(lam * step)
    b = lax.complex(b_re.astype(jnp.float32), b_im.astype(jnp.float32))
    b_bar = ((a_bar - 1) / lam)[..., None] * b
    return a_bar, b_bar


def _lin_combine(left, right):
    a_l, b_l = left
    a_r, b_r = right
    return a_r * a_l, a_r * b_l + b_r


def s5_scan(u, a_bar, b_bar, cmat, h0, want_y):
    T = u.shape[1]
    bu = jnp.einsum('gnc,btgc->tbgn', b_bar, u.astype(jnp.complex64))
    bu = bu.at[0].add(a_bar * h0)
    a = jnp.broadcast_to(a_bar, (T, 1) + a_bar.shape)
    _, h = lax.associative_scan(_lin_combine, (a, bu), axis=0)
    y = jnp.einsum('gcn,tbgn->btgc', cmat, h).real if want_y else None
    return y, h[-1]


def ssm_mixer(hl, hc, w_in, w_out, conv_w, conv_b, dt_bias, a_log, d_skip, norm_w,
              lam_re, lam_im, log_step, b_re, b_im, c_re, c_im, s5_dskip, glu_w, glu_b, need_ctx):
    def project(h):
        Bn, T, _ = h.shape
        z, xbc, dtr, u = jnp.split(h @ w_in, _points(SSM_SIZES), axis=-1)
        xbc = jax.nn.silu(dwconv_centred(xbc, conv_w, conv_b))
        xs, bm, cm = jnp.split(xbc, _points((SSD_INNER, SSD_GROUPS * SSD_STATE, SSD_GROUPS * SSD_STATE)), axis=-1)
        xs = xs.reshape(Bn, T, SSD_HEADS, SSD_HEAD_DIM).astype(jnp.float32)
        bm = bm.reshape(Bn, T, SSD_GROUPS, SSD_STATE).astype(jnp.float32)
        cm = cm.reshape(Bn, T, SSD_GROUPS, SSD_STATE).astype(jnp.float32)
        dt = jax.nn.softplus(dtr.reshape(Bn, T, 2, SSD_HEADS).astype(jnp.float32) + dt_bias.astype(jnp.float32))
        u = u.reshape(Bn, T, S5_GROUPS, S5_GROUP).astype(jnp.float32)
        return z, xs, bm, cm, dt, u

    z_l, xs_l, b_l, c_l, dt_l, u_l = project(hl)
    z_c, xs_c, b_c, c_c, dt_c, u_c = project(hc)
    A = -jnp.exp(a_log.astype(jnp.float32))
    Bn = hl.shape[0]
    ssd_l, ssd_c, s5_l, s5_c = [], [], [], []
    for dr in range(2):
        fl = (lambda t: jnp.flip(t, axis=1)) if dr else (lambda t: t)
        h0 = jnp.zeros((Bn, SSD_HEADS, SSD_HEAD_DIM, SSD_STATE), jnp.float32)
        y_c, h_ctx = ssd_scan(fl(xs_c), fl(dt_c[:, :, dr]), A[dr], fl(b_c), fl(c_c), h0, need_ctx)
        y_l, _ = ssd_scan(fl(xs_l), fl(dt_l[:, :, dr]), A[dr], fl(b_l), fl(c_l), h_ctx, True)
        ssd_l.append(fl(y_l))
        a_bar, b_bar = s5_discretise(lam_re[dr], lam_im[dr], log_step[dr], b_re[dr], b_im[dr])
        cmat = lax.complex(c_re[dr].astype(jnp.float32), c_im[dr].astype(jnp.float32))
        s0 = jnp.zeros((Bn, S5_GROUPS, S5_STATE), jnp.complex64)
        v_c, s_ctx = s5_scan(fl(u_c), a_bar, b_bar, cmat, s0, need_ctx)
        v_l, _ = s5_scan(fl(u_l), a_bar, b_bar, cmat, s_ctx, True)
        s5_l.append(fl(v_l))
        if need_ctx:
            ssd_c.append(fl(y_c))
            s5_c.append(fl(v_c))

    def finish(z, xs, ssd_y, u, s5_y):
        Bn_, T = z.shape[:2]
        y = (ssd_y + d_skip.astype(jnp.float32)[:, None] * xs).reshape(Bn_, T, SSD_INNER)
        y = rmsnorm(y * jax.nn.silu(z.astype(jnp.float32)), norm_w)
        v = (s5_y + s5_dskip.astype(jnp.float32).reshape(S5_GROUPS, S5_GROUP) * u).reshape(Bn_, T, S5_WIDTH)
        v = jax.nn.gelu(v)
        v = v * jax.nn.sigmoid(v @ glu_w.astype(jnp.float32) + glu_b.astype(jnp.float32))
        return jnp.concatenate([y, v], axis=-1).astype(w_out.dtype) @ w_out

    out_l = finish(z_l, xs_l, ssd_l[0] + ssd_l[1], u_l, s5_l[0] + s5_l[1])
    out_c = finish(z_c, xs_c, ssd_c[0] + ssd_c[1], u_c, s5_c[0] + s5_c[1]) if need_ctx else None
    return out_l, out_c


def hier_moe(h, w_group, b_group, w_expert, b_expert, w13, w2):
    N = h.shape[0]
    hf = h.astype(jnp.float32)
    pg = jax.nn.softmax(hf @ w_group.astype(jnp.float32) + b_group.astype(jnp.float32), axis=-1)
    g_idx = jnp.argmax(pg, axis=-1)
    p_top = jnp.max(pg, axis=-1)
    le = (hf @ w_expert.astype(jnp.float32) + b_expert.astype(jnp.float32)).reshape(N, MOE_GROUPS, MOE_EPG)
    le_sel = jnp.take_along_axis(le, g_idx[:, None, None], axis=1)[:, 0]
    top_v, top_i = lax.top_k(jax.nn.softmax(le_sel, axis=-1), MOE_TOPK)
    top_v = top_v / jnp.sum(top_v, axis=-1, keepdims=True)
    within = jnp.sum(jax.nn.one_hot(top_i, MOE_EPG, dtype=jnp.float32) * top_v[..., None], axis=1)
    combine = (jax.nn.one_hot(g_idx, MOE_GROUPS, dtype=jnp.float32)[:, :, None]
               * within[:, None, :] * p_top[:, None, None])
    out = jnp.zeros((N, h.shape[1]), jnp.float32)
    for g in range(MOE_GROUPS):
        sl = slice(g * MOE_EPG, (g + 1) * MOE_EPG)
        a, b = jnp.split(jnp.einsum('nd,edf->nef', h, w13[sl]), 2, axis=-1)
        act = jax.nn.silu(a) * b * combine[:, g, :, None].astype(h.dtype)
        out = out + jnp.einsum('nef,efd->nd', act, w2[sl]).astype(jnp.float32)
    return out.astype(h.dtype)


def setup_inputs(seed: int = 0) -> dict:
    key = jax.random.key(seed)
    ks = iter(jax.random.split(key, 64))
    nrm = lambda shape, s: jax.random.normal(next(ks), shape, jnp.float32) * s
    D = D_MODEL
    NE = (DEPTH + 1) // 2
    NO = DEPTH // 2
    dt0 = jnp.exp(jax.random.uniform(next(ks), (NO, 2, SSD_HEADS), minval=math.log(1e-3), maxval=math.log(1e-1)))
    n_idx = jnp.arange(S5_STATE, dtype=jnp.float32)
    return {
        'x': nrm((BATCH, SEQ, D), 1.0),
        'c': nrm((BATCH, D), 1.0),
        'ctx': nrm((BATCH, CTX_LEN, D), 1.0),
        'c_ctx': nrm((D,), 1.0),
        'mod_w': nrm((DEPTH, D, 6 * D), 0.5 * D ** -0.5),
        'mod_b': nrm((DEPTH, 6 * D), 0.02),
        'norm_mix': 1.0 + nrm((DEPTH, D), 0.1),
        'norm_ffn': 1.0 + nrm((DEPTH, D), 0.1),
        'att_w_in': nrm((NE, D, ATT_IN), D ** -0.5),
        'att_w_out': nrm((NE, ATT_WIDTH, D), ATT_WIDTH ** -0.5),
        'na_q_norm': 1.0 + nrm((NE, HEAD_DIM), 0.1),
        'na_k_norm': 1.0 + nrm((NE, HEAD_DIM), 0.1),
        'na_rel_bias': nrm((NE, NA_HEADS, 2 * NA_KH - 1, 2 * NA_KW - 1), 0.5),
        'wa_q_norm': 1.0 + nrm((NE, HEAD_DIM), 0.1),
        'wa_k_norm': 1.0 + nrm((NE, HEAD_DIM), 0.1),
        'wa_sink': nrm((NE, WA_HEADS), 0.5),
        'ssm_w_in': nrm((NO, D, SSM_IN), D ** -0.5),
        'ssm_w_out': nrm((NO, SSM_WIDTH, D), SSM_WIDTH ** -0.5),
        'ssd_conv_w': nrm((NO, CONV_K, SSD_CONV_CH), CONV_K ** -0.5),
        'ssd_conv_b': nrm((NO, SSD_CONV_CH), 0.01),
        'ssd_dt_bias': dt0 + jnp.log(-jnp.expm1(-dt0)),
        'ssd_a_log': jnp.log(jax.random.uniform(next(ks), (NO, 2, SSD_HEADS), minval=1.0, maxval=16.0)),
        'ssd_d': 1.0 + nrm((NO, SSD_HEADS), 0.1),
        'ssd_norm': 1.0 + nrm((NO, SSD_INNER), 0.1),
        's5_lambda_re': -0.5 + nrm((NO, 2, S5_GROUPS, S5_STATE), 0.01),
        's5_lambda_im': jnp.pi * n_idx + nrm((NO, 2, S5_GROUPS, S5_STATE), 0.01),
        's5_log_step': jax.random.uniform(next(ks), (NO, 2, S5_GROUPS), minval=math.log(1e-3), maxval=math.log(1e-1)),
        's5_b_re': nrm((NO, 2, S5_GROUPS, S5_STATE, S5_GROUP), (2 * S5_GROUP) ** -0.5),
        's5_b_im': nrm((NO, 2, S5_GROUPS, S5_STATE, S5_GROUP), (2 * S5_GROUP) ** -0.5),
        's5_c_re': nrm((NO, 2, S5_GROUPS, S5_GROUP, S5_STATE), (2 * S5_STATE) ** -0.5),
        's5_c_im': nrm((NO, 2, S5_GROUPS, S5_GROUP, S5_STATE), (2 * S5_STATE) ** -0.5),
        's5_d': nrm((NO, S5_WIDTH), 1.0),
        's5_glu_w': nrm((NO, S5_WIDTH, S5_WIDTH), S5_WIDTH ** -0.5),
        's5_glu_b': nrm((NO, S5_WIDTH), 0.01),
        'moe_w_group': nrm((DEPTH, D, MOE_GROUPS), D ** -0.5),
        'moe_b_group': nrm((DEPTH, MOE_GROUPS), 0.01),
        'moe_w_expert': nrm((DEPTH, D, MOE_EXPERTS), D ** -0.5),
        'moe_b_expert': nrm((DEPTH, MOE_EXPERTS), 0.01),
        'moe_w13': nrm((DEPTH, MOE_EXPERTS, D, 2 * MOE_FF), D ** -0.5),
        'moe_w2': nrm((DEPTH, MOE_EXPERTS, MOE_FF, D), MOE_FF ** -0.5),
    }


def reference(x, c, ctx, c_ctx, mod_w, mod_b, norm_mix, norm_ffn, att_w_in, att_w_out, na_q_norm, na_k_norm,
              na_rel_bias, wa_q_norm, wa_k_norm, wa_sink, ssm_w_in, ssm_w_out, ssd_conv_w, ssd_conv_b, ssd_dt_bias,
              ssd_a_log, ssd_d, ssd_norm, s5_lambda_re, s5_lambda_im, s5_log_step, s5_b_re, s5_b_im, s5_c_re, s5_c_im,
              s5_d, s5_glu_w, s5_glu_b, moe_w_group, moe_b_group, moe_w_expert, moe_b_expert, moe_w13, moe_w2):
    Bn, L, D = x.shape
    xl, xc = x, ctx
    for i in range(DEPTH):
        last = i == DEPTH - 1
        j = i // 2
        sh1_l, sc1_l, g1_l, sh2_l, sc2_l, g2_l = jnp.split(
            (jax.nn.silu(c) @ mod_w[i] + mod_b[i])[:, None, :], 6, axis=-1)
        sh1_c, sc1_c, g1_c, sh2_c, sc2_c, g2_c = jnp.split(
            (jax.nn.silu(c_ctx) @ mod_w[i] + mod_b[i])[None, None, :], 6, axis=-1)
        hl = ada_norm(xl, norm_mix[i], sh1_l, sc1_l)
        hc = ada_norm(xc, norm_mix[i], sh1_c, sc1_c)
        if i % 2 == 0:
            yl, yc = attention_mixer(hl, hc, att_w_in[j], att_w_out[j], na_q_norm[j], na_k_norm[j], na_rel_bias[j],
                                     wa_q_norm[j], wa_k_norm[j], wa_sink[j], not last)
        else:
            yl, yc = ssm_mixer(hl, hc, ssm_w_in[j], ssm_w_out[j], ssd_conv_w[j], ssd_conv_b[j], ssd_dt_bias[j],
                               ssd_a_log[j], ssd_d[j], ssd_norm[j], s5_lambda_re[j], s5_lambda_im[j],
                               s5_log_step[j], s5_b_re[j], s5_b_im[j], s5_c_re[j], s5_c_im[j], s5_d[j],
                               s5_glu_w[j], s5_glu_b[j], not last)
        xl = xl + g1_l * yl
        hl = ada_norm(xl, norm_ffn[i], sh2_l, sc2_l).reshape(Bn * L, D)
        moe_args = (moe_w_group[i], moe_b_group[i], moe_w_expert[i], moe_b_expert[i], moe_w13[i], moe_w2[i])
        if last:
            fl = hier_moe(hl, *moe_args)
        else:
            xc = xc + g1_c * yc
            hc = ada_norm(xc, norm_ffn[i], sh2_c, sc2_c).reshape(-1, D)
            f = hier_moe(jnp.concatenate([hl, hc], axis=0), *moe_args)
            fl = f[:Bn * L]
            xc = xc + g2_c * f[Bn * L:].reshape(xc.shape)
        xl = xl + g2_l * fl.reshape(Bn, L, D)
    return xl
```

```python
import os
import numpy as np
import concourse.bass as bass
import concourse.mybir as mybir
from concourse.bass_utils import run_bass_kernel_spmd

F32 = mybir.dt.float32
BF16 = mybir.dt.bfloat16
AF = mybir.ActivationFunctionType
ALU = mybir.AluOpType
AX = mybir.AxisListType

EPOCH = 12000
D = 1024
EPS = 1e-6


class Res:
    __slots__ = ("name", "w", "readers", "dsem", "dcnt")

    def __init__(self, name):
        self.name = name
        self.w = None
        self.readers = {}
        self.dsem = None
        self.dcnt = 0


class T:
    def __init__(self, h, name):
        self.h = h
        self.r = Res(name)

    def __getitem__(self, k):
        return self.h[k]


class Prog:
    def __init__(self, nc):
        self.nc = nc
        self.eng = {"pe": nc.tensor, "dve": nc.vector, "act": nc.scalar, "pool": nc.gpsimd, "sp": nc.sync}
        self.ops = {k: [] for k in self.eng}
        self.cnt = {k: 0 for k in self.eng}
        self.esems = {k: [] for k in self.eng}
        self.known = {k: {} for k in self.eng}
        self.dres = []
        self.nsem = 0
        self.nt = 0

    def sem(self, name):
        self.nsem += 1
        return self.nc.alloc_semaphore(name)

    def sb(self, shape, dt=F32, name=None):
        self.nt += 1
        name = name or f"t{self.nt}"
        return T(self.nc.alloc_sbuf_tensor(name, list(shape), dt), name)

    def ps(self, shape, dt=F32, name=None):
        self.nt += 1
        name = name or f"p{self.nt}"
        return T(self.nc.alloc_psum_tensor(name, list(shape), dt), name)

    def _esem(self, e, ep):
        while len(self.esems[e]) <= ep:
            self.esems[e].append(self.sem(f"s_{e}_{len(self.esems[e])}"))
        return self.esems[e][ep]

    def _need(self, e, ev, waits):
        sem, val, src = ev
        if src == "pe" and e == "pe":
            return
        k = id(sem)
        if self.known[e].get(k, 0) >= val:
            return
        self.known[e][k] = val
        waits.append((sem, val))

    def _deps(self, e, reads, writes):
        waits = []
        for t in reads:
            if t.r.w is not None:
                self._need(e, t.r.w, waits)
        for t in writes:
            if t.r.w is not None:
                self._need(e, t.r.w, waits)
            for ev in t.r.readers.values():
                self._need(e, ev, waits)
        return waits

    def _mark(self, ev, reads, writes):
        for t in writes:
            t.r.w = ev
            t.r.readers = {}
        for t in reads:
            if t not in writes:
                old = t.r.readers.get(id(ev[0]))
                if old is None or old[1] < ev[1]:
                    t.r.readers[id(ev[0])] = ev

    def op(self, e, fn, reads=(), writes=()):
        waits = self._deps(e, reads, writes)
        idx = self.cnt[e]
        self.cnt[e] += 1
        sem = self._esem(e, idx // EPOCH)
        ev = (sem, idx % EPOCH + 1, e)
        self._mark(ev, reads, writes)
        self.ops[e].append((waits, fn, (sem, 1)))

    def dma(self, e, out, in_, reads=(), writes=(), sres=None):
        waits = self._deps(e, reads, writes)
        t = sres or (writes[0] if writes else reads[0])
        r = t.r
        if r.dsem is None or r.dcnt + 16 > 30000:
            r.dsem = self.sem(f"d_{r.name}_{self.nsem}")
            r.dcnt = 0
            self.dres.append(r)
        r.dcnt += 16
        ev = (r.dsem, r.dcnt, "dma")
        self._mark(ev, reads, writes)
        self.ops[e].append((waits, lambda eng: eng.dma_start(out=out, in_=in_), (r.dsem, 16)))

    def finish(self):
        finals = {}
        for r in self.dres:
            finals[id(r.dsem)] = (r.dsem, max(finals.get(id(r.dsem), (None, 0))[1], r.dcnt))
        waits = []
        for sem, val in finals.values():
            if self.known["sp"].get(id(sem), 0) < val:
                waits.append((sem, val))
        for e in ("pe", "dve", "act", "pool"):
            n = self.cnt[e]
            if n:
                waits.append((self._esem(e, (n - 1) // EPOCH), (n - 1) % EPOCH + 1))
        self.ops["sp"].append((waits, None, None))

    def emit(self):
        self.finish()
        with self.nc.Block() as block:
            decos = {"sp": block.sync, "pe": block.tensor, "dve": block.vector, "act": block.scalar,
                     "pool": block.gpsimd}
            for e in ("sp", "pe", "dve", "act", "pool"):
                def body(engine, e=e):
                    for waits, fn, inc in self.ops[e]:
                        for sem, val in waits:
                            engine.wait_ge(sem, val)
                        if fn is not None:
                            fn(engine).then_inc(inc[0], inc[1])
                decos[e](body)

    def mm(self, out, lhsT, rhs, start, stop, reads, writes):
        self.op("pe", lambda g: g.matmul(out, lhsT, rhs, start=start, stop=stop), reads, writes)

    def tr(self, out, in_, ident, reads, writes):
        self.op("pe", lambda g: g.transpose(out, in_, ident), reads, writes)

    def act(self, out, in_, func, reads, writes, bias=None, scale=None, accum_out=None, e="act"):
        kw = {}
        if bias is not None:
            kw["bias"] = bias
        if scale is not None:
            kw["scale"] = scale
        if accum_out is not None:
            kw["accum_out"] = accum_out
        self.op("act", lambda g: g.activation(out, in_, func, **kw), reads, writes)

    def ts(self, e, out, in0, s1, s2, op0, op1, reads, writes):
        if op1 is None:
            self.op(e, lambda g: g.tensor_scalar(out, in0, s1, None, op0), reads, writes)
        else:
            self.op(e, lambda g: g.tensor_scalar(out, in0, s1, s2, op0, op1), reads, writes)

    def tt(self, e, out, in0, in1, op, reads, writes):
        self.op(e, lambda g: g.tensor_tensor(out, in0, in1, op), reads, writes)

    def stt(self, e, out, in0, sc, in1, op0, op1, reads, writes):
        self.op(e, lambda g: g.scalar_tensor_tensor(out, in0, sc, in1, op0, op1), reads, writes)

    def cp(self, e, out, in_, reads, writes):
        if e == "act":
            self.op(e, lambda g: g.copy(out, in_), reads, writes)
        else:
            self.op(e, lambda g: g.tensor_copy(out, in_), reads, writes)

    def memset(self, e, ap, v, writes):
        self.op(e, lambda g: g.memset(ap, v), (), writes)


def bcast_rows(ap, n=128):
    return ap.partition_broadcast(n)


def emit_mod_rows(p, cin_t, modw, modb, ncols, outs, psum, stage, cbc, ones, bstage, whichs=(0, 1)):
    nblk = ncols // 128
    for which in whichs:
        for kc in range(8):
            p.ts("dve", cbc[:, kc, :], ones[:, :], cin_t[:, which, kc:kc + 1], None, ALU.mult, None,
                 [ones, cin_t], [cbc])
        for j in range(nblk):
            p.dma("sp", stage[:, :, :], modw[:, j * 128:(j + 1) * 128].rearrange("(kc p) n -> p kc n", p=128),
                  (), [stage])
            p.dma("sp", bstage[:, :], bcast_rows(modb[0:1, j * 128:(j + 1) * 128]), (), [bstage])
            for kc in range(8):
                p.mm(psum[:, 0:128], cbc[:, kc, :], stage[:, kc, :], kc == 0, kc == 7, [cbc, stage], [psum])
            tile, ap = outs[which](j)
            p.tt("dve", ap, psum[:, 0:128], bstage[:, :], ALU.add, [psum, bstage], [tile])


def emit_adanorm_T(p, x_t, A_t, S_t, hb, ptr, hT_ap, hT_t, ident, small):
    ss, rs, junk = small["ss"], small["rs"], small["junk"]
    p.memset("dve", ss[:, 0:1], 0.0, [ss])
    p.act(junk[:, :], x_t[:, :], AF.Square, [x_t, ss], [junk, ss], accum_out=ss[:, 0:1])
    p.ts("dve", rs[:, 0:1], ss[:, 0:1], 1.0 / D, EPS, ALU.mult, ALU.add, [ss], [rs])
    p.op("act", lambda g: g.sqrt(rs[:, 1:2], rs[:, 0:1]), [rs], [rs])
    p.op("dve", lambda g: g.reciprocal(rs[:, 2:3], rs[:, 1:2]), [rs], [rs])
    p.stt("dve", junk[:, :], x_t[:, :], rs[:, 2:3], A_t[:, :], ALU.mult, ALU.mult, [x_t, rs, A_t], [junk])
    p.tt("dve", hb[:, :], junk[:, :], S_t[:, :], ALU.add, [junk, S_t], [hb])
    for kc in range(8):
        p.tr(ptr[:, kc, :], hb[:, kc * 128:(kc + 1) * 128], ident[:, :], [hb, ident], [ptr])
    p.cp("act", hT_ap, ptr[:, :, :], [ptr], [hT_t])


NT_MOE = 18
NLAT_MOE = 16


def build_moe(n_exp=32):
    NT = NT_MOE
    nc = bass.Bass("TRN2", target_bir_lowering=False)
    x = nc.dram_tensor("x", [NT * 128, D], F32, kind="ExternalInput").ap()
    cin = nc.dram_tensor("cin", [128, 2, 8], F32, kind="ExternalInput").ap()
    modw = nc.dram_tensor("modw", [D, 3 * D], F32, kind="ExternalInput").ap()
    modb = nc.dram_tensor("modb", [1, 3 * D], F32, kind="ExternalInput").ap()
    nrm = nc.dram_tensor("nrm", [1, D], F32, kind="ExternalInput").ap()
    wr = nc.dram_tensor("wr", [D, 36], F32, kind="ExternalInput").ap()
    br = nc.dram_tensor("br", [1, 36], F32, kind="ExternalInput").ap()
    w13 = nc.dram_tensor("w13", [32, D, D], F32, kind="ExternalInput").ap()
    w2 = nc.dram_tensor("w2", [32, 512, D], F32, kind="ExternalInput").ap()
    identd = nc.dram_tensor("identd", [128, 128], F32, kind="ExternalInput").ap()
    xo = nc.dram_tensor("xo", [NT * 128, D], F32, kind="ExternalOutput").ap()

    p = Prog(nc)
    hT = p.sb([128, 8, NT * 128], BF16, "hT")
    acc = p.sb([128, NT, D], F32, "acc")
    w13b = [p.sb([128, 8, D], BF16, f"w13b{i}") for i in range(2)]
    w2b = [p.sb([128, 4, D], BF16, "w2b0")]
    MB = [[p.sb([128, D], F32, f"mb{w}{v}") for v in range(3)] for w in range(2)]
    xt = p.sb([128, D], F32, "xt")
    hb = p.sb([128, D], BF16, "hb")
    stage = p.sb([128, 8, 128], F32, "stage")
    bstage = p.sb([128, 128], F32, "bstage")
    actT = [p.sb([128, 4, 512], BF16, f"actT{i}") for i in range(2)]
    sa = p.sb([128, 512], F32, "sa")
    cbc = p.sb([128, 8, 128], F32, "cbc")
    CW = p.sb([128, NT, 32], F32, "CW")
    ones = p.sb([128, 128], F32, "ones")
    identf = p.sb([128, 128], F32, "identf")
    ident = p.sb([128, 128], BF16, "ident")
    cin_t = p.sb([128, 2, 8], F32, "cin_t")
    nrm_t = xt
    wrf = p.sb([128, 8, 36], F32, "wrf")
    wrb = p.sb([128, 8, 36], BF16, "wrb")
    brb = p.sb([128, 36], F32, "brb")
    small = {"ss": p.sb([128, 4], F32, "ss"), "rs": p.sb([128, 4], F32, "rs"), "junk": p.sb([128, D], F32, "junk")}
    rt = p.sb([128, 256], F32, "rt")
    pbank = [p.ps([128, 512], F32, f"pb{i}") for i in range(6)]
    ptr = p.ps([128, 8, 128], BF16, "ptr")

    p.dma("sp", identf[:, :], identd[:, :], (), [identf])
    p.cp("dve", ident[:, :], identf[:, :], [identf], [ident])
    p.memset("pool", ones[:, :], 1.0, [ones])
    p.memset("pool", acc[:, :, :], 0.0, [acc])
    p.dma("sp", cin_t[:, :, :], cin[:, :, :], (), [cin_t])
    p.act(cin_t[:, :, :], cin_t[:, :, :], AF.Silu, [cin_t], [cin_t])
    p.dma("sp", nrm_t[:, :], bcast_rows(nrm[0:1, :]), (), [nrm_t])
    p.dma("sp", wrf[:, :, :], wr.rearrange("(kc p) n -> p kc n", p=128), (), [wrf])
    p.cp("dve", wrb[:, :, :], wrf[:, :, :], [wrf], [wrb])
    p.dma("sp", brb[:, :], bcast_rows(br[0:1, :]), (), [brb])

    def outsel(which):
        def f(j):
            v, c = divmod(j, 8)
            t = MB[which][v]
            return t, t[:, c * 128:(c + 1) * 128]
        return f
    emit_mod_rows(p, cin_t, modw, modb, 3 * D, [outsel(0), outsel(1)], pbank[0], stage, cbc, ones, bstage)
    for w in range(2):
        A = MB[w][1]
        p.stt("dve", A[:, :], A[:, :], 1.0, nrm_t[:, :], ALU.add, ALU.mult, [A, nrm_t], [A])

    for i in range(NT):
        w = 0 if i < NLAT_MOE else 1
        p.dma("sp", xt[:, :], x[i * 128:(i + 1) * 128, :], (), [xt])
        emit_adanorm_T(p, xt, MB[w][1], MB[w][0], hb, ptr, hT[:, :, i * 128:(i + 1) * 128], hT, ident, small)
        lgp = pbank[1]
        for kc in range(8):
            p.mm(lgp[:, 0:36], hT[:, kc, i * 128:(i + 1) * 128], wrb[:, kc, :], kc == 0, kc == 7, [hT, wrb], [lgp])
        lg = rt[:, 0:36]
        p.tt("dve", lg, lgp[:, 0:36], brb[:, :], ALU.add, [lgp, brb], [rt])
        R, W_ = [rt], [rt]
        gmax, nb, se, m1, m2, den = (rt[:, 40 + k:41 + k] for k in range(6))
        oh = rt[:, 48:52]
        pen = rt[:, 52:56]
        m32 = rt[:, 64:96]
        e32 = rt[:, 96:128]
        m32b = rt[:, 128:160]
        sel = rt[:, 160:192]
        gex = rt[:, 192:196]
        p.op("dve", lambda g, gmax=gmax, lg=lg: g.reduce_max(gmax, lg[:, 0:4], AX.X), R, W_)
        p.ts("dve", oh, lg[:, 0:4], gmax, None, ALU.is_ge, None, R, W_)
        p.ts("dve", nb, gmax, -1.0, None, ALU.mult, None, R, W_)
        p.act(gex, lg[:, 0:4], AF.Exp, R, W_, bias=nb, accum_out=se)
        p.ts("dve", pen, oh, 1e9, -1e9, ALU.mult, ALU.add, R, W_)
        p.tt("dve", m32.rearrange("p (g e) -> p g e", g=4), lg[:, 4:36].rearrange("p (g e) -> p g e", g=4),
             pen.to_broadcast([128, 4, 8]) if False else rt[:, 52:56].rearrange("p (g o) -> p g o", o=1).broadcast_to([128, 4, 8]),
             ALU.add, R, W_)
        p.op("dve", lambda g, m1=m1, m32=m32: g.reduce_max(m1, m32, AX.X), R, W_)
        p.ts("dve", nb, m1, -1.0, None, ALU.mult, None, R, W_)
        p.act(e32, m32, AF.Exp, R, W_, bias=nb)
        p.ts("dve", m32b, m32, m1, -1e9, ALU.is_ge, ALU.mult, R, W_)
        p.tt("dve", m32b, m32b, m32, ALU.add, R, W_)
        p.op("dve", lambda g, m2=m2, m32b=m32b: g.reduce_max(m2, m32b, AX.X), R, W_)
        p.ts("dve", sel, m32, m2, None, ALU.is_ge, None, R, W_)
        p.tt("dve", sel, sel, e32, ALU.mult, R, W_)
        p.op("dve", lambda g, sel=sel, den=den: g.reduce_sum(den, sel, AX.X), R, W_)
        p.tt("dve", den, den, se, ALU.mult, R, W_)
        p.op("dve", lambda g, den=den: g.reciprocal(den, den), R, W_)
        p.ts("dve", CW[:, i, :], sel, den, None, ALU.mult, None, [rt], [CW])

    blocks = [(b * 4, 4) for b in range(NLAT_MOE // 4)] + [(NLAT_MOE, NT - NLAT_MOE)]
    pa = [pbank[0], pbank[1]]
    pbb = [pbank[2], pbank[3]]
    po = [pbank[4], pbank[5]]
    cnt_ab = 0
    cnt_o = 0
    cnt_act = 0
    for e in range(n_exp):
        wa = w13b[e % 2]
        wb = w2b[0]
        p.dma("pool", wa[:, :, :], w13[e].rearrange("(kc p) n -> p kc n", p=128), (), [wa])
        p.dma("pool", wb[:, :, :], w2[e].rearrange("(kc p) n -> p kc n", p=128), (), [wb])
        for (t0, nt) in blocks:
            ntok = nt * 128
            at = actT[cnt_act % 2]
            cnt_act += 1
            for fc in range(4):
                A_, B_ = pa[cnt_ab % 2], pbb[cnt_ab % 2]
                cnt_ab += 1
                for kc in range(8):
                    p.mm(A_[:, 0:ntok], wa[:, kc, fc * 128:(fc + 1) * 128], hT[:, kc, t0 * 128:t0 * 128 + ntok],
                         kc == 0, kc == 7, [wa, hT], [A_])
                for kc in range(8):
                    p.mm(B_[:, 0:ntok], wa[:, kc, 512 + fc * 128:512 + (fc + 1) * 128],
                         hT[:, kc, t0 * 128:t0 * 128 + ntok], kc == 0, kc == 7, [wa, hT], [B_])
                p.act(sa[:, 0:ntok], A_[:, 0:ntok], AF.Silu, [A_], [sa])
                p.tt("dve", at[:, fc, 0:ntok], sa[:, 0:ntok], B_[:, 0:ntok], ALU.mult, [sa, B_], [at])
            for tt_ in range(nt):
                ti = t0 + tt_
                for half in range(2):
                    O_ = po[cnt_o % 2]
                    cnt_o += 1
                    for fc in range(4):
                        p.mm(O_[:, :], at[:, fc, tt_ * 128:(tt_ + 1) * 128], wb[:, fc, half * 512:(half + 1) * 512],
                             fc == 0, fc == 3, [at, wb], [O_])
                    accs = acc[:, ti, half * 512:(half + 1) * 512]
                    p.stt("dve", accs, O_[:, :], CW[:, ti, e:e + 1], accs, ALU.mult, ALU.add, [O_, CW, acc], [acc])

    for i in range(NT):
        w = 0 if i < NLAT_MOE else 1
        p.dma("sp", xt[:, :], x[i * 128:(i + 1) * 128, :], (), [xt])
        j = small["junk"]
        p.tt("dve", j[:, :], acc[:, i, :], MB[w][2][:, :], ALU.mult, [acc, MB[w][2]], [j])
        p.tt("dve", j[:, :], j[:, :], xt[:, :], ALU.add, [j, xt], [j])
        p.dma("sp", xo[i * 128:(i + 1) * 128, :], j[:, :], [j], ())
    p.emit()
    return nc


def moe_inputs(x_core, c_b, c_ctx, mod_w_i, mod_b_i, norm_ffn_i, wg, bg, we, be, w13, w2):
    cin = np.stack([c_b.reshape(8, 128).T, c_ctx.reshape(8, 128).T], axis=1)
    return {
        "x": np.ascontiguousarray(x_core, dtype=np.float32),
        "cin": np.ascontiguousarray(cin, dtype=np.float32),
        "modw": np.ascontiguousarray(mod_w_i[:, 3 * D:6 * D]),
        "modb": np.ascontiguousarray(mod_b_i[None, 3 * D:6 * D]),
        "nrm": np.ascontiguousarray(norm_ffn_i[None, :]),
        "wr": np.ascontiguousarray(np.concatenate([wg, we], axis=1)),
        "br": np.ascontiguousarray(np.concatenate([bg, be])[None, :]),
        "w13": w13, "w2": w2,
        "identd": np.eye(128, dtype=np.float32),
    }


NTA = 14
NLOC = 8
WCOLS = 2432
NEG = -30000.0


def alias(p, t, name):
    a = T(t.h, name)
    a.r.w = t.r.w
    a.r.readers = dict(t.r.readers)
    return a


def build_attn(ph=9, dbg=False):
    nc = bass.Bass("TRN2", target_bir_lowering=False)
    dt_in = lambda n, s: nc.dram_tensor(n, s, F32, kind="ExternalInput").ap()
    xe = dt_in("xe", [NTA * 128, D])
    cin = dt_in("cin", [128, 2, 8])
    modw = dt_in("modw", [D, 3 * D])
    modb = dt_in("modb", [1, 3 * D])
    nrm = dt_in("nrm", [1, D])
    win = dt_in("win", [D, WCOLS])
    wout = dt_in("wout", [D, D])
    gains = dt_in("gains", [128, 4])
    ropec = dt_in("ropec", [128, 1536])
    ropes = dt_in("ropes", [128, 1536])
    pmd = dt_in("pmd", [128, 128])
    onesd = dt_in("onesd", [128, 128])
    identd = dt_in("identd", [128, 128])
    nab = dt_in("nab", [8, 128, 27 * 128])
    wam = dt_in("wam", [128, 4 * 512])
    sink = dt_in("sink", [1, 8])
    xo = nc.dram_tensor("xo", [(NLOC + 2) * 128, D], F32, kind="ExternalOutput").ap()

    p = Prog(nc)
    NTOK = NTA * 128
    big = p.sb([128, 8 * NTOK], BF16, "big")
    hTv = big[:, :].rearrange("p (kc t) -> p kc t", kc=8)
    big2 = p.sb([128, 8 * WCOLS], BF16, "big2")
    winv = big2[:, :].rearrange("p (kc n) -> p kc n", kc=8)
    QT = p.sb([128, 14, NTOK], BF16, "QT")
    VA = p.sb([128, NTA, 8, 65], BF16, "VA")
    VB = p.sb([128, NTA, 2, 65], BF16, "VB")
    PT = [p.sb([128, 8, 128], BF16, f"PT{i}") for i in range(2)]
    MB = [[p.sb([128, D], F32, f"mb{w}{v}") for v in range(3)] for w in range(2)]
    xt = p.sb([128, D], F32, "xt")
    hb = p.sb([128, D], BF16, "hb")
    stage = p.sb([128, 8, 128], F32, "stage")
    bstage = p.sb([128, 128], F32, "bstage")
    cbc = p.sb([128, 8, 128], F32, "cbc")
    ones = p.sb([128, 128], F32, "ones")
    identf = p.sb([128, 128], F32, "identf")
    ident = p.sb([128, 128], BF16, "ident")
    onesb = p.sb([128, 128], BF16, "onesb")
    pm = p.sb([128, 128], BF16, "pm")
    cin_t = p.sb([128, 2, 8], F32, "cin_t")
    G = p.sb([128, 4], F32, "G")
    RC = p.sb([128, 1536], BF16, "RC")
    RS = p.sb([128, 1536], BF16, "RS")
    WM = p.sb([128, 4, 512], BF16, "WM")
    esink = p.sb([128, 8], F32, "esink")
    small = {"ss": p.sb([128, 4], F32, "ss"), "rs": p.sb([128, 4], F32, "rs"), "junk": p.sb([128, D], F32, "junk")}
    sq = p.sb([128, 512], BF16, "sq")
    rstd = p.sb([128, 512], F32, "rstd")
    qn = p.sb([128, 512], BF16, "qn")
    t1 = p.sb([128, 512], F32, "t1")
    rec = p.sb([128, 8], F32, "rec")
    oT = p.sb([128, 8, 128], BF16, "oT")
    pbank = [p.ps([128, 512], F32, f"pb{i}") for i in range(7)]
    ptr = p.ps([128, 8, 128], BF16, "ptr")

    p.dma("sp", identf[:, :], identd[:, :], (), [identf])
    p.cp("dve", ident[:, :], identf[:, :], [identf], [ident])
    p.dma("pool", onesb[:, :], onesd[:, :], (), [onesb])
    p.dma("pool", pm[:, :], pmd[:, :], (), [pm])
    p.dma("pool", RC[:, :], ropec[:, :], (), [RC])
    p.dma("pool", RS[:, :], ropes[:, :], (), [RS])
    p.dma("pool", WM[:, :, :], wam.rearrange("p (s q) -> p s q", s=4), (), [WM])
    p.dma("pool", winv, win.rearrange("(kc p) n -> p kc n", p=128), (), [big2])
    p.memset("pool", ones[:, :], 1.0, [ones])
    p.memset("pool", VA[:, :, :, :], 1.0, [VA])
    p.memset("pool", VB[:, :, :, :], 1.0, [VB])
    p.dma("sp", cin_t[:, :, :], cin[:, :, :], (), [cin_t])
    p.act(cin_t[:, :, :], cin_t[:, :, :], AF.Silu, [cin_t], [cin_t])
    p.dma("sp", xt[:, :], bcast_rows(nrm[0:1, :]), (), [xt])
    p.dma("sp", G[:, :], gains[:, :], (), [G])
    p.ts("dve", G[:, 0:1], G[:, 0:1], 0.125, None, ALU.mult, None, [G], [G])
    p.ts("dve", G[:, 2:3], G[:, 2:3], 0.125, None, ALU.mult, None, [G], [G])
    p.dma("sp", esink[:, :], bcast_rows(sink[0:1, :]), (), [esink])
    p.act(esink[:, :], esink[:, :], AF.Exp, [esink], [esink])

    def outsel(which):
        def f(j):
            v, c = divmod(j, 8)
            t = MB[which][v]
            return t, t[:, c * 128:(c + 1) * 128]
        return f
    emit_mod_rows(p, cin_t, modw, modb, 3 * D, [outsel(0), outsel(1)], pbank[0], stage, cbc, ones, bstage)
    for w in range(2):
        A = MB[w][1]
        p.stt("dve", A[:, :], A[:, :], 1.0, xt[:, :], ALU.add, ALU.mult, [A, xt], [A])

    for i in range(NTA):
        w = 0 if i < 12 else 1
        p.dma("sp", xt[:, :], xe[i * 128:(i + 1) * 128, :], (), [xt])
        emit_adanorm_T(p, xt, MB[w][1], MB[w][0], hb, ptr, hTv[:, :, i * 128:(i + 1) * 128], big, ident, small)

    blocks = [(0, 512), (512, 512), (1024, 512), (1536, 256)]
    cntp = 0
    for ch in range(14 if ph >= 2 else 0):
        gi = 0 if ch < 4 else 1 if ch < 8 else 2 if ch < 12 else 3
        rope = ch >= 8
        for (t0, n) in blocks:
            pq = pbank[cntp % 2]
            pmm = pbank[2 + cntp % 2]
            cntp += 1
            for kc in range(8):
                p.mm(pq[:, 0:n], winv[:, kc, ch * 128:(ch + 1) * 128], hTv[:, kc, t0:t0 + n], kc == 0, kc == 7,
                     [big2, big], [pq])
            p.act(sq[:, 0:n], pq[:, 0:n], AF.Square, [pq], [sq])
            p.mm(pmm[:, 0:n], onesb[:, :], sq[:, 0:n], True, True, [onesb, sq], [pmm])
            p.ts("dve", rstd[:, 0:n], pmm[:, 0:n], 1.0 / 64, EPS, ALU.mult, ALU.add, [pmm], [rstd])
            p.op("act", lambda g, n=n: g.sqrt(rstd[:, 0:n], rstd[:, 0:n]), [rstd], [rstd])
            p.op("dve", lambda g, n=n: g.reciprocal(rstd[:, 0:n], rstd[:, 0:n]), [rstd], [rstd])
            if rope and t0 < 1536:
                p.stt("dve", qn[:, 0:n], pq[:, 0:n], G[:, gi:gi + 1], rstd[:, 0:n], ALU.mult, ALU.mult,
                      [pq, G, rstd], [qn])
                pr = pbank[4 + cntp % 2]
                p.mm(pr[:, 0:n], pm[:, :], qn[:, 0:n], True, True, [pm, qn], [pr])
                p.tt("pool", t1[:, 0:n], qn[:, 0:n], RC[:, t0:t0 + n], ALU.mult, [qn, RC], [t1])
                p.tt("dve", rstd[:, 0:n], pr[:, 0:n], RS[:, t0:t0 + n], ALU.mult, [pr, RS], [rstd])
                p.tt("dve", QT[:, ch, t0:t0 + n], t1[:, 0:n], rstd[:, 0:n], ALU.add, [t1, rstd], [QT])
            else:
                p.stt("dve", QT[:, ch, t0:t0 + n], pq[:, 0:n], G[:, gi:gi + 1], rstd[:, 0:n], ALU.mult, ALU.mult,
                      [pq, G, rstd], [QT])

    for i in range(NTA if ph >= 3 else 0):
        pv, pv2 = pbank[cntp % 2], pbank[2 + cntp % 2]
        cntp += 1
        for kc in range(8):
            p.mm(pv[:, :], hTv[:, kc, i * 128:(i + 1) * 128], winv[:, kc, 1792:2304], kc == 0, kc == 7, [big, big2], [pv])
        for kc in range(8):
            p.mm(pv2[:, 0:128], hTv[:, kc, i * 128:(i + 1) * 128], winv[:, kc, 2304:2432], kc == 0, kc == 7,
                 [big, big2], [pv2])
        p.cp("act", VA[:, i, :, 0:64], pv[:, :].rearrange("p (h d) -> p h d", h=8), [pv], [VA])
        p.cp("dve", VB[:, i, :, 0:64], pv2[:, 0:128].rearrange("p (h d) -> p h d", h=2), [pv2], [VB])

    OAt = alias(p, big, "OA")
    OA = big[:, 0:(NLOC + 2) * 1024].rearrange("p (t d) -> p t d", d=1024)
    WO = alias(p, big2, "WO")
    wov = big2[:, 0:8192].rearrange("p (kc n) -> p kc n", kc=8)
    NABt = [alias(p, big2, f"NAB{i}") for i in range(2)]
    nabv = [big2[:, 8192 + i * 3456:8192 + (i + 1) * 3456].rearrange("p (s q) -> p s q", q=128) for i in range(2)]
    print("sbuf remaining", nc.sbuf_bytes_remaining)
    PTWt = p.sb([128, 5, 512], BF16, "PTWs")
    PTW = PTWt.h
    p.dma("pool", wov, wout.rearrange("(kc p) n -> p kc n", p=128), (), [WO])

    CT = [12, 13]

    def na_unit(h, qt, ktiles, slots, nabT, nabV, ot, u):
        half = slice((h % 2) * 64, (h % 2) * 64 + 64)
        qch, kch = h // 2, 4 + h // 2
        S = [pbank[(u % 2) * 2], pbank[(u % 2) * 2 + 1]]
        O = pbank[4 + u % 2]
        pt = PT[u % 2]
        allk = [(kt, sl) for kt, sl in zip(ktiles, slots)] + [(c, None) for c in CT]
        for c, (kt, sl) in enumerate(allk):
            bank = S[c // 4]
            o = bank[:, (c % 4) * 128:(c % 4 + 1) * 128]
            p.mm(o, QT[half, kch, kt * 128:(kt + 1) * 128], QT[half, qch, qt * 128:(qt + 1) * 128], True, sl is None,
                 [QT], [bank])
            if sl is not None:
                p.mm(o, ident[:, :], nabV[:, sl, :], False, True, [ident, nabT], [bank])
        nck = len(allk)
        n0 = min(nck, 4)
        p.act(pt[:, 0:n0, :], S[0][:, 0:n0 * 128].rearrange("p (c q) -> p c q", q=128), AF.Exp, [S[0]], [pt])
        if nck > 4:
            p.act(pt[:, 4:nck, :], S[1][:, 0:(nck - 4) * 128].rearrange("p (c q) -> p c q", q=128), AF.Exp, [S[1]], [pt])
        for c, (kt, sl) in enumerate(allk):
            p.mm(O[:, 0:65], pt[:, c, :], VA[:, kt, h, :], c == 0, c == nck - 1, [pt, VA], [O])
        p.op("dve", lambda g, O=O, h=h: g.reciprocal(rec[:, h:h + 1], O[:, 64:65]), [O], [rec])
        p.ts("dve", OA[:, ot, h * 64:(h + 1) * 64], O[:, 0:64], rec[:, h:h + 1], None, ALU.mult, None, [O, rec], [OAt])

    u = 0
    for h in range(8 if ph >= 4 else 0):
        nT, nV = NABt[h % 2], nabv[h % 2]
        p.dma("pool", nV, nab[h].rearrange("p (s q) -> p s q", q=128), (), [nT])
        for rp in range(NLOC):
            if rp == 0:
                kts, sls = list(range(0, 6)), list(range(5, 11))
            elif rp == 1:
                kts, sls = list(range(1, 6)), list(range(11, 16))
            elif rp == NLOC - 2:
                kts, sls = list(range(rp, rp + 5)), list(range(16, 21))
            elif rp == NLOC - 1:
                kts, sls = list(range(rp - 1, rp + 5)), list(range(21, 27))
            else:
                kts, sls = list(range(rp, rp + 5)), list(range(0, 5))
            na_unit(h, rp + 2, kts, sls, nT, nV, rp, u)
            u += 1
        for ci, ct in enumerate(CT):
            na_unit(h, ct, [], [], nT, nV, NLOC + ci, u)
            u += 1

    def wa_unit(qt, kvh, ktiles, mslots, ot, u):
        kch = 12 + kvh
        allk = [(kt, ms) for kt, ms in zip(ktiles, mslots)] + [(c, None) for c in CT]
        nck = len(allk)
        for c, (kt, ms) in enumerate(allk):
            for par in range(2):
                bank = pbank[((u * 5 + c) % 2) * 2 + par]
                half = slice(par * 64, par * 64 + 64)
                for jj in range(2):
                    j = 2 * jj + par
                    h = 4 * kvh + j
                    o = bank[:, jj * 128:(jj + 1) * 128]
                    p.mm(o, QT[half, kch, kt * 128:(kt + 1) * 128], QT[half, 8 + h // 2, qt * 128:(qt + 1) * 128],
                         True, ms is None, [QT], [bank])
                    if ms is not None:
                        p.mm(o, ident[:, :], WM[:, ms, 0:128], False, True, [ident, WM], [bank])
                p.act(PTW[:, c, par * 256:(par + 1) * 256], bank[:, 0:256], AF.Exp, [bank], [PTWt])
        O = pbank[4 + u % 2]
        WSUB = int(os.environ.get('WSUB', '9'))
        if WSUB < 1:
            return
        for j in range(4):
            pos = (j % 2) * 2 + j // 2
            for c, (kt, ms) in enumerate(allk):
                p.mm(O[:, j * 128:j * 128 + 65], PTW[:, c, pos * 128:(pos + 1) * 128], VB[:, kt, kvh, :], c == 0,
                     c == nck - 1, [PTWt, VB], [O])
        if WSUB < 2:
            return
        for j in range(4):
            h = 4 * kvh + j
            p.tt("dve", rec[:, h:h + 1], O[:, j * 128 + 64:j * 128 + 65], esink[:, h:h + 1], ALU.add, [O, esink], [rec])
            p.op("dve", lambda g, h=h: g.reciprocal(rec[:, h:h + 1], rec[:, h:h + 1]), [rec], [rec])
            p.ts("dve", OA[:, ot, 512 + h * 64:512 + (h + 1) * 64], O[:, j * 128:j * 128 + 64], rec[:, h:h + 1], None,
                 ALU.mult, None, [O, rec], [OAt])

    for n in range(int(os.environ.get('WN', NLOC)) if ph >= 5 else 0):
        for kvh in range(2):
            ms = [2 if n == 0 else 0, None, 3 if n == NLOC - 1 else 1]
            wa_unit(n + 2, kvh, [n + 1, n + 2, n + 3], ms, n, u)
            u += 1
    for ci, ct in enumerate(CT if ph >= 5 and int(os.environ.get('WC', 1)) else []):
        for kvh in range(2):
            wa_unit(ct, kvh, [], [], NLOC + ci, u)
            u += 1

    if dbg:
        dOA = nc.dram_tensor("dOA", [128, 10 * 1024], F32, kind="ExternalOutput").ap()
        dQT = nc.dram_tensor("dQT", [128, 14 * NTOK], F32, kind="ExternalOutput").ap()
        dVA = nc.dram_tensor("dVA", [128, NTA * 8 * 65], F32, kind="ExternalOutput").ap()
        p.dma("pool", dOA[:, :], big[:, 0:10 * 1024], [OAt], ())
        p.dma("pool", dQT[:, :], QT[:, :, :].rearrange("p c t -> p (c t)"), [QT], ())
        p.dma("pool", dVA[:, :], VA[:, :, :, :].rearrange("p t h d -> p (t h d)"), [VA], ())
    for o in range(NLOC + 2):
        w = 0 if o < NLOC else 1
        src = o + 2 if o < NLOC else 12 + (o - NLOC)
        for kc in range(8):
            p.tr(ptr[:, kc, :], OA[:, o, kc * 128:(kc + 1) * 128], ident[:, :], [OAt, ident], [ptr])
        p.cp("act", oT[:, :, :], ptr[:, :, :], [ptr], [oT])
        y0, y1 = pbank[(o % 2) * 2], pbank[(o % 2) * 2 + 1]
        for half, y in enumerate((y0, y1)):
            for kc in range(8):
                p.mm(y[:, :], oT[:, kc, :], wov[:, kc, half * 512:(half + 1) * 512], kc == 0, kc == 7, [oT, WO], [y])
        p.dma("sp", xt[:, :], xe[src * 128:(src + 1) * 128, :], (), [xt])
        j = small["junk"]
        g1 = MB[w][2]
        for half, y in enumerate((y0, y1)):
            sl = slice(half * 512, (half + 1) * 512)
            p.tt("dve", j[:, sl], y[:, :], g1[:, sl], ALU.mult, [y, g1], [j])
        p.tt("dve", j[:, :], j[:, :], xt[:, :], ALU.add, [j, xt], [j])
        p.dma("sp", xo[o * 128:(o + 1) * 128, :], j[:, :], [j], ())
    p.emit()
    return nc


def rope_tables(tok0):
    t = tok0 + np.arange(1536)
    row, col = (t // 64).astype(np.float32), (t % 64).astype(np.float32)
    inv = (10000.0 ** (-np.arange(16, dtype=np.float32) / 16)).astype(np.float32)
    C = np.zeros((64, 1536), np.float32)
    S = np.zeros((64, 1536), np.float32)
    for d in range(64):
        pos = row if d < 32 else col
        q = d % 32
        ang = (pos * inv[q % 16]).astype(np.float32)
        C[d] = np.cos(ang)
        S[d] = -np.sin(ang) if q < 16 else np.sin(ang)
    return np.concatenate([C, C], 0), np.concatenate([S, S], 0)


def perm_matrix():
    P = np.zeros((128, 128), np.float32)
    for m in range(128):
        blk, d = divmod(m, 64)
        q = d % 32
        partner = d + 16 if q < 16 else d - 16
        P[blk * 64 + partner, m] = 1.0
    return P


def na_bias_tables(rel_bias, R0):
    out = np.full((8, 128, 27, 128), NEG, np.float32)
    specs = []
    for c in range(5):
        specs.append((c, 2, 2 + c))
    for c in range(6):
        specs.append((5 + c, 0, c))
    for c in range(5):
        specs.append((11 + c, 1, 1 + c))
    for c in range(5):
        specs.append((16 + c, NLOC - 2, NLOC - 2 + c))
    for c in range(6):
        specs.append((21 + c, NLOC - 1, NLOC - 2 + c))
    kp = np.arange(128)
    qi = np.arange(128)
    for slot, rp, kt in specs:
        r = R0 + 2 * rp + qi // 64
        i = qi % 64
        kr = R0 - 4 + 2 * kt + kp // 64
        jc = kp % 64
        r0 = np.clip(r - 4, 0, 120)
        c0 = np.clip(i - 8, 0, 48)
        valid = ((kr[:, None] >= r0[None, :]) & (kr[:, None] < r0[None, :] + 8) & (kr[:, None] >= 0) & (kr[:, None] < 128)
                 & (jc[:, None] >= c0[None, :]) & (jc[:, None] < c0[None, :] + 16))
        dr = np.clip(kr[:, None] - r[None, :] + 7, 0, 14)
        dc = np.clip(jc[:, None] - i[None, :] + 15, 0, 30)
        vals = rel_bias[:, dr, dc]
        out[:, :, slot, :] = np.where(valid[None], vals, NEG)
    return out.reshape(8, 128, 27 * 128)


def wa_masks(gb0):
    kp = np.arange(128)[:, None]
    qi = np.arange(128)[None, :]
    prev = np.where(kp >= qi, 0.0, NEG).astype(np.float32)
    nxt = np.where(kp <= qi, 0.0, NEG).astype(np.float32)
    allneg = np.full((128, 128), NEG, np.float32)
    m = [prev, nxt, prev if gb0 > 0 else allneg, nxt if gb0 + NLOC < 64 else allneg]
    return np.concatenate([np.tile(x, (1, 4)) for x in m], axis=1)


def attn_inputs(inp, b, s):
    R0 = 16 * s
    x = inp["x"][b]
    xe = np.zeros((NTA * 128, D), np.float32)
    g0 = (R0 - 4) * 64
    lo, hi = max(g0, 0), min(g0 + 1536, 8192)
    xe[lo - g0:hi - g0] = x[lo:hi]
    xe[1536:] = inp["ctx"][b]
    w = inp["att_w_in"][0]
    win = np.concatenate([w[:, 0:512], w[:, 512:1024], w[:, 1536:2048], w[:, 2048:2112], w[:, 2048:2112],
                          w[:, 2112:2176], w[:, 2112:2176], w[:, 1024:1536], w[:, 2176:2304]], axis=1)
    gv = [inp["na_q_norm"][0], inp["na_k_norm"][0], inp["wa_q_norm"][0], inp["wa_k_norm"][0]]
    gains = np.stack([np.concatenate([g, g]) for g in gv], axis=1)
    rc, rs = rope_tables(g0)
    cin = np.stack([inp["c"][b].reshape(8, 128).T, inp["c_ctx"].reshape(8, 128).T], axis=1)
    ob = np.zeros((128, 128), np.float32)
    ob[:64, :64] = 1.0
    ob[64:, 64:] = 1.0
    return {
        "xe": xe, "cin": np.ascontiguousarray(cin, dtype=np.float32),
        "modw": np.ascontiguousarray(inp["mod_w"][0][:, 0:3 * D]), "modb": np.ascontiguousarray(inp["mod_b"][0][None, 0:3 * D]),
        "nrm": np.ascontiguousarray(inp["norm_mix"][0][None, :]),
        "win": np.ascontiguousarray(win), "wout": np.ascontiguousarray(inp["att_w_out"][0]),
        "gains": np.ascontiguousarray(gains, dtype=np.float32), "ropec": rc, "ropes": rs, "pmd": perm_matrix(),
        "onesd": ob, "identd": np.eye(128, dtype=np.float32),
        "nab": na_bias_tables(inp["na_rel_bias"][0], R0), "wam": wa_masks(R0 // 2),
        "sink": np.ascontiguousarray(inp["wa_sink"][0][None, :]),
    }


TS = 66
NSEQ = TS * 128
WS = 1056
TWO_PI = 2.0 * np.pi


def build_ssm(nt=TS, do_s5=True):
    nc = bass.Bass("TRN2", target_bir_lowering=False)
    dt_in = lambda n, s: nc.dram_tensor(n, s, F32, kind="ExternalInput").ap()
    xs = dt_in("xs", [NSEQ, D])
    cin = dt_in("cin", [128, 2, 8])
    modw = dt_in("modw", [D, 2 * D])
    modb = dt_in("modb", [1, 2 * D])
    nrm = dt_in("nrm", [1, D])
    wsel = dt_in("wsel", [D, WS])
    cw = dt_in("cw", [128, 6 * 3])
    cbias = dt_in("cbias", [128, 6])
    dtb = dt_in("dtb", [1, 8])
    alog = dt_in("alog", [1, 8])
    dsk = dt_in("dsk", [1, 8])
    identd = dt_in("identd", [128, 128])
    triud = dt_in("triud", [128, 128])
    iotad = dt_in("iotad", [128, 129])
    m01d = dt_in("m01d", [128, 512])
    lam = dt_in("lam", [128, 3 * 8])
    BLr = dt_in("BLr", [8, 128, 128])
    BLi = dt_in("BLi", [8, 128, 128])
    CLr = dt_in("CLr", [8, 128, 32])
    CLi = dt_in("CLi", [8, 128, 32])
    DL = dt_in("DL", [8, 128, 32])
    yssd = nc.dram_tensor("yssd", [NSEQ, 512], F32, kind="ExternalOutput").ap()
    ys5 = nc.dram_tensor("ys5", [256, NSEQ], F32, kind="ExternalOutput").ap()

    p = Prog(nc)
    UT = p.sb([128, 2, NSEQ], BF16, "UT")
    Z = p.sb([128, 2, NSEQ], F32, "Z")
    MB = [[p.sb([128, D], F32, f"mb{w}{v}") for v in range(2)] for w in range(2)]
    wsb = p.sb([128, 8, WS], BF16, "wsb")
    xt = p.sb([128, D], F32, "xt")
    hb = p.sb([128, D], BF16, "hb")
    hTi = p.sb([128, 8, 128], BF16, "hTi")
    stage = p.sb([128, 8, 128], F32, "stage")
    bstage = p.sb([128, 128], F32, "bstage")
    cbc = p.sb([128, 8, 128], F32, "cbc")
    ones = p.sb([128, 128], F32, "ones")
    identf = p.sb([128, 128], F32, "identf")
    ident = p.sb([128, 128], BF16, "ident")
    triu = p.sb([128, 128], F32, "triu")
    cin_t = p.sb([128, 2, 8], F32, "cin_t")
    small = {"ss": p.sb([128, 4], F32, "ss"), "rs": p.sb([128, 4], F32, "rs"), "junk": p.sb([128, D], F32, "junk")}
    CW = p.sb([128, 18], F32, "CWc")
    CBs = p.sb([128, 6], F32, "CBs")
    dtb_t = p.sb([128, 8], F32, "dtb_t")
    A_t = p.sb([128, 8], F32, "A_t")
    dsk_t = p.sb([128, 8], F32, "dsk_t")
    RAW = [p.sb([128, 6, 128], F32, f"raw{i}") for i in range(3)]
    DTs = [p.sb([128, 8], F32, f"dts{i}") for i in range(3)]
    CBUF = p.sb([128, 6, 130], F32, "CBUF")
    cacc = p.sb([128, 128], F32, "cacc")
    XC = p.sb([128, 6, 128], BF16, "XC")
    XTOK = p.sb([128, 512], BF16, "XTOK")
    BTOK = p.sb([128, 128], BF16, "BTOK")
    sm = p.sb([128, 64], F32, "sm")
    CBT = p.sb([128, 128], F32, "CBT")
    Rm = p.sb([128, 128], F32, "Rm")
    Lm = p.sb([128, 128], F32, "Lm")
    WT = [p.sb([128, 128], BF16, f"WT{i}") for i in range(2)]
    H = p.sb([128, 512], F32, "H")
    Hb = p.sb([128, 512], BF16, "Hb")
    XW = p.sb([128, 512], BF16, "XW")
    ysb = p.sb([128, 512], F32, "ysb")
    ytmp = p.sb([128, 512], F32, "ytmp")
    B0, B1, B2, B3, B4, B5, B6 = [p.ps([128, 512], F32, f"pb{i}") for i in range(7)]
    ptr = p.ps([128, 8, 128], BF16, "ptr")

    p.dma("sp", identf[:, :], identd[:, :], (), [identf])
    p.cp("dve", ident[:, :], identf[:, :], [identf], [ident])
    p.dma("sp", triu[:, :], triud[:, :], (), [triu])
    p.memset("pool", ones[:, :], 1.0, [ones])
    p.memset("pool", H[:, :], 0.0, [H])
    p.dma("pool", wsb[:, :, :], wsel.rearrange("(kc p) n -> p kc n", p=128), (), [wsb])
    p.dma("sp", cin_t[:, :, :], cin[:, :, :], (), [cin_t])
    p.act(cin_t[:, :, :], cin_t[:, :, :], AF.Silu, [cin_t], [cin_t])
    p.dma("sp", xt[:, :], bcast_rows(nrm[0:1, :]), (), [xt])
    p.dma("sp", CW[:, :], cw[:, :], (), [CW])
    p.dma("sp", CBs[:, :], cbias[:, :], (), [CBs])
    p.dma("sp", dtb_t[:, :], bcast_rows(dtb[0:1, :]), (), [dtb_t])
    p.dma("sp", A_t[:, :], bcast_rows(alog[0:1, :]), (), [A_t])
    p.act(A_t[:, :], A_t[:, :], AF.Exp, [A_t], [A_t])
    p.ts("dve", A_t[:, :], A_t[:, :], -1.0, None, ALU.mult, None, [A_t], [A_t])
    p.dma("sp", dsk_t[:, :], bcast_rows(dsk[0:1, :]), (), [dsk_t])

    def outsel(which):
        def f(j):
            v, c = divmod(j, 8)
            t = MB[which][v]
            return t, t[:, c * 128:(c + 1) * 128]
        return f
    emit_mod_rows(p, cin_t, modw, modb, 2 * D, [outsel(0), outsel(1)], B0, stage, cbc, ones, bstage)
    for w in range(2):
        A = MB[w][1]
        p.stt("dve", A[:, :], A[:, :], 1.0, xt[:, :], ALU.add, ALU.mult, [A, xt], [A])

    PSUB = int(os.environ.get('PSUB', '9'))

    def project(i):
        if PSUB < 1:
            return
        w = 1 if i < 2 else 0
        raw, dts = RAW[i % 3], DTs[i % 3]
        p.dma("sp", xt[:, :], xs[i * 128:(i + 1) * 128, :], (), [xt])
        emit_adanorm_T(p, xt, MB[w][1], MB[w][0], hb, ptr, hTi[:, :, :], hTi, ident, small)
        if PSUB < 2:
            return
        PQ = int(os.environ.get('PQ', '9'))
        for grp in range(2):
            if grp == 1 and PQ < 3:
                break
            for c4 in range(4):
                ch = grp * 4 + c4
                for kc in range(8):
                    p.mm(B0[:, c4 * 128:(c4 + 1) * 128], wsb[:, kc, ch * 128:(ch + 1) * 128], hTi[:, kc, :], kc == 0,
                         kc == 7, [wsb, hTi], [B0])
            if grp == 0:
                if PQ >= 2:
                    p.cp("act", raw[:, 0:4, :], B0[:, :].rearrange("p (c t) -> p c t", c=4), [B0], [raw])
            else:
                if PQ >= 4:
                    p.cp("act", raw[:, 4:6, :], B0[:, 0:256].rearrange("p (c t) -> p c t", c=2), [B0], [raw])
                if PQ >= 5:
                    for c2 in range(2):
                        p.cp("act", UT[:, c2, i * 128:(i + 1) * 128], B0[:, 256 + c2 * 128:384 + c2 * 128], [B0], [UT])
        if PSUB < 3:
            return
        for kc in range(8):
            p.mm(B1[:, 0:8], hTi[:, kc, :], wsb[:, kc, 1024:1032], kc == 0, kc == 7, [hTi, wsb], [B1])
        p.tt("dve", dts[:, :], B1[:, 0:8], dtb_t[:, :], ALU.add, [B1, dtb_t], [dts])
        p.act(dts[:, :], dts[:, :], AF.Exp, [dts], [dts])
        p.act(dts[:, :], dts[:, :], AF.Ln, [dts], [dts], bias=1.0)

    a_, acs, tot, eacs, wend, dec = (sm[:, 8 * k:8 * k + 8] for k in range(6))

    SSUB = int(os.environ.get('SSUB', '9'))

    def ssd_chunk(j):
        raw, dts = RAW[j % 3], DTs[j % 3]
        if SSUB < 1:
            return
        first = j in (0, 2)
        last = j in (1, nt - 1)
        if first:
            p.memset("pool", CBUF[:, :, 0:1], 0.0, [CBUF])
        else:
            p.cp("pool", CBUF[:, :, 0:1], RAW[(j - 1) % 3][:, :, 127:128], [RAW[(j - 1) % 3]], [CBUF])
        p.cp("pool", CBUF[:, :, 1:129], raw[:, :, :], [raw], [CBUF])
        if last:
            p.memset("pool", CBUF[:, :, 129:130], 0.0, [CBUF])
        else:
            p.cp("pool", CBUF[:, :, 129:130], RAW[(j + 1) % 3][:, :, 0:1], [RAW[(j + 1) % 3]], [CBUF])
        for ch in range(6):
            p.ts("dve", cacc[:, :], CBUF[:, ch, 0:128], CW[:, ch * 3:ch * 3 + 1], None, ALU.mult, None, [CBUF, CW], [cacc])
            p.stt("dve", cacc[:, :], CBUF[:, ch, 1:129], CW[:, ch * 3 + 1:ch * 3 + 2], cacc[:, :], ALU.mult, ALU.add,
                  [CBUF, CW, cacc], [cacc])
            p.stt("dve", cacc[:, :], CBUF[:, ch, 2:130], CW[:, ch * 3 + 2:ch * 3 + 3], cacc[:, :], ALU.mult, ALU.add,
                  [CBUF, CW, cacc], [cacc])
            p.act(XC[:, ch, :], cacc[:, :], AF.Silu, [cacc, CBs], [XC], bias=CBs[:, ch:ch + 1])
        if SSUB < 2:
            return
        for c in range(5):
            p.tr(ptr[:, c, :], XC[:, c, :], ident[:, :], [XC, ident], [ptr])
        p.cp("act", XTOK[:, :], ptr[:, 0:4, :].rearrange("p c t -> p (c t)"), [ptr], [XTOK])
        p.cp("act", BTOK[:, :], ptr[:, 4, :], [ptr], [BTOK])
        p.tt("dve", a_, dts[:, :], A_t[:, :], ALU.mult, [dts, A_t], [sm])
        p.mm(B1[:, 0:8], triu[:, :], a_, True, True, [triu, sm], [B1])
        p.mm(B1[:, 128:136], ones[:, :], a_, True, True, [ones, sm], [B1])
        p.cp("dve", acs, B1[:, 0:8], [B1], [sm])
        p.cp("dve", tot, B1[:, 128:136], [B1], [sm])
        if SSUB < 3:
            return
        p.mm(B2[:, 0:128], XC[:, 4, :], XC[:, 5, :], True, True, [XC], [B2])
        p.tt("dve", CBT[:, :], B2[:, 0:128], triu[:, :], ALU.mult, [B2, triu], [CBT])
        p.cp("act", Hb[:, :], H[:, :], [H], [Hb])
        for hd in range(8):
            wt = WT[hd % 2]
            p.ts("dve", Rm[:, :], triu[:, :], a_[:, hd:hd + 1], None, ALU.mult, None, [triu, sm], [Rm])
            p.mm(B3[:, 0:128], ones[:, :], Rm[:, :], True, True, [ones, Rm], [B3])
            p.ts("dve", Lm[:, :], B3[:, 0:128], acs[:, hd:hd + 1], 0.0, ALU.subtract, ALU.min, [B3, sm], [Lm])
            p.act(Lm[:, :], Lm[:, :], AF.Exp, [Lm], [Lm])
            p.stt("dve", wt[:, :], Lm[:, :], dts[:, hd:hd + 1], CBT[:, :], ALU.mult, ALU.mult, [Lm, dts, CBT], [wt])
            p.mm(B4[:, hd * 64:(hd + 1) * 64], wt[:, :], XTOK[:, hd * 64:(hd + 1) * 64], True, True, [wt, XTOK], [B4])
        if SSUB < 4:
            return
        p.mm(B5[:, :], XC[:, 5, :], Hb[:, :], True, True, [XC, Hb], [B5])
        p.act(eacs, acs, AF.Exp, [sm], [sm])
        v3 = lambda ap: ap.rearrange("p (h d) -> p h d", h=8)
        bc = lambda ap: ap.rearrange("p (h o) -> p h o", o=1).broadcast_to([128, 8, 64])
        p.tt("dve", v3(ytmp[:, :]), v3(B5[:, :]), bc(eacs), ALU.mult, [B5, sm], [ytmp])
        p.tt("dve", ysb[:, :], ytmp[:, :], B4[:, :], ALU.add, [ytmp, B4], [ysb])
        p.tt("dve", v3(ytmp[:, :]), v3(XTOK[:, :]), bc(dsk_t[:, :]), ALU.mult, [XTOK, dsk_t], [ytmp])
        p.tt("dve", ysb[:, :], ysb[:, :], ytmp[:, :], ALU.add, [ysb, ytmp], [ysb])
        p.dma("sp", yssd[j * 128:(j + 1) * 128, :], ysb[:, :], [ysb], ())
        if SSUB < 5:
            return
        p.tt("dve", wend, tot, acs, ALU.subtract, [sm], [sm])
        p.act(wend, wend, AF.Exp, [sm], [sm])
        p.tt("dve", wend, wend, dts[:, :], ALU.mult, [sm, dts], [sm])
        p.act(dec, tot, AF.Exp, [sm], [sm])
        p.tt("dve", v3(XW[:, :]), v3(XTOK[:, :]), bc(wend), ALU.mult, [XTOK, sm], [XW])
        p.mm(B6[:, :], BTOK[:, :], XW[:, :], True, True, [BTOK, XW], [B6])
        p.tt("dve", v3(H[:, :]), v3(H[:, :]), bc(dec), ALU.mult, [H, sm], [H])
        p.tt("dve", H[:, :], H[:, :], B6[:, :], ALU.add, [H, B6], [H])

    for i in range(nt + 1):
        if i < nt:
            project(i)
        if i >= 1:
            ssd_chunk(i - 1)

    if do_s5:
        LAM = p.sb([128, 24], F32, "LAM")
        dsc = p.sb([128, 8, 16], F32, "dsc")
        iota = p.sb([128, 129], F32, "iota")
        ang = p.sb([128, 129], F32, "ang")
        mag = p.sb([128, 129], F32, "mag")
        cs_ = p.sb([128, 129], F32, "cs_")
        sn_ = p.sb([128, 129], F32, "sn_")
        EP = p.sb([128, 2, 512], F32, "EP")
        EN = p.sb([128, 2, 512], F32, "EN")
        m01 = p.sb([128, 512], F32, "m01")
        blr = p.sb([128, 128], BF16, "blr")
        bli = p.sb([128, 128], BF16, "bli")
        clr = p.sb([128, 32], BF16, "clr")
        cli = p.sb([128, 32], BF16, "cli")
        dl = p.sb([128, 32], BF16, "dl")
        xr = p.sb([128, 512], F32, "xr")
        xi = p.sb([128, 512], F32, "xi")
        q1 = p.sb([128, 512], F32, "q1")
        q2 = p.sb([128, 512], F32, "q2")
        wr_ = p.sb([128, 512], BF16, "wr_")
        wi_ = p.sb([128, 512], BF16, "wi_")
        gst = p.sb([128, 8], F32, "gst")
        yo = p.sb([32, 512], F32, "yo")
        twopi = p.sb([128, 129], F32, "twopi")
        kint = p.sb([128, 129], mybir.dt.int32, "kint")
        p.dma("sp", LAM[:, :], lam[:, :], (), [LAM])
        p.dma("sp", iota[:, :], iotad[:, :], (), [iota])
        p.dma("sp", m01[:, :], m01d[:, :], (), [m01])
        nblk = (nt * 128 + 511) // 512
        for pr in range(8):
            lr, li, ls = LAM[:, pr:pr + 1], LAM[:, 8 + pr:9 + pr], LAM[:, 16 + pr:17 + pr]
            sc = lambda k: dsc[:, pr, k:k + 1]
            R, W_ = [LAM, dsc, iota, ang, mag, cs_, sn_], [dsc]
            step, lrs, th, den, cr, ci, t0_, t1_ = (sc(k) for k in range(8))
            p.act(step, ls, AF.Exp, [LAM], [dsc])
            p.tt("dve", lrs, lr, step, ALU.mult, [LAM, dsc], [dsc])
            p.tt("dve", th, li, step, ALU.mult, [LAM, dsc], [dsc])
            p.ts("dve", ang[:, :], iota[:, :], th, None, ALU.mult, None, [iota, dsc], [ang])
            p.ts("dve", cs_[:, :], ang[:, :], 1.5 * np.pi, None, ALU.add, None, [ang], [cs_])
            p.ts("dve", twopi[:, :], cs_[:, :], 1.0 / TWO_PI, None, ALU.mult, None, [cs_], [twopi])
            p.cp("dve", kint[:, :], twopi[:, :], [twopi], [kint])
            p.cp("dve", twopi[:, :], kint[:, :], [kint], [twopi])
            p.stt("dve", cs_[:, :], twopi[:, :], -TWO_PI, cs_[:, :], ALU.mult, ALU.add, [twopi, cs_], [cs_])
            p.ts("dve", twopi[:, :], cs_[:, :], 0.0, None, ALU.is_lt, None, [cs_], [twopi])
            p.stt("dve", cs_[:, :], twopi[:, :], TWO_PI, cs_[:, :], ALU.mult, ALU.add, [twopi, cs_], [cs_])
            p.ts("dve", sn_[:, :], ang[:, :], np.pi, None, ALU.add, None, [ang], [sn_])
            p.ts("dve", twopi[:, :], sn_[:, :], 1.0 / TWO_PI, None, ALU.mult, None, [sn_], [twopi])
            p.cp("dve", kint[:, :], twopi[:, :], [twopi], [kint])
            p.cp("dve", twopi[:, :], kint[:, :], [kint], [twopi])
            p.stt("dve", sn_[:, :], twopi[:, :], -TWO_PI, sn_[:, :], ALU.mult, ALU.add, [twopi, sn_], [sn_])
            p.ts("dve", twopi[:, :], sn_[:, :], 0.0, None, ALU.is_lt, None, [sn_], [twopi])
            p.stt("dve", sn_[:, :], twopi[:, :], TWO_PI, sn_[:, :], ALU.mult, ALU.add, [twopi, sn_], [sn_])
            p.act(cs_[:, :], cs_[:, :], AF.Sin, [cs_], [cs_], bias=-np.pi)
            p.act(sn_[:, :], sn_[:, :], AF.Sin, [sn_], [sn_], bias=-np.pi)
            p.act(mag[:, :], iota[:, :], AF.Exp, [iota, dsc], [mag], scale=lrs)
            ar, ai, a128r, a128i = (sc(k) for k in range(8, 12))
            p.tt("dve", ar, mag[:, 1:2], cs_[:, 1:2], ALU.mult, [mag, cs_], [dsc])
            p.tt("dve", ai, mag[:, 1:2], sn_[:, 1:2], ALU.mult, [mag, sn_], [dsc])
            p.tt("dve", a128r, mag[:, 128:129], cs_[:, 128:129], ALU.mult, [mag, cs_], [dsc])
            p.tt("dve", a128i, mag[:, 128:129], sn_[:, 128:129], ALU.mult, [mag, sn_], [dsc])
            p.tt("dve", den, lr, lr, ALU.mult, [LAM], [dsc])
            p.stt("dve", den, li, li, den, ALU.mult, ALU.add, [LAM, dsc], [dsc])
            p.op("dve", lambda g, den=den: g.reciprocal(den, den), [dsc], [dsc])
            p.ts("dve", t0_, ar, -1.0, None, ALU.add, None, [dsc], [dsc])
            p.tt("dve", t1_, ai, li, ALU.mult, [dsc, LAM], [dsc])
            p.stt("dve", cr, t0_, lr, t1_, ALU.mult, ALU.add, [dsc, LAM], [dsc])
            p.tt("dve", cr, cr, den, ALU.mult, [dsc], [dsc])
            p.tt("dve", t1_, t0_, li, ALU.mult, [dsc, LAM], [dsc])
            p.stt("dve", ci, ai, lr, t1_, ALU.mult, ALU.subtract, [dsc, LAM], [dsc])
            p.tt("dve", ci, ci, den, ALU.mult, [dsc], [dsc])
            p.tt("dve", EP[:, 0, 0:128], mag[:, 0:128], cs_[:, 0:128], ALU.mult, [mag, cs_], [EP])
            p.tt("dve", EP[:, 1, 0:128], mag[:, 0:128], sn_[:, 0:128], ALU.mult, [mag, sn_], [EP])
            p.op("dve", lambda g: g.reciprocal(mag[:, :], mag[:, :]), [mag], [mag])
            p.tt("dve", cs_[:, :], cs_[:, :], mag[:, :], ALU.mult, [cs_, mag], [cs_])
            p.tt("dve", sn_[:, :], sn_[:, :], mag[:, :], ALU.mult, [sn_, mag], [sn_])
            p.ts("dve", ang[:, :], sn_[:, :], ci, None, ALU.mult, None, [sn_, dsc], [ang])
            p.stt("dve", EN[:, 0, 0:128], cs_[:, 0:128], cr, ang[:, 0:128], ALU.mult, ALU.add, [cs_, dsc, ang], [EN])
            p.ts("dve", ang[:, :], sn_[:, :], cr, None, ALU.mult, None, [sn_, dsc], [ang])
            p.stt("dve", EN[:, 1, 0:128], cs_[:, 0:128], ci, ang[:, 0:128], ALU.mult, ALU.subtract, [cs_, dsc, ang], [EN])
            for rep in range(1, 4):
                p.cp("pool", EP[:, :, rep * 128:(rep + 1) * 128], EP[:, :, 0:128], [EP], [EP])
                p.cp("pool", EN[:, :, rep * 128:(rep + 1) * 128], EN[:, :, 0:128], [EN], [EN])
            p.dma("pool", blr[:, :], BLr[pr], (), [blr])
            p.dma("pool", bli[:, :], BLi[pr], (), [bli])
            p.dma("pool", clr[:, :], CLr[pr], (), [clr])
            p.dma("pool", cli[:, :], CLi[pr], (), [cli])
            p.ts("dve", cli[:, :], cli[:, :], -1.0, None, ALU.mult, None, [cli], [cli])
            p.dma("pool", dl[:, :], DL[pr], (), [dl])
            uc = pr // 4
            for b in range(nblk):
                t0 = b * 512
                n = min(512, nt * 128 - t0)
                p.mm(B0[:, 0:n], blr[:, :], UT[:, uc, t0:t0 + n], True, True, [blr, UT], [B0])
                p.mm(B1[:, 0:n], bli[:, :], UT[:, uc, t0:t0 + n], True, True, [bli, UT], [B1])
                p.cp("act", xr[:, 0:n], B0[:, 0:n], [B0], [xr])
                p.cp("act", xi[:, 0:n], B1[:, 0:n], [B1], [xi])
                p.tt("dve", q1[:, 0:n], xr[:, 0:n], EN[:, 0, 0:n], ALU.mult, [xr, EN], [q1])
                p.tt("pool", q2[:, 0:n], xi[:, 0:n], EN[:, 1, 0:n], ALU.mult, [xi, EN], [q2])
                p.tt("dve", q1[:, 0:n], q1[:, 0:n], q2[:, 0:n], ALU.subtract, [q1, q2], [q1])
                p.op("dve", lambda g, t0=t0, n=n: g.tensor_tensor_scan(Z[:, 0, t0:t0 + n], m01[:, 0:n], q1[:, 0:n], 0.0,
                                                                        ALU.mult, ALU.add), [m01, q1], [Z])
                p.tt("pool", q2[:, 0:n], xi[:, 0:n], EN[:, 0, 0:n], ALU.mult, [xi, EN], [q2])
                p.tt("dve", q1[:, 0:n], xr[:, 0:n], EN[:, 1, 0:n], ALU.mult, [xr, EN], [q1])
                p.tt("dve", q1[:, 0:n], q1[:, 0:n], q2[:, 0:n], ALU.add, [q1, q2], [q1])
                p.op("dve", lambda g, t0=t0, n=n: g.tensor_tensor_scan(Z[:, 1, t0:t0 + n], m01[:, 0:n], q1[:, 0:n], 0.0,
                                                                        ALU.mult, ALU.add), [m01, q1], [Z])
            gr, gi_, tq = gst[:, 0:1], gst[:, 1:2], gst[:, 2:3]
            p.memset("dve", gst[:, :], 0.0, [gst])
            for c in range(nt):
                zr, zi = Z[:, 0, c * 128:(c + 1) * 128], Z[:, 1, c * 128:(c + 1) * 128]
                if c > 0:
                    p.ts("dve", zr, zr, gr, None, ALU.add, None, [Z, gst], [Z])
                    p.ts("dve", zi, zi, gi_, None, ALU.add, None, [Z, gst], [Z])
                if c < nt - 1:
                    lr_, li_ = Z[:, 0, c * 128 + 127:c * 128 + 128], Z[:, 1, c * 128 + 127:c * 128 + 128]
                    p.tt("dve", tq, li_, a128i, ALU.mult, [Z, dsc], [gst])
                    p.stt("dve", gr, lr_, a128r, tq, ALU.mult, ALU.subtract, [Z, dsc, gst], [gst])
                    p.tt("dve", tq, lr_, a128i, ALU.mult, [Z, dsc], [gst])
                    p.stt("dve", gi_, li_, a128r, tq, ALU.mult, ALU.add, [Z, dsc, gst], [gst])
            for b in range(nblk):
                t0 = b * 512
                n = min(512, nt * 128 - t0)
                p.tt("dve", q1[:, 0:n], Z[:, 0, t0:t0 + n], EP[:, 0, 0:n], ALU.mult, [Z, EP], [q1])
                p.tt("pool", q2[:, 0:n], Z[:, 1, t0:t0 + n], EP[:, 1, 0:n], ALU.mult, [Z, EP], [q2])
                p.tt("dve", wr_[:, 0:n], q1[:, 0:n], q2[:, 0:n], ALU.subtract, [q1, q2], [wr_])
                p.tt("pool", q2[:, 0:n], Z[:, 1, t0:t0 + n], EP[:, 0, 0:n], ALU.mult, [Z, EP], [q2])
                p.tt("dve", q1[:, 0:n], Z[:, 0, t0:t0 + n], EP[:, 1, 0:n], ALU.mult, [Z, EP], [q1])
                p.tt("dve", wi_[:, 0:n], q1[:, 0:n], q2[:, 0:n], ALU.add, [q1, q2], [wi_])
                p.mm(B2[0:32, 0:n], clr[:, :], wr_[:, 0:n], True, False, [clr, wr_], [B2])
                p.mm(B2[0:32, 0:n], cli[:, :], wi_[:, 0:n], False, False, [cli, wi_], [B2])
                p.mm(B2[0:32, 0:n], dl[:, :], UT[:, uc, t0:t0 + n], False, True, [dl, UT], [B2])
                p.cp("act", yo[:, 0:n], B2[0:32, 0:n], [B2], [yo])
                p.dma("sp", ys5[pr * 32:(pr + 1) * 32, t0:t0 + n], yo[:, 0:n], [yo], ())
    p.emit()
    return nc


def ssm_inputs(inp, xseq, b, dr, hf, nt=TS):
    w = inp["ssm_w_in"][0]
    cols = np.concatenate([np.arange(1024 + 512 * hf, 1024 + 512 * hf + 512), np.arange(2048 + 128 * hf, 2048 + 128 * hf + 128),
                           np.arange(2304 + 128 * hf, 2304 + 128 * hf + 128), np.arange(2592 + 256 * hf, 2592 + 256 * hf + 256),
                           np.arange(2560 + 16 * dr + 8 * hf, 2560 + 16 * dr + 8 * hf + 8)])
    cch = np.concatenate([np.arange(512 * hf, 512 * hf + 512), np.arange(1024 + 128 * hf, 1024 + 128 * hf + 128),
                          np.arange(1280 + 128 * hf, 1280 + 128 * hf + 128)])
    cwf = inp["ssd_conv_w"][0][:, cch]
    if dr == 1:
        cwf = cwf[::-1]
    cw = np.ascontiguousarray(cwf.T.reshape(6, 128, 3).transpose(1, 0, 2).reshape(128, 18))
    cb = np.ascontiguousarray(inp["ssd_conv_b"][0][cch].reshape(6, 128).T)
    hs = slice(8 * hf, 8 * hf + 8)
    zero8 = np.zeros((1, 8), np.float32)
    gs = 16 * hf
    lam = np.zeros((128, 24), np.float32)
    BLr = np.zeros((8, 128, 128), np.float32)
    BLi = np.zeros((8, 128, 128), np.float32)
    CLr = np.zeros((8, 128, 32), np.float32)
    CLi = np.zeros((8, 128, 32), np.float32)
    DLm = np.zeros((8, 128, 32), np.float32)
    sd = inp["s5_d"][0]
    for pr in range(8):
        for k in range(2):
            g = gs + 2 * pr + k
            rows = slice(64 * k, 64 * k + 64)
            lam[rows, pr] = inp["s5_lambda_re"][0, dr, g]
            lam[rows, 8 + pr] = inp["s5_lambda_im"][0, dr, g]
            lam[rows, 16 + pr] = inp["s5_log_step"][0, dr, g]
            ur = 32 * (pr % 4) + 16 * k
            BLr[pr, ur:ur + 16, rows] = inp["s5_b_re"][0, dr, g].T
            BLi[pr, ur:ur + 16, rows] = inp["s5_b_im"][0, dr, g].T
            CLr[pr, rows, 16 * k:16 * k + 16] = inp["s5_c_re"][0, dr, g].T
            CLi[pr, rows, 16 * k:16 * k + 16] = inp["s5_c_im"][0, dr, g].T
            if dr == 0:
                for c in range(16):
                    DLm[pr, ur + c, 16 * k + c] = sd[g * 16 + c]
    m01 = np.ones((128, 512), np.float32)
    m01[:, ::128] = 0.0
    cin = np.stack([inp["c"][b].reshape(8, 128).T, inp["c_ctx"].reshape(8, 128).T], axis=1)
    return {
        "xs": np.ascontiguousarray(xseq, dtype=np.float32), "cin": np.ascontiguousarray(cin, dtype=np.float32),
        "modw": np.ascontiguousarray(inp["mod_w"][1][:, 0:2 * D]), "modb": np.ascontiguousarray(inp["mod_b"][1][None, 0:2 * D]),
        "nrm": np.ascontiguousarray(inp["norm_mix"][1][None, :]),
        "wsel": np.ascontiguousarray(np.concatenate([w[:, cols], np.zeros((D, WS - 1032), np.float32)], axis=1)), "cw": cw, "cbias": cb,
        "dtb": np.ascontiguousarray(inp["ssd_dt_bias"][0, dr, hs][None, :]),
        "alog": np.ascontiguousarray(inp["ssd_a_log"][0, dr, hs][None, :]),
        "dsk": np.ascontiguousarray(inp["ssd_d"][0, hs][None, :]) if dr == 0 else zero8,
        "identd": np.eye(128, dtype=np.float32), "triud": np.triu(np.ones((128, 128), np.float32)),
        "iotad": np.tile(np.arange(129, dtype=np.float32)[None, :], (128, 1)), "m01d": m01,
        "lam": lam, "BLr": BLr, "BLi": BLi, "CLr": CLr, "CLi": CLi, "DL": DLm,
    }


NTT = 16
GELU_C = 1.5957691216057308


def build_fin():
    nc = bass.Bass("TRN2", target_bir_lowering=False)
    dt_in = lambda n, s: nc.dram_tensor(n, s, F32, kind="ExternalInput").ap()
    x = dt_in("x", [NTT * 128, D])
    cin = dt_in("cin", [128, 2, 8])
    modw = dt_in("modw", [D, 3 * D])
    modb = dt_in("modb", [1, 3 * D])
    nrm = dt_in("nrm", [1, D])
    wz = dt_in("wz", [D, D])
    yf = dt_in("yf", [NTT * 128, D])
    yb = dt_in("yb", [NTT * 128, D])
    vf = dt_in("vf", [NTT * 128, 512])
    vb = dt_in("vb", [NTT * 128, 512])
    snorm = dt_in("snorm", [1, D])
    gluw = dt_in("gluw", [512, 512])
    glub = dt_in("glub", [1, 512])
    wout = dt_in("wout", [1536, D])
    identd = dt_in("identd", [128, 128])
    xo = nc.dram_tensor("xo", [NTT * 128, D], F32, kind="ExternalOutput").ap()

    p = Prog(nc)
    wzb = p.sb([128, 8, D], BF16, "wzb")
    woutb = p.sb([128, 12, D], BF16, "woutb")
    gwb = p.sb([128, 4, 512], BF16, "gwb")
    MB = [p.sb([128, D], F32, f"mb{v}") for v in range(3)]
    xt = p.sb([128, D], F32, "xt")
    hb = p.sb([128, D], BF16, "hb")
    hTi = p.sb([128, 8, 128], BF16, "hTi")
    stage = p.sb([128, 8, 128], F32, "stage")
    bstage = p.sb([128, 128], F32, "bstage")
    cbc = p.sb([128, 8, 128], F32, "cbc")
    ones = p.sb([128, 128], F32, "ones")
    identf = p.sb([128, 128], F32, "identf")
    ident = p.sb([128, 128], BF16, "ident")
    cin_t = p.sb([128, 2, 8], F32, "cin_t")
    small = {"ss": p.sb([128, 4], F32, "ss"), "rs": p.sb([128, 4], F32, "rs"), "junk": p.sb([128, D], F32, "junk")}
    sn_bc = p.sb([128, D], F32, "sn_bc")
    gb_bc = p.sb([128, 512], F32, "gb_bc")
    zs = p.sb([128, D], F32, "zs")
    ya = p.sb([128, D], F32, "ya")
    ybt = p.sb([128, D], F32, "ybt")
    va = p.sb([128, 512], F32, "va")
    vbt = p.sb([128, 512], F32, "vbt")
    v2 = p.sb([128, 512], F32, "v2")
    gvb = p.sb([128, 512], BF16, "gvb")
    cat = p.sb([128, 1536], BF16, "cat")
    gT = p.sb([128, 4, 128], BF16, "gT")
    cT = p.sb([128, 12, 128], BF16, "cT")
    st2 = p.sb([128, 4], F32, "st2")
    Z0, Z1, G, O0, O1, B0 = [p.ps([128, 512], F32, f"pb{i}") for i in range(6)]
    ptr = p.ps([128, 8, 128], BF16, "ptr")

    p.dma("sp", identf[:, :], identd[:, :], (), [identf])
    p.cp("dve", ident[:, :], identf[:, :], [identf], [ident])
    p.memset("pool", ones[:, :], 1.0, [ones])
    p.dma("pool", wzb[:, :, :], wz.rearrange("(kc p) n -> p kc n", p=128), (), [wzb])
    p.dma("pool", gwb[:, :, :], gluw.rearrange("(kc p) n -> p kc n", p=128), (), [gwb])
    p.dma("pool", woutb[:, :, :], wout.rearrange("(kc p) n -> p kc n", p=128), (), [woutb])
    p.dma("sp", cin_t[:, :, :], cin[:, :, :], (), [cin_t])
    p.act(cin_t[:, :, :], cin_t[:, :, :], AF.Silu, [cin_t], [cin_t])
    p.dma("sp", xt[:, :], bcast_rows(nrm[0:1, :]), (), [xt])
    p.dma("sp", sn_bc[:, :], bcast_rows(snorm[0:1, :]), (), [sn_bc])
    p.dma("sp", gb_bc[:, :], bcast_rows(glub[0:1, :]), (), [gb_bc])

    def outsel(j):
        v, c = divmod(j, 8)
        t = MB[v]
        return t, t[:, c * 128:(c + 1) * 128]
    emit_mod_rows(p, cin_t, modw, modb, 3 * D, [outsel, outsel], B0, stage, cbc, ones, bstage, whichs=(0,))
    A = MB[1]
    p.stt("dve", A[:, :], A[:, :], 1.0, xt[:, :], ALU.add, ALU.mult, [A, xt], [A])

    for i in range(NTT):
        rows = slice(i * 128, (i + 1) * 128)
        p.dma("sp", xt[:, :], x[rows, :], (), [xt])
        emit_adanorm_T(p, xt, MB[1], MB[0], hb, ptr, hTi[:, :, :], hTi, ident, small)
        for half, Zp in enumerate((Z0, Z1)):
            for kc in range(8):
                p.mm(Zp[:, :], hTi[:, kc, :], wzb[:, kc, half * 512:(half + 1) * 512], kc == 0, kc == 7, [hTi, wzb], [Zp])
            p.act(zs[:, half * 512:(half + 1) * 512], Zp[:, :], AF.Silu, [Zp], [zs])
        p.dma("sp", ya[:, :], yf[rows, :], (), [ya])
        p.dma("sp", ybt[:, :], yb[rows, :], (), [ybt])
        p.tt("pool", ya[:, :], ya[:, :], ybt[:, :], ALU.add, [ya, ybt], [ya])
        p.tt("dve", ya[:, :], ya[:, :], zs[:, :], ALU.mult, [ya, zs], [ya])
        j = small["junk"]
        p.memset("dve", st2[:, 0:1], 0.0, [st2])
        p.act(j[:, :], ya[:, :], AF.Square, [ya, st2], [j, st2], accum_out=st2[:, 0:1])
        p.ts("dve", st2[:, 1:2], st2[:, 0:1], 1.0 / D, EPS, ALU.mult, ALU.add, [st2], [st2])
        p.op("act", lambda g: g.sqrt(st2[:, 2:3], st2[:, 1:2]), [st2], [st2])
        p.op("dve", lambda g: g.reciprocal(st2[:, 3:4], st2[:, 2:3]), [st2], [st2])
        p.stt("dve", cat[:, 0:1024], ya[:, :], st2[:, 3:4], sn_bc[:, :], ALU.mult, ALU.mult, [ya, st2, sn_bc], [cat])
        p.dma("sp", va[:, :], vf[rows, :], (), [va])
        p.dma("sp", vbt[:, :], vb[rows, :], (), [vbt])
        p.tt("pool", va[:, :], va[:, :], vbt[:, :], ALU.add, [va, vbt], [va])
        p.tt("dve", v2[:, :], va[:, :], va[:, :], ALU.mult, [va], [v2])
        p.ts("dve", v2[:, :], v2[:, :], 0.044715, 1.0, ALU.mult, ALU.add, [v2], [v2])
        p.tt("dve", v2[:, :], v2[:, :], va[:, :], ALU.mult, [v2, va], [v2])
        p.act(v2[:, :], v2[:, :], AF.Sigmoid, [v2], [v2], scale=GELU_C)
        p.tt("dve", va[:, :], va[:, :], v2[:, :], ALU.mult, [va, v2], [va])
        p.cp("dve", gvb[:, :], va[:, :], [va], [gvb])
        for c in range(4):
            p.tr(ptr[:, c, :], gvb[:, c * 128:(c + 1) * 128], ident[:, :], [gvb, ident], [ptr])
        p.cp("act", gT[:, :, :], ptr[:, 0:4, :], [ptr], [gT])
        for c in range(4):
            p.mm(G[:, :], gT[:, c, :], gwb[:, c, :], c == 0, c == 3, [gT, gwb], [G])
        p.tt("dve", v2[:, :], G[:, :], gb_bc[:, :], ALU.add, [G, gb_bc], [v2])
        p.act(v2[:, :], v2[:, :], AF.Sigmoid, [v2], [v2])
        p.tt("dve", cat[:, 1024:1536], va[:, :], v2[:, :], ALU.mult, [va, v2], [cat])
        for c in range(8):
            p.tr(ptr[:, c, :], cat[:, c * 128:(c + 1) * 128], ident[:, :], [cat, ident], [ptr])
        p.cp("act", cT[:, 0:8, :], ptr[:, :, :], [ptr], [cT])
        for c in range(4):
            p.tr(ptr[:, c, :], cat[:, 1024 + c * 128:1024 + (c + 1) * 128], ident[:, :], [cat, ident], [ptr])
        p.cp("act", cT[:, 8:12, :], ptr[:, 0:4, :], [ptr], [cT])
        for half, Op in enumerate((O0, O1)):
            for c in range(12):
                p.mm(Op[:, :], cT[:, c, :], woutb[:, c, half * 512:(half + 1) * 512], c == 0, c == 11, [cT, woutb], [Op])
        for half, Op in enumerate((O0, O1)):
            sl = slice(half * 512, (half + 1) * 512)
            p.tt("dve", j[:, sl], Op[:, :], MB[2][:, sl], ALU.mult, [Op, MB[2]], [j])
        p.tt("dve", j[:, :], j[:, :], xt[:, :], ALU.add, [j, xt], [j])
        p.dma("sp", xo[rows, :], j[:, :], [j], ())
    p.emit()
    return nc


def fin_inputs(inp, b, xcore, yfc, ybc, vfc, vbc):
    cin = np.stack([inp["c"][b].reshape(8, 128).T, inp["c_ctx"].reshape(8, 128).T], axis=1)
    ca = lambda a: np.ascontiguousarray(a, dtype=np.float32)
    return {
        "x": ca(xcore), "cin": ca(cin), "modw": ca(inp["mod_w"][1][:, 0:3 * D]), "modb": ca(inp["mod_b"][1][None, 0:3 * D]),
        "nrm": ca(inp["norm_mix"][1][None, :]), "wz": ca(inp["ssm_w_in"][0][:, 0:1024]),
        "yf": ca(yfc), "yb": ca(ybc), "vf": ca(vfc), "vb": ca(vbc),
        "snorm": ca(inp["ssd_norm"][0][None, :]), "gluw": ca(inp["s5_glu_w"][0]), "glub": ca(inp["s5_glu_b"][0][None, :]),
        "wout": ca(inp["ssm_w_out"][0]), "identd": np.eye(128, dtype=np.float32),
    }


_CACHE = {}


def _prog(name, fn):
    if name not in _CACHE:
        _CACHE[name] = fn()
    return _CACHE[name]


def kernel(**inputs):
    inp = {k: np.asarray(v) for k, v in inputs.items()}
    C8 = list(range(8))
    xl = np.empty((2, 8192, D), np.float32)
    xc = np.empty((2, 256, D), np.float32)
    for half in range(2):
        vcs = [(b, s) for b in range(2) for s in range(4 * half, 4 * half + 4)]
        res = run_bass_kernel_spmd(build_attn(), [attn_inputs(inp, b, s) for b, s in vcs], core_ids=C8)
        for (b, s), r in zip(vcs, res.results):
            xl[b, s * 1024:(s + 1) * 1024] = r["xo"][:1024]
            if s == 0:
                xc[b] = r["xo"][1024:]

    def moe(L, xl_in, xc_in):
        cores = [(b, q) for b in range(2) for q in range(4)]
        ims = [moe_inputs(np.concatenate([xl_in[b, q * 2048:(q + 1) * 2048], xc_in[b]], axis=0), inp["c"][b], inp["c_ctx"],
                          inp["mod_w"][L], inp["mod_b"][L], inp["norm_ffn"][L], inp["moe_w_group"][L], inp["moe_b_group"][L],
                          inp["moe_w_expert"][L], inp["moe_b_expert"][L], inp["moe_w13"][L], inp["moe_w2"][L])
               for b, q in cores]
        res = run_bass_kernel_spmd(build_moe(), ims, core_ids=C8)
        xo = np.empty_like(xl_in)
        xco = np.empty_like(xc_in)
        for (b, q), r in zip(cores, res.results):
            xo[b, q * 2048:(q + 1) * 2048] = r["xo"][:2048]
            if q == 0:
                xco[b] = r["xo"][2048:]
        return xo, xco

    xl, xc = moe(0, xl, xc)
    cores = [(b, dr, hf) for b in range(2) for dr in range(2) for hf in range(2)]
    ims = []
    for b, dr, hf in cores:
        seq = np.concatenate([xc[b], xl[b]], axis=0) if dr == 0 else np.concatenate([xc[b][::-1], xl[b][::-1]], axis=0)
        ims.append(ssm_inputs(inp, seq, b, dr, hf))
    res = run_bass_kernel_spmd(build_ssm(), ims, core_ids=C8)
    Y = np.empty((2, 2, 8192, D), np.float32)
    V = np.empty((2, 2, 8192, 512), np.float32)
    for (b, dr, hf), r in zip(cores, res.results):
        y = r["yssd"][256:]
        v = r["ys5"][:, 256:].T
        if dr == 1:
            y, v = y[::-1], v[::-1]
        Y[b, dr, :, hf * 512:(hf + 1) * 512] = y
        V[b, dr, :, hf * 256:(hf + 1) * 256] = v
    cores = [(b, q) for b in range(2) for q in range(4)]
    ims = []
    for b, q in cores:
        sl = slice(q * 2048, (q + 1) * 2048)
        ims.append(fin_inputs(inp, b, xl[b, sl], Y[b, 0, sl], Y[b, 1, sl], V[b, 0, sl], V[b, 1, sl]))
    res = run_bass_kernel_spmd(build_fin(), ims, core_ids=C8)
    for (b, q), r in zip(cores, res.results):
        xl[b, q * 2048:(q + 1) * 2048] = r["xo"]
    xl, _ = moe(1, xl, np.zeros_like(xc))
    return xl
```

```python
import os
import numpy as np
import concourse.bass as bass
import concourse.mybir as mybir
from concourse.bass_utils import run_bass_kernel_spmd

F32 = mybir.dt.float32
BF16 = mybir.dt.bfloat16
AF = mybir.ActivationFunctionType
ALU = mybir.AluOpType
AX = mybir.AxisListType

EPOCH = 12000
D = 1024
EPS = 1e-6


class Res:
    __slots__ = ("name", "w", "readers", "dsem", "dcnt")

    def __init__(self, name):
        self.name = name
        self.w = None
        self.readers = {}
        self.dsem = None
        self.dcnt = 0


class T:
    def __init__(self, h, name):
        self.h = h
        self.r = Res(name)

    def __getitem__(self, k):
        return self.h[k]


class Prog:
    def __init__(self, nc):
        self.nc = nc
        self.eng = {"pe": nc.tensor, "dve": nc.vector, "act": nc.scalar, "pool": nc.gpsimd, "sp": nc.sync}
        self.ops = {k: [] for k in self.eng}
        self.cnt = {k: 0 for k in self.eng}
        self.esems = {k: [] for k in self.eng}
        self.known = {k: {} for k in self.eng}
        self.dres = []
        self.nsem = 0
        self.nt = 0

    def sem(self, name):
        self.nsem += 1
        return self.nc.alloc_semaphore(name)

    def sb(self, shape, dt=F32, name=None):
        self.nt += 1
        name = name or f"t{self.nt}"
        return T(self.nc.alloc_sbuf_tensor(name, list(shape), dt), name)

    def ps(self, shape, dt=F32, name=None):
        self.nt += 1
        name = name or f"p{self.nt}"
        return T(self.nc.alloc_psum_tensor(name, list(shape), dt), name)

    def _esem(self, e, ep):
        while len(self.esems[e]) <= ep:
            self.esems[e].append(self.sem(f"s_{e}_{len(self.esems[e])}"))
        return self.esems[e][ep]

    def _need(self, e, ev, waits):
        sem, val, src = ev
        if src == "pe" and e == "pe":
            return
        k = id(sem)
        if self.known[e].get(k, 0) >= val:
            return
        self.known[e][k] = val
        waits.append((sem, val))

    def _deps(self, e, reads, writes):
        waits = []
        for t in reads:
            if t.r.w is not None:
                self._need(e, t.r.w, waits)
        for t in writes:
            if t.r.w is not None:
                self._need(e, t.r.w, waits)
            for ev in t.r.readers.values():
                self._need(e, ev, waits)
        return waits

    def _mark(self, ev, reads, writes):
        for t in writes:
            t.r.w = ev
            t.r.readers = {}
        for t in reads:
            if t not in writes:
                old = t.r.readers.get(id(ev[0]))
                if old is None or old[1] < ev[1]:
                    t.r.readers[id(ev[0])] = ev

    def op(self, e, fn, reads=(), writes=()):
        waits = self._deps(e, reads, writes)
        idx = self.cnt[e]
        self.cnt[e] += 1
        sem = self._esem(e, idx // EPOCH)
        ev = (sem, idx % EPOCH + 1, e)
        self._mark(ev, reads, writes)
        self.ops[e].append((waits, fn, (sem, 1)))

    def dma(self, e, out, in_, reads=(), writes=(), sres=None):
        waits = self._deps(e, reads, writes)
        t = sres or (writes[0] if writes else reads[0])
        r = t.r
        if r.dsem is None or r.dcnt + 16 > 30000:
            r.dsem = self.sem(f"d_{r.name}_{self.nsem}")
            r.dcnt = 0
            self.dres.append(r)
        r.dcnt += 16
        ev = (r.dsem, r.dcnt, "dma")
        self._mark(ev, reads, writes)
        self.ops[e].append((waits, lambda eng: eng.dma_start(out=out, in_=in_), (r.dsem, 16)))

    def finish(self):
        finals = {}
        for r in self.dres:
            finals[id(r.dsem)] = (r.dsem, max(finals.get(id(r.dsem), (None, 0))[1], r.dcnt))
        waits = []
        for sem, val in finals.values():
            if self.known["sp"].get(id(sem), 0) < val:
                waits.append((sem, val))
        for e in ("pe", "dve", "act", "pool"):
            n = self.cnt[e]
            if n:
                waits.append((self._esem(e, (n - 1) // EPOCH), (n - 1) % EPOCH + 1))
        self.ops["sp"].append((waits, None, None))

    def emit(self):
        self.finish()
        with self.nc.Block() as block:
            decos = {"sp": block.sync, "pe": block.tensor, "dve": block.vector, "act": block.scalar,
                     "pool": block.gpsimd}
            for e in ("sp", "pe", "dve", "act", "pool"):
                def body(engine, e=e):
                    for waits, fn, inc in self.ops[e]:
                        for sem, val in waits:
                            engine.wait_ge(sem, val)
                        if fn is not None:
                            fn(engine).then_inc(inc[0], inc[1])
                decos[e](body)

    def mm(self, out, lhsT, rhs, start, stop, reads, writes):
        self.op("pe", lambda g: g.matmul(out, lhsT, rhs, start=start, stop=stop), reads, writes)

    def tr(self, out, in_, ident, reads, writes):
        self.op("pe", lambda g: g.transpose(out, in_, ident), reads, writes)

    def act(self, out, in_, func, reads, writes, bias=None, scale=None, accum_out=None, e="act"):
        kw = {}
        if bias is not None:
            kw["bias"] = bias
        if scale is not None:
            kw["scale"] = scale
        if accum_out is not None:
            kw["accum_out"] = accum_out
        self.op("act", lambda g: g.activation(out, in_, func, **kw), reads, writes)

    def ts(self, e, out, in0, s1, s2, op0, op1, reads, writes):
        if op1 is None:
            self.op(e, lambda g: g.tensor_scalar(out, in0, s1, None, op0), reads, writes)
        else:
            self.op(e, lambda g: g.tensor_scalar(out, in0, s1, s2, op0, op1), reads, writes)

    def tt(self, e, out, in0, in1, op, reads, writes):
        self.op(e, lambda g: g.tensor_tensor(out, in0, in1, op), reads, writes)

    def stt(self, e, out, in0, sc, in1, op0, op1, reads, writes):
        self.op(e, lambda g: g.scalar_tensor_tensor(out, in0, sc, in1, op0, op1), reads, writes)

    def cp(self, e, out, in_, reads, writes):
        if e == "act":
            self.op(e, lambda g: g.copy(out, in_), reads, writes)
        else:
            self.op(e, lambda g: g.tensor_copy(out, in_), reads, writes)

    def memset(self, e, ap, v, writes):
        self.op(e, lambda g: g.memset(ap, v), (), writes)


def bcast_rows(ap, n=128):
    return ap.partition_broadcast(n)


def emit_mod_rows(p, cin_t, modw, modb, ncols, outs, psum, stage, cbc, ones, bstage, whichs=(0, 1)):
    nblk = ncols // 128
    for which in whichs:
        for kc in range(8):
            p.ts("dve", cbc[:, kc, :], ones[:, :], cin_t[:, which, kc:kc + 1], None, ALU.mult, None,
                 [ones, cin_t], [cbc])
        for j in range(nblk):
            p.dma("sp", stage[:, :, :], modw[:, j * 128:(j + 1) * 128].rearrange("(kc p) n -> p kc n", p=128),
                  (), [stage])
            p.dma("sp", bstage[:, :], bcast_rows(modb[0:1, j * 128:(j + 1) * 128]), (), [bstage])
            for kc in range(8):
                p.mm(psum[:, 0:128], cbc[:, kc, :], stage[:, kc, :], kc == 0, kc == 7, [cbc, stage], [psum])
            tile, ap = outs[which](j)
            p.tt("dve", ap, psum[:, 0:128], bstage[:, :], ALU.add, [psum, bstage], [tile])


def emit_adanorm_T(p, x_t, A_t, S_t, hb, ptr, hT_ap, hT_t, ident, small):
    ss, rs, junk = small["ss"], small["rs"], small["junk"]
    p.memset("dve", ss[:, 0:1], 0.0, [ss])
    p.act(junk[:, :], x_t[:, :], AF.Square, [x_t, ss], [junk, ss], accum_out=ss[:, 0:1])
    p.ts("dve", rs[:, 0:1], ss[:, 0:1], 1.0 / D, EPS, ALU.mult, ALU.add, [ss], [rs])
    p.op("act", lambda g: g.sqrt(rs[:, 1:2], rs[:, 0:1]), [rs], [rs])
    p.op("dve", lambda g: g.reciprocal(rs[:, 2:3], rs[:, 1:2]), [rs], [rs])
    p.stt("dve", junk[:, :], x_t[:, :], rs[:, 2:3], A_t[:, :], ALU.mult, ALU.mult, [x_t, rs, A_t], [junk])
    p.tt("dve", hb[:, :], junk[:, :], S_t[:, :], ALU.add, [junk, S_t], [hb])
    for kc in range(8):
        p.tr(ptr[:, kc, :], hb[:, kc * 128:(kc + 1) * 128], ident[:, :], [hb, ident], [ptr])
    p.cp("act", hT_ap, ptr[:, :, :], [ptr], [hT_t])


NT_MOE = 18
NLAT_MOE = 16


def build_moe(n_exp=32):
    NT = NT_MOE
    nc = bass.Bass("TRN2", target_bir_lowering=False)
    x = nc.dram_tensor("x", [NT * 128, D], F32, kind="ExternalInput").ap()
    cin = nc.dram_tensor("cin", [128, 2, 8], F32, kind="ExternalInput").ap()
    modw = nc.dram_tensor("modw", [D, 3 * D], F32, kind="ExternalInput").ap()
    modb = nc.dram_tensor("modb", [1, 3 * D], F32, kind="ExternalInput").ap()
    nrm = nc.dram_tensor("nrm", [1, D], F32, kind="ExternalInput").ap()
    wr = nc.dram_tensor("wr", [D, 36], F32, kind="ExternalInput").ap()
    br = nc.dram_tensor("br", [1, 36], F32, kind="ExternalInput").ap()
    w13 = nc.dram_tensor("w13", [32, D, D], F32, kind="ExternalInput").ap()
    w2 = nc.dram_tensor("w2", [32, 512, D], F32, kind="ExternalInput").ap()
    identd = nc.dram_tensor("identd", [128, 128], F32, kind="ExternalInput").ap()
    xo = nc.dram_tensor("xo", [NT * 128, D], F32, kind="ExternalOutput").ap()

    p = Prog(nc)
    hT = p.sb([128, 8, NT * 128], BF16, "hT")
    acc = p.sb([128, NT, D], F32, "acc")
    w13b = [p.sb([128, 8, D], BF16, f"w13b{i}") for i in range(2)]
    w2b = [p.sb([128, 4, D], BF16, "w2b0")]
    MB = [[p.sb([128, D], F32, f"mb{w}{v}") for v in range(3)] for w in range(2)]
    xt = p.sb([128, D], F32, "xt")
    hb = p.sb([128, D], BF16, "hb")
    stage = p.sb([128, 8, 128], F32, "stage")
    bstage = p.sb([128, 128], F32, "bstage")
    actT = [p.sb([128, 4, 512], BF16, f"actT{i}") for i in range(2)]
    sa = p.sb([128, 512], F32, "sa")
    cbc = p.sb([128, 8, 128], F32, "cbc")
    CW = p.sb([128, NT, 32], F32, "CW")
    ones = p.sb([128, 128], F32, "ones")
    identf = p.sb([128, 128], F32, "identf")
    ident = p.sb([128, 128], BF16, "ident")
    cin_t = p.sb([128, 2, 8], F32, "cin_t")
    nrm_t = xt
    wrf = p.sb([128, 8, 36], F32, "wrf")
    wrb = p.sb([128, 8, 36], BF16, "wrb")
    brb = p.sb([128, 36], F32, "brb")
    small = {"ss": p.sb([128, 4], F32, "ss"), "rs": p.sb([128, 4], F32, "rs"), "junk": p.sb([128, D], F32, "junk")}
    rt = p.sb([128, 256], F32, "rt")
    pbank = [p.ps([128, 512], F32, f"pb{i}") for i in range(6)]
    ptr = p.ps([128, 8, 128], BF16, "ptr")

    p.dma("sp", identf[:, :], identd[:, :], (), [identf])
    p.cp("dve", ident[:, :], identf[:, :], [identf], [ident])
    p.memset("pool", ones[:, :], 1.0, [ones])
    p.memset("pool", acc[:, :, :], 0.0, [acc])
    p.dma("sp", cin_t[:, :, :], cin[:, :, :], (), [cin_t])
    p.act(cin_t[:, :, :], cin_t[:, :, :], AF.Silu, [cin_t], [cin_t])
    p.dma("sp", nrm_t[:, :], bcast_rows(nrm[0:1, :]), (), [nrm_t])
    p.dma("sp", wrf[:, :, :], wr.rearrange("(kc p) n -> p kc n", p=128), (), [wrf])
    p.cp("dve", wrb[:, :, :], wrf[:, :, :], [wrf], [wrb])
    p.dma("sp", brb[:, :], bcast_rows(br[0:1, :]), (), [brb])

    def outsel(which):
        def f(j):
            v, c = divmod(j, 8)
            t = MB[which][v]
            return t, t[:, c * 128:(c + 1) * 128]
        return f
    emit_mod_rows(p, cin_t, modw, modb, 3 * D, [outsel(0), outsel(1)], pbank[0], stage, cbc, ones, bstage)
    for w in range(2):
        A = MB[w][1]
        p.stt("dve", A[:, :], A[:, :], 1.0, nrm_t[:, :], ALU.add, ALU.mult, [A, nrm_t], [A])

    for i in range(NT):
        w = 0 if i < NLAT_MOE else 1
        p.dma("sp", xt[:, :], x[i * 128:(i + 1) * 128, :], (), [xt])
        emit_adanorm_T(p, xt, MB[w][1], MB[w][0], hb, ptr, hT[:, :, i * 128:(i + 1) * 128], hT, ident, small)
        lgp = pbank[1]
        for kc in range(8):
            p.mm(lgp[:, 0:36], hT[:, kc, i * 128:(i + 1) * 128], wrb[:, kc, :], kc == 0, kc == 7, [hT, wrb], [lgp])
        lg = rt[:, 0:36]
        p.tt("dve", lg, lgp[:, 0:36], brb[:, :], ALU.add, [lgp, brb], [rt])
        R, W_ = [rt], [rt]
        gmax, nb, se, m1, m2, den = (rt[:, 40 + k:41 + k] for k in range(6))
        oh = rt[:, 48:52]
        pen = rt[:, 52:56]
        m32 = rt[:, 64:96]
        e32 = rt[:, 96:128]
        m32b = rt[:, 128:160]
        sel = rt[:, 160:192]
        gex = rt[:, 192:196]
        p.op("dve", lambda g, gmax=gmax, lg=lg: g.reduce_max(gmax, lg[:, 0:4], AX.X), R, W_)
        p.ts("dve", oh, lg[:, 0:4], gmax, None, ALU.is_ge, None, R, W_)
        p.ts("dve", nb, gmax, -1.0, None, ALU.mult, None, R, W_)
        p.act(gex, lg[:, 0:4], AF.Exp, R, W_, bias=nb, accum_out=se)
        p.ts("dve", pen, oh, 1e9, -1e9, ALU.mult, ALU.add, R, W_)
        p.tt("dve", m32.rearrange("p (g e) -> p g e", g=4), lg[:, 4:36].rearrange("p (g e) -> p g e", g=4),
             pen.to_broadcast([128, 4, 8]) if False else rt[:, 52:56].rearrange("p (g o) -> p g o", o=1).broadcast_to([128, 4, 8]),
             ALU.add, R, W_)
        p.op("dve", lambda g, m1=m1, m32=m32: g.reduce_max(m1, m32, AX.X), R, W_)
        p.ts("dve", nb, m1, -1.0, None, ALU.mult, None, R, W_)
        p.act(e32, m32, AF.Exp, R, W_, bias=nb)
        p.ts("dve", m32b, m32, m1, -1e9, ALU.is_ge, ALU.mult, R, W_)
        p.tt("dve", m32b, m32b, m32, ALU.add, R, W_)
        p.op("dve", lambda g, m2=m2, m32b=m32b: g.reduce_max(m2, m32b, AX.X), R, W_)
        p.ts("dve", sel, m32, m2, None, ALU.is_ge, None, R, W_)
        p.tt("dve", sel, sel, e32, ALU.mult, R, W_)
        p.op("dve", lambda g, sel=sel, den=den: g.reduce_sum(den, sel, AX.X), R, W_)
        p.tt("dve", den, den, se, ALU.mult, R, W_)
        p.op("dve", lambda g, den=den: g.reciprocal(den, den), R, W_)
        p.ts("dve", CW[:, i, :], sel, den, None, ALU.mult, None, [rt], [CW])

    blocks = [(b * 4, 4) for b in range(NLAT_MOE // 4)] + [(NLAT_MOE, NT - NLAT_MOE)]
    pa = [pbank[0], pbank[1]]
    pbb = [pbank[2], pbank[3]]
    po = [pbank[4], pbank[5]]
    cnt_ab = 0
    cnt_o = 0
    cnt_act = 0
    for e in range(n_exp):
        wa = w13b[e % 2]
        wb = w2b[0]
        p.dma("pool", wa[:, :, :], w13[e].rearrange("(kc p) n -> p kc n", p=128), (), [wa])
        p.dma("pool", wb[:, :, :], w2[e].rearrange("(kc p) n -> p kc n", p=128), (), [wb])
        for (t0, nt) in blocks:
            ntok = nt * 128
            at = actT[cnt_act % 2]
            cnt_act += 1
            for fc in range(4):
                A_, B_ = pa[cnt_ab % 2], pbb[cnt_ab % 2]
                cnt_ab += 1
                for kc in range(8):
                    p.mm(A_[:, 0:ntok], wa[:, kc, fc * 128:(fc + 1) * 128], hT[:, kc, t0 * 128:t0 * 128 + ntok],
                         kc == 0, kc == 7, [wa, hT], [A_])
                for kc in range(8):
                    p.mm(B_[:, 0:ntok], wa[:, kc, 512 + fc * 128:512 + (fc + 1) * 128],
                         hT[:, kc, t0 * 128:t0 * 128 + ntok], kc == 0, kc == 7, [wa, hT], [B_])
                p.act(sa[:, 0:ntok], A_[:, 0:ntok], AF.Silu, [A_], [sa])
                p.tt("dve", at[:, fc, 0:ntok], sa[:, 0:ntok], B_[:, 0:ntok], ALU.mult, [sa, B_], [at])
            for tt_ in range(nt):
                ti = t0 + tt_
                for half in range(2):
                    O_ = po[cnt_o % 2]
                    cnt_o += 1
                    for fc in range(4):
                        p.mm(O_[:, :], at[:, fc, tt_ * 128:(tt_ + 1) * 128], wb[:, fc, half * 512:(half + 1) * 512],
                             fc == 0, fc == 3, [at, wb], [O_])
                    accs = acc[:, ti, half * 512:(half + 1) * 512]
                    p.stt("dve", accs, O_[:, :], CW[:, ti, e:e + 1], accs, ALU.mult, ALU.add, [O_, CW, acc], [acc])

    for i in range(NT):
        w = 0 if i < NLAT_MOE else 1
        p.dma("sp", xt[:, :], x[i * 128:(i + 1) * 128, :], (), [xt])
        j = small["junk"]
        p.tt("dve", j[:, :], acc[:, i, :], MB[w][2][:, :], ALU.mult, [acc, MB[w][2]], [j])
        p.tt("dve", j[:, :], j[:, :], xt[:, :], ALU.add, [j, xt], [j])
        p.dma("sp", xo[i * 128:(i + 1) * 128, :], j[:, :], [j], ())
    p.emit()
    return nc


def moe_inputs(x_core, c_b, c_ctx, mod_w_i, mod_b_i, norm_ffn_i, wg, bg, we, be, w13, w2):
    cin = np.stack([c_b.reshape(8, 128).T, c_ctx.reshape(8, 128).T], axis=1)
    return {
        "x": np.ascontiguousarray(x_core, dtype=np.float32),
        "cin": np.ascontiguousarray(cin, dtype=np.float32),
        "modw": np.ascontiguousarray(mod_w_i[:, 3 * D:6 * D]),
        "modb": np.ascontiguousarray(mod_b_i[None, 3 * D:6 * D]),
        "nrm": np.ascontiguousarray(norm_ffn_i[None, :]),
        "wr": np.ascontiguousarray(np.concatenate([wg, we], axis=1)),
        "br": np.ascontiguousarray(np.concatenate([bg, be])[None, :]),
        "w13": w13, "w2": w2,
        "identd": np.eye(128, dtype=np.float32),
    }


NTA = 14
NLOC = 8
WCOLS = 2432
NEG = -30000.0


def alias(p, t, name):
    a = T(t.h, name)
    a.r.w = t.r.w
    a.r.readers = dict(t.r.readers)
    return a


def build_attn(ph=9, dbg=False):
    nc = bass.Bass("TRN2", target_bir_lowering=False)
    dt_in = lambda n, s: nc.dram_tensor(n, s, F32, kind="ExternalInput").ap()
    xe = dt_in("xe", [NTA * 128, D])
    cin = dt_in("cin", [128, 2, 8])
    modw = dt_in("modw", [D, 3 * D])
    modb = dt_in("modb", [1, 3 * D])
    nrm = dt_in("nrm", [1, D])
    win = dt_in("win", [D, WCOLS])
    wout = dt_in("wout", [D, D])
    gains = dt_in("gains", [128, 4])
    ropec = dt_in("ropec", [128, 1536])
    ropes = dt_in("ropes", [128, 1536])
    pmd = dt_in("pmd", [128, 128])
    onesd = dt_in("onesd", [128, 128])
    identd = dt_in("identd", [128, 128])
    nab = dt_in("nab", [8, 128, 27 * 128])
    wam = dt_in("wam", [128, 4 * 512])
    sink = dt_in("sink", [1, 8])
    xo = nc.dram_tensor("xo", [(NLOC + 2) * 128, D], F32, kind="ExternalOutput").ap()

    p = Prog(nc)
    NTOK = NTA * 128
    big = p.sb([128, 8 * NTOK], BF16, "big")
    hTv = big[:, :].rearrange("p (kc t) -> p kc t", kc=8)
    big2 = p.sb([128, 8 * WCOLS], BF16, "big2")
    winv = big2[:, :].rearrange("p (kc n) -> p kc n", kc=8)
    QT = p.sb([128, 14, NTOK], BF16, "QT")
    VA = p.sb([128, NTA, 8, 65], BF16, "VA")
    VB = p.sb([128, NTA, 2, 65], BF16, "VB")
    PT = [p.sb([128, 8, 128], BF16, f"PT{i}") for i in range(2)]
    MB = [[p.sb([128, D], F32, f"mb{w}{v}") for v in range(3)] for w in range(2)]
    xt = p.sb([128, D], F32, "xt")
    hb = p.sb([128, D], BF16, "hb")
    stage = p.sb([128, 8, 128], F32, "stage")
    bstage = p.sb([128, 128], F32, "bstage")
    cbc = p.sb([128, 8, 128], F32, "cbc")
    ones = p.sb([128, 128], F32, "ones")
    identf = p.sb([128, 128], F32, "identf")
    ident = p.sb([128, 128], BF16, "ident")
    onesb = p.sb([128, 128], BF16, "onesb")
    pm = p.sb([128, 128], BF16, "pm")
    cin_t = p.sb([128, 2, 8], F32, "cin_t")
    G = p.sb([128, 4], F32, "G")
    RC = p.sb([128, 1536], BF16, "RC")
    RS = p.sb([128, 1536], BF16, "RS")
    WM = p.sb([128, 4, 512], BF16, "WM")
    esink = p.sb([128, 8], F32, "esink")
    small = {"ss": p.sb([128, 4], F32, "ss"), "rs": p.sb([128, 4], F32, "rs"), "junk": p.sb([128, D], F32, "junk")}
    sq = p.sb([128, 512], BF16, "sq")
    rstd = p.sb([128, 512], F32, "rstd")
    qn = p.sb([128, 512], BF16, "qn")
    t1 = p.sb([128, 512], F32, "t1")
    rec = p.sb([128, 8], F32, "rec")
    oT = p.sb([128, 8, 128], BF16, "oT")
    pbank = [p.ps([128, 512], F32, f"pb{i}") for i in range(7)]
    ptr = p.ps([128, 8, 128], BF16, "ptr")

    p.dma("sp", identf[:, :], identd[:, :], (), [identf])
    p.cp("dve", ident[:, :], identf[:, :], [identf], [ident])
    p.dma("pool", onesb[:, :], onesd[:, :], (), [onesb])
    p.dma("pool", pm[:, :], pmd[:, :], (), [pm])
    p.dma("pool", RC[:, :], ropec[:, :], (), [RC])
    p.dma("pool", RS[:, :], ropes[:, :], (), [RS])
    p.dma("pool", WM[:, :, :], wam.rearrange("p (s q) -> p s q", s=4), (), [WM])
    p.dma("pool", winv, win.rearrange("(kc p) n -> p kc n", p=128), (), [big2])
    p.memset("pool", ones[:, :], 1.0, [ones])
    p.memset("pool", VA[:, :, :, :], 1.0, [VA])
    p.memset("pool", VB[:, :, :, :], 1.0, [VB])
    p.dma("sp", cin_t[:, :, :], cin[:, :, :], (), [cin_t])
    p.act(cin_t[:, :, :], cin_t[:, :, :], AF.Silu, [cin_t], [cin_t])
    p.dma("sp", xt[:, :], bcast_rows(nrm[0:1, :]), (), [xt])
    p.dma("sp", G[:, :], gains[:, :], (), [G])
    p.ts("dve", G[:, 0:1], G[:, 0:1], 0.125, None, ALU.mult, None, [G], [G])
    p.ts("dve", G[:, 2:3], G[:, 2:3], 0.125, None, ALU.mult, None, [G], [G])
    p.dma("sp", esink[:, :], bcast_rows(sink[0:1, :]), (), [esink])
    p.act(esink[:, :], esink[:, :], AF.Exp, [esink], [esink])

    def outsel(which):
        def f(j):
            v, c = divmod(j, 8)
            t = MB[which][v]
            return t, t[:, c * 128:(c + 1) * 128]
        return f
    emit_mod_rows(p, cin_t, modw, modb, 3 * D, [outsel(0), outsel(1)], pbank[0], stage, cbc, ones, bstage)
    for w in range(2):
        A = MB[w][1]
        p.stt("dve", A[:, :], A[:, :], 1.0, xt[:, :], ALU.add, ALU.mult, [A, xt], [A])

    for i in range(NTA):
        w = 0 if i < 12 else 1
        p.dma("sp", xt[:, :], xe[i * 128:(i + 1) * 128, :], (), [xt])
        emit_adanorm_T(p, xt, MB[w][1], MB[w][0], hb, ptr, hTv[:, :, i * 128:(i + 1) * 128], big, ident, small)

    blocks = [(0, 512), (512, 512), (1024, 512), (1536, 256)]
    cntp = 0
    for ch in range(14 if ph >= 2 else 0):
        gi = 0 if ch < 4 else 1 if ch < 8 else 2 if ch < 12 else 3
        rope = ch >= 8
        for (t0, n) in blocks:
            pq = pbank[cntp % 2]
            pmm = pbank[2 + cntp % 2]
            cntp += 1
            for kc in range(8):
                p.mm(pq[:, 0:n], winv[:, kc, ch * 128:(ch + 1) * 128], hTv[:, kc, t0:t0 + n], kc == 0, kc == 7,
                     [big2, big], [pq])
            p.act(sq[:, 0:n], pq[:, 0:n], AF.Square, [pq], [sq])
            p.mm(pmm[:, 0:n], onesb[:, :], sq[:, 0:n], True, True, [onesb, sq], [pmm])
            p.ts("dve", rstd[:, 0:n], pmm[:, 0:n], 1.0 / 64, EPS, ALU.mult, ALU.add, [pmm], [rstd])
            p.op("act", lambda g, n=n: g.sqrt(rstd[:, 0:n], rstd[:, 0:n]), [rstd], [rstd])
            p.op("dve", lambda g, n=n: g.reciprocal(rstd[:, 0:n], rstd[:, 0:n]), [rstd], [rstd])
            if rope and t0 < 1536:
                p.stt("dve", qn[:, 0:n], pq[:, 0:n], G[:, gi:gi + 1], rstd[:, 0:n], ALU.mult, ALU.mult,
                      [pq, G, rstd], [qn])
                pr = pbank[4 + cntp % 2]
                p.mm(pr[:, 0:n], pm[:, :], qn[:, 0:n], True, True, [pm, qn], [pr])
                p.tt("pool", t1[:, 0:n], qn[:, 0:n], RC[:, t0:t0 + n], ALU.mult, [qn, RC], [t1])
                p.tt("dve", rstd[:, 0:n], pr[:, 0:n], RS[:, t0:t0 + n], ALU.mult, [pr, RS], [rstd])
                p.tt("dve", QT[:, ch, t0:t0 + n], t1[:, 0:n], rstd[:, 0:n], ALU.add, [t1, rstd], [QT])
            else:
                p.stt("dve", QT[:, ch, t0:t0 + n], pq[:, 0:n], G[:, gi:gi + 1], rstd[:, 0:n], ALU.mult, ALU.mult,
                      [pq, G, rstd], [QT])

    for i in range(NTA if ph >= 3 else 0):
        pv, pv2 = pbank[cntp % 2], pbank[2 + cntp % 2]
        cntp += 1
        for kc in range(8):
            p.mm(pv[:, :], hTv[:, kc, i * 128:(i + 1) * 128], winv[:, kc, 1792:2304], kc == 0, kc == 7, [big, big2], [pv])
        for kc in range(8):
            p.mm(pv2[:, 0:128], hTv[:, kc, i * 128:(i + 1) * 128], winv[:, kc, 2304:2432], kc == 0, kc == 7,
                 [big, big2], [pv2])
        p.cp("act", VA[:, i, :, 0:64], pv[:, :].rearrange("p (h d) -> p h d", h=8), [pv], [VA])
        p.cp("dve", VB[:, i, :, 0:64], pv2[:, 0:128].rearrange("p (h d) -> p h d", h=2), [pv2], [VB])

    OAt = alias(p, big, "OA")
    OA = big[:, 0:(NLOC + 2) * 1024].rearrange("p (t d) -> p t d", d=1024)
    WO = alias(p, big2, "WO")
    wov = big2[:, 0:8192].rearrange("p (kc n) -> p kc n", kc=8)
    NABt = [alias(p, big2, f"NAB{i}") for i in range(2)]
    nabv = [big2[:, 8192 + i * 3456:8192 + (i + 1) * 3456].rearrange("p (s q) -> p s q", q=128) for i in range(2)]
    print("sbuf remaining", nc.sbuf_bytes_remaining)
    PTWt = p.sb([128, 5, 512], BF16, "PTWs")
    PTW = PTWt.h
    p.dma("pool", wov, wout.rearrange("(kc p) n -> p kc n", p=128), (), [WO])

    CT = [12, 13]

    def na_unit(h, qt, ktiles, slots, nabT, nabV, ot, u):
        half = slice((h % 2) * 64, (h % 2) * 64 + 64)
        qch, kch = h // 2, 4 + h // 2
        S = [pbank[(u % 2) * 2], pbank[(u % 2) * 2 + 1]]
        O = pbank[4 + u % 2]
        pt = PT[u % 2]
        allk = [(kt, sl) for kt, sl in zip(ktiles, slots)] + [(c, None) for c in CT]
        for c, (kt, sl) in enumerate(allk):
            bank = S[c // 4]
            o = bank[:, (c % 4) * 128:(c % 4 + 1) * 128]
            p.mm(o, QT[half, kch, kt * 128:(kt + 1) * 128], QT[half, qch, qt * 128:(qt + 1) * 128], True, sl is None,
                 [QT], [bank])
            if sl is not None:
                p.mm(o, ident[:, :], nabV[:, sl, :], False, True, [ident, nabT], [bank])
        nck = len(allk)
        n0 = min(nck, 4)
        p.act(pt[:, 0:n0, :], S[0][:, 0:n0 * 128].rearrange("p (c q) -> p c q", q=128), AF.Exp, [S[0]], [pt])
        if nck > 4:
            p.act(pt[:, 4:nck, :], S[1][:, 0:(nck - 4) * 128].rearrange("p (c q) -> p c q", q=128), AF.Exp, [S[1]], [pt])
        for c, (kt, sl) in enumerate(allk):
            p.mm(O[:, 0:65], pt[:, c, :], VA[:, kt, h, :], c == 0, c == nck - 1, [pt, VA], [O])
        p.op("dve", lambda g, O=O, h=h: g.reciprocal(rec[:, h:h + 1], O[:, 64:65]), [O], [rec])
        p.ts("dve", OA[:, ot, h * 64:(h + 1) * 64], O[:, 0:64], rec[:, h:h + 1], None, ALU.mult, None, [O, rec], [OAt])

    u = 0
    for h in range(8 if ph >= 4 else 0):
        nT, nV = NABt[h % 2], nabv[h % 2]
        p.dma("pool", nV, nab[h].rearrange("p (s q) -> p s q", q=128), (), [nT])
        for rp in range(NLOC):
            if rp == 0:
                kts, sls = list(range(0, 6)), list(range(5, 11))
            elif rp == 1:
                kts, sls = list(range(1, 6)), list(range(11, 16))
            elif rp == NLOC - 2:
                kts, sls = list(range(rp, rp + 5)), list(range(16, 21))
            elif rp == NLOC - 1:
                kts, sls = list(range(rp - 1, rp + 5)), list(range(21, 27))
            else:
                kts, sls = list(range(rp, rp + 5)), list(range(0, 5))
            na_unit(h, rp + 2, kts, sls, nT, nV, rp, u)
            u += 1
        for ci, ct in enumerate(CT):
            na_unit(h, ct, [], [], nT, nV, NLOC + ci, u)
            u += 1

    def wa_unit(qt, kvh, ktiles, mslots, ot, u):
        kch = 12 + kvh
        allk = [(kt, ms) for kt, ms in zip(ktiles, mslots)] + [(c, None) for c in CT]
        nck = len(allk)
        for c, (kt, ms) in enumerate(allk):
            for par in range(2):
                bank = pbank[((u * 5 + c) % 2) * 2 + par]
                half = slice(par * 64, par * 64 + 64)
                for jj in range(2):
                    j = 2 * jj + par
                    h = 4 * kvh + j
                    o = bank[:, jj * 128:(jj + 1) * 128]
                    p.mm(o, QT[half, kch, kt * 128:(kt + 1) * 128], QT[half, 8 + h // 2, qt * 128:(qt + 1) * 128],
                         True, ms is None, [QT], [bank])
                    if ms is not None:
                        p.mm(o, ident[:, :], WM[:, ms, 0:128], False, True, [ident, WM], [bank])
                p.act(PTW[:, c, par * 256:(par + 1) * 256], bank[:, 0:256], AF.Exp, [bank], [PTWt])
        O = pbank[4 + u % 2]
        WSUB = int(os.environ.get('WSUB', '9'))
        if WSUB < 1:
            return
        for j in range(4):
            pos = (j % 2) * 2 + j // 2
            for c, (kt, ms) in enumerate(allk):
                p.mm(O[:, j * 128:j * 128 + 65], PTW[:, c, pos * 128:(pos + 1) * 128], VB[:, kt, kvh, :], c == 0,
                     c == nck - 1, [PTWt, VB], [O])
        if WSUB < 2:
            return
        for j in range(4):
            h = 4 * kvh + j
            p.tt("dve", rec[:, h:h + 1], O[:, j * 128 + 64:j * 128 + 65], esink[:, h:h + 1], ALU.add, [O, esink], [rec])
            p.op("dve", lambda g, h=h: g.reciprocal(rec[:, h:h + 1], rec[:, h:h + 1]), [rec], [rec])
            p.ts("dve", OA[:, ot, 512 + h * 64:512 + (h + 1) * 64], O[:, j * 128:j * 128 + 64], rec[:, h:h + 1], None,
                 ALU.mult, None, [O, rec], [OAt])

    for n in range(int(os.environ.get('WN', NLOC)) if ph >= 5 else 0):
        for kvh in range(2):
            ms = [2 if n == 0 else 0, None, 3 if n == NLOC - 1 else 1]
            wa_unit(n + 2, kvh, [n + 1, n + 2, n + 3], ms, n, u)
            u += 1
    for ci, ct in enumerate(CT if ph >= 5 and int(os.environ.get('WC', 1)) else []):
        for kvh in range(2):
            wa_unit(ct, kvh, [], [], NLOC + ci, u)
            u += 1

    if dbg:
        dOA = nc.dram_tensor("dOA", [128, 10 * 1024], F32, kind="ExternalOutput").ap()
        dQT = nc.dram_tensor("dQT", [128, 14 * NTOK], F32, kind="ExternalOutput").ap()
        dVA = nc.dram_tensor("dVA", [128, NTA * 8 * 65], F32, kind="ExternalOutput").ap()
        p.dma("pool", dOA[:, :], big[:, 0:10 * 1024], [OAt], ())
        p.dma("pool", dQT[:, :], QT[:, :, :].rearrange("p c t -> p (c t)"), [QT], ())
        p.dma("pool", dVA[:, :], VA[:, :, :, :].rearrange("p t h d -> p (t h d)"), [VA], ())
    for o in range(NLOC + 2):
        w = 0 if o < NLOC else 1
        src = o + 2 if o < NLOC else 12 + (o - NLOC)
        for kc in range(8):
            p.tr(ptr[:, kc, :], OA[:, o, kc * 128:(kc + 1) * 128], ident[:, :], [OAt, ident], [ptr])
        p.cp("act", oT[:, :, :], ptr[:, :, :], [ptr], [oT])
        y0, y1 = pbank[(o % 2) * 2], pbank[(o % 2) * 2 + 1]
        for half, y in enumerate((y0, y1)):
            for kc in range(8):
                p.mm(y[:, :], oT[:, kc, :], wov[:, kc, half * 512:(half + 1) * 512], kc == 0, kc == 7, [oT, WO], [y])
        p.dma("sp", xt[:, :], xe[src * 128:(src + 1) * 128, :], (), [xt])
        j = small["junk"]
        g1 = MB[w][2]
        for half, y in enumerate((y0, y1)):
            sl = slice(half * 512, (half + 1) * 512)
            p.tt("dve", j[:, sl], y[:, :], g1[:, sl], ALU.mult, [y, g1], [j])
        p.tt("dve", j[:, :], j[:, :], xt[:, :], ALU.add, [j, xt], [j])
        p.dma("sp", xo[o * 128:(o + 1) * 128, :], j[:, :], [j], ())
    p.emit()
    return nc


def rope_tables(tok0):
    t = tok0 + np.arange(1536)
    row, col = (t // 64).astype(np.float32), (t % 64).astype(np.float32)
    inv = (10000.0 ** (-np.arange(16, dtype=np.float32) / 16)).astype(np.float32)
    C = np.zeros((64, 1536), np.float32)
    S = np.zeros((64, 1536), np.float32)
    for d in range(64):
        pos = row if d < 32 else col
        q = d % 32
        ang = (pos * inv[q % 16]).astype(np.float32)
        C[d] = np.cos(ang)
        S[d] = -np.sin(ang) if q < 16 else np.sin(ang)
    return np.concatenate([C, C], 0), np.concatenate([S, S], 0)


def perm_matrix():
    P = np.zeros((128, 128), np.float32)
    for m in range(128):
        blk, d = divmod(m, 64)
        q = d % 32
        partner = d + 16 if q < 16 else d - 16
        P[blk * 64 + partner, m] = 1.0
    return P


def na_bias_tables(rel_bias, R0):
    out = np.full((8, 128, 27, 128), NEG, np.float32)
    specs = []
    for c in range(5):
        specs.append((c, 2, 2 + c))
    for c in range(6):
        specs.append((5 + c, 0, c))
    for c in range(5):
        specs.append((11 + c, 1, 1 + c))
    for c in range(5):
        specs.append((16 + c, NLOC - 2, NLOC - 2 + c))
    for c in range(6):
        specs.append((21 + c, NLOC - 1, NLOC - 2 + c))
    kp = np.arange(128)
    qi = np.arange(128)
    for slot, rp, kt in specs:
        r = R0 + 2 * rp + qi // 64
        i = qi % 64
        kr = R0 - 4 + 2 * kt + kp // 64
        jc = kp % 64
        r0 = np.clip(r - 4, 0, 120)
        c0 = np.clip(i - 8, 0, 48)
        valid = ((kr[:, None] >= r0[None, :]) & (kr[:, None] < r0[None, :] + 8) & (kr[:, None] >= 0) & (kr[:, None] < 128)
                 & (jc[:, None] >= c0[None, :]) & (jc[:, None] < c0[None, :] + 16))
        dr = np.clip(kr[:, None] - r[None, :] + 7, 0, 14)
        dc = np.clip(jc[:, None] - i[None, :] + 15, 0, 30)
        vals = rel_bias[:, dr, dc]
        out[:, :, slot, :] = np.where(valid[None], vals, NEG)
    return out.reshape(8, 128, 27 * 128)


def wa_masks(gb0):
    kp = np.arange(128)[:, None]
    qi = np.arange(128)[None, :]
    prev = np.where(kp >= qi, 0.0, NEG).astype(np.float32)
    nxt = np.where(kp <= qi, 0.0, NEG).astype(np.float32)
    allneg = np.full((128, 128), NEG, np.float32)
    m = [prev, nxt, prev if gb0 > 0 else allneg, nxt if gb0 + NLOC < 64 else allneg]
    return np.concatenate([np.tile(x, (1, 4)) for x in m], axis=1)


def attn_inputs(inp, b, s):
    R0 = 16 * s
    x = inp["x"][b]
    xe = np.zeros((NTA * 128, D), np.float32)
    g0 = (R0 - 4) * 64
    lo, hi = max(g0, 0), min(g0 + 1536, 8192)
    xe[lo - g0:hi - g0] = x[lo:hi]
    xe[1536:] = inp["ctx"][b]
    w = inp["att_w_in"][0]
    win = np.concatenate([w[:, 0:512], w[:, 512:1024], w[:, 1536:2048], w[:, 2048:2112], w[:, 2048:2112],
                          w[:, 2112:2176], w[:, 2112:2176], w[:, 1024:1536], w[:, 2176:2304]], axis=1)
    gv = [inp["na_q_norm"][0], inp["na_k_norm"][0], inp["wa_q_norm"][0], inp["wa_k_norm"][0]]
    gains = np.stack([np.concatenate([g, g]) for g in gv], axis=1)
    rc, rs = rope_tables(g0)
    cin = np.stack([inp["c"][b].reshape(8, 128).T, inp["c_ctx"].reshape(8, 128).T], axis=1)
    ob = np.zeros((128, 128), np.float32)
    ob[:64, :64] = 1.0
    ob[64:, 64:] = 1.0
    return {
        "xe": xe, "cin": np.ascontiguousarray(cin, dtype=np.float32),
        "modw": np.ascontiguousarray(inp["mod_w"][0][:, 0:3 * D]), "modb": np.ascontiguousarray(inp["mod_b"][0][None, 0:3 * D]),
        "nrm": np.ascontiguousarray(inp["norm_mix"][0][None, :]),
        "win": np.ascontiguousarray(win), "wout": np.ascontiguousarray(inp["att_w_out"][0]),
        "gains": np.ascontiguousarray(gains, dtype=np.float32), "ropec": rc, "ropes": rs, "pmd": perm_matrix(),
        "onesd": ob, "identd": np.eye(128, dtype=np.float32),
        "nab": na_bias_tables(inp["na_rel_bias"][0], R0), "wam": wa_masks(R0 // 2),
        "sink": np.ascontiguousarray(inp["wa_sink"][0][None, :]),
    }


TS = 66
NSEQ = TS * 128
WS = 1056
TWO_PI = 2.0 * np.pi


def build_ssm(nt=TS, do_s5=True):
    nc = bass.Bass("TRN2", target_bir_lowering=False)
    dt_in = lambda n, s: nc.dram_tensor(n, s, F32, kind="ExternalInput").ap()
    xs = dt_in("xs", [NSEQ, D])
    cin = dt_in("cin", [128, 2, 8])
    modw = dt_in("modw", [D, 2 * D])
    modb = dt_in("modb", [1, 2 * D])
    nrm = dt_in("nrm", [1, D])
    wsel = dt_in("wsel", [D, WS])
    cw = dt_in("cw", [128, 6 * 3])
    cbias = dt_in("cbias", [128, 6])
    dtb = dt_in("dtb", [1, 8])
    alog = dt_in("alog", [1, 8])
    dsk = dt_in("dsk", [1, 8])
    identd = dt_in("identd", [128, 128])
    triud = dt_in("triud", [128, 128])
    iotad = dt_in("iotad", [128, 129])
    m01d = dt_in("m01d", [128, 512])
    lam = dt_in("lam", [128, 3 * 8])
    BLr = dt_in("BLr", [8, 128, 128])
    BLi = dt_in("BLi", [8, 128, 128])
    CLr = dt_in("CLr", [8, 128, 32])
    CLi = dt_in("CLi", [8, 128, 32])
    DL = dt_in("DL", [8, 128, 32])
    yssd = nc.dram_tensor("yssd", [NSEQ, 512], F32, kind="ExternalOutput").ap()
    ys5 = nc.dram_tensor("ys5", [256, NSEQ], F32, kind="ExternalOutput").ap()

    p = Prog(nc)
    UT = p.sb([128, 2, NSEQ], BF16, "UT")
    Z = p.sb([128, 2, NSEQ], F32, "Z")
    MB = [[p.sb([128, D], F32, f"mb{w}{v}") for v in range(2)] for w in range(2)]
    wsb = p.sb([128, 8, WS], BF16, "wsb")
    xt = p.sb([128, D], F32, "xt")
    hb = p.sb([128, D], BF16, "hb")
    hTi = p.sb([128, 8, 128], BF16, "hTi")
    stage = p.sb([128, 8, 128], F32, "stage")
    bstage = p.sb([128, 128], F32, "bstage")
    cbc = p.sb([128, 8, 128], F32, "cbc")
    ones = p.sb([128, 128], F32, "ones")
    identf = p.sb([128, 128], F32, "identf")
    ident = p.sb([128, 128], BF16, "ident")
    triu = p.sb([128, 128], F32, "triu")
    cin_t = p.sb([128, 2, 8], F32, "cin_t")
    small = {"ss": p.sb([128, 4], F32, "ss"), "rs": p.sb([128, 4], F32, "rs"), "junk": p.sb([128, D], F32, "junk")}
    CW = p.sb([128, 18], F32, "CWc")
    CBs = p.sb([128, 6], F32, "CBs")
    dtb_t = p.sb([128, 8], F32, "dtb_t")
    A_t = p.sb([128, 8], F32, "A_t")
    dsk_t = p.sb([128, 8], F32, "dsk_t")
    RAW = [p.sb([128, 6, 128], BF16, f"raw{i}") for i in range(3)]
    DTs = [p.sb([128, 8], F32, f"dts{i}") for i in range(3)]
    CBUF = p.sb([128, 6, 130], BF16, "CBUF")
    cacc6 = p.sb([128, 6, 128], F32, "cacc6")
    tmp6 = p.sb([128, 6, 128], F32, "tmp6")
    XC = p.sb([128, 6, 128], BF16, "XC")
    XTOK = p.sb([128, 512], BF16, "XTOK")
    BTOK = p.sb([128, 128], BF16, "BTOK")
    sm = p.sb([128, 64], F32, "sm")
    CBT = p.sb([128, 128], F32, "CBT")
    WT4 = [p.sb([128, 512], BF16, f"WT4{i}") for i in range(2)]
    H = p.sb([128, 512], F32, "H")
    Hb = p.sb([128, 512], BF16, "Hb")
    XW = p.sb([128, 512], BF16, "XW")
    ysb = p.sb([128, 512], F32, "ysb")
    ytmp = p.sb([128, 512], F32, "ytmp")
    B0, B1, B2, B3, B4, B5, B6 = [p.ps([128, 512], F32, f"pb{i}") for i in range(7)]
    ptr = p.ps([128, 8, 128], BF16, "ptr")

    p.dma("sp", identf[:, :], identd[:, :], (), [identf])
    p.cp("dve", ident[:, :], identf[:, :], [identf], [ident])
    p.dma("sp", triu[:, :], triud[:, :], (), [triu])
    p.memset("pool", ones[:, :], 1.0, [ones])
    p.memset("pool", H[:, :], 0.0, [H])
    p.dma("pool", wsb[:, :, :], wsel.rearrange("(kc p) n -> p kc n", p=128), (), [wsb])
    p.dma("sp", cin_t[:, :, :], cin[:, :, :], (), [cin_t])
    p.act(cin_t[:, :, :], cin_t[:, :, :], AF.Silu, [cin_t], [cin_t])
    p.dma("sp", xt[:, :], bcast_rows(nrm[0:1, :]), (), [xt])
    p.dma("sp", CW[:, :], cw[:, :], (), [CW])
    p.dma("sp", CBs[:, :], cbias[:, :], (), [CBs])
    p.dma("sp", dtb_t[:, :], bcast_rows(dtb[0:1, :]), (), [dtb_t])
    p.dma("sp", A_t[:, :], bcast_rows(alog[0:1, :]), (), [A_t])
    p.act(A_t[:, :], A_t[:, :], AF.Exp, [A_t], [A_t])
    p.ts("dve", A_t[:, :], A_t[:, :], -1.0, None, ALU.mult, None, [A_t], [A_t])
    p.dma("sp", dsk_t[:, :], bcast_rows(dsk[0:1, :]), (), [dsk_t])

    def outsel(which):
        def f(j):
            v, c = divmod(j, 8)
            t = MB[which][v]
            return t, t[:, c * 128:(c + 1) * 128]
        return f
    emit_mod_rows(p, cin_t, modw, modb, 2 * D, [outsel(0), outsel(1)], B0, stage, cbc, ones, bstage)
    for w in range(2):
        A = MB[w][1]
        p.stt("dve", A[:, :], A[:, :], 1.0, xt[:, :], ALU.add, ALU.mult, [A, xt], [A])

    PSUB = int(os.environ.get('PSUB', '9'))

    RMt = [alias(p, stage, f"RM{i}") for i in range(2)]
    RMv = [stage[:, 4 * i:4 * i + 4, :].rearrange("p c t -> p (c t)") for i in range(2)]
    LMt = [alias(p, cbc, f"LM{i}") for i in range(2)]
    LMv = [cbc[:, 4 * i:4 * i + 4, :].rearrange("p c t -> p (c t)") for i in range(2)]

    def project(i):
        if PSUB < 1:
            return
        w = 1 if i < 2 else 0
        raw, dts = RAW[i % 3], DTs[i % 3]
        p.dma("sp", xt[:, :], xs[i * 128:(i + 1) * 128, :], (), [xt])
        emit_adanorm_T(p, xt, MB[w][1], MB[w][0], hb, ptr, hTi[:, :, :], hTi, ident, small)
        if PSUB < 2:
            return
        PQ = int(os.environ.get('PQ', '9'))
        for grp in range(2):
            if grp == 1 and PQ < 3:
                break
            for c4 in range(4):
                ch = grp * 4 + c4
                for kc in range(8):
                    p.mm(B0[:, c4 * 128:(c4 + 1) * 128], wsb[:, kc, ch * 128:(ch + 1) * 128], hTi[:, kc, :], kc == 0,
                         kc == 7, [wsb, hTi], [B0])
            if grp == 0:
                if PQ >= 2:
                    p.cp("act", raw[:, 0:4, :], B0[:, :].rearrange("p (c t) -> p c t", c=4), [B0], [raw])
            else:
                if PQ >= 4:
                    p.cp("act", raw[:, 4:6, :], B0[:, 0:256].rearrange("p (c t) -> p c t", c=2), [B0], [raw])
                if PQ >= 5:
                    for c2 in range(2):
                        p.cp("act", UT[:, c2, i * 128:(i + 1) * 128], B0[:, 256 + c2 * 128:384 + c2 * 128], [B0], [UT])
        if PSUB < 3:
            return
        for kc in range(8):
            p.mm(B1[:, 0:8], hTi[:, kc, :], wsb[:, kc, 1024:1032], kc == 0, kc == 7, [hTi, wsb], [B1])
        p.tt("dve", dts[:, :], B1[:, 0:8], dtb_t[:, :], ALU.add, [B1, dtb_t], [dts])
        p.act(dts[:, :], dts[:, :], AF.Exp, [dts], [dts])
        p.act(dts[:, :], dts[:, :], AF.Ln, [dts], [dts], bias=1.0)

    a_, acs, tot, eacs, wend, dec = (sm[:, 8 * k:8 * k + 8] for k in range(6))

    SSUB = int(os.environ.get('SSUB', '9'))

    def ssd_chunk(j):
        raw, dts = RAW[j % 3], DTs[j % 3]
        if SSUB < 1:
            return
        first = j in (0, 2)
        last = j in (1, nt - 1)
        if first:
            p.memset("pool", CBUF[:, :, 0:1], 0.0, [CBUF])
        else:
            p.cp("pool", CBUF[:, :, 0:1], RAW[(j - 1) % 3][:, :, 127:128], [RAW[(j - 1) % 3]], [CBUF])
        p.cp("pool", CBUF[:, :, 1:129], raw[:, :, :], [raw], [CBUF])
        if last:
            p.memset("pool", CBUF[:, :, 129:130], 0.0, [CBUF])
        else:
            p.cp("pool", CBUF[:, :, 129:130], RAW[(j + 1) % 3][:, :, 0:1], [RAW[(j + 1) % 3]], [CBUF])
        cw3 = CW[:, :].rearrange("p (c k) -> p c k", k=3)
        wk = lambda k: cw3[:, :, k:k + 1].broadcast_to([128, 6, 128])
        p.tt("dve", cacc6[:, :, :], CBUF[:, :, 0:128], wk(0), ALU.mult, [CBUF, CW], [cacc6])
        p.tt("pool", tmp6[:, :, :], CBUF[:, :, 1:129], wk(1), ALU.mult, [CBUF, CW], [tmp6])
        p.tt("dve", cacc6[:, :, :], cacc6[:, :, :], tmp6[:, :, :], ALU.add, [cacc6, tmp6], [cacc6])
        p.tt("pool", tmp6[:, :, :], CBUF[:, :, 2:130], wk(2), ALU.mult, [CBUF, CW], [tmp6])
        p.tt("dve", cacc6[:, :, :], cacc6[:, :, :], tmp6[:, :, :], ALU.add, [cacc6, tmp6], [cacc6])
        p.tt("dve", cacc6[:, :, :], cacc6[:, :, :], CBs[:, :].rearrange("p (c o) -> p c o", o=1).broadcast_to([128, 6, 128]),
             ALU.add, [cacc6, CBs], [cacc6])
        p.act(XC[:, :, :], cacc6[:, :, :], AF.Silu, [cacc6], [XC])
        if SSUB < 2:
            return
        for c in range(5):
            p.tr(ptr[:, c, :], XC[:, c, :], ident[:, :], [XC, ident], [ptr])
        p.cp("act", XTOK[:, :], ptr[:, 0:4, :].rearrange("p c t -> p (c t)"), [ptr], [XTOK])
        p.cp("act", BTOK[:, :], ptr[:, 4, :], [ptr], [BTOK])
        p.tt("dve", a_, dts[:, :], A_t[:, :], ALU.mult, [dts, A_t], [sm])
        p.mm(B1[:, 0:8], triu[:, :], a_, True, True, [triu, sm], [B1])
        p.mm(B1[:, 128:136], ones[:, :], a_, True, True, [ones, sm], [B1])
        p.cp("dve", acs, B1[:, 0:8], [B1], [sm])
        p.cp("dve", tot, B1[:, 128:136], [B1], [sm])
        if SSUB < 3:
            return
        p.mm(B2[:, 0:128], XC[:, 4, :], XC[:, 5, :], True, True, [XC], [B2])
        p.tt("dve", CBT[:, :], B2[:, 0:128], triu[:, :], ALU.mult, [B2, triu], [CBT])
        p.cp("act", Hb[:, :], H[:, :], [H], [Hb])
        v4 = lambda ap: ap.rearrange("p (h t) -> p h t", h=4)
        b4 = lambda ap: ap.rearrange("p (h o) -> p h o", o=1).broadcast_to([128, 4, 128])
        o4 = lambda ap: ap.rearrange("p (o t) -> p o t", o=1).broadcast_to([128, 4, 128])
        for g4 in range(2):
            hs = slice(g4 * 4, g4 * 4 + 4)
            rt_, rv_, lt_, lv_, wt4, Pb = RMt[g4], RMv[g4], LMt[g4], LMv[g4], WT4[g4], (B3, B2)[g4]
            p.tt("dve", v4(rv_), o4(triu[:, :]), b4(a_[:, hs]), ALU.mult, [triu, sm], [rt_])
            p.mm(Pb[:, :], ones[:, :], rv_, True, True, [ones, rt_], [Pb])
            p.tt("dve", v4(lv_), v4(Pb[:, :]), b4(acs[:, hs]), ALU.subtract, [Pb, sm], [lt_])
            p.ts("dve", lv_, lv_, 0.0, None, ALU.min, None, [lt_], [lt_])
            p.act(lv_, lv_, AF.Exp, [lt_], [lt_])
            p.tt("dve", v4(lv_), v4(lv_), b4(dts[:, hs]), ALU.mult, [lt_, dts], [lt_])
            p.tt("dve", v4(wt4[:, :]), v4(lv_), o4(CBT[:, :]), ALU.mult, [lt_, CBT], [wt4])
            for hh in range(4):
                hd = g4 * 4 + hh
                p.mm(B4[:, hd * 64:(hd + 1) * 64], wt4[:, hh * 128:(hh + 1) * 128], XTOK[:, hd * 64:(hd + 1) * 64], True, True,
                     [wt4, XTOK], [B4])
        if SSUB < 4:
            return
        p.mm(B5[:, :], XC[:, 5, :], Hb[:, :], True, True, [XC, Hb], [B5])
        p.act(eacs, acs, AF.Exp, [sm], [sm])
        v3 = lambda ap: ap.rearrange("p (h d) -> p h d", h=8)
        bc = lambda ap: ap.rearrange("p (h o) -> p h o", o=1).broadcast_to([128, 8, 64])
        p.tt("dve", v3(ytmp[:, :]), v3(B5[:, :]), bc(eacs), ALU.mult, [B5, sm], [ytmp])
        p.tt("dve", ysb[:, :], ytmp[:, :], B4[:, :], ALU.add, [ytmp, B4], [ysb])
        p.tt("dve", v3(ytmp[:, :]), v3(XTOK[:, :]), bc(dsk_t[:, :]), ALU.mult, [XTOK, dsk_t], [ytmp])
        p.tt("dve", ysb[:, :], ysb[:, :], ytmp[:, :], ALU.add, [ysb, ytmp], [ysb])
        p.dma("sp", yssd[j * 128:(j + 1) * 128, :], ysb[:, :], [ysb], ())
        if SSUB < 5:
            return
        p.tt("dve", wend, tot, acs, ALU.subtract, [sm], [sm])
        p.act(wend, wend, AF.Exp, [sm], [sm])
        p.tt("dve", wend, wend, dts[:, :], ALU.mult, [sm, dts], [sm])
        p.act(dec, tot, AF.Exp, [sm], [sm])
        p.tt("dve", v3(XW[:, :]), v3(XTOK[:, :]), bc(wend), ALU.mult, [XTOK, sm], [XW])
        p.mm(B6[:, :], BTOK[:, :], XW[:, :], True, True, [BTOK, XW], [B6])
        p.tt("dve", v3(H[:, :]), v3(H[:, :]), bc(dec), ALU.mult, [H, sm], [H])
        p.tt("dve", H[:, :], H[:, :], B6[:, :], ALU.add, [H, B6], [H])

    for i in range(nt + 1):
        if i < nt:
            project(i)
        if i >= 1:
            ssd_chunk(i - 1)

    if do_s5:
        LAM = p.sb([128, 24], F32, "LAM")
        dsc = p.sb([128, 8, 16], F32, "dsc")
        iota = p.sb([128, 129], F32, "iota")
        ang = p.sb([128, 129], F32, "ang")
        mag = p.sb([128, 129], F32, "mag")
        cs_ = p.sb([128, 129], F32, "cs_")
        sn_ = p.sb([128, 129], F32, "sn_")
        EP = p.sb([128, 2, 512], F32, "EP")
        EN = p.sb([128, 2, 512], F32, "EN")
        m01 = p.sb([128, 512], F32, "m01")
        blr = p.sb([128, 128], BF16, "blr")
        bli = p.sb([128, 128], BF16, "bli")
        clr = p.sb([128, 32], BF16, "clr")
        cli = p.sb([128, 32], BF16, "cli")
        dl = p.sb([128, 32], BF16, "dl")
        xr = p.sb([128, 512], F32, "xr")
        xi = p.sb([128, 512], F32, "xi")
        q1 = p.sb([128, 512], F32, "q1")
        q2 = p.sb([128, 512], F32, "q2")
        wr_ = p.sb([128, 512], BF16, "wr_")
        wi_ = p.sb([128, 512], BF16, "wi_")
        gst = p.sb([128, 8], F32, "gst")
        yo = p.sb([32, 512], F32, "yo")
        twopi = p.sb([128, 129], F32, "twopi")
        kint = p.sb([128, 129], mybir.dt.int32, "kint")
        p.dma("sp", LAM[:, :], lam[:, :], (), [LAM])
        p.dma("sp", iota[:, :], iotad[:, :], (), [iota])
        p.dma("sp", m01[:, :], m01d[:, :], (), [m01])
        nblk = (nt * 128 + 511) // 512
        for pr in range(8):
            lr, li, ls = LAM[:, pr:pr + 1], LAM[:, 8 + pr:9 + pr], LAM[:, 16 + pr:17 + pr]
            sc = lambda k: dsc[:, pr, k:k + 1]
            R, W_ = [LAM, dsc, iota, ang, mag, cs_, sn_], [dsc]
            step, lrs, th, den, cr, ci, t0_, t1_ = (sc(k) for k in range(8))
            p.act(step, ls, AF.Exp, [LAM], [dsc])
            p.tt("dve", lrs, lr, step, ALU.mult, [LAM, dsc], [dsc])
            p.tt("dve", th, li, step, ALU.mult, [LAM, dsc], [dsc])
            p.ts("dve", ang[:, :], iota[:, :], th, None, ALU.mult, None, [iota, dsc], [ang])
            p.ts("dve", cs_[:, :], ang[:, :], 1.5 * np.pi, None, ALU.add, None, [ang], [cs_])
            p.ts("dve", twopi[:, :], cs_[:, :], 1.0 / TWO_PI, None, ALU.mult, None, [cs_], [twopi])
            p.cp("dve", kint[:, :], twopi[:, :], [twopi], [kint])
            p.cp("dve", twopi[:, :], kint[:, :], [kint], [twopi])
            p.stt("dve", cs_[:, :], twopi[:, :], -TWO_PI, cs_[:, :], ALU.mult, ALU.add, [twopi, cs_], [cs_])
            p.ts("dve", twopi[:, :], cs_[:, :], 0.0, None, ALU.is_lt, None, [cs_], [twopi])
            p.stt("dve", cs_[:, :], twopi[:, :], TWO_PI, cs_[:, :], ALU.mult, ALU.add, [twopi, cs_], [cs_])
            p.ts("dve", sn_[:, :], ang[:, :], np.pi, None, ALU.add, None, [ang], [sn_])
            p.ts("dve", twopi[:, :], sn_[:, :], 1.0 / TWO_PI, None, ALU.mult, None, [sn_], [twopi])
            p.cp("dve", kint[:, :], twopi[:, :], [twopi], [kint])
            p.cp("dve", twopi[:, :], kint[:, :], [kint], [twopi])
            p.stt("dve", sn_[:, :], twopi[:, :], -TWO_PI, sn_[:, :], ALU.mult, ALU.add, [twopi, sn_], [sn_])
            p.ts("dve", twopi[:, :], sn_[:, :], 0.0, None, ALU.is_lt, None, [sn_], [twopi])
            p.stt("dve", sn_[:, :], twopi[:, :], TWO_PI, sn_[:, :], ALU.mult, ALU.add, [twopi, sn_], [sn_])
            p.act(cs_[:, :], cs_[:, :], AF.Sin, [cs_], [cs_], bias=-np.pi)
            p.act(sn_[:, :], sn_[:, :], AF.Sin, [sn_], [sn_], bias=-np.pi)
            p.act(mag[:, :], iota[:, :], AF.Exp, [iota, dsc], [mag], scale=lrs)
            ar, ai, a128r, a128i = (sc(k) for k in range(8, 12))
            p.tt("dve", ar, mag[:, 1:2], cs_[:, 1:2], ALU.mult, [mag, cs_], [dsc])
            p.tt("dve", ai, mag[:, 1:2], sn_[:, 1:2], ALU.mult, [mag, sn_], [dsc])
            p.tt("dve", a128r, mag[:, 128:129], cs_[:, 128:129], ALU.mult, [mag, cs_], [dsc])
            p.tt("dve", a128i, mag[:, 128:129], sn_[:, 128:129], ALU.mult, [mag, sn_], [dsc])
            p.tt("dve", den, lr, lr, ALU.mult, [LAM], [dsc])
            p.stt("dve", den, li, li, den, ALU.mult, ALU.add, [LAM, dsc], [dsc])
            p.op("dve", lambda g, den=den: g.reciprocal(den, den), [dsc], [dsc])
            p.ts("dve", t0_, ar, -1.0, None, ALU.add, None, [dsc], [dsc])
            p.tt("dve", t1_, ai, li, ALU.mult, [dsc, LAM], [dsc])
            p.stt("dve", cr, t0_, lr, t1_, ALU.mult, ALU.add, [dsc, LAM], [dsc])
            p.tt("dve", cr, cr, den, ALU.mult, [dsc], [dsc])
            p.tt("dve", t1_, t0_, li, ALU.mult, [dsc, LAM], [dsc])
            p.stt("dve", ci, ai, lr, t1_, ALU.mult, ALU.subtract, [dsc, LAM], [dsc])
            p.tt("dve", ci, ci, den, ALU.mult, [dsc], [dsc])
            p.tt("dve", EP[:, 0, 0:128], mag[:, 0:128], cs_[:, 0:128], ALU.mult, [mag, cs_], [EP])
            p.tt("dve", EP[:, 1, 0:128], mag[:, 0:128], sn_[:, 0:128], ALU.mult, [mag, sn_], [EP])
            p.op("dve", lambda g: g.reciprocal(mag[:, :], mag[:, :]), [mag], [mag])
            p.tt("dve", cs_[:, :], cs_[:, :], mag[:, :], ALU.mult, [cs_, mag], [cs_])
            p.tt("dve", sn_[:, :], sn_[:, :], mag[:, :], ALU.mult, [sn_, mag], [sn_])
            p.ts("dve", ang[:, :], sn_[:, :], ci, None, ALU.mult, None, [sn_, dsc], [ang])
            p.stt("dve", EN[:, 0, 0:128], cs_[:, 0:128], cr, ang[:, 0:128], ALU.mult, ALU.add, [cs_, dsc, ang], [EN])
            p.ts("dve", ang[:, :], sn_[:, :], cr, None, ALU.mult, None, [sn_, dsc], [ang])
            p.stt("dve", EN[:, 1, 0:128], cs_[:, 0:128], ci, ang[:, 0:128], ALU.mult, ALU.subtract, [cs_, dsc, ang], [EN])
            for rep in range(1, 4):
                p.cp("pool", EP[:, :, rep * 128:(rep + 1) * 128], EP[:, :, 0:128], [EP], [EP])
                p.cp("pool", EN[:, :, rep * 128:(rep + 1) * 128], EN[:, :, 0:128], [EN], [EN])
            p.dma("pool", blr[:, :], BLr[pr], (), [blr])
            p.dma("pool", bli[:, :], BLi[pr], (), [bli])
            p.dma("pool", clr[:, :], CLr[pr], (), [clr])
            p.dma("pool", cli[:, :], CLi[pr], (), [cli])
            p.ts("dve", cli[:, :], cli[:, :], -1.0, None, ALU.mult, None, [cli], [cli])
            p.dma("pool", dl[:, :], DL[pr], (), [dl])
            uc = pr // 4
            for b in range(nblk):
                t0 = b * 512
                n = min(512, nt * 128 - t0)
                p.mm(B0[:, 0:n], blr[:, :], UT[:, uc, t0:t0 + n], True, True, [blr, UT], [B0])
                p.mm(B1[:, 0:n], bli[:, :], UT[:, uc, t0:t0 + n], True, True, [bli, UT], [B1])
                p.cp("act", xr[:, 0:n], B0[:, 0:n], [B0], [xr])
                p.cp("act", xi[:, 0:n], B1[:, 0:n], [B1], [xi])
                p.tt("dve", q1[:, 0:n], xr[:, 0:n], EN[:, 0, 0:n], ALU.mult, [xr, EN], [q1])
                p.tt("pool", q2[:, 0:n], xi[:, 0:n], EN[:, 1, 0:n], ALU.mult, [xi, EN], [q2])
                p.tt("dve", q1[:, 0:n], q1[:, 0:n], q2[:, 0:n], ALU.subtract, [q1, q2], [q1])
                p.op("dve", lambda g, t0=t0, n=n: g.tensor_tensor_scan(Z[:, 0, t0:t0 + n], m01[:, 0:n], q1[:, 0:n], 0.0,
                                                                        ALU.mult, ALU.add), [m01, q1], [Z])
                p.tt("pool", q2[:, 0:n], xi[:, 0:n], EN[:, 0, 0:n], ALU.mult, [xi, EN], [q2])
                p.tt("dve", q1[:, 0:n], xr[:, 0:n], EN[:, 1, 0:n], ALU.mult, [xr, EN], [q1])
                p.tt("dve", q1[:, 0:n], q1[:, 0:n], q2[:, 0:n], ALU.add, [q1, q2], [q1])
                p.op("dve", lambda g, t0=t0, n=n: g.tensor_tensor_scan(Z[:, 1, t0:t0 + n], m01[:, 0:n], q1[:, 0:n], 0.0,
                                                                        ALU.mult, ALU.add), [m01, q1], [Z])
            gr, gi_, tq = gst[:, 0:1], gst[:, 1:2], gst[:, 2:3]
            p.memset("dve", gst[:, :], 0.0, [gst])
            for c in range(nt):
                zr, zi = Z[:, 0, c * 128:(c + 1) * 128], Z[:, 1, c * 128:(c + 1) * 128]
                if c > 0:
                    p.ts("dve", zr, zr, gr, None, ALU.add, None, [Z, gst], [Z])
                    p.ts("dve", zi, zi, gi_, None, ALU.add, None, [Z, gst], [Z])
                if c < nt - 1:
                    lr_, li_ = Z[:, 0, c * 128 + 127:c * 128 + 128], Z[:, 1, c * 128 + 127:c * 128 + 128]
                    p.tt("dve", tq, li_, a128i, ALU.mult, [Z, dsc], [gst])
                    p.stt("dve", gr, lr_, a128r, tq, ALU.mult, ALU.subtract, [Z, dsc, gst], [gst])
                    p.tt("dve", tq, lr_, a128i, ALU.mult, [Z, dsc], [gst])
                    p.stt("dve", gi_, li_, a128r, tq, ALU.mult, ALU.add, [Z, dsc, gst], [gst])
            for b in range(nblk):
                t0 = b * 512
                n = min(512, nt * 128 - t0)
                p.tt("dve", q1[:, 0:n], Z[:, 0, t0:t0 + n], EP[:, 0, 0:n], ALU.mult, [Z, EP], [q1])
                p.tt("pool", q2[:, 0:n], Z[:, 1, t0:t0 + n], EP[:, 1, 0:n], ALU.mult, [Z, EP], [q2])
                p.tt("dve", wr_[:, 0:n], q1[:, 0:n], q2[:, 0:n], ALU.subtract, [q1, q2], [wr_])
                p.tt("pool", q2[:, 0:n], Z[:, 1, t0:t0 + n], EP[:, 0, 0:n], ALU.mult, [Z, EP], [q2])
                p.tt("dve", q1[:, 0:n], Z[:, 0, t0:t0 + n], EP[:, 1, 0:n], ALU.mult, [Z, EP], [q1])
                p.tt("dve", wi_[:, 0:n], q1[:, 0:n], q2[:, 0:n], ALU.add, [q1, q2], [wi_])
                p.mm(B2[0:32, 0:n], clr[:, :], wr_[:, 0:n], True, False, [clr, wr_], [B2])
                p.mm(B2[0:32, 0:n], cli[:, :], wi_[:, 0:n], False, False, [cli, wi_], [B2])
                p.mm(B2[0:32, 0:n], dl[:, :], UT[:, uc, t0:t0 + n], False, True, [dl, UT], [B2])
                p.cp("act", yo[:, 0:n], B2[0:32, 0:n], [B2], [yo])
                p.dma("sp", ys5[pr * 32:(pr + 1) * 32, t0:t0 + n], yo[:, 0:n], [yo], ())
    p.emit()
    return nc


def ssm_inputs(inp, xseq, b, dr, hf, nt=TS):
    w = inp["ssm_w_in"][0]
    cols = np.concatenate([np.arange(1024 + 512 * hf, 1024 + 512 * hf + 512), np.arange(2048 + 128 * hf, 2048 + 128 * hf + 128),
                           np.arange(2304 + 128 * hf, 2304 + 128 * hf + 128), np.arange(2592 + 256 * hf, 2592 + 256 * hf + 256),
                           np.arange(2560 + 16 * dr + 8 * hf, 2560 + 16 * dr + 8 * hf + 8)])
    cch = np.concatenate([np.arange(512 * hf, 512 * hf + 512), np.arange(1024 + 128 * hf, 1024 + 128 * hf + 128),
                          np.arange(1280 + 128 * hf, 1280 + 128 * hf + 128)])
    cwf = inp["ssd_conv_w"][0][:, cch]
    if dr == 1:
        cwf = cwf[::-1]
    cw = np.ascontiguousarray(cwf.T.reshape(6, 128, 3).transpose(1, 0, 2).reshape(128, 18))
    cb = np.ascontiguousarray(inp["ssd_conv_b"][0][cch].reshape(6, 128).T)
    hs = slice(8 * hf, 8 * hf + 8)
    zero8 = np.zeros((1, 8), np.float32)
    gs = 16 * hf
    lam = np.zeros((128, 24), np.float32)
    BLr = np.zeros((8, 128, 128), np.float32)
    BLi = np.zeros((8, 128, 128), np.float32)
    CLr = np.zeros((8, 128, 32), np.float32)
    CLi = np.zeros((8, 128, 32), np.float32)
    DLm = np.zeros((8, 128, 32), np.float32)
    sd = inp["s5_d"][0]
    for pr in range(8):
        for k in range(2):
            g = gs + 2 * pr + k
            rows = slice(64 * k, 64 * k + 64)
            lam[rows, pr] = inp["s5_lambda_re"][0, dr, g]
            lam[rows, 8 + pr] = inp["s5_lambda_im"][0, dr, g]
            lam[rows, 16 + pr] = inp["s5_log_step"][0, dr, g]
            ur = 32 * (pr % 4) + 16 * k
            BLr[pr, ur:ur + 16, rows] = inp["s5_b_re"][0, dr, g].T
            BLi[pr, ur:ur + 16, rows] = inp["s5_b_im"][0, dr, g].T
            CLr[pr, rows, 16 * k:16 * k + 16] = inp["s5_c_re"][0, dr, g].T
            CLi[pr, rows, 16 * k:16 * k + 16] = inp["s5_c_im"][0, dr, g].T
            if dr == 0:
                for c in range(16):
                    DLm[pr, ur + c, 16 * k + c] = sd[g * 16 + c]
    m01 = np.ones((128, 512), np.float32)
    m01[:, ::128] = 0.0
    cin = np.stack([inp["c"][b].reshape(8, 128).T, inp["c_ctx"].reshape(8, 128).T], axis=1)
    return {
        "xs": np.ascontiguousarray(xseq, dtype=np.float32), "cin": np.ascontiguousarray(cin, dtype=np.float32),
        "modw": np.ascontiguousarray(inp["mod_w"][1][:, 0:2 * D]), "modb": np.ascontiguousarray(inp["mod_b"][1][None, 0:2 * D]),
        "nrm": np.ascontiguousarray(inp["norm_mix"][1][None, :]),
        "wsel": np.ascontiguousarray(np.concatenate([w[:, cols], np.zeros((D, WS - 1032), np.float32)], axis=1)), "cw": cw, "cbias": cb,
        "dtb": np.ascontiguousarray(inp["ssd_dt_bias"][0, dr, hs][None, :]),
        "alog": np.ascontiguousarray(inp["ssd_a_log"][0, dr, hs][None, :]),
        "dsk": np.ascontiguousarray(inp["ssd_d"][0, hs][None, :]) if dr == 0 else zero8,
        "identd": np.eye(128, dtype=np.float32), "triud": np.triu(np.ones((128, 128), np.float32)),
        "iotad": np.tile(np.arange(129, dtype=np.float32)[None, :], (128, 1)), "m01d": m01,
        "lam": lam, "BLr": BLr, "BLi": BLi, "CLr": CLr, "CLi": CLi, "DL": DLm,
    }


NTT = 16
GELU_C = 1.5957691216057308


def build_fin():
    nc = bass.Bass("TRN2", target_bir_lowering=False)
    dt_in = lambda n, s: nc.dram_tensor(n, s, F32, kind="ExternalInput").ap()
    x = dt_in("x", [NTT * 128, D])
    cin = dt_in("cin", [128, 2, 8])
    modw = dt_in("modw", [D, 3 * D])
    modb = dt_in("modb", [1, 3 * D])
    nrm = dt_in("nrm", [1, D])
    wz = dt_in("wz", [D, D])
    yf = dt_in("yf", [NTT * 128, D])
    yb = dt_in("yb", [NTT * 128, D])
    vf = dt_in("vf", [NTT * 128, 512])
    vb = dt_in("vb", [NTT * 128, 512])
    snorm = dt_in("snorm", [1, D])
    gluw = dt_in("gluw", [512, 512])
    glub = dt_in("glub", [1, 512])
    wout = dt_in("wout", [1536, D])
    identd = dt_in("identd", [128, 128])
    xo = nc.dram_tensor("xo", [NTT * 128, D], F32, kind="ExternalOutput").ap()

    p = Prog(nc)
    wzb = p.sb([128, 8, D], BF16, "wzb")
    woutb = p.sb([128, 12, D], BF16, "woutb")
    gwb = p.sb([128, 4, 512], BF16, "gwb")
    MB = [p.sb([128, D], F32, f"mb{v}") for v in range(3)]
    xt = p.sb([128, D], F32, "xt")
    hb = p.sb([128, D], BF16, "hb")
    hTi = p.sb([128, 8, 128], BF16, "hTi")
    stage = p.sb([128, 8, 128], F32, "stage")
    bstage = p.sb([128, 128], F32, "bstage")
    cbc = p.sb([128, 8, 128], F32, "cbc")
    ones = p.sb([128, 128], F32, "ones")
    identf = p.sb([128, 128], F32, "identf")
    ident = p.sb([128, 128], BF16, "ident")
    cin_t = p.sb([128, 2, 8], F32, "cin_t")
    small = {"ss": p.sb([128, 4], F32, "ss"), "rs": p.sb([128, 4], F32, "rs"), "junk": p.sb([128, D], F32, "junk")}
    sn_bc = p.sb([128, D], F32, "sn_bc")
    gb_bc = p.sb([128, 512], F32, "gb_bc")
    zs = p.sb([128, D], F32, "zs")
    ya = p.sb([128, D], F32, "ya")
    ybt = p.sb([128, D], F32, "ybt")
    va = p.sb([128, 512], F32, "va")
    vbt = p.sb([128, 512], F32, "vbt")
    v2 = p.sb([128, 512], F32, "v2")
    gvb = p.sb([128, 512], BF16, "gvb")
    cat = p.sb([128, 1536], BF16, "cat")
    gT = p.sb([128, 4, 128], BF16, "gT")
    cT = p.sb([128, 12, 128], BF16, "cT")
    st2 = p.sb([128, 4], F32, "st2")
    Z0, Z1, G, O0, O1, B0 = [p.ps([128, 512], F32, f"pb{i}") for i in range(6)]
    ptr = p.ps([128, 8, 128], BF16, "ptr")

    p.dma("sp", identf[:, :], identd[:, :], (), [identf])
    p.cp("dve", ident[:, :], identf[:, :], [identf], [ident])
    p.memset("pool", ones[:, :], 1.0, [ones])
    p.dma("pool", wzb[:, :, :], wz.rearrange("(kc p) n -> p kc n", p=128), (), [wzb])
    p.dma("pool", gwb[:, :, :], gluw.rearrange("(kc p) n -> p kc n", p=128), (), [gwb])
    p.dma("pool", woutb[:, :, :], wout.rearrange("(kc p) n -> p kc n", p=128), (), [woutb])
    p.dma("sp", cin_t[:, :, :], cin[:, :, :], (), [cin_t])
    p.act(cin_t[:, :, :], cin_t[:, :, :], AF.Silu, [cin_t], [cin_t])
    p.dma("sp", xt[:, :], bcast_rows(nrm[0:1, :]), (), [xt])
    p.dma("sp", sn_bc[:, :], bcast_rows(snorm[0:1, :]), (), [sn_bc])
    p.dma("sp", gb_bc[:, :], bcast_rows(glub[0:1, :]), (), [gb_bc])

    def outsel(j):
        v, c = divmod(j, 8)
        t = MB[v]
        return t, t[:, c * 128:(c + 1) * 128]
    emit_mod_rows(p, cin_t, modw, modb, 3 * D, [outsel, outsel], B0, stage, cbc, ones, bstage, whichs=(0,))
    A = MB[1]
    p.stt("dve", A[:, :], A[:, :], 1.0, xt[:, :], ALU.add, ALU.mult, [A, xt], [A])

    for i in range(NTT):
        rows = slice(i * 128, (i + 1) * 128)
        p.dma("sp", xt[:, :], x[rows, :], (), [xt])
        emit_adanorm_T(p, xt, MB[1], MB[0], hb, ptr, hTi[:, :, :], hTi, ident, small)
        for half, Zp in enumerate((Z0, Z1)):
            for kc in range(8):
                p.mm(Zp[:, :], hTi[:, kc, :], wzb[:, kc, half * 512:(half + 1) * 512], kc == 0, kc == 7, [hTi, wzb], [Zp])
            p.act(zs[:, half * 512:(half + 1) * 512], Zp[:, :], AF.Silu, [Zp], [zs])
        p.dma("sp", ya[:, :], yf[rows, :], (), [ya])
        p.dma("sp", ybt[:, :], yb[rows, :], (), [ybt])
        p.tt("pool", ya[:, :], ya[:, :], ybt[:, :], ALU.add, [ya, ybt], [ya])
        p.tt("dve", ya[:, :], ya[:, :], zs[:, :], ALU.mult, [ya, zs], [ya])
        j = small["junk"]
        p.memset("dve", st2[:, 0:1], 0.0, [st2])
        p.act(j[:, :], ya[:, :], AF.Square, [ya, st2], [j, st2], accum_out=st2[:, 0:1])
        p.ts("dve", st2[:, 1:2], st2[:, 0:1], 1.0 / D, EPS, ALU.mult, ALU.add, [st2], [st2])
        p.op("act", lambda g: g.sqrt(st2[:, 2:3], st2[:, 1:2]), [st2], [st2])
        p.op("dve", lambda g: g.reciprocal(st2[:, 3:4], st2[:, 2:3]), [st2], [st2])
        p.stt("dve", cat[:, 0:1024], ya[:, :], st2[:, 3:4], sn_bc[:, :], ALU.mult, ALU.mult, [ya, st2, sn_bc], [cat])
        p.dma("sp", va[:, :], vf[rows, :], (), [va])
        p.dma("sp", vbt[:, :], vb[rows, :], (), [vbt])
        p.tt("pool", va[:, :], va[:, :], vbt[:, :], ALU.add, [va, vbt], [va])
        p.tt("dve", v2[:, :], va[:, :], va[:, :], ALU.mult, [va], [v2])
        p.ts("dve", v2[:, :], v2[:, :], 0.044715, 1.0, ALU.mult, ALU.add, [v2], [v2])
        p.tt("dve", v2[:, :], v2[:, :], va[:, :], ALU.mult, [v2, va], [v2])
        p.act(v2[:, :], v2[:, :], AF.Sigmoid, [v2], [v2], scale=GELU_C)
        p.tt("dve", va[:, :], va[:, :], v2[:, :], ALU.mult, [va, v2], [va])
        p.cp("dve", gvb[:, :], va[:, :], [va], [gvb])
        for c in range(4):
            p.tr(ptr[:, c, :], gvb[:, c * 128:(c + 1) * 128], ident[:, :], [gvb, ident], [ptr])
        p.cp("act", gT[:, :, :], ptr[:, 0:4, :], [ptr], [gT])
        for c in range(4):
            p.mm(G[:, :], gT[:, c, :], gwb[:, c, :], c == 0, c == 3, [gT, gwb], [G])
        p.tt("dve", v2[:, :], G[:, :], gb_bc[:, :], ALU.add, [G, gb_bc], [v2])
        p.act(v2[:, :], v2[:, :], AF.Sigmoid, [v2], [v2])
        p.tt("dve", cat[:, 1024:1536], va[:, :], v2[:, :], ALU.mult, [va, v2], [cat])
        for c in range(8):
            p.tr(ptr[:, c, :], cat[:, c * 128:(c + 1) * 128], ident[:, :], [cat, ident], [ptr])
        p.cp("act", cT[:, 0:8, :], ptr[:, :, :], [ptr], [cT])
        for c in range(4):
            p.tr(ptr[:, c, :], cat[:, 1024 + c * 128:1024 + (c + 1) * 128], ident[:, :], [cat, ident], [ptr])
        p.cp("act", cT[:, 8:12, :], ptr[:, 0:4, :], [ptr], [cT])
        for half, Op in enumerate((O0, O1)):
            for c in range(12):
                p.mm(Op[:, :], cT[:, c, :], woutb[:, c, half * 512:(half + 1) * 512], c == 0, c == 11, [cT, woutb], [Op])
        for half, Op in enumerate((O0, O1)):
            sl = slice(half * 512, (half + 1) * 512)
            p.tt("dve", j[:, sl], Op[:, :], MB[2][:, sl], ALU.mult, [Op, MB[2]], [j])
        p.tt("dve", j[:, :], j[:, :], xt[:, :], ALU.add, [j, xt], [j])
        p.dma("sp", xo[rows, :], j[:, :], [j], ())
    p.emit()
    return nc


def fin_inputs(inp, b, xcore, yfc, ybc, vfc, vbc):
    cin = np.stack([inp["c"][b].reshape(8, 128).T, inp["c_ctx"].reshape(8, 128).T], axis=1)
    ca = lambda a: np.ascontiguousarray(a, dtype=np.float32)
    return {
        "x": ca(xcore), "cin": ca(cin), "modw": ca(inp["mod_w"][1][:, 0:3 * D]), "modb": ca(inp["mod_b"][1][None, 0:3 * D]),
        "nrm": ca(inp["norm_mix"][1][None, :]), "wz": ca(inp["ssm_w_in"][0][:, 0:1024]),
        "yf": ca(yfc), "yb": ca(ybc), "vf": ca(vfc), "vb": ca(vbc),
        "snorm": ca(inp["ssd_norm"][0][None, :]), "gluw": ca(inp["s5_glu_w"][0]), "glub": ca(inp["s5_glu_b"][0][None, :]),
        "wout": ca(inp["ssm_w_out"][0]), "identd": np.eye(128, dtype=np.float32),
    }


_CACHE = {}


def _prog(name, fn):
    if name not in _CACHE:
        _CACHE[name] = fn()
    return _CACHE[name]


def kernel(**inputs):
    inp = {k: np.asarray(v) for k, v in inputs.items()}
    C8 = list(range(8))
    xl = np.empty((2, 8192, D), np.float32)
    xc = np.empty((2, 256, D), np.float32)
    for half in range(2):
        vcs = [(b, s) for b in range(2) for s in range(4 * half, 4 * half + 4)]
        res = run_bass_kernel_spmd(build_attn(), [attn_inputs(inp, b, s) for b, s in vcs], core_ids=C8)
        for (b, s), r in zip(vcs, res.results):
            xl[b, s * 1024:(s + 1) * 1024] = r["xo"][:1024]
            if s == 0:
                xc[b] = r["xo"][1024:]

    def moe(L, xl_in, xc_in):
        cores = [(b, q) for b in range(2) for q in range(4)]
        ims = [moe_inputs(np.concatenate([xl_in[b, q * 2048:(q + 1) * 2048], xc_in[b]], axis=0), inp["c"][b], inp["c_ctx"],
                          inp["mod_w"][L], inp["mod_b"][L], inp["norm_ffn"][L], inp["moe_w_group"][L], inp["moe_b_group"][L],
                          inp["moe_w_expert"][L], inp["moe_b_expert"][L], inp["moe_w13"][L], inp["moe_w2"][L])
               for b, q in cores]
        res = run_bass_kernel_spmd(build_moe(), ims, core_ids=C8)
        xo = np.empty_like(xl_in)
        xco = np.empty_like(xc_in)
        for (b, q), r in zip(cores, res.results):
            xo[b, q * 2048:(q + 1) * 2048] = r["xo"][:2048]
            if q == 0:
                xco[b] = r["xo"][2048:]
        return xo, xco

    xl, xc = moe(0, xl, xc)
    cores = [(b, dr, hf) for b in range(2) for dr in range(2) for hf in range(2)]
    ims = []
    for b, dr, hf in cores:
        seq = np.concatenate([xc[b], xl[b]], axis=0) if dr == 0 else np.concatenate([xc[b][::-1], xl[b][::-1]], axis=0)
        ims.append(ssm_inputs(inp, seq, b, dr, hf))
    res = run_bass_kernel_spmd(build_ssm(), ims, core_ids=C8)
    Y = np.empty((2, 2, 8192, D), np.float32)
    V = np.empty((2, 2, 8192, 512), np.float32)
    for (b, dr, hf), r in zip(cores, res.results):
        y = r["yssd"][256:]
        v = r["ys5"][:, 256:].T
        if dr == 1:
            y, v = y[::-1], v[::-1]
        Y[b, dr, :, hf * 512:(hf + 1) * 512] = y
        V[b, dr, :, hf * 256:(hf + 1) * 256] = v
    cores = [(b, q) for b in range(2) for q in range(4)]
    ims = []
    for b, q in cores:
        sl = slice(q * 2048, (q + 1) * 2048)
        ims.append(fin_inputs(inp, b, xl[b, sl], Y[b, 0, sl], Y[b, 1, sl], V[b, 0, sl], V[b, 1, sl]))
    res = run_bass_kernel_spmd(build_fin(), ims, core_ids=C8)
    for (b, q), r in zip(cores, res.results):
        xl[b, q * 2048:(q + 1) * 2048] = r["xo"]
    xl, _ = moe(1, xl, np.zeros_like(xc))
    return xl
```

```python
import os
import numpy as np
import concourse.bass as bass
import concourse.mybir as mybir
from concourse.bass_utils import run_bass_kernel_spmd

F32 = mybir.dt.float32
BF16 = mybir.dt.bfloat16
AF = mybir.ActivationFunctionType
ALU = mybir.AluOpType
AX = mybir.AxisListType

EPOCH = 12000
D = 1024
EPS = 1e-6


class Res:
    __slots__ = ("name", "w", "readers", "dsem", "dcnt")

    def __init__(self, name):
        self.name = name
        self.w = None
        self.readers = {}
        self.dsem = None
        self.dcnt = 0


class T:
    def __init__(self, h, name):
        self.h = h
        self.r = Res(name)

    def __getitem__(self, k):
        return self.h[k]


class Prog:
    def __init__(self, nc):
        self.nc = nc
        self.eng = {"pe": nc.tensor, "dve": nc.vector, "act": nc.scalar, "pool": nc.gpsimd, "sp": nc.sync}
        self.ops = {k: [] for k in self.eng}
        self.cnt = {k: 0 for k in self.eng}
        self.esems = {k: [] for k in self.eng}
        self.known = {k: {} for k in self.eng}
        self.dres = []
        self.nsem = 0
        self.nt = 0

    def sem(self, name):
        self.nsem += 1
        return self.nc.alloc_semaphore(name)

    def sb(self, shape, dt=F32, name=None):
        self.nt += 1
        name = name or f"t{self.nt}"
        return T(self.nc.alloc_sbuf_tensor(name, list(shape), dt), name)

    def ps(self, shape, dt=F32, name=None):
        self.nt += 1
        name = name or f"p{self.nt}"
        return T(self.nc.alloc_psum_tensor(name, list(shape), dt), name)

    def _esem(self, e, ep):
        while len(self.esems[e]) <= ep:
            self.esems[e].append(self.sem(f"s_{e}_{len(self.esems[e])}"))
        return self.esems[e][ep]

    def _need(self, e, ev, waits):
        sem, val, src = ev
        if src == "pe" and e == "pe":
            return
        k = id(sem)
        if self.known[e].get(k, 0) >= val:
            return
        self.known[e][k] = val
        waits.append((sem, val))

    def _deps(self, e, reads, writes):
        waits = []
        for t in reads:
            if t.r.w is not None:
                self._need(e, t.r.w, waits)
        for t in writes:
            if t.r.w is not None:
                self._need(e, t.r.w, waits)
            for ev in t.r.readers.values():
                self._need(e, ev, waits)
        return waits

    def _mark(self, ev, reads, writes):
        for t in writes:
            t.r.w = ev
            t.r.readers = {}
        for t in reads:
            if t not in writes:
                old = t.r.readers.get(id(ev[0]))
                if old is None or old[1] < ev[1]:
                    t.r.readers[id(ev[0])] = ev

    def op(self, e, fn, reads=(), writes=()):
        waits = self._deps(e, reads, writes)
        idx = self.cnt[e]
        self.cnt[e] += 1
        sem = self._esem(e, idx // EPOCH)
        ev = (sem, idx % EPOCH + 1, e)
        self._mark(ev, reads, writes)
        self.ops[e].append((waits, fn, (sem, 1)))

    def dma(self, e, out, in_, reads=(), writes=(), sres=None):
        waits = self._deps(e, reads, writes)
        t = sres or (writes[0] if writes else reads[0])
        r = t.r
        if r.dsem is None or r.dcnt + 16 > 30000:
            r.dsem = self.sem(f"d_{r.name}_{self.nsem}")
            r.dcnt = 0
            self.dres.append(r)
        r.dcnt += 16
        ev = (r.dsem, r.dcnt, "dma")
        self._mark(ev, reads, writes)
        self.ops[e].append((waits, lambda eng: eng.dma_start(out=out, in_=in_), (r.dsem, 16)))

    def finish(self):
        finals = {}
        for r in self.dres:
            finals[id(r.dsem)] = (r.dsem, max(finals.get(id(r.dsem), (None, 0))[1], r.dcnt))
        waits = []
        for sem, val in finals.values():
            if self.known["sp"].get(id(sem), 0) < val:
                waits.append((sem, val))
        for e in ("pe", "dve", "act", "pool"):
            n = self.cnt[e]
            if n:
                waits.append((self._esem(e, (n - 1) // EPOCH), (n - 1) % EPOCH + 1))
        self.ops["sp"].append((waits, None, None))

    def emit(self):
        self.finish()
        with self.nc.Block() as block:
            decos = {"sp": block.sync, "pe": block.tensor, "dve": block.vector, "act": block.scalar,
                     "pool": block.gpsimd}
            for e in ("sp", "pe", "dve", "act", "pool"):
                def body(engine, e=e):
                    for waits, fn, inc in self.ops[e]:
                        for sem, val in waits:
                            engine.wait_ge(sem, val)
                        if fn is not None:
                            fn(engine).then_inc(inc[0], inc[1])
                decos[e](body)

    def mm(self, out, lhsT, rhs, start, stop, reads, writes):
        self.op("pe", lambda g: g.matmul(out, lhsT, rhs, start=start, stop=stop), reads, writes)

    def tr(self, out, in_, ident, reads, writes):
        self.op("pe", lambda g: g.transpose(out, in_, ident), reads, writes)

    def act(self, out, in_, func, reads, writes, bias=None, scale=None, accum_out=None, e="act"):
        kw = {}
        if bias is not None:
            kw["bias"] = bias
        if scale is not None:
            kw["scale"] = scale
        if accum_out is not None:
            kw["accum_out"] = accum_out
        self.op("act", lambda g: g.activation(out, in_, func, **kw), reads, writes)

    def ts(self, e, out, in0, s1, s2, op0, op1, reads, writes):
        if op1 is None:
            self.op(e, lambda g: g.tensor_scalar(out, in0, s1, None, op0), reads, writes)
        else:
            self.op(e, lambda g: g.tensor_scalar(out, in0, s1, s2, op0, op1), reads, writes)

    def tt(self, e, out, in0, in1, op, reads, writes):
        self.op(e, lambda g: g.tensor_tensor(out, in0, in1, op), reads, writes)

    def stt(self, e, out, in0, sc, in1, op0, op1, reads, writes):
        self.op(e, lambda g: g.scalar_tensor_tensor(out, in0, sc, in1, op0, op1), reads, writes)

    def cp(self, e, out, in_, reads, writes):
        if e == "act":
            self.op(e, lambda g: g.copy(out, in_), reads, writes)
        else:
            self.op(e, lambda g: g.tensor_copy(out, in_), reads, writes)

    def memset(self, e, ap, v, writes):
        self.op(e, lambda g: g.memset(ap, v), (), writes)


def bcast_rows(ap, n=128):
    return ap.partition_broadcast(n)


def emit_mod_rows(p, cin_t, modw, modb, ncols, outs, psum, stage, cbc, ones, bstage, whichs=(0, 1)):
    nblk = ncols // 128
    for which in whichs:
        for kc in range(8):
            p.ts("dve", cbc[:, kc, :], ones[:, :], cin_t[:, which, kc:kc + 1], None, ALU.mult, None,
                 [ones, cin_t], [cbc])
        for j in range(nblk):
            p.dma("sp", stage[:, :, :], modw[:, j * 128:(j + 1) * 128].rearrange("(kc p) n -> p kc n", p=128),
                  (), [stage])
            p.dma("sp", bstage[:, :], bcast_rows(modb[0:1, j * 128:(j + 1) * 128]), (), [bstage])
            for kc in range(8):
                p.mm(psum[:, 0:128], cbc[:, kc, :], stage[:, kc, :], kc == 0, kc == 7, [cbc, stage], [psum])
            tile, ap = outs[which](j)
            p.tt("dve", ap, psum[:, 0:128], bstage[:, :], ALU.add, [psum, bstage], [tile])


def emit_adanorm_T(p, x_t, A_t, S_t, hb, ptr, hT_ap, hT_t, ident, small):
    ss, rs, junk = small["ss"], small["rs"], small["junk"]
    p.memset("dve", ss[:, 0:1], 0.0, [ss])
    p.act(junk[:, :], x_t[:, :], AF.Square, [x_t, ss], [junk, ss], accum_out=ss[:, 0:1])
    p.ts("dve", rs[:, 0:1], ss[:, 0:1], 1.0 / D, EPS, ALU.mult, ALU.add, [ss], [rs])
    p.op("act", lambda g: g.sqrt(rs[:, 1:2], rs[:, 0:1]), [rs], [rs])
    p.op("dve", lambda g: g.reciprocal(rs[:, 2:3], rs[:, 1:2]), [rs], [rs])
    p.stt("dve", junk[:, :], x_t[:, :], rs[:, 2:3], A_t[:, :], ALU.mult, ALU.mult, [x_t, rs, A_t], [junk])
    p.tt("dve", hb[:, :], junk[:, :], S_t[:, :], ALU.add, [junk, S_t], [hb])
    for kc in range(8):
        p.tr(ptr[:, kc, :], hb[:, kc * 128:(kc + 1) * 128], ident[:, :], [hb, ident], [ptr])
    p.cp("act", hT_ap, ptr[:, :, :], [ptr], [hT_t])


NT_MOE = 18
NLAT_MOE = 16


def build_moe(n_exp=32):
    NT = NT_MOE
    nc = bass.Bass("TRN2", target_bir_lowering=False)
    x = nc.dram_tensor("x", [NT * 128, D], F32, kind="ExternalInput").ap()
    cin = nc.dram_tensor("cin", [128, 2, 8], F32, kind="ExternalInput").ap()
    modw = nc.dram_tensor("modw", [D, 3 * D], F32, kind="ExternalInput").ap()
    modb = nc.dram_tensor("modb", [1, 3 * D], F32, kind="ExternalInput").ap()
    nrm = nc.dram_tensor("nrm", [1, D], F32, kind="ExternalInput").ap()
    wr = nc.dram_tensor("wr", [D, 36], F32, kind="ExternalInput").ap()
    br = nc.dram_tensor("br", [1, 36], F32, kind="ExternalInput").ap()
    w13 = nc.dram_tensor("w13", [32, D, D], F32, kind="ExternalInput").ap()
    w2 = nc.dram_tensor("w2", [32, 512, D], F32, kind="ExternalInput").ap()
    identd = nc.dram_tensor("identd", [128, 128], F32, kind="ExternalInput").ap()
    xo = nc.dram_tensor("xo", [NT * 128, D], F32, kind="ExternalOutput").ap()

    p = Prog(nc)
    hT = p.sb([128, 8, NT * 128], BF16, "hT")
    acc = p.sb([128, NT, D], F32, "acc")
    w13b = [p.sb([128, 8, D], BF16, f"w13b{i}") for i in range(2)]
    w2b = [p.sb([128, 4, D], BF16, "w2b0")]
    MB = [[p.sb([128, D], F32, f"mb{w}{v}") for v in range(3)] for w in range(2)]
    xt = p.sb([128, D], F32, "xt")
    hb = p.sb([128, D], BF16, "hb")
    stage = p.sb([128, 8, 128], F32, "stage")
    bstage = p.sb([128, 128], F32, "bstage")
    actT = [p.sb([128, 4, 512], BF16, f"actT{i}") for i in range(2)]
    sa = p.sb([128, 512], F32, "sa")
    cbc = p.sb([128, 8, 128], F32, "cbc")
    CW = p.sb([128, NT, 32], F32, "CW")
    ones = p.sb([128, 128], F32, "ones")
    identf = p.sb([128, 128], F32, "identf")
    ident = p.sb([128, 128], BF16, "ident")
    cin_t = p.sb([128, 2, 8], F32, "cin_t")
    nrm_t = xt
    wrf = p.sb([128, 8, 36], F32, "wrf")
    wrb = p.sb([128, 8, 36], BF16, "wrb")
    brb = p.sb([128, 36], F32, "brb")
    small = {"ss": p.sb([128, 4], F32, "ss"), "rs": p.sb([128, 4], F32, "rs"), "junk": p.sb([128, D], F32, "junk")}
    rt = p.sb([128, 256], F32, "rt")
    pbank = [p.ps([128, 512], F32, f"pb{i}") for i in range(6)]
    ptr = p.ps([128, 8, 128], BF16, "ptr")

    p.dma("sp", identf[:, :], identd[:, :], (), [identf])
    p.cp("dve", ident[:, :], identf[:, :], [identf], [ident])
    p.memset("pool", ones[:, :], 1.0, [ones])
    p.memset("pool", acc[:, :, :], 0.0, [acc])
    p.dma("sp", cin_t[:, :, :], cin[:, :, :], (), [cin_t])
    p.act(cin_t[:, :, :], cin_t[:, :, :], AF.Silu, [cin_t], [cin_t])
    p.dma("sp", nrm_t[:, :], bcast_rows(nrm[0:1, :]), (), [nrm_t])
    p.dma("sp", wrf[:, :, :], wr.rearrange("(kc p) n -> p kc n", p=128), (), [wrf])
    p.cp("dve", wrb[:, :, :], wrf[:, :, :], [wrf], [wrb])
    p.dma("sp", brb[:, :], bcast_rows(br[0:1, :]), (), [brb])

    def outsel(which):
        def f(j):
            v, c = divmod(j, 8)
            t = MB[which][v]
            return t, t[:, c * 128:(c + 1) * 128]
        return f
    emit_mod_rows(p, cin_t, modw, modb, 3 * D, [outsel(0), outsel(1)], pbank[0], stage, cbc, ones, bstage)
    for w in range(2):
        A = MB[w][1]
        p.stt("dve", A[:, :], A[:, :], 1.0, nrm_t[:, :], ALU.add, ALU.mult, [A, nrm_t], [A])

    for i in range(NT):
        w = 0 if i < NLAT_MOE else 1
        p.dma("sp", xt[:, :], x[i * 128:(i + 1) * 128, :], (), [xt])
        emit_adanorm_T(p, xt, MB[w][1], MB[w][0], hb, ptr, hT[:, :, i * 128:(i + 1) * 128], hT, ident, small)
        lgp = pbank[1]
        for kc in range(8):
            p.mm(lgp[:, 0:36], hT[:, kc, i * 128:(i + 1) * 128], wrb[:, kc, :], kc == 0, kc == 7, [hT, wrb], [lgp])
        lg = rt[:, 0:36]
        p.tt("dve", lg, lgp[:, 0:36], brb[:, :], ALU.add, [lgp, brb], [rt])
        R, W_ = [rt], [rt]
        gmax, nb, se, m1, m2, den = (rt[:, 40 + k:41 + k] for k in range(6))
        oh = rt[:, 48:52]
        pen = rt[:, 52:56]
        m32 = rt[:, 64:96]
        e32 = rt[:, 96:128]
        m32b = rt[:, 128:160]
        sel = rt[:, 160:192]
        gex = rt[:, 192:196]
        p.op("dve", lambda g, gmax=gmax, lg=lg: g.reduce_max(gmax, lg[:, 0:4], AX.X), R, W_)
        p.ts("dve", oh, lg[:, 0:4], gmax, None, ALU.is_ge, None, R, W_)
        p.ts("dve", nb, gmax, -1.0, None, ALU.mult, None, R, W_)
        p.act(gex, lg[:, 0:4], AF.Exp, R, W_, bias=nb, accum_out=se)
        p.ts("dve", pen, oh, 1e9, -1e9, ALU.mult, ALU.add, R, W_)
        p.tt("dve", m32.rearrange("p (g e) -> p g e", g=4), lg[:, 4:36].rearrange("p (g e) -> p g e", g=4),
             pen.to_broadcast([128, 4, 8]) if False else rt[:, 52:56].rearrange("p (g o) -> p g o", o=1).broadcast_to([128, 4, 8]),
             ALU.add, R, W_)
        p.op("dve", lambda g, m1=m1, m32=m32: g.reduce_max(m1, m32, AX.X), R, W_)
        p.ts("dve", nb, m1, -1.0, None, ALU.mult, None, R, W_)
        p.act(e32, m32, AF.Exp, R, W_, bias=nb)
        p.ts("dve", m32b, m32, m1, -1e9, ALU.is_ge, ALU.mult, R, W_)
        p.tt("dve", m32b, m32b, m32, ALU.add, R, W_)
        p.op("dve", lambda g, m2=m2, m32b=m32b: g.reduce_max(m2, m32b, AX.X), R, W_)
        p.ts("dve", sel, m32, m2, None, ALU.is_ge, None, R, W_)
        p.tt("dve", sel, sel, e32, ALU.mult, R, W_)
        p.op("dve", lambda g, sel=sel, den=den: g.reduce_sum(den, sel, AX.X), R, W_)
        p.tt("dve", den, den, se, ALU.mult, R, W_)
        p.op("dve", lambda g, den=den: g.reciprocal(den, den), R, W_)
        p.ts("dve", CW[:, i, :], sel, den, None, ALU.mult, None, [rt], [CW])

    blocks = [(b * 4, 4) for b in range(NLAT_MOE // 4)] + [(NLAT_MOE, NT - NLAT_MOE)]
    pa = [pbank[0], pbank[1]]
    pbb = [pbank[2], pbank[3]]
    po = [pbank[4], pbank[5]]
    cnt_ab = 0
    cnt_o = 0
    cnt_act = 0
    for e in range(n_exp):
        wa = w13b[e % 2]
        p.dma("pool", wa[:, :, :], w13[e].rearrange("(kc p) n -> p kc n", p=128), (), [wa])
        if e % 2 == 0:
            wb = w2b[0]
            p.dma("pool", wb[:, :, :], w2[e].rearrange("(kc p) n -> p kc n", p=128), (), [wb])
            wbv = [(wb, wb[:, fc, :]) for fc in range(4)]
        else:
            wbv = []
            for hh, tl in enumerate((stage, cbc)):
                v = tl.h.bitcast(BF16)[:, :, :].rearrange("p a b -> p (a b)").rearrange("p (f n) -> p f n", f=2)
                p.dma("pool", v, w2[e][hh * 256:(hh + 1) * 256, :].rearrange("(kc p) n -> p kc n", p=128), (), [tl])
                wbv += [(tl, v[:, 0, :]), (tl, v[:, 1, :])]
        for (t0, nt) in blocks:
            ntok = nt * 128
            at = actT[cnt_act % 2]
            cnt_act += 1
            for fc in range(4):
                A_, B_ = pa[cnt_ab % 2], pbb[cnt_ab % 2]
                cnt_ab += 1
                for kc in range(8):
                    p.mm(A_[:, 0:ntok], wa[:, kc, fc * 128:(fc + 1) * 128], hT[:, kc, t0 * 128:t0 * 128 + ntok],
                         kc == 0, kc == 7, [wa, hT], [A_])
                for kc in range(8):
                    p.mm(B_[:, 0:ntok], wa[:, kc, 512 + fc * 128:512 + (fc + 1) * 128],
                         hT[:, kc, t0 * 128:t0 * 128 + ntok], kc == 0, kc == 7, [wa, hT], [B_])
                p.act(sa[:, 0:ntok], A_[:, 0:ntok], AF.Silu, [A_], [sa])
                p.tt("dve", at[:, fc, 0:ntok], sa[:, 0:ntok], B_[:, 0:ntok], ALU.mult, [sa, B_], [at])
            for tt_ in range(nt):
                ti = t0 + tt_
                for half in range(2):
                    O_ = po[cnt_o % 2]
                    cnt_o += 1
                    for fc in range(4):
                        wt_, wv_ = wbv[fc]
                        p.mm(O_[:, :], at[:, fc, tt_ * 128:(tt_ + 1) * 128], wv_[:, half * 512:(half + 1) * 512],
                             fc == 0, fc == 3, [at, wt_], [O_])
                    accs = acc[:, ti, half * 512:(half + 1) * 512]
                    p.stt("dve", accs, O_[:, :], CW[:, ti, e:e + 1], accs, ALU.mult, ALU.add, [O_, CW, acc], [acc])

    for i in range(NT):
        w = 0 if i < NLAT_MOE else 1
        p.dma("sp", xt[:, :], x[i * 128:(i + 1) * 128, :], (), [xt])
        j = small["junk"]
        p.tt("dve", j[:, :], acc[:, i, :], MB[w][2][:, :], ALU.mult, [acc, MB[w][2]], [j])
        p.tt("dve", j[:, :], j[:, :], xt[:, :], ALU.add, [j, xt], [j])
        p.dma("sp", xo[i * 128:(i + 1) * 128, :], j[:, :], [j], ())
    p.emit()
    return nc


def moe_inputs(x_core, c_b, c_ctx, mod_w_i, mod_b_i, norm_ffn_i, wg, bg, we, be, w13, w2):
    cin = np.stack([c_b.reshape(8, 128).T, c_ctx.reshape(8, 128).T], axis=1)
    return {
        "x": np.ascontiguousarray(x_core, dtype=np.float32),
        "cin": np.ascontiguousarray(cin, dtype=np.float32),
        "modw": np.ascontiguousarray(mod_w_i[:, 3 * D:6 * D]),
        "modb": np.ascontiguousarray(mod_b_i[None, 3 * D:6 * D]),
        "nrm": np.ascontiguousarray(norm_ffn_i[None, :]),
        "wr": np.ascontiguousarray(np.concatenate([wg, we], axis=1)),
        "br": np.ascontiguousarray(np.concatenate([bg, be])[None, :]),
        "w13": w13, "w2": w2,
        "identd": np.eye(128, dtype=np.float32),
    }


NTA = 14
NLOC = 8
WCOLS = 2432
NEG = -30000.0


def alias(p, t, name):
    a = T(t.h, name)
    a.r.w = t.r.w
    a.r.readers = dict(t.r.readers)
    return a


def build_attn(ph=9, dbg=False):
    nc = bass.Bass("TRN2", target_bir_lowering=False)
    dt_in = lambda n, s: nc.dram_tensor(n, s, F32, kind="ExternalInput").ap()
    xe = dt_in("xe", [NTA * 128, D])
    cin = dt_in("cin", [128, 2, 8])
    modw = dt_in("modw", [D, 3 * D])
    modb = dt_in("modb", [1, 3 * D])
    nrm = dt_in("nrm", [1, D])
    win = dt_in("win", [D, WCOLS])
    wout = dt_in("wout", [D, D])
    gains = dt_in("gains", [128, 4])
    ropec = dt_in("ropec", [128, 1536])
    ropes = dt_in("ropes", [128, 1536])
    pmd = dt_in("pmd", [128, 128])
    onesd = dt_in("onesd", [128, 128])
    identd = dt_in("identd", [128, 128])
    nab = dt_in("nab", [8, 128, 27 * 128])
    wam = dt_in("wam", [128, 4 * 512])
    sink = dt_in("sink", [1, 8])
    xo = nc.dram_tensor("xo", [(NLOC + 2) * 128, D], F32, kind="ExternalOutput").ap()

    p = Prog(nc)
    NTOK = NTA * 128
    big = p.sb([128, 8 * NTOK], BF16, "big")
    hTv = big[:, :].rearrange("p (kc t) -> p kc t", kc=8)
    big2 = p.sb([128, 8 * WCOLS], BF16, "big2")
    winv = big2[:, :].rearrange("p (kc n) -> p kc n", kc=8)
    QT = p.sb([128, 14, NTOK], BF16, "QT")
    VA = p.sb([128, NTA, 8, 65], BF16, "VA")
    VB = p.sb([128, NTA, 2, 65], BF16, "VB")
    PT = [p.sb([128, 8, 128], BF16, f"PT{i}") for i in range(2)]
    MB = [[p.sb([128, D], F32, f"mb{w}{v}") for v in range(3)] for w in range(2)]
    xt = p.sb([128, D], F32, "xt")
    hb = p.sb([128, D], BF16, "hb")
    stage = p.sb([128, 8, 128], F32, "stage")
    bstage = p.sb([128, 128], F32, "bstage")
    cbc = p.sb([128, 8, 128], F32, "cbc")
    ones = p.sb([128, 128], F32, "ones")
    identf = p.sb([128, 128], F32, "identf")
    ident = p.sb([128, 128], BF16, "ident")
    onesb = p.sb([128, 128], BF16, "onesb")
    pm = p.sb([128, 128], BF16, "pm")
    cin_t = p.sb([128, 2, 8], F32, "cin_t")
    G = p.sb([128, 4], F32, "G")
    RC = p.sb([128, 1536], BF16, "RC")
    RS = p.sb([128, 1536], BF16, "RS")
    WM = p.sb([128, 4, 512], BF16, "WM")
    esink = p.sb([128, 8], F32, "esink")
    small = {"ss": p.sb([128, 4], F32, "ss"), "rs": p.sb([128, 4], F32, "rs"), "junk": p.sb([128, D], F32, "junk")}
    sq = p.sb([128, 512], BF16, "sq")
    rstd = p.sb([128, 512], F32, "rstd")
    qn = p.sb([128, 512], BF16, "qn")
    t1 = p.sb([128, 512], F32, "t1")
    rec = p.sb([128, 8], F32, "rec")
    oT = p.sb([128, 8, 128], BF16, "oT")
    pbank = [p.ps([128, 512], F32, f"pb{i}") for i in range(7)]
    ptr = p.ps([128, 8, 128], BF16, "ptr")

    p.dma("sp", identf[:, :], identd[:, :], (), [identf])
    p.cp("dve", ident[:, :], identf[:, :], [identf], [ident])
    p.dma("pool", onesb[:, :], onesd[:, :], (), [onesb])
    p.dma("pool", pm[:, :], pmd[:, :], (), [pm])
    p.dma("pool", RC[:, :], ropec[:, :], (), [RC])
    p.dma("pool", RS[:, :], ropes[:, :], (), [RS])
    p.dma("pool", WM[:, :, :], wam.rearrange("p (s q) -> p s q", s=4), (), [WM])
    p.dma("pool", winv, win.rearrange("(kc p) n -> p kc n", p=128), (), [big2])
    p.memset("pool", ones[:, :], 1.0, [ones])
    p.memset("pool", VA[:, :, :, :], 1.0, [VA])
    p.memset("pool", VB[:, :, :, :], 1.0, [VB])
    p.dma("sp", cin_t[:, :, :], cin[:, :, :], (), [cin_t])
    p.act(cin_t[:, :, :], cin_t[:, :, :], AF.Silu, [cin_t], [cin_t])
    p.dma("sp", xt[:, :], bcast_rows(nrm[0:1, :]), (), [xt])
    p.dma("sp", G[:, :], gains[:, :], (), [G])
    p.ts("dve", G[:, 0:1], G[:, 0:1], 0.125, None, ALU.mult, None, [G], [G])
    p.ts("dve", G[:, 2:3], G[:, 2:3], 0.125, None, ALU.mult, None, [G], [G])
    p.dma("sp", esink[:, :], bcast_rows(sink[0:1, :]), (), [esink])
    p.act(esink[:, :], esink[:, :], AF.Exp, [esink], [esink])

    def outsel(which):
        def f(j):
            v, c = divmod(j, 8)
            t = MB[which][v]
            return t, t[:, c * 128:(c + 1) * 128]
        return f
    emit_mod_rows(p, cin_t, modw, modb, 3 * D, [outsel(0), outsel(1)], pbank[0], stage, cbc, ones, bstage)
    for w in range(2):
        A = MB[w][1]
        p.stt("dve", A[:, :], A[:, :], 1.0, xt[:, :], ALU.add, ALU.mult, [A, xt], [A])

    for i in range(NTA):
        w = 0 if i < 12 else 1
        p.dma("sp", xt[:, :], xe[i * 128:(i + 1) * 128, :], (), [xt])
        emit_adanorm_T(p, xt, MB[w][1], MB[w][0], hb, ptr, hTv[:, :, i * 128:(i + 1) * 128], big, ident, small)

    blocks = [(0, 512), (512, 512), (1024, 512), (1536, 256)]
    cntp = 0
    for ch in range(14 if ph >= 2 else 0):
        gi = 0 if ch < 4 else 1 if ch < 8 else 2 if ch < 12 else 3
        rope = ch >= 8
        for (t0, n) in blocks:
            pq = pbank[cntp % 2]
            pmm = pbank[2 + cntp % 2]
            cntp += 1
            for kc in range(8):
                p.mm(pq[:, 0:n], winv[:, kc, ch * 128:(ch + 1) * 128], hTv[:, kc, t0:t0 + n], kc == 0, kc == 7,
                     [big2, big], [pq])
            p.act(sq[:, 0:n], pq[:, 0:n], AF.Square, [pq], [sq])
            p.mm(pmm[:, 0:n], onesb[:, :], sq[:, 0:n], True, True, [onesb, sq], [pmm])
            p.ts("dve", rstd[:, 0:n], pmm[:, 0:n], 1.0 / 64, EPS, ALU.mult, ALU.add, [pmm], [rstd])
            p.op("act", lambda g, n=n: g.sqrt(rstd[:, 0:n], rstd[:, 0:n]), [rstd], [rstd])
            p.op("dve", lambda g, n=n: g.reciprocal(rstd[:, 0:n], rstd[:, 0:n]), [rstd], [rstd])
            if rope and t0 < 1536:
                p.stt("dve", qn[:, 0:n], pq[:, 0:n], G[:, gi:gi + 1], rstd[:, 0:n], ALU.mult, ALU.mult,
                      [pq, G, rstd], [qn])
                pr = pbank[4 + cntp % 2]
                p.mm(pr[:, 0:n], pm[:, :], qn[:, 0:n], True, True, [pm, qn], [pr])
                p.tt("pool", t1[:, 0:n], qn[:, 0:n], RC[:, t0:t0 + n], ALU.mult, [qn, RC], [t1])
                p.tt("dve", rstd[:, 0:n], pr[:, 0:n], RS[:, t0:t0 + n], ALU.mult, [pr, RS], [rstd])
                p.tt("dve", QT[:, ch, t0:t0 + n], t1[:, 0:n], rstd[:, 0:n], ALU.add, [t1, rstd], [QT])
            else:
                p.stt("dve", QT[:, ch, t0:t0 + n], pq[:, 0:n], G[:, gi:gi + 1], rstd[:, 0:n], ALU.mult, ALU.mult,
                      [pq, G, rstd], [QT])

    for i in range(NTA if ph >= 3 else 0):
        pv, pv2 = pbank[cntp % 2], pbank[2 + cntp % 2]
        cntp += 1
        for kc in range(8):
            p.mm(pv[:, :], hTv[:, kc, i * 128:(i + 1) * 128], winv[:, kc, 1792:2304], kc == 0, kc == 7, [big, big2], [pv])
        for kc in range(8):
            p.mm(pv2[:, 0:128], hTv[:, kc, i * 128:(i + 1) * 128], winv[:, kc, 2304:2432], kc == 0, kc == 7,
                 [big, big2], [pv2])
        p.cp("act", VA[:, i, :, 0:64], pv[:, :].rearrange("p (h d) -> p h d", h=8), [pv], [VA])
        p.cp("dve", VB[:, i, :, 0:64], pv2[:, 0:128].rearrange("p (h d) -> p h d", h=2), [pv2], [VB])

    OAt = alias(p, big, "OA")
    OA = big[:, 0:(NLOC + 2) * 1024].rearrange("p (t d) -> p t d", d=1024)
    WO = alias(p, big2, "WO")
    wov = big2[:, 0:8192].rearrange("p (kc n) -> p kc n", kc=8)
    NABt = [alias(p, big2, f"NAB{i}") for i in range(2)]
    nabv = [big2[:, 8192 + i * 3456:8192 + (i + 1) * 3456].rearrange("p (s q) -> p s q", q=128) for i in range(2)]
    print("sbuf remaining", nc.sbuf_bytes_remaining)
    PTWt = p.sb([128, 5, 512], BF16, "PTWs")
    PTW = PTWt.h
    p.dma("pool", wov, wout.rearrange("(kc p) n -> p kc n", p=128), (), [WO])

    CT = [12, 13]

    def na_unit(h, qt, ktiles, slots, nabT, nabV, ot, u):
        half = slice((h % 2) * 64, (h % 2) * 64 + 64)
        qch, kch = h // 2, 4 + h // 2
        S = [pbank[(u % 2) * 2], pbank[(u % 2) * 2 + 1]]
        O = pbank[4 + u % 2]
        pt = PT[u % 2]
        allk = [(kt, sl) for kt, sl in zip(ktiles, slots)] + [(c, None) for c in CT]
        for c, (kt, sl) in enumerate(allk):
            bank = S[c // 4]
            o = bank[:, (c % 4) * 128:(c % 4 + 1) * 128]
            p.mm(o, QT[half, kch, kt * 128:(kt + 1) * 128], QT[half, qch, qt * 128:(qt + 1) * 128], True, sl is None,
                 [QT], [bank])
            if sl is not None:
                p.mm(o, ident[:, :], nabV[:, sl, :], False, True, [ident, nabT], [bank])
        nck = len(allk)
        n0 = min(nck, 4)
        p.act(pt[:, 0:n0, :], S[0][:, 0:n0 * 128].rearrange("p (c q) -> p c q", q=128), AF.Exp, [S[0]], [pt])
        if nck > 4:
            p.act(pt[:, 4:nck, :], S[1][:, 0:(nck - 4) * 128].rearrange("p (c q) -> p c q", q=128), AF.Exp, [S[1]], [pt])
        for c, (kt, sl) in enumerate(allk):
            p.mm(O[:, 0:65], pt[:, c, :], VA[:, kt, h, :], c == 0, c == nck - 1, [pt, VA], [O])
        p.op("dve", lambda g, O=O, h=h: g.reciprocal(rec[:, h:h + 1], O[:, 64:65]), [O], [rec])
        p.ts("dve", OA[:, ot, h * 64:(h + 1) * 64], O[:, 0:64], rec[:, h:h + 1], None, ALU.mult, None, [O, rec], [OAt])

    u = 0
    for h in range(8 if ph >= 4 else 0):
        nT, nV = NABt[h % 2], nabv[h % 2]
        p.dma("pool", nV, nab[h].rearrange("p (s q) -> p s q", q=128), (), [nT])
        for rp in range(NLOC):
            if rp == 0:
                kts, sls = list(range(0, 6)), list(range(5, 11))
            elif rp == 1:
                kts, sls = list(range(1, 6)), list(range(11, 16))
            elif rp == NLOC - 2:
                kts, sls = list(range(rp, rp + 5)), list(range(16, 21))
            elif rp == NLOC - 1:
                kts, sls = list(range(rp - 1, rp + 5)), list(range(21, 27))
            else:
                kts, sls = list(range(rp, rp + 5)), list(range(0, 5))
            na_unit(h, rp + 2, kts, sls, nT, nV, rp, u)
            u += 1
        for ci, ct in enumerate(CT):
            na_unit(h, ct, [], [], nT, nV, NLOC + ci, u)
            u += 1

    def wa_unit(qt, kvh, ktiles, mslots, ot, u):
        kch = 12 + kvh
        allk = [(kt, ms) for kt, ms in zip(ktiles, mslots)] + [(c, None) for c in CT]
        nck = len(allk)
        for c, (kt, ms) in enumerate(allk):
            for par in range(2):
                bank = pbank[((u * 5 + c) % 2) * 2 + par]
                half = slice(par * 64, par * 64 + 64)
                for jj in range(2):
                    j = 2 * jj + par
                    h = 4 * kvh + j
                    o = bank[:, jj * 128:(jj + 1) * 128]
                    p.mm(o, QT[half, kch, kt * 128:(kt + 1) * 128], QT[half, 8 + h // 2, qt * 128:(qt + 1) * 128],
                         True, ms is None, [QT], [bank])
                    if ms is not None:
                        p.mm(o, ident[:, :], WM[:, ms, 0:128], False, True, [ident, WM], [bank])
                p.act(PTW[:, c, par * 256:(par + 1) * 256], bank[:, 0:256], AF.Exp, [bank], [PTWt])
        O = pbank[4 + u % 2]
        WSUB = int(os.environ.get('WSUB', '9'))
        if WSUB < 1:
            return
        for j in range(4):
            pos = (j % 2) * 2 + j // 2
            for c, (kt, ms) in enumerate(allk):
                p.mm(O[:, j * 128:j * 128 + 65], PTW[:, c, pos * 128:(pos + 1) * 128], VB[:, kt, kvh, :], c == 0,
                     c == nck - 1, [PTWt, VB], [O])
        if WSUB < 2:
            return
        for j in range(4):
            h = 4 * kvh + j
            p.tt("dve", rec[:, h:h + 1], O[:, j * 128 + 64:j * 128 + 65], esink[:, h:h + 1], ALU.add, [O, esink], [rec])
            p.op("dve", lambda g, h=h: g.reciprocal(rec[:, h:h + 1], rec[:, h:h + 1]), [rec], [rec])
            p.ts("dve", OA[:, ot, 512 + h * 64:512 + (h + 1) * 64], O[:, j * 128:j * 128 + 64], rec[:, h:h + 1], None,
                 ALU.mult, None, [O, rec], [OAt])

    for n in range(int(os.environ.get('WN', NLOC)) if ph >= 5 else 0):
        for kvh in range(2):
            ms = [2 if n == 0 else 0, None, 3 if n == NLOC - 1 else 1]
            wa_unit(n + 2, kvh, [n + 1, n + 2, n + 3], ms, n, u)
            u += 1
    for ci, ct in enumerate(CT if ph >= 5 and int(os.environ.get('WC', 1)) else []):
        for kvh in range(2):
            wa_unit(ct, kvh, [], [], NLOC + ci, u)
            u += 1

    if dbg:
        dOA = nc.dram_tensor("dOA", [128, 10 * 1024], F32, kind="ExternalOutput").ap()
        dQT = nc.dram_tensor("dQT", [128, 14 * NTOK], F32, kind="ExternalOutput").ap()
        dVA = nc.dram_tensor("dVA", [128, NTA * 8 * 65], F32, kind="ExternalOutput").ap()
        p.dma("pool", dOA[:, :], big[:, 0:10 * 1024], [OAt], ())
        p.dma("pool", dQT[:, :], QT[:, :, :].rearrange("p c t -> p (c t)"), [QT], ())
        p.dma("pool", dVA[:, :], VA[:, :, :, :].rearrange("p t h d -> p (t h d)"), [VA], ())
    for o in range(NLOC + 2):
        w = 0 if o < NLOC else 1
        src = o + 2 if o < NLOC else 12 + (o - NLOC)
        for kc in range(8):
            p.tr(ptr[:, kc, :], OA[:, o, kc * 128:(kc + 1) * 128], ident[:, :], [OAt, ident], [ptr])
        p.cp("act", oT[:, :, :], ptr[:, :, :], [ptr], [oT])
        y0, y1 = pbank[(o % 2) * 2], pbank[(o % 2) * 2 + 1]
        for half, y in enumerate((y0, y1)):
            for kc in range(8):
                p.mm(y[:, :], oT[:, kc, :], wov[:, kc, half * 512:(half + 1) * 512], kc == 0, kc == 7, [oT, WO], [y])
        p.dma("sp", xt[:, :], xe[src * 128:(src + 1) * 128, :], (), [xt])
        j = small["junk"]
        g1 = MB[w][2]
        for half, y in enumerate((y0, y1)):
            sl = slice(half * 512, (half + 1) * 512)
            p.tt("dve", j[:, sl], y[:, :], g1[:, sl], ALU.mult, [y, g1], [j])
        p.tt("dve", j[:, :], j[:, :], xt[:, :], ALU.add, [j, xt], [j])
        p.dma("sp", xo[o * 128:(o + 1) * 128, :], j[:, :], [j], ())
    p.emit()
    return nc


def rope_tables(tok0):
    t = tok0 + np.arange(1536)
    row, col = (t // 64).astype(np.float32), (t % 64).astype(np.float32)
    inv = (10000.0 ** (-np.arange(16, dtype=np.float32) / 16)).astype(np.float32)
    C = np.zeros((64, 1536), np.float32)
    S = np.zeros((64, 1536), np.float32)
    for d in range(64):
        pos = row if d < 32 else col
        q = d % 32
        ang = (pos * inv[q % 16]).astype(np.float32)
        C[d] = np.cos(ang)
        S[d] = -np.sin(ang) if q < 16 else np.sin(ang)
    return np.concatenate([C, C], 0), np.concatenate([S, S], 0)


def perm_matrix():
    P = np.zeros((128, 128), np.float32)
    for m in range(128):
        blk, d = divmod(m, 64)
        q = d % 32
        partner = d + 16 if q < 16 else d - 16
        P[blk * 64 + partner, m] = 1.0
    return P


def na_bias_tables(rel_bias, R0):
    out = np.full((8, 128, 27, 128), NEG, np.float32)
    specs = []
    for c in range(5):
        specs.append((c, 2, 2 + c))
    for c in range(6):
        specs.append((5 + c, 0, c))
    for c in range(5):
        specs.append((11 + c, 1, 1 + c))
    for c in range(5):
        specs.append((16 + c, NLOC - 2, NLOC - 2 + c))
    for c in range(6):
        specs.append((21 + c, NLOC - 1, NLOC - 2 + c))
    kp = np.arange(128)
    qi = np.arange(128)
    for slot, rp, kt in specs:
        r = R0 + 2 * rp + qi // 64
        i = qi % 64
        kr = R0 - 4 + 2 * kt + kp // 64
        jc = kp % 64
        r0 = np.clip(r - 4, 0, 120)
        c0 = np.clip(i - 8, 0, 48)
        valid = ((kr[:, None] >= r0[None, :]) & (kr[:, None] < r0[None, :] + 8) & (kr[:, None] >= 0) & (kr[:, None] < 128)
                 & (jc[:, None] >= c0[None, :]) & (jc[:, None] < c0[None, :] + 16))
        dr = np.clip(kr[:, None] - r[None, :] + 7, 0, 14)
        dc = np.clip(jc[:, None] - i[None, :] + 15, 0, 30)
        vals = rel_bias[:, dr, dc]
        out[:, :, slot, :] = np.where(valid[None], vals, NEG)
    return out.reshape(8, 128, 27 * 128)


def wa_masks(gb0):
    kp = np.arange(128)[:, None]
    qi = np.arange(128)[None, :]
    prev = np.where(kp >= qi, 0.0, NEG).astype(np.float32)
    nxt = np.where(kp <= qi, 0.0, NEG).astype(np.float32)
    allneg = np.full((128, 128), NEG, np.float32)
    m = [prev, nxt, prev if gb0 > 0 else allneg, nxt if gb0 + NLOC < 64 else allneg]
    return np.concatenate([np.tile(x, (1, 4)) for x in m], axis=1)


def attn_inputs(inp, b, s):
    R0 = 16 * s
    x = inp["x"][b]
    xe = np.zeros((NTA * 128, D), np.float32)
    g0 = (R0 - 4) * 64
    lo, hi = max(g0, 0), min(g0 + 1536, 8192)
    xe[lo - g0:hi - g0] = x[lo:hi]
    xe[1536:] = inp["ctx"][b]
    w = inp["att_w_in"][0]
    win = np.concatenate([w[:, 0:512], w[:, 512:1024], w[:, 1536:2048], w[:, 2048:2112], w[:, 2048:2112],
                          w[:, 2112:2176], w[:, 2112:2176], w[:, 1024:1536], w[:, 2176:2304]], axis=1)
    gv = [inp["na_q_norm"][0], inp["na_k_norm"][0], inp["wa_q_norm"][0], inp["wa_k_norm"][0]]
    gains = np.stack([np.concatenate([g, g]) for g in gv], axis=1)
    rc, rs = rope_tables(g0)
    cin = np.stack([inp["c"][b].reshape(8, 128).T, inp["c_ctx"].reshape(8, 128).T], axis=1)
    ob = np.zeros((128, 128), np.float32)
    ob[:64, :64] = 1.0
    ob[64:, 64:] = 1.0
    return {
        "xe": xe, "cin": np.ascontiguousarray(cin, dtype=np.float32),
        "modw": np.ascontiguousarray(inp["mod_w"][0][:, 0:3 * D]), "modb": np.ascontiguousarray(inp["mod_b"][0][None, 0:3 * D]),
        "nrm": np.ascontiguousarray(inp["norm_mix"][0][None, :]),
        "win": np.ascontiguousarray(win), "wout": np.ascontiguousarray(inp["att_w_out"][0]),
        "gains": np.ascontiguousarray(gains, dtype=np.float32), "ropec": rc, "ropes": rs, "pmd": perm_matrix(),
        "onesd": ob, "identd": np.eye(128, dtype=np.float32),
        "nab": na_bias_tables(inp["na_rel_bias"][0], R0), "wam": wa_masks(R0 // 2),
        "sink": np.ascontiguousarray(inp["wa_sink"][0][None, :]),
    }


TS = 66
NSEQ = TS * 128
WS = 1056
TWO_PI = 2.0 * np.pi


def build_ssm(nt=TS, do_s5=True):
    nc = bass.Bass("TRN2", target_bir_lowering=False)
    dt_in = lambda n, s: nc.dram_tensor(n, s, F32, kind="ExternalInput").ap()
    xs = dt_in("xs", [NSEQ, D])
    cin = dt_in("cin", [128, 2, 8])
    modw = dt_in("modw", [D, 2 * D])
    modb = dt_in("modb", [1, 2 * D])
    nrm = dt_in("nrm", [1, D])
    wsel = dt_in("wsel", [D, WS])
    cw = dt_in("cw", [128, 6 * 3])
    cbias = dt_in("cbias", [128, 6])
    dtb = dt_in("dtb", [1, 8])
    alog = dt_in("alog", [1, 8])
    dsk = dt_in("dsk", [1, 8])
    identd = dt_in("identd", [128, 128])
    triud = dt_in("triud", [128, 128])
    iotad = dt_in("iotad", [128, 129])
    m01d = dt_in("m01d", [128, 512])
    lam = dt_in("lam", [128, 3 * 8])
    BLr = dt_in("BLr", [8, 128, 128])
    BLi = dt_in("BLi", [8, 128, 128])
    CLr = dt_in("CLr", [8, 128, 32])
    CLi = dt_in("CLi", [8, 128, 32])
    DL = dt_in("DL", [8, 128, 32])
    yssd = nc.dram_tensor("yssd", [NSEQ, 512], F32, kind="ExternalOutput").ap()
    ys5 = nc.dram_tensor("ys5", [256, NSEQ], F32, kind="ExternalOutput").ap()

    p = Prog(nc)
    UT = p.sb([128, 2, NSEQ], BF16, "UT")
    Z = p.sb([128, 2, NSEQ], F32, "Z")
    MB = [[p.sb([128, D], F32, f"mb{w}{v}") for v in range(2)] for w in range(2)]
    wsb = p.sb([128, 8, WS], BF16, "wsb")
    xt = p.sb([128, D], F32, "xt")
    hb = p.sb([128, D], BF16, "hb")
    hTi = p.sb([128, 8, 128], BF16, "hTi")
    stage = p.sb([128, 8, 128], F32, "stage")
    bstage = p.sb([128, 128], F32, "bstage")
    cbc = p.sb([128, 8, 128], F32, "cbc")
    ones = p.sb([128, 128], F32, "ones")
    identf = p.sb([128, 128], F32, "identf")
    ident = p.sb([128, 128], BF16, "ident")
    triu = p.sb([128, 128], F32, "triu")
    cin_t = p.sb([128, 2, 8], F32, "cin_t")
    small = {"ss": p.sb([128, 4], F32, "ss"), "rs": p.sb([128, 4], F32, "rs"), "junk": p.sb([128, D], F32, "junk")}
    CW = p.sb([128, 18], F32, "CWc")
    CBs = p.sb([128, 6], F32, "CBs")
    dtb_t = p.sb([128, 8], F32, "dtb_t")
    A_t = p.sb([128, 8], F32, "A_t")
    dsk_t = p.sb([128, 8], F32, "dsk_t")
    RAW = [p.sb([128, 6, 128], BF16, f"raw{i}") for i in range(3)]
    DTs = [p.sb([128, 8], F32, f"dts{i}") for i in range(3)]
    CBUF = p.sb([128, 6, 130], BF16, "CBUF")
    cacc6 = p.sb([128, 6, 128], F32, "cacc6")
    tmp6 = p.sb([128, 6, 128], F32, "tmp6")
    XC = p.sb([128, 6, 128], BF16, "XC")
    XTOK = p.sb([128, 512], BF16, "XTOK")
    BTOK = p.sb([128, 128], BF16, "BTOK")
    sm = p.sb([128, 64], F32, "sm")
    CBT = p.sb([128, 128], F32, "CBT")
    WT4 = [p.sb([128, 512], BF16, f"WT4{i}") for i in range(2)]
    H = p.sb([128, 512], F32, "H")
    Hb = p.sb([128, 512], BF16, "Hb")
    XW = p.sb([128, 512], BF16, "XW")
    ysb = p.sb([128, 512], F32, "ysb")
    ytmp = p.sb([128, 512], F32, "ytmp")
    B0, B1, B2, B3, B4, B5, B6 = [p.ps([128, 512], F32, f"pb{i}") for i in range(7)]
    ptr = p.ps([128, 8, 128], BF16, "ptr")

    p.dma("sp", identf[:, :], identd[:, :], (), [identf])
    p.cp("dve", ident[:, :], identf[:, :], [identf], [ident])
    p.dma("sp", triu[:, :], triud[:, :], (), [triu])
    p.memset("pool", ones[:, :], 1.0, [ones])
    p.memset("pool", H[:, :], 0.0, [H])
    p.dma("pool", wsb[:, :, :], wsel.rearrange("(kc p) n -> p kc n", p=128), (), [wsb])
    p.dma("sp", cin_t[:, :, :], cin[:, :, :], (), [cin_t])
    p.act(cin_t[:, :, :], cin_t[:, :, :], AF.Silu, [cin_t], [cin_t])
    p.dma("sp", xt[:, :], bcast_rows(nrm[0:1, :]), (), [xt])
    p.dma("sp", CW[:, :], cw[:, :], (), [CW])
    p.dma("sp", CBs[:, :], cbias[:, :], (), [CBs])
    p.dma("sp", dtb_t[:, :], bcast_rows(dtb[0:1, :]), (), [dtb_t])
    p.dma("sp", A_t[:, :], bcast_rows(alog[0:1, :]), (), [A_t])
    p.act(A_t[:, :], A_t[:, :], AF.Exp, [A_t], [A_t])
    p.ts("dve", A_t[:, :], A_t[:, :], -1.0, None, ALU.mult, None, [A_t], [A_t])
    p.dma("sp", dsk_t[:, :], bcast_rows(dsk[0:1, :]), (), [dsk_t])

    def outsel(which):
        def f(j):
            v, c = divmod(j, 8)
            t = MB[which][v]
            return t, t[:, c * 128:(c + 1) * 128]
        return f
    emit_mod_rows(p, cin_t, modw, modb, 2 * D, [outsel(0), outsel(1)], B0, stage, cbc, ones, bstage)
    for w in range(2):
        A = MB[w][1]
        p.stt("dve", A[:, :], A[:, :], 1.0, xt[:, :], ALU.add, ALU.mult, [A, xt], [A])

    PSUB = int(os.environ.get('PSUB', '9'))

    RMt = [alias(p, stage, f"RM{i}") for i in range(2)]
    RMv = [stage[:, 4 * i:4 * i + 4, :].rearrange("p c t -> p (c t)") for i in range(2)]
    LMt = [alias(p, cbc, f"LM{i}") for i in range(2)]
    LMv = [cbc[:, 4 * i:4 * i + 4, :].rearrange("p c t -> p (c t)") for i in range(2)]

    def project(i):
        if PSUB < 1:
            return
        w = 1 if i < 2 else 0
        raw, dts = RAW[i % 3], DTs[i % 3]
        p.dma("sp", xt[:, :], xs[i * 128:(i + 1) * 128, :], (), [xt])
        emit_adanorm_T(p, xt, MB[w][1], MB[w][0], hb, ptr, hTi[:, :, :], hTi, ident, small)
        if PSUB < 2:
            return
        PQ = int(os.environ.get('PQ', '9'))
        for grp in range(2):
            if grp == 1 and PQ < 3:
                break
            for c4 in range(4):
                ch = grp * 4 + c4
                for kc in range(8):
                    p.mm(B0[:, c4 * 128:(c4 + 1) * 128], wsb[:, kc, ch * 128:(ch + 1) * 128], hTi[:, kc, :], kc == 0,
                         kc == 7, [wsb, hTi], [B0])
            if grp == 0:
                if PQ >= 2:
                    p.cp("act", raw[:, 0:4, :], B0[:, :].rearrange("p (c t) -> p c t", c=4), [B0], [raw])
            else:
                if PQ >= 4:
                    p.cp("act", raw[:, 4:6, :], B0[:, 0:256].rearrange("p (c t) -> p c t", c=2), [B0], [raw])
                if PQ >= 5:
                    for c2 in range(2):
                        p.cp("act", UT[:, c2, i * 128:(i + 1) * 128], B0[:, 256 + c2 * 128:384 + c2 * 128], [B0], [UT])
        if PSUB < 3:
            return
        for kc in range(8):
            p.mm(B1[:, 0:8], hTi[:, kc, :], wsb[:, kc, 1024:1032], kc == 0, kc == 7, [hTi, wsb], [B1])
        p.tt("dve", dts[:, :], B1[:, 0:8], dtb_t[:, :], ALU.add, [B1, dtb_t], [dts])
        p.act(dts[:, :], dts[:, :], AF.Exp, [dts], [dts])
        p.act(dts[:, :], dts[:, :], AF.Ln, [dts], [dts], bias=1.0)

    a_, acs, tot, eacs, wend, dec = (sm[:, 8 * k:8 * k + 8] for k in range(6))

    SSUB = int(os.environ.get('SSUB', '9'))

    def ssd_chunk(j):
        raw, dts = RAW[j % 3], DTs[j % 3]
        if SSUB < 1:
            return
        first = j in (0, 2)
        last = j in (1, nt - 1)
        if first:
            p.memset("pool", CBUF[:, :, 0:1], 0.0, [CBUF])
        else:
            p.cp("pool", CBUF[:, :, 0:1], RAW[(j - 1) % 3][:, :, 127:128], [RAW[(j - 1) % 3]], [CBUF])
        p.cp("pool", CBUF[:, :, 1:129], raw[:, :, :], [raw], [CBUF])
        if last:
            p.memset("pool", CBUF[:, :, 129:130], 0.0, [CBUF])
        else:
            p.cp("pool", CBUF[:, :, 129:130], RAW[(j + 1) % 3][:, :, 0:1], [RAW[(j + 1) % 3]], [CBUF])
        cw3 = CW[:, :].rearrange("p (c k) -> p c k", k=3)
        wk = lambda k: cw3[:, :, k:k + 1].broadcast_to([128, 6, 128])
        p.tt("dve", cacc6[:, :, :], CBUF[:, :, 0:128], wk(0), ALU.mult, [CBUF, CW], [cacc6])
        p.tt("pool", tmp6[:, :, :], CBUF[:, :, 1:129], wk(1), ALU.mult, [CBUF, CW], [tmp6])
        p.tt("dve", cacc6[:, :, :], cacc6[:, :, :], tmp6[:, :, :], ALU.add, [cacc6, tmp6], [cacc6])
        p.tt("pool", tmp6[:, :, :], CBUF[:, :, 2:130], wk(2), ALU.mult, [CBUF, CW], [tmp6])
        p.tt("dve", cacc6[:, :, :], cacc6[:, :, :], tmp6[:, :, :], ALU.add, [cacc6, tmp6], [cacc6])
        p.tt("dve", cacc6[:, :, :], cacc6[:, :, :], CBs[:, :].rearrange("p (c o) -> p c o", o=1).broadcast_to([128, 6, 128]),
             ALU.add, [cacc6, CBs], [cacc6])
        p.act(XC[:, :, :], cacc6[:, :, :], AF.Silu, [cacc6], [XC])
        if SSUB < 2:
            return
        for c in range(5):
            p.tr(ptr[:, c, :], XC[:, c, :], ident[:, :], [XC, ident], [ptr])
        p.cp("act", XTOK[:, :], ptr[:, 0:4, :].rearrange("p c t -> p (c t)"), [ptr], [XTOK])
        p.cp("act", BTOK[:, :], ptr[:, 4, :], [ptr], [BTOK])
        p.tt("dve", a_, dts[:, :], A_t[:, :], ALU.mult, [dts, A_t], [sm])
        p.mm(B1[:, 0:8], triu[:, :], a_, True, True, [triu, sm], [B1])
        p.mm(B1[:, 128:136], ones[:, :], a_, True, True, [ones, sm], [B1])
        p.cp("dve", acs, B1[:, 0:8], [B1], [sm])
        p.cp("dve", tot, B1[:, 128:136], [B1], [sm])
        if SSUB < 3:
            return
        p.mm(B2[:, 0:128], XC[:, 4, :], XC[:, 5, :], True, True, [XC], [B2])
        p.tt("dve", CBT[:, :], B2[:, 0:128], triu[:, :], ALU.mult, [B2, triu], [CBT])
        p.cp("act", Hb[:, :], H[:, :], [H], [Hb])
        v4 = lambda ap: ap.rearrange("p (h t) -> p h t", h=4)
        b4 = lambda ap: ap.rearrange("p (h o) -> p h o", o=1).broadcast_to([128, 4, 128])
        o4 = lambda ap: ap.rearrange("p (o t) -> p o t", o=1).broadcast_to([128, 4, 128])
        for g4 in range(2):
            hs = slice(g4 * 4, g4 * 4 + 4)
            rt_, rv_, lt_, lv_, wt4, Pb = RMt[g4], RMv[g4], LMt[g4], LMv[g4], WT4[g4], (B3, B2)[g4]
            p.tt("dve", v4(rv_), o4(triu[:, :]), b4(a_[:, hs]), ALU.mult, [triu, sm], [rt_])
            p.mm(Pb[:, :], ones[:, :], rv_, True, True, [ones, rt_], [Pb])
            p.tt("dve", v4(lv_), v4(Pb[:, :]), b4(acs[:, hs]), ALU.subtract, [Pb, sm], [lt_])
            p.ts("dve", lv_, lv_, 0.0, None, ALU.min, None, [lt_], [lt_])
            p.act(lv_, lv_, AF.Exp, [lt_], [lt_])
            p.tt("dve", v4(lv_), v4(lv_), b4(dts[:, hs]), ALU.mult, [lt_, dts], [lt_])
            p.tt("dve", v4(wt4[:, :]), v4(lv_), o4(CBT[:, :]), ALU.mult, [lt_, CBT], [wt4])
            for hh in range(4):
                hd = g4 * 4 + hh
                p.mm(B4[:, hd * 64:(hd + 1) * 64], wt4[:, hh * 128:(hh + 1) * 128], XTOK[:, hd * 64:(hd + 1) * 64], True, True,
                     [wt4, XTOK], [B4])
        if SSUB < 4:
            return
        p.mm(B5[:, :], XC[:, 5, :], Hb[:, :], True, True, [XC, Hb], [B5])
        p.act(eacs, acs, AF.Exp, [sm], [sm])
        v3 = lambda ap: ap.rearrange("p (h d) -> p h d", h=8)
        bc = lambda ap: ap.rearrange("p (h o) -> p h o", o=1).broadcast_to([128, 8, 64])
        p.tt("dve", v3(ytmp[:, :]), v3(B5[:, :]), bc(eacs), ALU.mult, [B5, sm], [ytmp])
        p.tt("dve", ysb[:, :], ytmp[:, :], B4[:, :], ALU.add, [ytmp, B4], [ysb])
        p.tt("dve", v3(ytmp[:, :]), v3(XTOK[:, :]), bc(dsk_t[:, :]), ALU.mult, [XTOK, dsk_t], [ytmp])
        p.tt("dve", ysb[:, :], ysb[:, :], ytmp[:, :], ALU.add, [ysb, ytmp], [ysb])
        p.dma("sp", yssd[j * 128:(j + 1) * 128, :], ysb[:, :], [ysb], ())
        if SSUB < 5:
            return
        p.tt("dve", wend, tot, acs, ALU.subtract, [sm], [sm])
        p.act(wend, wend, AF.Exp, [sm], [sm])
        p.tt("dve", wend, wend, dts[:, :], ALU.mult, [sm, dts], [sm])
        p.act(dec, tot, AF.Exp, [sm], [sm])
        p.tt("dve", v3(XW[:, :]), v3(XTOK[:, :]), bc(wend), ALU.mult, [XTOK, sm], [XW])
        p.mm(B6[:, :], BTOK[:, :], XW[:, :], True, True, [BTOK, XW], [B6])
        p.tt("dve", v3(H[:, :]), v3(H[:, :]), bc(dec), ALU.mult, [H, sm], [H])
        p.tt("dve", H[:, :], H[:, :], B6[:, :], ALU.add, [H, B6], [H])

    for i in range(nt + 1):
        if i < nt:
            project(i)
        if i >= 1:
            ssd_chunk(i - 1)

    if do_s5:
        LAM = p.sb([128, 24], F32, "LAM")
        dsc = p.sb([128, 16], F32, "dsc")
        iota = p.sb([128, 129], F32, "iota")
        ang = p.sb([128, 129], F32, "ang")
        mag = p.sb([128, 129], F32, "mag")
        cs_ = p.sb([128, 129], F32, "cs_")
        sn_ = p.sb([128, 129], F32, "sn_")
        EP = p.sb([128, 2, 512], F32, "EP")
        EN = p.sb([128, 2, 512], F32, "EN")
        m01 = p.sb([128, 512], F32, "m01")
        blr = p.sb([128, 128], BF16, "blr")
        bli = p.sb([128, 128], BF16, "bli")
        clr = p.sb([128, 32], BF16, "clr")
        cli = p.sb([128, 32], BF16, "cli")
        dl = p.sb([128, 32], BF16, "dl")
        q1 = p.sb([128, 512], F32, "q1")
        q2 = p.sb([128, 512], F32, "q2")
        WR = [p.sb([128, 512], BF16, f"wr_{i}") for i in range(2)]
        WI = [p.sb([128, 512], BF16, f"wi_{i}") for i in range(2)]
        XA = p.sb([128, 2, 66], F32, "XA")
        XB = p.sb([128, 2, 66], F32, "XB")
        tq66 = ang
        pw = p.sb([128, 2, 8], F32, "pw")
        yo = p.sb([32, 512], F32, "yo")
        twopi = p.sb([128, 129], F32, "twopi")
        kint = p.sb([128, 129], mybir.dt.int32, "kint")
        p.dma("sp", LAM[:, :], lam[:, :], (), [LAM])
        p.dma("sp", iota[:, :], iotad[:, :], (), [iota])
        p.dma("sp", m01[:, :], m01d[:, :], (), [m01])
        nblk = (nt * 128 + 511) // 512
        for pr in range(8):
            lr, li, ls = LAM[:, pr:pr + 1], LAM[:, 8 + pr:9 + pr], LAM[:, 16 + pr:17 + pr]
            sc = lambda k: dsc[:, k:k + 1]
            R, W_ = [LAM, dsc, iota, ang, mag, cs_, sn_], [dsc]
            step, lrs, th, den, cr, ci, t0_, t1_ = (sc(k) for k in range(8))
            p.act(step, ls, AF.Exp, [LAM], [dsc])
            p.tt("dve", lrs, lr, step, ALU.mult, [LAM, dsc], [dsc])
            p.tt("dve", th, li, step, ALU.mult, [LAM, dsc], [dsc])
            p.ts("dve", ang[:, :], iota[:, :], th, None, ALU.mult, None, [iota, dsc], [ang])
            p.ts("dve", cs_[:, :], ang[:, :], 1.5 * np.pi, None, ALU.add, None, [ang], [cs_])
            p.ts("dve", twopi[:, :], cs_[:, :], 1.0 / TWO_PI, None, ALU.mult, None, [cs_], [twopi])
            p.cp("dve", kint[:, :], twopi[:, :], [twopi], [kint])
            p.cp("dve", twopi[:, :], kint[:, :], [kint], [twopi])
            p.stt("dve", cs_[:, :], twopi[:, :], -TWO_PI, cs_[:, :], ALU.mult, ALU.add, [twopi, cs_], [cs_])
            p.ts("dve", twopi[:, :], cs_[:, :], 0.0, None, ALU.is_lt, None, [cs_], [twopi])
            p.stt("dve", cs_[:, :], twopi[:, :], TWO_PI, cs_[:, :], ALU.mult, ALU.add, [twopi, cs_], [cs_])
            p.ts("dve", sn_[:, :], ang[:, :], np.pi, None, ALU.add, None, [ang], [sn_])
            p.ts("dve", twopi[:, :], sn_[:, :], 1.0 / TWO_PI, None, ALU.mult, None, [sn_], [twopi])
            p.cp("dve", kint[:, :], twopi[:, :], [twopi], [kint])
            p.cp("dve", twopi[:, :], kint[:, :], [kint], [twopi])
            p.stt("dve", sn_[:, :], twopi[:, :], -TWO_PI, sn_[:, :], ALU.mult, ALU.add, [twopi, sn_], [sn_])
            p.ts("dve", twopi[:, :], sn_[:, :], 0.0, None, ALU.is_lt, None, [sn_], [twopi])
            p.stt("dve", sn_[:, :], twopi[:, :], TWO_PI, sn_[:, :], ALU.mult, ALU.add, [twopi, sn_], [sn_])
            p.act(cs_[:, :], cs_[:, :], AF.Sin, [cs_], [cs_], bias=-np.pi)
            p.act(sn_[:, :], sn_[:, :], AF.Sin, [sn_], [sn_], bias=-np.pi)
            p.act(mag[:, :], iota[:, :], AF.Exp, [iota, dsc], [mag], scale=lrs)
            ar, ai, a128r, a128i = (sc(k) for k in range(8, 12))
            p.tt("dve", ar, mag[:, 1:2], cs_[:, 1:2], ALU.mult, [mag, cs_], [dsc])
            p.tt("dve", ai, mag[:, 1:2], sn_[:, 1:2], ALU.mult, [mag, sn_], [dsc])
            p.tt("dve", a128r, mag[:, 128:129], cs_[:, 128:129], ALU.mult, [mag, cs_], [dsc])
            p.tt("dve", a128i, mag[:, 128:129], sn_[:, 128:129], ALU.mult, [mag, sn_], [dsc])
            p.tt("dve", den, lr, lr, ALU.mult, [LAM], [dsc])
            p.stt("dve", den, li, li, den, ALU.mult, ALU.add, [LAM, dsc], [dsc])
            p.op("dve", lambda g, den=den: g.reciprocal(den, den), [dsc], [dsc])
            p.ts("dve", t0_, ar, -1.0, None, ALU.add, None, [dsc], [dsc])
            p.tt("dve", t1_, ai, li, ALU.mult, [dsc, LAM], [dsc])
            p.stt("dve", cr, t0_, lr, t1_, ALU.mult, ALU.add, [dsc, LAM], [dsc])
            p.tt("dve", cr, cr, den, ALU.mult, [dsc], [dsc])
            p.tt("dve", t1_, t0_, li, ALU.mult, [dsc, LAM], [dsc])
            p.stt("dve", ci, ai, lr, t1_, ALU.mult, ALU.subtract, [dsc, LAM], [dsc])
            p.tt("dve", ci, ci, den, ALU.mult, [dsc], [dsc])
            p.tt("dve", EP[:, 0, 0:128], mag[:, 0:128], cs_[:, 0:128], ALU.mult, [mag, cs_], [EP])
            p.tt("dve", EP[:, 1, 0:128], mag[:, 0:128], sn_[:, 0:128], ALU.mult, [mag, sn_], [EP])
            p.op("dve", lambda g: g.reciprocal(mag[:, :], mag[:, :]), [mag], [mag])
            p.tt("dve", cs_[:, :], cs_[:, :], mag[:, :], ALU.mult, [cs_, mag], [cs_])
            p.tt("dve", sn_[:, :], sn_[:, :], mag[:, :], ALU.mult, [sn_, mag], [sn_])
            p.ts("dve", ang[:, :], sn_[:, :], ci, None, ALU.mult, None, [sn_, dsc], [ang])
            p.stt("dve", EN[:, 0, 0:128], cs_[:, 0:128], cr, ang[:, 0:128], ALU.mult, ALU.add, [cs_, dsc, ang], [EN])
            p.ts("dve", ang[:, :], sn_[:, :], cr, None, ALU.mult, None, [sn_, dsc], [ang])
            p.stt("dve", EN[:, 1, 0:128], cs_[:, 0:128], ci, ang[:, 0:128], ALU.mult, ALU.subtract, [cs_, dsc, ang], [EN])
            for rep in range(1, 4):
                p.cp("pool", EP[:, :, rep * 128:(rep + 1) * 128], EP[:, :, 0:128], [EP], [EP])
                p.cp("pool", EN[:, :, rep * 128:(rep + 1) * 128], EN[:, :, 0:128], [EN], [EN])
            p.dma("pool", blr[:, :], BLr[pr], (), [blr])
            p.dma("pool", bli[:, :], BLi[pr], (), [bli])
            p.dma("pool", clr[:, :], CLr[pr], (), [clr])
            p.dma("pool", cli[:, :], CLi[pr], (), [cli])
            p.ts("dve", cli[:, :], cli[:, :], -1.0, None, ALU.mult, None, [cli], [cli])
            p.dma("pool", dl[:, :], DL[pr], (), [dl])
            uc = pr // 4
            for b in range(nblk):
                t0 = b * 512
                n = min(512, nt * 128 - t0)
                Pr, Pi = ((B0, B1), (B3, B4))[b % 2]
                p.mm(Pr[:, 0:n], blr[:, :], UT[:, uc, t0:t0 + n], True, True, [blr, UT], [Pr])
                p.mm(Pi[:, 0:n], bli[:, :], UT[:, uc, t0:t0 + n], True, True, [bli, UT], [Pi])
                p.tt("dve", q1[:, 0:n], Pr[:, 0:n], EN[:, 0, 0:n], ALU.mult, [Pr, EN], [q1])
                p.tt("dve", q2[:, 0:n], Pi[:, 0:n], EN[:, 1, 0:n], ALU.mult, [Pi, EN], [q2])
                p.tt("dve", q1[:, 0:n], q1[:, 0:n], q2[:, 0:n], ALU.subtract, [q1, q2], [q1])
                p.op("dve", lambda g, t0=t0, n=n: g.tensor_tensor_scan(Z[:, 0, t0:t0 + n], m01[:, 0:n], q1[:, 0:n], 0.0,
                                                                        ALU.mult, ALU.add), [m01, q1], [Z])
                p.tt("dve", q2[:, 0:n], Pi[:, 0:n], EN[:, 0, 0:n], ALU.mult, [Pi, EN], [q2])
                p.tt("dve", q1[:, 0:n], Pr[:, 0:n], EN[:, 1, 0:n], ALU.mult, [Pr, EN], [q1])
                p.tt("dve", q1[:, 0:n], q1[:, 0:n], q2[:, 0:n], ALU.add, [q1, q2], [q1])
                p.op("dve", lambda g, t0=t0, n=n: g.tensor_tensor_scan(Z[:, 1, t0:t0 + n], m01[:, 0:n], q1[:, 0:n], 0.0,
                                                                        ALU.mult, ALU.add), [m01, q1], [Z])
            zend = lambda ri: Z[:, ri, 0:nt * 128].rearrange("p (c t) -> p c t", t=128)[:, :, 127]
            cur, nxt = XA, XB
            CH = [Z, dsc, XA, XB, tq66, pw]
            p.ts("dve", tq66[:, 0:nt], zend(1), a128i, None, ALU.mult, None, CH, [tq66])
            p.stt("dve", cur[:, 0, 0:nt], zend(0), a128r, tq66[:, 0:nt], ALU.mult, ALU.subtract, CH, [cur])
            p.ts("dve", tq66[:, 0:nt], zend(0), a128i, None, ALU.mult, None, CH, [tq66])
            p.stt("dve", cur[:, 1, 0:nt], zend(1), a128r, tq66[:, 0:nt], ALU.mult, ALU.add, CH, [cur])
            p.cp("dve", pw[:, 0, 0:1], a128r, [dsc], [pw])
            p.cp("dve", pw[:, 1, 0:1], a128i, [dsc], [pw])
            k = 0
            while (1 << k) < nt:
                sft = 1 << k
                n = nt - sft
                Ar, Ai = pw[:, 0, k:k + 1], pw[:, 1, k:k + 1]
                p.cp("dve", nxt[:, :, 0:sft], cur[:, :, 0:sft], [cur], [nxt])
                p.ts("dve", tq66[:, 0:n], cur[:, 1, 0:n], Ai, None, ALU.mult, None, [cur, pw], [tq66])
                p.stt("dve", nxt[:, 0, sft:nt], cur[:, 0, 0:n], Ar, tq66[:, 0:n], ALU.mult, ALU.subtract, [cur, pw, tq66], [nxt])
                p.tt("dve", nxt[:, 0, sft:nt], nxt[:, 0, sft:nt], cur[:, 0, sft:nt], ALU.add, [cur, nxt], [nxt])
                p.ts("dve", tq66[:, 0:n], cur[:, 0, 0:n], Ai, None, ALU.mult, None, [cur, pw], [tq66])
                p.stt("dve", nxt[:, 1, sft:nt], cur[:, 1, 0:n], Ar, tq66[:, 0:n], ALU.mult, ALU.add, [cur, pw, tq66], [nxt])
                p.tt("dve", nxt[:, 1, sft:nt], nxt[:, 1, sft:nt], cur[:, 1, sft:nt], ALU.add, [cur, nxt], [nxt])
                p.tt("dve", pw[:, 0, k + 1:k + 2], Ai, Ai, ALU.mult, [pw], [pw])
                p.stt("dve", pw[:, 0, k + 1:k + 2], Ar, Ar, pw[:, 0, k + 1:k + 2], ALU.mult, ALU.subtract, [pw], [pw])
                p.stt("dve", pw[:, 1, k + 1:k + 2], Ar, 2.0, Ai, ALU.mult, ALU.mult, [pw], [pw])
                cur, nxt = nxt, cur
                k += 1
            if nt > 1:
                for ri, eng in ((0, "dve"), (1, "pool")):
                    zv = Z[:, ri, 128:nt * 128].rearrange("p (c t) -> p c t", t=128)
                    gb = cur[:, ri, 0:nt - 1].rearrange("p (c o) -> p c o", o=1).broadcast_to([128, nt - 1, 128])
                    p.tt(eng, zv, zv, gb, ALU.add, [Z, cur], [Z])
            for b in range(nblk):
                t0 = b * 512
                n = min(512, nt * 128 - t0)
                wr_, wi_ = WR[b % 2], WI[b % 2]
                p.tt("dve", q1[:, 0:n], Z[:, 0, t0:t0 + n], EP[:, 0, 0:n], ALU.mult, [Z, EP], [q1])
                p.tt("dve", q2[:, 0:n], Z[:, 1, t0:t0 + n], EP[:, 1, 0:n], ALU.mult, [Z, EP], [q2])
                p.tt("dve", wr_[:, 0:n], q1[:, 0:n], q2[:, 0:n], ALU.subtract, [q1, q2], [wr_])
                p.tt("dve", q2[:, 0:n], Z[:, 1, t0:t0 + n], EP[:, 0, 0:n], ALU.mult, [Z, EP], [q2])
                p.tt("dve", q1[:, 0:n], Z[:, 0, t0:t0 + n], EP[:, 1, 0:n], ALU.mult, [Z, EP], [q1])
                p.tt("dve", wi_[:, 0:n], q1[:, 0:n], q2[:, 0:n], ALU.add, [q1, q2], [wi_])
                Py = (B2, B5)[b % 2]
                p.mm(Py[0:32, 0:n], clr[:, :], wr_[:, 0:n], True, False, [clr, wr_], [Py])
                p.mm(Py[0:32, 0:n], cli[:, :], wi_[:, 0:n], False, False, [cli, wi_], [Py])
                p.mm(Py[0:32, 0:n], dl[:, :], UT[:, uc, t0:t0 + n], False, True, [dl, UT], [Py])
                p.cp("act", yo[:, 0:n], Py[0:32, 0:n], [Py], [yo])
                p.dma("sp", ys5[pr * 32:(pr + 1) * 32, t0:t0 + n], yo[:, 0:n], [yo], ())
    p.emit()
    return nc


def ssm_inputs(inp, xseq, b, dr, hf, nt=TS):
    w = inp["ssm_w_in"][0]
    cols = np.concatenate([np.arange(1024 + 512 * hf, 1024 + 512 * hf + 512), np.arange(2048 + 128 * hf, 2048 + 128 * hf + 128),
                           np.arange(2304 + 128 * hf, 2304 + 128 * hf + 128), np.arange(2592 + 256 * hf, 2592 + 256 * hf + 256),
                           np.arange(2560 + 16 * dr + 8 * hf, 2560 + 16 * dr + 8 * hf + 8)])
    cch = np.concatenate([np.arange(512 * hf, 512 * hf + 512), np.arange(1024 + 128 * hf, 1024 + 128 * hf + 128),
                          np.arange(1280 + 128 * hf, 1280 + 128 * hf + 128)])
    cwf = inp["ssd_conv_w"][0][:, cch]
    if dr == 1:
        cwf = cwf[::-1]
    cw = np.ascontiguousarray(cwf.T.reshape(6, 128, 3).transpose(1, 0, 2).reshape(128, 18))
    cb = np.ascontiguousarray(inp["ssd_conv_b"][0][cch].reshape(6, 128).T)
    hs = slice(8 * hf, 8 * hf + 8)
    zero8 = np.zeros((1, 8), np.float32)
    gs = 16 * hf
    lam = np.zeros((128, 24), np.float32)
    BLr = np.zeros((8, 128, 128), np.float32)
    BLi = np.zeros((8, 128, 128), np.float32)
    CLr = np.zeros((8, 128, 32), np.float32)
    CLi = np.zeros((8, 128, 32), np.float32)
    DLm = np.zeros((8, 128, 32), np.float32)
    sd = inp["s5_d"][0]
    for pr in range(8):
        for k in range(2):
            g = gs + 2 * pr + k
            rows = slice(64 * k, 64 * k + 64)
            lam[rows, pr] = inp["s5_lambda_re"][0, dr, g]
            lam[rows, 8 + pr] = inp["s5_lambda_im"][0, dr, g]
            lam[rows, 16 + pr] = inp["s5_log_step"][0, dr, g]
            ur = 32 * (pr % 4) + 16 * k
            BLr[pr, ur:ur + 16, rows] = inp["s5_b_re"][0, dr, g].T
            BLi[pr, ur:ur + 16, rows] = inp["s5_b_im"][0, dr, g].T
            CLr[pr, rows, 16 * k:16 * k + 16] = inp["s5_c_re"][0, dr, g].T
            CLi[pr, rows, 16 * k:16 * k + 16] = inp["s5_c_im"][0, dr, g].T
            if dr == 0:
                for c in range(16):
                    DLm[pr, ur + c, 16 * k + c] = sd[g * 16 + c]
    m01 = np.ones((128, 512), np.float32)
    m01[:, ::128] = 0.0
    cin = np.stack([inp["c"][b].reshape(8, 128).T, inp["c_ctx"].reshape(8, 128).T], axis=1)
    return {
        "xs": np.ascontiguousarray(xseq, dtype=np.float32), "cin": np.ascontiguousarray(cin, dtype=np.float32),
        "modw": np.ascontiguousarray(inp["mod_w"][1][:, 0:2 * D]), "modb": np.ascontiguousarray(inp["mod_b"][1][None, 0:2 * D]),
        "nrm": np.ascontiguousarray(inp["norm_mix"][1][None, :]),
        "wsel": np.ascontiguousarray(np.concatenate([w[:, cols], np.zeros((D, WS - 1032), np.float32)], axis=1)), "cw": cw, "cbias": cb,
        "dtb": np.ascontiguousarray(inp["ssd_dt_bias"][0, dr, hs][None, :]),
        "alog": np.ascontiguousarray(inp["ssd_a_log"][0, dr, hs][None, :]),
        "dsk": np.ascontiguousarray(inp["ssd_d"][0, hs][None, :]) if dr == 0 else zero8,
        "identd": np.eye(128, dtype=np.float32), "triud": np.triu(np.ones((128, 128), np.float32)),
        "iotad": np.tile(np.arange(129, dtype=np.float32)[None, :], (128, 1)), "m01d": m01,
        "lam": lam, "BLr": BLr, "BLi": BLi, "CLr": CLr, "CLi": CLi, "DL": DLm,
    }


NTT = 16
GELU_C = 1.5957691216057308


def build_fin():
    nc = bass.Bass("TRN2", target_bir_lowering=False)
    dt_in = lambda n, s: nc.dram_tensor(n, s, F32, kind="ExternalInput").ap()
    x = dt_in("x", [NTT * 128, D])
    cin = dt_in("cin", [128, 2, 8])
    modw = dt_in("modw", [D, 3 * D])
    modb = dt_in("modb", [1, 3 * D])
    nrm = dt_in("nrm", [1, D])
    wz = dt_in("wz", [D, D])
    yf = dt_in("yf", [NTT * 128, D])
    yb = dt_in("yb", [NTT * 128, D])
    vf = dt_in("vf", [NTT * 128, 512])
    vb = dt_in("vb", [NTT * 128, 512])
    snorm = dt_in("snorm", [1, D])
    gluw = dt_in("gluw", [512, 512])
    glub = dt_in("glub", [1, 512])
    wout = dt_in("wout", [1536, D])
    identd = dt_in("identd", [128, 128])
    xo = nc.dram_tensor("xo", [NTT * 128, D], F32, kind="ExternalOutput").ap()

    p = Prog(nc)
    wzb = p.sb([128, 8, D], BF16, "wzb")
    woutb = p.sb([128, 12, D], BF16, "woutb")
    gwb = p.sb([128, 4, 512], BF16, "gwb")
    MB = [p.sb([128, D], F32, f"mb{v}") for v in range(3)]
    xt = p.sb([128, D], F32, "xt")
    hb = p.sb([128, D], BF16, "hb")
    hTi = p.sb([128, 8, 128], BF16, "hTi")
    stage = p.sb([128, 8, 128], F32, "stage")
    bstage = p.sb([128, 128], F32, "bstage")
    cbc = p.sb([128, 8, 128], F32, "cbc")
    ones = p.sb([128, 128], F32, "ones")
    identf = p.sb([128, 128], F32, "identf")
    ident = p.sb([128, 128], BF16, "ident")
    cin_t = p.sb([128, 2, 8], F32, "cin_t")
    small = {"ss": p.sb([128, 4], F32, "ss"), "rs": p.sb([128, 4], F32, "rs"), "junk": p.sb([128, D], F32, "junk")}
    sn_bc = p.sb([128, D], F32, "sn_bc")
    gb_bc = p.sb([128, 512], F32, "gb_bc")
    zs = p.sb([128, D], F32, "zs")
    ya = p.sb([128, D], F32, "ya")
    ybt = p.sb([128, D], F32, "ybt")
    va = p.sb([128, 512], F32, "va")
    vbt = p.sb([128, 512], F32, "vbt")
    v2 = p.sb([128, 512], F32, "v2")
    gvb = p.sb([128, 512], BF16, "gvb")
    cat = p.sb([128, 1536], BF16, "cat")
    gT = p.sb([128, 4, 128], BF16, "gT")
    cT = p.sb([128, 12, 128], BF16, "cT")
    st2 = p.sb([128, 4], F32, "st2")
    Z0, Z1, G, O0, O1, B0 = [p.ps([128, 512], F32, f"pb{i}") for i in range(6)]
    ptr = p.ps([128, 8, 128], BF16, "ptr")

    p.dma("sp", identf[:, :], identd[:, :], (), [identf])
    p.cp("dve", ident[:, :], identf[:, :], [identf], [ident])
    p.memset("pool", ones[:, :], 1.0, [ones])
    p.dma("pool", wzb[:, :, :], wz.rearrange("(kc p) n -> p kc n", p=128), (), [wzb])
    p.dma("pool", gwb[:, :, :], gluw.rearrange("(kc p) n -> p kc n", p=128), (), [gwb])
    p.dma("pool", woutb[:, :, :], wout.rearrange("(kc p) n -> p kc n", p=128), (), [woutb])
    p.dma("sp", cin_t[:, :, :], cin[:, :, :], (), [cin_t])
    p.act(cin_t[:, :, :], cin_t[:, :, :], AF.Silu, [cin_t], [cin_t])
    p.dma("sp", xt[:, :], bcast_rows(nrm[0:1, :]), (), [xt])
    p.dma("sp", sn_bc[:, :], bcast_rows(snorm[0:1, :]), (), [sn_bc])
    p.dma("sp", gb_bc[:, :], bcast_rows(glub[0:1, :]), (), [gb_bc])

    def outsel(j):
        v, c = divmod(j, 8)
        t = MB[v]
        return t, t[:, c * 128:(c + 1) * 128]
    emit_mod_rows(p, cin_t, modw, modb, 3 * D, [outsel, outsel], B0, stage, cbc, ones, bstage, whichs=(0,))
    A = MB[1]
    p.stt("dve", A[:, :], A[:, :], 1.0, xt[:, :], ALU.add, ALU.mult, [A, xt], [A])

    for i in range(NTT):
        rows = slice(i * 128, (i + 1) * 128)
        p.dma("sp", xt[:, :], x[rows, :], (), [xt])
        emit_adanorm_T(p, xt, MB[1], MB[0], hb, ptr, hTi[:, :, :], hTi, ident, small)
        for half, Zp in enumerate((Z0, Z1)):
            for kc in range(8):
                p.mm(Zp[:, :], hTi[:, kc, :], wzb[:, kc, half * 512:(half + 1) * 512], kc == 0, kc == 7, [hTi, wzb], [Zp])
            p.act(zs[:, half * 512:(half + 1) * 512], Zp[:, :], AF.Silu, [Zp], [zs])
        p.dma("sp", ya[:, :], yf[rows, :], (), [ya])
        p.dma("sp", ybt[:, :], yb[rows, :], (), [ybt])
        p.tt("pool", ya[:, :], ya[:, :], ybt[:, :], ALU.add, [ya, ybt], [ya])
        p.tt("dve", ya[:, :], ya[:, :], zs[:, :], ALU.mult, [ya, zs], [ya])
        j = small["junk"]
        p.memset("dve", st2[:, 0:1], 0.0, [st2])
        p.act(j[:, :], ya[:, :], AF.Square, [ya, st2], [j, st2], accum_out=st2[:, 0:1])
        p.ts("dve", st2[:, 1:2], st2[:, 0:1], 1.0 / D, EPS, ALU.mult, ALU.add, [st2], [st2])
        p.op("act", lambda g: g.sqrt(st2[:, 2:3], st2[:, 1:2]), [st2], [st2])
        p.op("dve", lambda g: g.reciprocal(st2[:, 3:4], st2[:, 2:3]), [st2], [st2])
        p.stt("dve", cat[:, 0:1024], ya[:, :], st2[:, 3:4], sn_bc[:, :], ALU.mult, ALU.mult, [ya, st2, sn_bc], [cat])
        p.dma("sp", va[:, :], vf[rows, :], (), [va])
        p.dma("sp", vbt[:, :], vb[rows, :], (), [vbt])
        p.tt("pool", va[:, :], va[:, :], vbt[:, :], ALU.add, [va, vbt], [va])
        p.tt("dve", v2[:, :], va[:, :], va[:, :], ALU.mult, [va], [v2])
        p.ts("dve", v2[:, :], v2[:, :], 0.044715, 1.0, ALU.mult, ALU.add, [v2], [v2])
        p.tt("dve", v2[:, :], v2[:, :], va[:, :], ALU.mult, [v2, va], [v2])
        p.act(v2[:, :], v2[:, :], AF.Sigmoid, [v2], [v2], scale=GELU_C)
        p.tt("dve", va[:, :], va[:, :], v2[:, :], ALU.mult, [va, v2], [va])
        p.cp("dve", gvb[:, :], va[:, :], [va], [gvb])
        for c in range(4):
            p.tr(ptr[:, c, :], gvb[:, c * 128:(c + 1) * 128], ident[:, :], [gvb, ident], [ptr])
        p.cp("act", gT[:, :, :], ptr[:, 0:4, :], [ptr], [gT])
        for c in range(4):
            p.mm(G[:, :], gT[:, c, :], gwb[:, c, :], c == 0, c == 3, [gT, gwb], [G])
        p.tt("dve", v2[:, :], G[:, :], gb_bc[:, :], ALU.add, [G, gb_bc], [v2])
        p.act(v2[:, :], v2[:, :], AF.Sigmoid, [v2], [v2])
        p.tt("dve", cat[:, 1024:1536], va[:, :], v2[:, :], ALU.mult, [va, v2], [cat])
        for c in range(8):
            p.tr(ptr[:, c, :], cat[:, c * 128:(c + 1) * 128], ident[:, :], [cat, ident], [ptr])
        p.cp("act", cT[:, 0:8, :], ptr[:, :, :], [ptr], [cT])
        for c in range(4):
            p.tr(ptr[:, c, :], cat[:, 1024 + c * 128:1024 + (c + 1) * 128], ident[:, :], [cat, ident], [ptr])
        p.cp("act", cT[:, 8:12, :], ptr[:, 0:4, :], [ptr], [cT])
        for half, Op in enumerate((O0, O1)):
            for c in range(12):
                p.mm(Op[:, :], cT[:, c, :], woutb[:, c, half * 512:(half + 1) * 512], c == 0, c == 11, [cT, woutb], [Op])
        for half, Op in enumerate((O0, O1)):
            sl = slice(half * 512, (half + 1) * 512)
            p.tt("dve", j[:, sl], Op[:, :], MB[2][:, sl], ALU.mult, [Op, MB[2]], [j])
        p.tt("dve", j[:, :], j[:, :], xt[:, :], ALU.add, [j, xt], [j])
        p.dma("sp", xo[rows, :], j[:, :], [j], ())
    p.emit()
    return nc


def fin_inputs(inp, b, xcore, yfc, ybc, vfc, vbc):
    cin = np.stack([inp["c"][b].reshape(8, 128).T, inp["c_ctx"].reshape(8, 128).T], axis=1)
    ca = lambda a: np.ascontiguousarray(a, dtype=np.float32)
    return {
        "x": ca(xcore), "cin": ca(cin), "modw": ca(inp["mod_w"][1][:, 0:3 * D]), "modb": ca(inp["mod_b"][1][None, 0:3 * D]),
        "nrm": ca(inp["norm_mix"][1][None, :]), "wz": ca(inp["ssm_w_in"][0][:, 0:1024]),
        "yf": ca(yfc), "yb": ca(ybc), "vf": ca(vfc), "vb": ca(vbc),
        "snorm": ca(inp["ssd_norm"][0][None, :]), "gluw": ca(inp["s5_glu_w"][0]), "glub": ca(inp["s5_glu_b"][0][None, :]),
        "wout": ca(inp["ssm_w_out"][0]), "identd": np.eye(128, dtype=np.float32),
    }


_CACHE = {}


def _prog(name, fn):
    if name not in _CACHE:
        _CACHE[name] = fn()
    return _CACHE[name]


def kernel(**inputs):
    inp = {k: np.asarray(v) for k, v in inputs.items()}
    C8 = list(range(8))
    xl = np.empty((2, 8192, D), np.float32)
    xc = np.empty((2, 256, D), np.float32)
    for half in range(2):
        vcs = [(b, s) for b in range(2) for s in range(4 * half, 4 * half + 4)]
        res = run_bass_kernel_spmd(build_attn(), [attn_inputs(inp, b, s) for b, s in vcs], core_ids=C8)
        for (b, s), r in zip(vcs, res.results):
            xl[b, s * 1024:(s + 1) * 1024] = r["xo"][:1024]
            if s == 0:
                xc[b] = r["xo"][1024:]

    def moe(L, xl_in, xc_in):
        cores = [(b, q) for b in range(2) for q in range(4)]
        ims = [moe_inputs(np.concatenate([xl_in[b, q * 2048:(q + 1) * 2048], xc_in[b]], axis=0), inp["c"][b], inp["c_ctx"],
                          inp["mod_w"][L], inp["mod_b"][L], inp["norm_ffn"][L], inp["moe_w_group"][L], inp["moe_b_group"][L],
                          inp["moe_w_expert"][L], inp["moe_b_expert"][L], inp["moe_w13"][L], inp["moe_w2"][L])
               for b, q in cores]
        res = run_bass_kernel_spmd(build_moe(), ims, core_ids=C8)
        xo = np.empty_like(xl_in)
        xco = np.empty_like(xc_in)
        for (b, q), r in zip(cores, res.results):
            xo[b, q * 2048:(q + 1) * 2048] = r["xo"][:2048]
            if q == 0:
                xco[b] = r["xo"][2048:]
        return xo, xco

    xl, xc = moe(0, xl, xc)
    cores = [(b, dr, hf) for b in range(2) for dr in range(2) for hf in range(2)]
    ims = []
    for b, dr, hf in cores:
        seq = np.concatenate([xc[b], xl[b]], axis=0) if dr == 0 else np.concatenate([xc[b][::-1], xl[b][::-1]], axis=0)
        ims.append(ssm_inputs(inp, seq, b, dr, hf))
    res = run_bass_kernel_spmd(build_ssm(), ims, core_ids=C8)
    Y = np.empty((2, 2, 8192, D), np.float32)
    V = np.empty((2, 2, 8192, 512), np.float32)
    for (b, dr, hf), r in zip(cores, res.results):
        y = r["yssd"][256:]
        v = r["ys5"][:, 256:].T
        if dr == 1:
            y, v = y[::-1], v[::-1]
        Y[b, dr, :, hf * 512:(hf + 1) * 512] = y
        V[b, dr, :, hf * 256:(hf + 1) * 256] = v
    cores = [(b, q) for b in range(2) for q in range(4)]
    ims = []
    for b, q in cores:
        sl = slice(q * 2048, (q + 1) * 2048)
        ims.append(fin_inputs(inp, b, xl[b, sl], Y[b, 0, sl], Y[b, 1, sl], V[b, 0, sl], V[b, 1, sl]))
    res = run_bass_kernel_spmd(build_fin(), ims, core_ids=C8)
    for (b, q), r in zip(cores, res.results):
        xl[b, q * 2048:(q + 1) * 2048] = r["xo"]
    xl, _ = moe(1, xl, np.zeros_like(xc))
    return xl
```

```python
import os
import numpy as np
import concourse.bass as bass
import concourse.mybir as mybir
from concourse.bass_utils import run_bass_kernel_spmd

F32 = mybir.dt.float32
BF16 = mybir.dt.bfloat16
AF = mybir.ActivationFunctionType
ALU = mybir.AluOpType
AX = mybir.AxisListType

EPOCH = 12000
D = 1024
EPS = 1e-6


class Res:
    __slots__ = ("name", "w", "readers", "dsem", "dcnt")

    def __init__(self, name):
        self.name = name
        self.w = None
        self.readers = {}
        self.dsem = None
        self.dcnt = 0


class T:
    def __init__(self, h, name):
        self.h = h
        self.r = Res(name)

    def __getitem__(self, k):
        return self.h[k]


class Prog:
    def __init__(self, nc):
        self.nc = nc
        self.eng = {"pe": nc.tensor, "dve": nc.vector, "act": nc.scalar, "pool": nc.gpsimd, "sp": nc.sync}
        self.ops = {k: [] for k in self.eng}
        self.cnt = {k: 0 for k in self.eng}
        self.esems = {k: [] for k in self.eng}
        self.known = {k: {} for k in self.eng}
        self.dres = []
        self.nsem = 0
        self.nt = 0

    def sem(self, name):
        self.nsem += 1
        return self.nc.alloc_semaphore(name)

    def sb(self, shape, dt=F32, name=None):
        self.nt += 1
        name = name or f"t{self.nt}"
        return T(self.nc.alloc_sbuf_tensor(name, list(shape), dt), name)

    def ps(self, shape, dt=F32, name=None):
        self.nt += 1
        name = name or f"p{self.nt}"
        return T(self.nc.alloc_psum_tensor(name, list(shape), dt), name)

    def _esem(self, e, ep):
        while len(self.esems[e]) <= ep:
            self.esems[e].append(self.sem(f"s_{e}_{len(self.esems[e])}"))
        return self.esems[e][ep]

    def _need(self, e, ev, waits):
        sem, val, src = ev
        if src == "pe" and e == "pe":
            return
        k = id(sem)
        if self.known[e].get(k, 0) >= val:
            return
        self.known[e][k] = val
        waits.append((sem, val))

    def _deps(self, e, reads, writes):
        waits = []
        for t in reads:
            if t.r.w is not None:
                self._need(e, t.r.w, waits)
        for t in writes:
            if t.r.w is not None:
                self._need(e, t.r.w, waits)
            for ev in t.r.readers.values():
                self._need(e, ev, waits)
        return waits

    def _mark(self, ev, reads, writes):
        for t in writes:
            t.r.w = ev
            t.r.readers = {}
        for t in reads:
            if t not in writes:
                old = t.r.readers.get(id(ev[0]))
                if old is None or old[1] < ev[1]:
                    t.r.readers[id(ev[0])] = ev

    def op(self, e, fn, reads=(), writes=()):
        waits = self._deps(e, reads, writes)
        idx = self.cnt[e]
        self.cnt[e] += 1
        sem = self._esem(e, idx // EPOCH)
        ev = (sem, idx % EPOCH + 1, e)
        self._mark(ev, reads, writes)
        self.ops[e].append((waits, fn, (sem, 1)))

    def dma(self, e, out, in_, reads=(), writes=(), sres=None):
        waits = self._deps(e, reads, writes)
        t = sres or (writes[0] if writes else reads[0])
        r = t.r
        if r.dsem is None or r.dcnt + 16 > 30000:
            r.dsem = self.sem(f"d_{r.name}_{self.nsem}")
            r.dcnt = 0
            self.dres.append(r)
        r.dcnt += 16
        ev = (r.dsem, r.dcnt, "dma")
        self._mark(ev, reads, writes)
        self.ops[e].append((waits, lambda eng: eng.dma_start(out=out, in_=in_), (r.dsem, 16)))

    def finish(self):
        finals = {}
        for r in self.dres:
            finals[id(r.dsem)] = (r.dsem, max(finals.get(id(r.dsem), (None, 0))[1], r.dcnt))
        waits = []
        for sem, val in finals.values():
            if self.known["sp"].get(id(sem), 0) < val:
                waits.append((sem, val))
        for e in ("pe", "dve", "act", "pool"):
            n = self.cnt[e]
            if n:
                waits.append((self._esem(e, (n - 1) // EPOCH), (n - 1) % EPOCH + 1))
        self.ops["sp"].append((waits, None, None))

    def emit(self):
        self.finish()
        with self.nc.Block() as block:
            decos = {"sp": block.sync, "pe": block.tensor, "dve": block.vector, "act": block.scalar,
                     "pool": block.gpsimd}
            for e in ("sp", "pe", "dve", "act", "pool"):
                def body(engine, e=e):
                    for waits, fn, inc in self.ops[e]:
                        for sem, val in waits:
                            engine.wait_ge(sem, val)
                        if fn is not None:
                            fn(engine).then_inc(inc[0], inc[1])
                decos[e](body)

    def mm(self, out, lhsT, rhs, start, stop, reads, writes):
        self.op("pe", lambda g: g.matmul(out, lhsT, rhs, start=start, stop=stop), reads, writes)

    def tr(self, out, in_, ident, reads, writes):
        self.op("pe", lambda g: g.transpose(out, in_, ident), reads, writes)

    def act(self, out, in_, func, reads, writes, bias=None, scale=None, accum_out=None, e="act"):
        kw = {}
        if bias is not None:
            kw["bias"] = bias
        if scale is not None:
            kw["scale"] = scale
        if accum_out is not None:
            kw["accum_out"] = accum_out
        self.op("act", lambda g: g.activation(out, in_, func, **kw), reads, writes)

    def ts(self, e, out, in0, s1, s2, op0, op1, reads, writes):
        if op1 is None:
            self.op(e, lambda g: g.tensor_scalar(out, in0, s1, None, op0), reads, writes)
        else:
            self.op(e, lambda g: g.tensor_scalar(out, in0, s1, s2, op0, op1), reads, writes)

    def tt(self, e, out, in0, in1, op, reads, writes):
        self.op(e, lambda g: g.tensor_tensor(out, in0, in1, op), reads, writes)

    def stt(self, e, out, in0, sc, in1, op0, op1, reads, writes):
        self.op(e, lambda g: g.scalar_tensor_tensor(out, in0, sc, in1, op0, op1), reads, writes)

    def cp(self, e, out, in_, reads, writes):
        if e == "act":
            self.op(e, lambda g: g.copy(out, in_), reads, writes)
        else:
            self.op(e, lambda g: g.tensor_copy(out, in_), reads, writes)

    def memset(self, e, ap, v, writes):
        self.op(e, lambda g: g.memset(ap, v), (), writes)


def bcast_rows(ap, n=128):
    return ap.partition_broadcast(n)


def emit_mod_rows(p, cin_t, modw, modb, ncols, outs, psum, stage, cbc, ones, bstage, whichs=(0, 1)):
    nblk = ncols // 128
    for which in whichs:
        for kc in range(8):
            p.ts("dve", cbc[:, kc, :], ones[:, :], cin_t[:, which, kc:kc + 1], None, ALU.mult, None,
                 [ones, cin_t], [cbc])
        for j in range(nblk):
            p.dma("sp", stage[:, :, :], modw[:, j * 128:(j + 1) * 128].rearrange("(kc p) n -> p kc n", p=128),
                  (), [stage])
            p.dma("sp", bstage[:, :], bcast_rows(modb[0:1, j * 128:(j + 1) * 128]), (), [bstage])
            for kc in range(8):
                p.mm(psum[:, 0:128], cbc[:, kc, :], stage[:, kc, :], kc == 0, kc == 7, [cbc, stage], [psum])
            tile, ap = outs[which](j)
            p.tt("dve", ap, psum[:, 0:128], bstage[:, :], ALU.add, [psum, bstage], [tile])


def emit_adanorm_T(p, x_t, A_t, S_t, hb, ptr, hT_ap, hT_t, ident, small):
    ss, rs, junk = small["ss"], small["rs"], small["junk"]
    p.memset("dve", ss[:, 0:1], 0.0, [ss])
    p.act(junk[:, :], x_t[:, :], AF.Square, [x_t, ss], [junk, ss], accum_out=ss[:, 0:1])
    p.ts("dve", rs[:, 0:1], ss[:, 0:1], 1.0 / D, EPS, ALU.mult, ALU.add, [ss], [rs])
    p.op("act", lambda g: g.sqrt(rs[:, 1:2], rs[:, 0:1]), [rs], [rs])
    p.op("dve", lambda g: g.reciprocal(rs[:, 2:3], rs[:, 1:2]), [rs], [rs])
    p.stt("dve", junk[:, :], x_t[:, :], rs[:, 2:3], A_t[:, :], ALU.mult, ALU.mult, [x_t, rs, A_t], [junk])
    p.tt("dve", hb[:, :], junk[:, :], S_t[:, :], ALU.add, [junk, S_t], [hb])
    for kc in range(8):
        p.tr(ptr[:, kc, :], hb[:, kc * 128:(kc + 1) * 128], ident[:, :], [hb, ident], [ptr])
    p.cp("act", hT_ap, ptr[:, :, :], [ptr], [hT_t])


NT_MOE = 18
NLAT_MOE = 16


def build_moe(n_exp=32, NT=NT_MOE):
    nc = bass.Bass("TRN2", target_bir_lowering=False)
    x = nc.dram_tensor("x", [NT * 128, D], F32, kind="ExternalInput").ap()
    cin = nc.dram_tensor("cin", [128, 2, 8], F32, kind="ExternalInput").ap()
    modw = nc.dram_tensor("modw", [D, 3 * D], F32, kind="ExternalInput").ap()
    modb = nc.dram_tensor("modb", [1, 3 * D], F32, kind="ExternalInput").ap()
    nrm = nc.dram_tensor("nrm", [1, D], F32, kind="ExternalInput").ap()
    wr = nc.dram_tensor("wr", [D, 36], F32, kind="ExternalInput").ap()
    br = nc.dram_tensor("br", [1, 36], F32, kind="ExternalInput").ap()
    w13 = nc.dram_tensor("w13", [32, D, D], F32, kind="ExternalInput").ap()
    w2 = nc.dram_tensor("w2", [32, 512, D], F32, kind="ExternalInput").ap()
    identd = nc.dram_tensor("identd", [128, 128], F32, kind="ExternalInput").ap()
    xo = nc.dram_tensor("xo", [NT * 128, D], F32, kind="ExternalOutput").ap()

    p = Prog(nc)
    hT = p.sb([128, 8, NT * 128], BF16, "hT")
    acc = p.sb([128, NT, D], F32, "acc")
    w13b = [p.sb([128, 8, D], BF16, f"w13b{i}") for i in range(2)]
    w2b = [p.sb([128, 4, D], BF16, "w2b0")]
    MB = [[p.sb([128, D], F32, f"mb{w}{v}") for v in range(3)] for w in range(2)]
    xt = p.sb([128, D], F32, "xt")
    hb = p.sb([128, D], BF16, "hb")
    stage = p.sb([128, 8, 128], F32, "stage")
    bstage = p.sb([128, 128], F32, "bstage")
    actT = [p.sb([128, 4, 512], BF16, f"actT{i}") for i in range(2)]
    sa = p.sb([128, 512], F32, "sa")
    cbc = p.sb([128, 8, 128], F32, "cbc")
    CW = p.sb([128, NT, 32], F32, "CW")
    ones = p.sb([128, 128], F32, "ones")
    identf = p.sb([128, 128], F32, "identf")
    ident = p.sb([128, 128], BF16, "ident")
    cin_t = p.sb([128, 2, 8], F32, "cin_t")
    nrm_t = xt
    wrf = p.sb([128, 8, 36], F32, "wrf")
    wrb = p.sb([128, 8, 36], BF16, "wrb")
    brb = p.sb([128, 36], F32, "brb")
    small = {"ss": p.sb([128, 4], F32, "ss"), "rs": p.sb([128, 4], F32, "rs"), "junk": p.sb([128, D], F32, "junk")}
    rt = p.sb([128, 256], F32, "rt")
    pbank = [p.ps([128, 512], F32, f"pb{i}") for i in range(6)]
    ptr = p.ps([128, 8, 128], BF16, "ptr")

    p.dma("sp", identf[:, :], identd[:, :], (), [identf])
    p.cp("dve", ident[:, :], identf[:, :], [identf], [ident])
    p.memset("pool", ones[:, :], 1.0, [ones])
    p.memset("pool", acc[:, :, :], 0.0, [acc])
    p.dma("sp", cin_t[:, :, :], cin[:, :, :], (), [cin_t])
    p.act(cin_t[:, :, :], cin_t[:, :, :], AF.Silu, [cin_t], [cin_t])
    p.dma("sp", nrm_t[:, :], bcast_rows(nrm[0:1, :]), (), [nrm_t])
    p.dma("sp", wrf[:, :, :], wr.rearrange("(kc p) n -> p kc n", p=128), (), [wrf])
    p.cp("dve", wrb[:, :, :], wrf[:, :, :], [wrf], [wrb])
    p.dma("sp", brb[:, :], bcast_rows(br[0:1, :]), (), [brb])

    def outsel(which):
        def f(j):
            v, c = divmod(j, 8)
            t = MB[which][v]
            return t, t[:, c * 128:(c + 1) * 128]
        return f
    emit_mod_rows(p, cin_t, modw, modb, 3 * D, [outsel(0), outsel(1)], pbank[0], stage, cbc, ones, bstage)
    for w in range(2):
        A = MB[w][1]
        p.stt("dve", A[:, :], A[:, :], 1.0, nrm_t[:, :], ALU.add, ALU.mult, [A, nrm_t], [A])

    for i in range(NT):
        w = 0 if i < NLAT_MOE else 1
        p.dma("sp", xt[:, :], x[i * 128:(i + 1) * 128, :], (), [xt])
        emit_adanorm_T(p, xt, MB[w][1], MB[w][0], hb, ptr, hT[:, :, i * 128:(i + 1) * 128], hT, ident, small)
        lgp = pbank[1]
        for kc in range(8):
            p.mm(lgp[:, 0:36], hT[:, kc, i * 128:(i + 1) * 128], wrb[:, kc, :], kc == 0, kc == 7, [hT, wrb], [lgp])
        lg = rt[:, 0:36]
        p.tt("dve", lg, lgp[:, 0:36], brb[:, :], ALU.add, [lgp, brb], [rt])
        R, W_ = [rt], [rt]
        gmax, nb, se, m1, m2, den = (rt[:, 40 + k:41 + k] for k in range(6))
        oh = rt[:, 48:52]
        pen = rt[:, 52:56]
        m32 = rt[:, 64:96]
        e32 = rt[:, 96:128]
        m32b = rt[:, 128:160]
        sel = rt[:, 160:192]
        gex = rt[:, 192:196]
        p.op("dve", lambda g, gmax=gmax, lg=lg: g.reduce_max(gmax, lg[:, 0:4], AX.X), R, W_)
        p.ts("dve", oh, lg[:, 0:4], gmax, None, ALU.is_ge, None, R, W_)
        p.ts("dve", nb, gmax, -1.0, None, ALU.mult, None, R, W_)
        p.act(gex, lg[:, 0:4], AF.Exp, R, W_, bias=nb, accum_out=se)
        p.ts("dve", pen, oh, 1e9, -1e9, ALU.mult, ALU.add, R, W_)
        p.tt("dve", m32.rearrange("p (g e) -> p g e", g=4), lg[:, 4:36].rearrange("p (g e) -> p g e", g=4),
             pen.to_broadcast([128, 4, 8]) if False else rt[:, 52:56].rearrange("p (g o) -> p g o", o=1).broadcast_to([128, 4, 8]),
             ALU.add, R, W_)
        p.op("dve", lambda g, m1=m1, m32=m32: g.reduce_max(m1, m32, AX.X), R, W_)
        p.ts("dve", nb, m1, -1.0, None, ALU.mult, None, R, W_)
        p.act(e32, m32, AF.Exp, R, W_, bias=nb)
        p.ts("dve", m32b, m32, m1, -1e9, ALU.is_ge, ALU.mult, R, W_)
        p.tt("dve", m32b, m32b, m32, ALU.add, R, W_)
        p.op("dve", lambda g, m2=m2, m32b=m32b: g.reduce_max(m2, m32b, AX.X), R, W_)
        p.ts("dve", sel, m32, m2, None, ALU.is_ge, None, R, W_)
        p.tt("dve", sel, sel, e32, ALU.mult, R, W_)
        p.op("dve", lambda g, sel=sel, den=den: g.reduce_sum(den, sel, AX.X), R, W_)
        p.tt("dve", den, den, se, ALU.mult, R, W_)
        p.op("dve", lambda g, den=den: g.reciprocal(den, den), R, W_)
        p.ts("dve", CW[:, i, :], sel, den, None, ALU.mult, None, [rt], [CW])

    blocks = [(b * 4, 4) for b in range(NLAT_MOE // 4)] + ([(NLAT_MOE, NT - NLAT_MOE)] if NT > NLAT_MOE else [])
    pa = [pbank[0], pbank[1]]
    pbb = [pbank[2], pbank[3]]
    po = [pbank[4], pbank[5]]
    cnt_ab = 0
    cnt_o = 0
    cnt_act = 0
    for e in range(n_exp):
        wa = w13b[e % 2]
        p.dma("pool", wa[:, :, :], w13[e].rearrange("(kc p) n -> p kc n", p=128), (), [wa])
        if e % 2 == 0:
            wb = w2b[0]
            p.dma("pool", wb[:, :, :], w2[e].rearrange("(kc p) n -> p kc n", p=128), (), [wb])
            wbv = [(wb, wb[:, fc, :]) for fc in range(4)]
        else:
            wbv = []
            for hh, tl in enumerate((stage, cbc)):
                v = tl.h.bitcast(BF16)[:, :, :].rearrange("p a b -> p (a b)").rearrange("p (f n) -> p f n", f=2)
                p.dma("pool", v, w2[e][hh * 256:(hh + 1) * 256, :].rearrange("(kc p) n -> p kc n", p=128), (), [tl])
                wbv += [(tl, v[:, 0, :]), (tl, v[:, 1, :])]
        for (t0, nt) in blocks:
            ntok = nt * 128
            at = actT[cnt_act % 2]
            cnt_act += 1
            for fc in range(4):
                A_, B_ = pa[cnt_ab % 2], pbb[cnt_ab % 2]
                cnt_ab += 1
                for kc in range(8):
                    p.mm(A_[:, 0:ntok], wa[:, kc, fc * 128:(fc + 1) * 128], hT[:, kc, t0 * 128:t0 * 128 + ntok],
                         kc == 0, kc == 7, [wa, hT], [A_])
                for kc in range(8):
                    p.mm(B_[:, 0:ntok], wa[:, kc, 512 + fc * 128:512 + (fc + 1) * 128],
                         hT[:, kc, t0 * 128:t0 * 128 + ntok], kc == 0, kc == 7, [wa, hT], [B_])
                p.act(sa[:, 0:ntok], A_[:, 0:ntok], AF.Silu, [A_], [sa])
                p.tt("dve", at[:, fc, 0:ntok], sa[:, 0:ntok], B_[:, 0:ntok], ALU.mult, [sa, B_], [at])
            for tt_ in range(nt):
                ti = t0 + tt_
                for half in range(2):
                    O_ = po[cnt_o % 2]
                    cnt_o += 1
                    for fc in range(4):
                        wt_, wv_ = wbv[fc]
                        p.mm(O_[:, :], at[:, fc, tt_ * 128:(tt_ + 1) * 128], wv_[:, half * 512:(half + 1) * 512],
                             fc == 0, fc == 3, [at, wt_], [O_])
                    accs = acc[:, ti, half * 512:(half + 1) * 512]
                    p.stt("dve", accs, O_[:, :], CW[:, ti, e:e + 1], accs, ALU.mult, ALU.add, [O_, CW, acc], [acc])

    for i in range(NT):
        w = 0 if i < NLAT_MOE else 1
        p.dma("sp", xt[:, :], x[i * 128:(i + 1) * 128, :], (), [xt])
        j = small["junk"]
        p.tt("dve", j[:, :], acc[:, i, :], MB[w][2][:, :], ALU.mult, [acc, MB[w][2]], [j])
        p.tt("dve", j[:, :], j[:, :], xt[:, :], ALU.add, [j, xt], [j])
        p.dma("sp", xo[i * 128:(i + 1) * 128, :], j[:, :], [j], ())
    p.emit()
    return nc


def moe_inputs(x_core, c_b, c_ctx, mod_w_i, mod_b_i, norm_ffn_i, wg, bg, we, be, w13, w2):
    cin = np.stack([c_b.reshape(8, 128).T, c_ctx.reshape(8, 128).T], axis=1)
    return {
        "x": np.ascontiguousarray(x_core, dtype=np.float32),
        "cin": np.ascontiguousarray(cin, dtype=np.float32),
        "modw": np.ascontiguousarray(mod_w_i[:, 3 * D:6 * D]),
        "modb": np.ascontiguousarray(mod_b_i[None, 3 * D:6 * D]),
        "nrm": np.ascontiguousarray(norm_ffn_i[None, :]),
        "wr": np.ascontiguousarray(np.concatenate([wg, we], axis=1)),
        "br": np.ascontiguousarray(np.concatenate([bg, be])[None, :]),
        "w13": w13, "w2": w2,
        "identd": np.eye(128, dtype=np.float32),
    }


NTA = 14
NLOC = 8
WCOLS = 2432
NEG = -30000.0


def alias(p, t, name):
    a = T(t.h, name)
    a.r.w = t.r.w
    a.r.readers = dict(t.r.readers)
    return a


def build_attn(ph=9, dbg=False, NSLAB=2):
    nc = bass.Bass("TRN2", target_bir_lowering=False)
    dt_in = lambda n, s: nc.dram_tensor(n, s, F32, kind="ExternalInput").ap()
    xe = dt_in("xe", [NSLAB * NTA * 128, D])
    cin = dt_in("cin", [128, 2, 8])
    modw = dt_in("modw", [D, 3 * D])
    modb = dt_in("modb", [1, 3 * D])
    nrm = dt_in("nrm", [1, D])
    win = dt_in("win", [D, WCOLS])
    wout = dt_in("wout", [D, D])
    gains = dt_in("gains", [128, 4])
    ropec = dt_in("ropec", [128, NSLAB * 1536])
    ropes = dt_in("ropes", [128, NSLAB * 1536])
    pmd = dt_in("pmd", [128, 128])
    onesd = dt_in("onesd", [128, 128])
    identd = dt_in("identd", [128, 128])
    nab = dt_in("nab", [NSLAB * 8, 128, 27 * 128])
    wam = dt_in("wam", [128, NSLAB * 4 * 512])
    sink = dt_in("sink", [1, 8])
    xo = nc.dram_tensor("xo", [NSLAB * (NLOC + 2) * 128, D], F32, kind="ExternalOutput").ap()

    p = Prog(nc)
    NTOK = NTA * 128
    big = p.sb([128, 8 * NTOK], BF16, "big")
    hTv = big[:, :].rearrange("p (kc t) -> p kc t", kc=8)
    big2 = p.sb([128, 8 * WCOLS], BF16, "big2")
    winv = big2[:, :].rearrange("p (kc n) -> p kc n", kc=8)
    QT = p.sb([128, 14, NTOK], BF16, "QT")
    VA = p.sb([128, NTA, 8, 65], BF16, "VA")
    VB = p.sb([128, NTA, 2, 65], BF16, "VB")
    PT = [p.sb([128, 8, 128], BF16, f"PT{i}") for i in range(2)]
    PTWt = p.sb([128, 5, 512], BF16, "PTWs")
    PTW = PTWt.h
    MB = [[p.sb([128, D], F32, f"mb{w}{v}") for v in range(3)] for w in range(2)]
    xt = p.sb([128, D], F32, "xt")
    hb = p.sb([128, D], BF16, "hb")
    stage = p.sb([128, 8, 128], F32, "stage")
    bstage = p.sb([128, 128], F32, "bstage")
    cbc = p.sb([128, 8, 128], F32, "cbc")
    ones = p.sb([128, 128], F32, "ones")
    identf = p.sb([128, 128], F32, "identf")
    ident = p.sb([128, 128], BF16, "ident")
    onesb = p.sb([128, 128], BF16, "onesb")
    pm = p.sb([128, 128], BF16, "pm")
    cin_t = p.sb([128, 2, 8], F32, "cin_t")
    G = p.sb([128, 4], F32, "G")
    RC = p.sb([128, 1536], BF16, "RC")
    RS = p.sb([128, 1536], BF16, "RS")
    WM = p.sb([128, 4, 512], BF16, "WM")
    esink = p.sb([128, 8], F32, "esink")
    small = {"ss": p.sb([128, 4], F32, "ss"), "rs": p.sb([128, 4], F32, "rs"), "junk": p.sb([128, D], F32, "junk")}
    sq = p.sb([128, 512], BF16, "sq")
    rstd = p.sb([128, 512], F32, "rstd")
    qn = p.sb([128, 512], BF16, "qn")
    t1 = p.sb([128, 512], F32, "t1")
    rec = p.sb([128, 8], F32, "rec")
    oT = p.sb([128, 8, 128], BF16, "oT")
    pbank = [p.ps([128, 512], F32, f"pb{i}") for i in range(7)]
    ptr = p.ps([128, 8, 128], BF16, "ptr")

    p.dma("sp", identf[:, :], identd[:, :], (), [identf])
    p.cp("dve", ident[:, :], identf[:, :], [identf], [ident])
    p.dma("pool", onesb[:, :], onesd[:, :], (), [onesb])
    p.dma("pool", pm[:, :], pmd[:, :], (), [pm])
    p.memset("pool", ones[:, :], 1.0, [ones])
    p.memset("pool", VA[:, :, :, :], 1.0, [VA])
    p.memset("pool", VB[:, :, :, :], 1.0, [VB])
    p.dma("sp", cin_t[:, :, :], cin[:, :, :], (), [cin_t])
    p.act(cin_t[:, :, :], cin_t[:, :, :], AF.Silu, [cin_t], [cin_t])
    p.dma("sp", xt[:, :], bcast_rows(nrm[0:1, :]), (), [xt])
    p.dma("sp", G[:, :], gains[:, :], (), [G])
    p.ts("dve", G[:, 0:1], G[:, 0:1], 0.125, None, ALU.mult, None, [G], [G])
    p.ts("dve", G[:, 2:3], G[:, 2:3], 0.125, None, ALU.mult, None, [G], [G])
    p.dma("sp", esink[:, :], bcast_rows(sink[0:1, :]), (), [esink])
    p.act(esink[:, :], esink[:, :], AF.Exp, [esink], [esink])

    def outsel(which):
        def f(j):
            v, c = divmod(j, 8)
            t = MB[which][v]
            return t, t[:, c * 128:(c + 1) * 128]
        return f
    emit_mod_rows(p, cin_t, modw, modb, 3 * D, [outsel(0), outsel(1)], pbank[0], stage, cbc, ones, bstage)
    for w in range(2):
        A = MB[w][1]
        p.stt("dve", A[:, :], A[:, :], 1.0, xt[:, :], ALU.add, ALU.mult, [A, xt], [A])

    prev_alias = []
    for slab in range(NSLAB):
        for base, al in prev_alias:
            for ev in ([al.r.w] if al.r.w is not None else []) + list(al.r.readers.values()):
                old = base.r.readers.get(id(ev[0]))
                if old is None or old[1] < ev[1]:
                    base.r.readers[id(ev[0])] = ev
        xe_s = xe[slab * NTA * 128:(slab + 1) * NTA * 128, :]
        p.dma("pool", RC[:, :], ropec[:, slab * 1536:(slab + 1) * 1536], (), [RC])
        p.dma("pool", RS[:, :], ropes[:, slab * 1536:(slab + 1) * 1536], (), [RS])
        p.dma("pool", WM[:, :, :], wam[:, slab * 2048:(slab + 1) * 2048].rearrange("p (s q) -> p s q", s=4), (), [WM])
        p.dma("pool", winv, win.rearrange("(kc p) n -> p kc n", p=128), (), [big2])
        for i in range(NTA):
            w = 0 if i < 12 else 1
            p.dma("sp", xt[:, :], xe_s[i * 128:(i + 1) * 128, :], (), [xt])
            emit_adanorm_T(p, xt, MB[w][1], MB[w][0], hb, ptr, hTv[:, :, i * 128:(i + 1) * 128], big, ident, small)

        blocks = [(0, 512), (512, 512), (1024, 512), (1536, 256)]
        cntp = 0
        for ch in range(14 if ph >= 2 else 0):
            gi = 0 if ch < 4 else 1 if ch < 8 else 2 if ch < 12 else 3
            rope = ch >= 8
            for (t0, n) in blocks:
                pq = pbank[cntp % 2]
                pmm = pbank[2 + cntp % 2]
                cntp += 1
                for kc in range(8):
                    p.mm(pq[:, 0:n], winv[:, kc, ch * 128:(ch + 1) * 128], hTv[:, kc, t0:t0 + n], kc == 0, kc == 7,
                         [big2, big], [pq])
                p.act(sq[:, 0:n], pq[:, 0:n], AF.Square, [pq], [sq])
                p.mm(pmm[:, 0:n], onesb[:, :], sq[:, 0:n], True, True, [onesb, sq], [pmm])
                p.ts("dve", rstd[:, 0:n], pmm[:, 0:n], 1.0 / 64, EPS, ALU.mult, ALU.add, [pmm], [rstd])
                p.op("act", lambda g, n=n: g.sqrt(rstd[:, 0:n], rstd[:, 0:n]), [rstd], [rstd])
                p.op("dve", lambda g, n=n: g.reciprocal(rstd[:, 0:n], rstd[:, 0:n]), [rstd], [rstd])
                if rope and t0 < 1536:
                    p.stt("dve", qn[:, 0:n], pq[:, 0:n], G[:, gi:gi + 1], rstd[:, 0:n], ALU.mult, ALU.mult,
                          [pq, G, rstd], [qn])
                    pr = pbank[4 + cntp % 2]
                    p.mm(pr[:, 0:n], pm[:, :], qn[:, 0:n], True, True, [pm, qn], [pr])
                    p.tt("pool", t1[:, 0:n], qn[:, 0:n], RC[:, t0:t0 + n], ALU.mult, [qn, RC], [t1])
                    p.tt("dve", rstd[:, 0:n], pr[:, 0:n], RS[:, t0:t0 + n], ALU.mult, [pr, RS], [rstd])
                    p.tt("dve", QT[:, ch, t0:t0 + n], t1[:, 0:n], rstd[:, 0:n], ALU.add, [t1, rstd], [QT])
                else:
                    p.stt("dve", QT[:, ch, t0:t0 + n], pq[:, 0:n], G[:, gi:gi + 1], rstd[:, 0:n], ALU.mult, ALU.mult,
                          [pq, G, rstd], [QT])

        for i in range(NTA if ph >= 3 else 0):
            pv, pv2 = pbank[cntp % 2], pbank[2 + cntp % 2]
            cntp += 1
            for kc in range(8):
                p.mm(pv[:, :], hTv[:, kc, i * 128:(i + 1) * 128], winv[:, kc, 1792:2304], kc == 0, kc == 7, [big, big2], [pv])
            for kc in range(8):
                p.mm(pv2[:, 0:128], hTv[:, kc, i * 128:(i + 1) * 128], winv[:, kc, 2304:2432], kc == 0, kc == 7,
                     [big, big2], [pv2])
            p.cp("act", VA[:, i, :, 0:64], pv[:, :].rearrange("p (h d) -> p h d", h=8), [pv], [VA])
            p.cp("dve", VB[:, i, :, 0:64], pv2[:, 0:128].rearrange("p (h d) -> p h d", h=2), [pv2], [VB])

        OAt = alias(p, big, "OA")
        OA = big[:, 0:(NLOC + 2) * 1024].rearrange("p (t d) -> p t d", d=1024)
        WO = alias(p, big2, "WO")
        wov = big2[:, 0:8192].rearrange("p (kc n) -> p kc n", kc=8)
        NABt = [alias(p, big2, f"NAB{i}") for i in range(2)]
        nabv = [big2[:, 8192 + i * 3456:8192 + (i + 1) * 3456].rearrange("p (s q) -> p s q", q=128) for i in range(2)]
        p.dma("pool", wov, wout.rearrange("(kc p) n -> p kc n", p=128), (), [WO])

        CT = [12, 13]

        def na_unit(h, qt, ktiles, slots, nabT, nabV, ot, u):
            half = slice((h % 2) * 64, (h % 2) * 64 + 64)
            qch, kch = h // 2, 4 + h // 2
            S = [pbank[(u % 2) * 2], pbank[(u % 2) * 2 + 1]]
            O = pbank[4 + u % 2]
            pt = PT[u % 2]
            allk = [(kt, sl) for kt, sl in zip(ktiles, slots)] + [(c, None) for c in CT]
            for c, (kt, sl) in enumerate(allk):
                bank = S[c // 4]
                o = bank[:, (c % 4) * 128:(c % 4 + 1) * 128]
                p.mm(o, QT[half, kch, kt * 128:(kt + 1) * 128], QT[half, qch, qt * 128:(qt + 1) * 128], True, sl is None,
                     [QT], [bank])
                if sl is not None:
                    p.mm(o, ident[:, :], nabV[:, sl, :], False, True, [ident, nabT], [bank])
            nck = len(allk)
            n0 = min(nck, 4)
            p.act(pt[:, 0:n0, :], S[0][:, 0:n0 * 128].rearrange("p (c q) -> p c q", q=128), AF.Exp, [S[0]], [pt])
            if nck > 4:
                p.act(pt[:, 4:nck, :], S[1][:, 0:(nck - 4) * 128].rearrange("p (c q) -> p c q", q=128), AF.Exp, [S[1]], [pt])
            for c, (kt, sl) in enumerate(allk):
                p.mm(O[:, 0:65], pt[:, c, :], VA[:, kt, h, :], c == 0, c == nck - 1, [pt, VA], [O])
            p.op("dve", lambda g, O=O, h=h: g.reciprocal(rec[:, h:h + 1], O[:, 64:65]), [O], [rec])
            p.ts("dve", OA[:, ot, h * 64:(h + 1) * 64], O[:, 0:64], rec[:, h:h + 1], None, ALU.mult, None, [O, rec], [OAt])

        u = 0
        for h in range(8 if ph >= 4 else 0):
            nT, nV = NABt[h % 2], nabv[h % 2]
            p.dma("pool", nV, nab[slab * 8 + h].rearrange("p (s q) -> p s q", q=128), (), [nT])
            for rp in range(NLOC):
                if rp == 0:
                    kts, sls = list(range(0, 6)), list(range(5, 11))
                elif rp == 1:
                    kts, sls = list(range(1, 6)), list(range(11, 16))
                elif rp == NLOC - 2:
                    kts, sls = list(range(rp, rp + 5)), list(range(16, 21))
                elif rp == NLOC - 1:
                    kts, sls = list(range(rp - 1, rp + 5)), list(range(21, 27))
                else:
                    kts, sls = list(range(rp, rp + 5)), list(range(0, 5))
                na_unit(h, rp + 2, kts, sls, nT, nV, rp, u)
                u += 1
            for ci, ct in enumerate(CT):
                na_unit(h, ct, [], [], nT, nV, NLOC + ci, u)
                u += 1

        def wa_unit(qt, kvh, ktiles, mslots, ot, u):
            kch = 12 + kvh
            allk = [(kt, ms) for kt, ms in zip(ktiles, mslots)] + [(c, None) for c in CT]
            nck = len(allk)
            for c, (kt, ms) in enumerate(allk):
                for par in range(2):
                    bank = pbank[((u * 5 + c) % 2) * 2 + par]
                    half = slice(par * 64, par * 64 + 64)
                    for jj in range(2):
                        j = 2 * jj + par
                        h = 4 * kvh + j
                        o = bank[:, jj * 128:(jj + 1) * 128]
                        p.mm(o, QT[half, kch, kt * 128:(kt + 1) * 128], QT[half, 8 + h // 2, qt * 128:(qt + 1) * 128],
                             True, ms is None, [QT], [bank])
                        if ms is not None:
                            p.mm(o, ident[:, :], WM[:, ms, 0:128], False, True, [ident, WM], [bank])
                    p.act(PTW[:, c, par * 256:(par + 1) * 256], bank[:, 0:256], AF.Exp, [bank], [PTWt])
            O = pbank[4 + u % 2]
            WSUB = int(os.environ.get('WSUB', '9'))
            if WSUB < 1:
                return
            for j in range(4):
                pos = (j % 2) * 2 + j // 2
                for c, (kt, ms) in enumerate(allk):
                    p.mm(O[:, j * 128:j * 128 + 65], PTW[:, c, pos * 128:(pos + 1) * 128], VB[:, kt, kvh, :], c == 0,
                         c == nck - 1, [PTWt, VB], [O])
            if WSUB < 2:
                return
            for j in range(4):
                h = 4 * kvh + j
                p.tt("dve", rec[:, h:h + 1], O[:, j * 128 + 64:j * 128 + 65], esink[:, h:h + 1], ALU.add, [O, esink], [rec])
                p.op("dve", lambda g, h=h: g.reciprocal(rec[:, h:h + 1], rec[:, h:h + 1]), [rec], [rec])
                p.ts("dve", OA[:, ot, 512 + h * 64:512 + (h + 1) * 64], O[:, j * 128:j * 128 + 64], rec[:, h:h + 1], None,
                     ALU.mult, None, [O, rec], [OAt])

        for n in range(int(os.environ.get('WN', NLOC)) if ph >= 5 else 0):
            for kvh in range(2):
                ms = [2 if n == 0 else 0, None, 3 if n == NLOC - 1 else 1]
                wa_unit(n + 2, kvh, [n + 1, n + 2, n + 3], ms, n, u)
                u += 1
        for ci, ct in enumerate(CT if ph >= 5 and int(os.environ.get('WC', 1)) else []):
            for kvh in range(2):
                wa_unit(ct, kvh, [], [], NLOC + ci, u)
                u += 1

        if dbg:
            dOA = nc.dram_tensor("dOA", [128, 10 * 1024], F32, kind="ExternalOutput").ap()
            dQT = nc.dram_tensor("dQT", [128, 14 * NTOK], F32, kind="ExternalOutput").ap()
            dVA = nc.dram_tensor("dVA", [128, NTA * 8 * 65], F32, kind="ExternalOutput").ap()
            p.dma("pool", dOA[:, :], big[:, 0:10 * 1024], [OAt], ())
            p.dma("pool", dQT[:, :], QT[:, :, :].rearrange("p c t -> p (c t)"), [QT], ())
            p.dma("pool", dVA[:, :], VA[:, :, :, :].rearrange("p t h d -> p (t h d)"), [VA], ())
        for o in range(NLOC + 2):
            w = 0 if o < NLOC else 1
            src = o + 2 if o < NLOC else 12 + (o - NLOC)
            for kc in range(8):
                p.tr(ptr[:, kc, :], OA[:, o, kc * 128:(kc + 1) * 128], ident[:, :], [OAt, ident], [ptr])
            p.cp("act", oT[:, :, :], ptr[:, :, :], [ptr], [oT])
            y0, y1 = pbank[(o % 2) * 2], pbank[(o % 2) * 2 + 1]
            for half, y in enumerate((y0, y1)):
                for kc in range(8):
                    p.mm(y[:, :], oT[:, kc, :], wov[:, kc, half * 512:(half + 1) * 512], kc == 0, kc == 7, [oT, WO], [y])
            p.dma("sp", xt[:, :], xe_s[src * 128:(src + 1) * 128, :], (), [xt])
            j = small["junk"]
            g1 = MB[w][2]
            for half, y in enumerate((y0, y1)):
                sl = slice(half * 512, (half + 1) * 512)
                p.tt("dve", j[:, sl], y[:, :], g1[:, sl], ALU.mult, [y, g1], [j])
            p.tt("dve", j[:, :], j[:, :], xt[:, :], ALU.add, [j, xt], [j])
            p.dma("sp", xo[(slab * (NLOC + 2) + o) * 128:(slab * (NLOC + 2) + o + 1) * 128, :], j[:, :], [j], ())
        prev_alias = [(big, OAt), (big2, WO), (big2, NABt[0]), (big2, NABt[1])]
    p.emit()
    return nc


def rope_tables(tok0):
    t = tok0 + np.arange(1536)
    row, col = (t // 64).astype(np.float32), (t % 64).astype(np.float32)
    inv = (10000.0 ** (-np.arange(16, dtype=np.float32) / 16)).astype(np.float32)
    C = np.zeros((64, 1536), np.float32)
    S = np.zeros((64, 1536), np.float32)
    for d in range(64):
        pos = row if d < 32 else col
        q = d % 32
        ang = (pos * inv[q % 16]).astype(np.float32)
        C[d] = np.cos(ang)
        S[d] = -np.sin(ang) if q < 16 else np.sin(ang)
    return np.concatenate([C, C], 0), np.concatenate([S, S], 0)


def perm_matrix():
    P = np.zeros((128, 128), np.float32)
    for m in range(128):
        blk, d = divmod(m, 64)
        q = d % 32
        partner = d + 16 if q < 16 else d - 16
        P[blk * 64 + partner, m] = 1.0
    return P


def na_bias_tables(rel_bias, R0):
    out = np.full((8, 128, 27, 128), NEG, np.float32)
    specs = []
    for c in range(5):
        specs.append((c, 2, 2 + c))
    for c in range(6):
        specs.append((5 + c, 0, c))
    for c in range(5):
        specs.append((11 + c, 1, 1 + c))
    for c in range(5):
        specs.append((16 + c, NLOC - 2, NLOC - 2 + c))
    for c in range(6):
        specs.append((21 + c, NLOC - 1, NLOC - 2 + c))
    kp = np.arange(128)
    qi = np.arange(128)
    for slot, rp, kt in specs:
        r = R0 + 2 * rp + qi // 64
        i = qi % 64
        kr = R0 - 4 + 2 * kt + kp // 64
        jc = kp % 64
        r0 = np.clip(r - 4, 0, 120)
        c0 = np.clip(i - 8, 0, 48)
        valid = ((kr[:, None] >= r0[None, :]) & (kr[:, None] < r0[None, :] + 8) & (kr[:, None] >= 0) & (kr[:, None] < 128)
                 & (jc[:, None] >= c0[None, :]) & (jc[:, None] < c0[None, :] + 16))
        dr = np.clip(kr[:, None] - r[None, :] + 7, 0, 14)
        dc = np.clip(jc[:, None] - i[None, :] + 15, 0, 30)
        vals = rel_bias[:, dr, dc]
        out[:, :, slot, :] = np.where(valid[None], vals, NEG)
    return out.reshape(8, 128, 27 * 128)


def wa_masks(gb0):
    kp = np.arange(128)[:, None]
    qi = np.arange(128)[None, :]
    prev = np.where(kp >= qi, 0.0, NEG).astype(np.float32)
    nxt = np.where(kp <= qi, 0.0, NEG).astype(np.float32)
    allneg = np.full((128, 128), NEG, np.float32)
    m = [prev, nxt, prev if gb0 > 0 else allneg, nxt if gb0 + NLOC < 64 else allneg]
    return np.concatenate([np.tile(x, (1, 4)) for x in m], axis=1)


def attn_inputs1(inp, b, s):
    R0 = 16 * s
    x = inp["x"][b]
    xe = np.zeros((NTA * 128, D), np.float32)
    g0 = (R0 - 4) * 64
    lo, hi = max(g0, 0), min(g0 + 1536, 8192)
    xe[lo - g0:hi - g0] = x[lo:hi]
    xe[1536:] = inp["ctx"][b]
    w = inp["att_w_in"][0]
    win = np.concatenate([w[:, 0:512], w[:, 512:1024], w[:, 1536:2048], w[:, 2048:2112], w[:, 2048:2112],
                          w[:, 2112:2176], w[:, 2112:2176], w[:, 1024:1536], w[:, 2176:2304]], axis=1)
    gv = [inp["na_q_norm"][0], inp["na_k_norm"][0], inp["wa_q_norm"][0], inp["wa_k_norm"][0]]
    gains = np.stack([np.concatenate([g, g]) for g in gv], axis=1)
    rc, rs = rope_tables(g0)
    cin = np.stack([inp["c"][b].reshape(8, 128).T, inp["c_ctx"].reshape(8, 128).T], axis=1)
    ob = np.zeros((128, 128), np.float32)
    ob[:64, :64] = 1.0
    ob[64:, 64:] = 1.0
    return {
        "xe": xe, "cin": np.ascontiguousarray(cin, dtype=np.float32),
        "modw": np.ascontiguousarray(inp["mod_w"][0][:, 0:3 * D]), "modb": np.ascontiguousarray(inp["mod_b"][0][None, 0:3 * D]),
        "nrm": np.ascontiguousarray(inp["norm_mix"][0][None, :]),
        "win": np.ascontiguousarray(win), "wout": np.ascontiguousarray(inp["att_w_out"][0]),
        "gains": np.ascontiguousarray(gains, dtype=np.float32), "ropec": rc, "ropes": rs, "pmd": perm_matrix(),
        "onesd": ob, "identd": np.eye(128, dtype=np.float32),
        "nab": na_bias_tables(inp["na_rel_bias"][0], R0), "wam": wa_masks(R0 // 2),
        "sink": np.ascontiguousarray(inp["wa_sink"][0][None, :]),
    }


TS = 66
NSEQ = TS * 128
WS = 1056
TWO_PI = 2.0 * np.pi


def build_ssm(nt=TS, do_s5=True):
    nc = bass.Bass("TRN2", target_bir_lowering=False)
    dt_in = lambda n, s: nc.dram_tensor(n, s, F32, kind="ExternalInput").ap()
    xs = dt_in("xs", [NSEQ, D])
    cin = dt_in("cin", [128, 2, 8])
    modw = dt_in("modw", [D, 2 * D])
    modb = dt_in("modb", [1, 2 * D])
    nrm = dt_in("nrm", [1, D])
    wsel = dt_in("wsel", [D, WS])
    cw = dt_in("cw", [128, 6 * 3])
    cbias = dt_in("cbias", [128, 6])
    dtb = dt_in("dtb", [1, 8])
    alog = dt_in("alog", [1, 8])
    dsk = dt_in("dsk", [1, 8])
    identd = dt_in("identd", [128, 128])
    triud = dt_in("triud", [128, 128])
    iotad = dt_in("iotad", [128, 129])
    m01d = dt_in("m01d", [128, 512])
    lam = dt_in("lam", [128, 3 * 8])
    BLr = dt_in("BLr", [8, 128, 128])
    BLi = dt_in("BLi", [8, 128, 128])
    CLr = dt_in("CLr", [8, 128, 32])
    CLi = dt_in("CLi", [8, 128, 32])
    DL = dt_in("DL", [8, 128, 32])
    yssd = nc.dram_tensor("yssd", [NSEQ, 512], F32, kind="ExternalOutput").ap()
    ys5 = nc.dram_tensor("ys5", [256, NSEQ], F32, kind="ExternalOutput").ap()

    p = Prog(nc)
    UT = p.sb([128, 2, NSEQ], BF16, "UT")
    Z = p.sb([128, 2, NSEQ], F32, "Z")
    MB = [[p.sb([128, D], F32, f"mb{w}{v}") for v in range(2)] for w in range(2)]
    wsb = p.sb([128, 8, WS], BF16, "wsb")
    xt = p.sb([128, D], F32, "xt")
    hb = p.sb([128, D], BF16, "hb")
    hTi = p.sb([128, 8, 128], BF16, "hTi")
    stage = p.sb([128, 8, 128], F32, "stage")
    bstage = p.sb([128, 128], F32, "bstage")
    cbc = p.sb([128, 8, 128], F32, "cbc")
    ones = p.sb([128, 128], F32, "ones")
    identf = p.sb([128, 128], F32, "identf")
    ident = p.sb([128, 128], BF16, "ident")
    triu = p.sb([128, 128], F32, "triu")
    cin_t = p.sb([128, 2, 8], F32, "cin_t")
    small = {"ss": p.sb([128, 4], F32, "ss"), "rs": p.sb([128, 4], F32, "rs"), "junk": p.sb([128, D], F32, "junk")}
    CW = p.sb([128, 18], F32, "CWc")
    CBs = p.sb([128, 6], F32, "CBs")
    dtb_t = p.sb([128, 8], F32, "dtb_t")
    A_t = p.sb([128, 8], F32, "A_t")
    dsk_t = p.sb([128, 8], F32, "dsk_t")
    RAW = [p.sb([128, 6, 128], BF16, f"raw{i}") for i in range(3)]
    DTs = [p.sb([128, 8], F32, f"dts{i}") for i in range(3)]
    CBUF = p.sb([128, 6, 130], BF16, "CBUF")
    cacc6 = p.sb([128, 6, 128], F32, "cacc6")
    tmp6 = p.sb([128, 6, 128], F32, "tmp6")
    XC = p.sb([128, 6, 128], BF16, "XC")
    XTOK = p.sb([128, 512], BF16, "XTOK")
    BTOK = p.sb([128, 128], BF16, "BTOK")
    sm = p.sb([128, 64], F32, "sm")
    CBT = p.sb([128, 128], F32, "CBT")
    WT4 = [p.sb([128, 512], BF16, f"WT4{i}") for i in range(2)]
    H = p.sb([128, 512], F32, "H")
    Hb = p.sb([128, 512], BF16, "Hb")
    XW = p.sb([128, 512], BF16, "XW")
    ysb = p.sb([128, 512], F32, "ysb")
    ytmp = p.sb([128, 512], F32, "ytmp")
    B0, B1, B2, B3, B4, B5, B6 = [p.ps([128, 512], F32, f"pb{i}") for i in range(7)]
    ptr = p.ps([128, 8, 128], BF16, "ptr")

    p.dma("sp", identf[:, :], identd[:, :], (), [identf])
    p.cp("dve", ident[:, :], identf[:, :], [identf], [ident])
    p.dma("sp", triu[:, :], triud[:, :], (), [triu])
    p.memset("pool", ones[:, :], 1.0, [ones])
    p.memset("pool", H[:, :], 0.0, [H])
    p.dma("pool", wsb[:, :, :], wsel.rearrange("(kc p) n -> p kc n", p=128), (), [wsb])
    p.dma("sp", cin_t[:, :, :], cin[:, :, :], (), [cin_t])
    p.act(cin_t[:, :, :], cin_t[:, :, :], AF.Silu, [cin_t], [cin_t])
    p.dma("sp", xt[:, :], bcast_rows(nrm[0:1, :]), (), [xt])
    p.dma("sp", CW[:, :], cw[:, :], (), [CW])
    p.dma("sp", CBs[:, :], cbias[:, :], (), [CBs])
    p.dma("sp", dtb_t[:, :], bcast_rows(dtb[0:1, :]), (), [dtb_t])
    p.dma("sp", A_t[:, :], bcast_rows(alog[0:1, :]), (), [A_t])
    p.act(A_t[:, :], A_t[:, :], AF.Exp, [A_t], [A_t])
    p.ts("dve", A_t[:, :], A_t[:, :], -1.0, None, ALU.mult, None, [A_t], [A_t])
    p.dma("sp", dsk_t[:, :], bcast_rows(dsk[0:1, :]), (), [dsk_t])

    def outsel(which):
        def f(j):
            v, c = divmod(j, 8)
            t = MB[which][v]
            return t, t[:, c * 128:(c + 1) * 128]
        return f
    emit_mod_rows(p, cin_t, modw, modb, 2 * D, [outsel(0), outsel(1)], B0, stage, cbc, ones, bstage)
    for w in range(2):
        A = MB[w][1]
        p.stt("dve", A[:, :], A[:, :], 1.0, xt[:, :], ALU.add, ALU.mult, [A, xt], [A])

    PSUB = int(os.environ.get('PSUB', '9'))

    RMt = [alias(p, stage, f"RM{i}") for i in range(2)]
    RMv = [stage[:, 4 * i:4 * i + 4, :].rearrange("p c t -> p (c t)") for i in range(2)]
    LMt = [alias(p, cbc, f"LM{i}") for i in range(2)]
    LMv = [cbc[:, 4 * i:4 * i + 4, :].rearrange("p c t -> p (c t)") for i in range(2)]

    def project(i):
        if PSUB < 1:
            return
        w = 1 if i < 2 else 0
        raw, dts = RAW[i % 3], DTs[i % 3]
        p.dma("sp", xt[:, :], xs[i * 128:(i + 1) * 128, :], (), [xt])
        emit_adanorm_T(p, xt, MB[w][1], MB[w][0], hb, ptr, hTi[:, :, :], hTi, ident, small)
        if PSUB < 2:
            return
        PQ = int(os.environ.get('PQ', '9'))
        for grp in range(2):
            if grp == 1 and PQ < 3:
                break
            for c4 in range(4):
                ch = grp * 4 + c4
                for kc in range(8):
                    p.mm(B0[:, c4 * 128:(c4 + 1) * 128], wsb[:, kc, ch * 128:(ch + 1) * 128], hTi[:, kc, :], kc == 0,
                         kc == 7, [wsb, hTi], [B0])
            if grp == 0:
                if PQ >= 2:
                    p.cp("act", raw[:, 0:4, :], B0[:, :].rearrange("p (c t) -> p c t", c=4), [B0], [raw])
            else:
                if PQ >= 4:
                    p.cp("act", raw[:, 4:6, :], B0[:, 0:256].rearrange("p (c t) -> p c t", c=2), [B0], [raw])
                if PQ >= 5:
                    for c2 in range(2):
                        p.cp("act", UT[:, c2, i * 128:(i + 1) * 128], B0[:, 256 + c2 * 128:384 + c2 * 128], [B0], [UT])
        if PSUB < 3:
            return
        for kc in range(8):
            p.mm(B1[:, 0:8], hTi[:, kc, :], wsb[:, kc, 1024:1032], kc == 0, kc == 7, [hTi, wsb], [B1])
        p.tt("dve", dts[:, :], B1[:, 0:8], dtb_t[:, :], ALU.add, [B1, dtb_t], [dts])
        p.act(dts[:, :], dts[:, :], AF.Exp, [dts], [dts])
        p.act(dts[:, :], dts[:, :], AF.Ln, [dts], [dts], bias=1.0)

    a_, acs, tot, eacs, wend, dec = (sm[:, 8 * k:8 * k + 8] for k in range(6))

    SSUB = int(os.environ.get('SSUB', '9'))

    def ssd_chunk(j):
        raw, dts = RAW[j % 3], DTs[j % 3]
        if SSUB < 1:
            return
        first = j in (0, 2)
        last = j in (1, nt - 1)
        if first:
            p.memset("pool", CBUF[:, :, 0:1], 0.0, [CBUF])
        else:
            p.cp("pool", CBUF[:, :, 0:1], RAW[(j - 1) % 3][:, :, 127:128], [RAW[(j - 1) % 3]], [CBUF])
        p.cp("pool", CBUF[:, :, 1:129], raw[:, :, :], [raw], [CBUF])
        if last:
            p.memset("pool", CBUF[:, :, 129:130], 0.0, [CBUF])
        else:
            p.cp("pool", CBUF[:, :, 129:130], RAW[(j + 1) % 3][:, :, 0:1], [RAW[(j + 1) % 3]], [CBUF])
        cw3 = CW[:, :].rearrange("p (c k) -> p c k", k=3)
        wk = lambda k: cw3[:, :, k:k + 1].broadcast_to([128, 6, 128])
        p.tt("dve", cacc6[:, :, :], CBUF[:, :, 0:128], wk(0), ALU.mult, [CBUF, CW], [cacc6])
        p.tt("pool", tmp6[:, :, :], CBUF[:, :, 1:129], wk(1), ALU.mult, [CBUF, CW], [tmp6])
        p.tt("dve", cacc6[:, :, :], cacc6[:, :, :], tmp6[:, :, :], ALU.add, [cacc6, tmp6], [cacc6])
        p.tt("pool", tmp6[:, :, :], CBUF[:, :, 2:130], wk(2), ALU.mult, [CBUF, CW], [tmp6])
        p.tt("dve", cacc6[:, :, :], cacc6[:, :, :], tmp6[:, :, :], ALU.add, [cacc6, tmp6], [cacc6])
        p.tt("dve", cacc6[:, :, :], cacc6[:, :, :], CBs[:, :].rearrange("p (c o) -> p c o", o=1).broadcast_to([128, 6, 128]),
             ALU.add, [cacc6, CBs], [cacc6])
        p.act(XC[:, :, :], cacc6[:, :, :], AF.Silu, [cacc6], [XC])
        if SSUB < 2:
            return
        for c in range(5):
            p.tr(ptr[:, c, :], XC[:, c, :], ident[:, :], [XC, ident], [ptr])
        p.cp("act", XTOK[:, :], ptr[:, 0:4, :].rearrange("p c t -> p (c t)"), [ptr], [XTOK])
        p.cp("act", BTOK[:, :], ptr[:, 4, :], [ptr], [BTOK])
        p.tt("dve", a_, dts[:, :], A_t[:, :], ALU.mult, [dts, A_t], [sm])
        p.mm(B1[:, 0:8], triu[:, :], a_, True, True, [triu, sm], [B1])
        p.mm(B1[:, 128:136], ones[:, :], a_, True, True, [ones, sm], [B1])
        p.cp("dve", acs, B1[:, 0:8], [B1], [sm])
        p.cp("dve", tot, B1[:, 128:136], [B1], [sm])
        if SSUB < 3:
            return
        p.mm(B2[:, 0:128], XC[:, 4, :], XC[:, 5, :], True, True, [XC], [B2])
        p.tt("dve", CBT[:, :], B2[:, 0:128], triu[:, :], ALU.mult, [B2, triu], [CBT])
        p.cp("act", Hb[:, :], H[:, :], [H], [Hb])
        v4 = lambda ap: ap.rearrange("p (h t) -> p h t", h=4)
        b4 = lambda ap: ap.rearrange("p (h o) -> p h o", o=1).broadcast_to([128, 4, 128])
        o4 = lambda ap: ap.rearrange("p (o t) -> p o t", o=1).broadcast_to([128, 4, 128])
        for g4 in range(2):
            hs = slice(g4 * 4, g4 * 4 + 4)
            rt_, rv_, lt_, lv_, wt4, Pb = RMt[g4], RMv[g4], LMt[g4], LMv[g4], WT4[g4], (B3, B2)[g4]
            p.tt("dve", v4(rv_), o4(triu[:, :]), b4(a_[:, hs]), ALU.mult, [triu, sm], [rt_])
            p.mm(Pb[:, :], ones[:, :], rv_, True, True, [ones, rt_], [Pb])
            p.tt("dve", v4(lv_), v4(Pb[:, :]), b4(acs[:, hs]), ALU.subtract, [Pb, sm], [lt_])
            p.ts("dve", lv_, lv_, 0.0, None, ALU.min, None, [lt_], [lt_])
            p.act(lv_, lv_, AF.Exp, [lt_], [lt_])
            p.tt("dve", v4(lv_), v4(lv_), b4(dts[:, hs]), ALU.mult, [lt_, dts], [lt_])
            p.tt("dve", v4(wt4[:, :]), v4(lv_), o4(CBT[:, :]), ALU.mult, [lt_, CBT], [wt4])
            for hh in range(4):
                hd = g4 * 4 + hh
                p.mm(B4[:, hd * 64:(hd + 1) * 64], wt4[:, hh * 128:(hh + 1) * 128], XTOK[:, hd * 64:(hd + 1) * 64], True, True,
                     [wt4, XTOK], [B4])
        if SSUB < 4:
            return
        p.mm(B5[:, :], XC[:, 5, :], Hb[:, :], True, True, [XC, Hb], [B5])
        p.act(eacs, acs, AF.Exp, [sm], [sm])
        v3 = lambda ap: ap.rearrange("p (h d) -> p h d", h=8)
        bc = lambda ap: ap.rearrange("p (h o) -> p h o", o=1).broadcast_to([128, 8, 64])
        p.tt("dve", v3(ytmp[:, :]), v3(B5[:, :]), bc(eacs), ALU.mult, [B5, sm], [ytmp])
        p.tt("dve", ysb[:, :], ytmp[:, :], B4[:, :], ALU.add, [ytmp, B4], [ysb])
        p.tt("dve", v3(ytmp[:, :]), v3(XTOK[:, :]), bc(dsk_t[:, :]), ALU.mult, [XTOK, dsk_t], [ytmp])
        p.tt("dve", ysb[:, :], ysb[:, :], ytmp[:, :], ALU.add, [ysb, ytmp], [ysb])
        p.dma("sp", yssd[j * 128:(j + 1) * 128, :], ysb[:, :], [ysb], ())
        if SSUB < 5:
            return
        p.tt("dve", wend, tot, acs, ALU.subtract, [sm], [sm])
        p.act(wend, wend, AF.Exp, [sm], [sm])
        p.tt("dve", wend, wend, dts[:, :], ALU.mult, [sm, dts], [sm])
        p.act(dec, tot, AF.Exp, [sm], [sm])
        p.tt("dve", v3(XW[:, :]), v3(XTOK[:, :]), bc(wend), ALU.mult, [XTOK, sm], [XW])
        p.mm(B6[:, :], BTOK[:, :], XW[:, :], True, True, [BTOK, XW], [B6])
        p.tt("dve", v3(H[:, :]), v3(H[:, :]), bc(dec), ALU.mult, [H, sm], [H])
        p.tt("dve", H[:, :], H[:, :], B6[:, :], ALU.add, [H, B6], [H])

    for i in range(nt + 1):
        if i < nt:
            project(i)
        if i >= 1:
            ssd_chunk(i - 1)

    if do_s5:
        LAM = p.sb([128, 24], F32, "LAM")
        dsc = p.sb([128, 16], F32, "dsc")
        iota = p.sb([128, 129], F32, "iota")
        ang = p.sb([128, 129], F32, "ang")
        mag = p.sb([128, 129], F32, "mag")
        cs_ = p.sb([128, 129], F32, "cs_")
        sn_ = p.sb([128, 129], F32, "sn_")
        EP = p.sb([128, 2, 512], F32, "EP")
        EN = p.sb([128, 2, 512], F32, "EN")
        m01 = p.sb([128, 512], F32, "m01")
        blr = p.sb([128, 128], BF16, "blr")
        bli = p.sb([128, 128], BF16, "bli")
        clr = p.sb([128, 32], BF16, "clr")
        cli = p.sb([128, 32], BF16, "cli")
        dl = p.sb([128, 32], BF16, "dl")
        q1 = p.sb([128, 512], F32, "q1")
        q2 = p.sb([128, 512], F32, "q2")
        WR = [p.sb([128, 512], BF16, f"wr_{i}") for i in range(2)]
        WI = [p.sb([128, 512], BF16, f"wi_{i}") for i in range(2)]
        XA = p.sb([128, 2, 66], F32, "XA")
        XB = p.sb([128, 2, 66], F32, "XB")
        tq66 = ang
        pw = p.sb([128, 2, 8], F32, "pw")
        yo = p.sb([32, 512], F32, "yo")
        twopi = p.sb([128, 129], F32, "twopi")
        kint = p.sb([128, 129], mybir.dt.int32, "kint")
        p.dma("sp", LAM[:, :], lam[:, :], (), [LAM])
        p.dma("sp", iota[:, :], iotad[:, :], (), [iota])
        p.dma("sp", m01[:, :], m01d[:, :], (), [m01])
        nblk = (nt * 128 + 511) // 512
        for pr in range(8):
            lr, li, ls = LAM[:, pr:pr + 1], LAM[:, 8 + pr:9 + pr], LAM[:, 16 + pr:17 + pr]
            sc = lambda k: dsc[:, k:k + 1]
            R, W_ = [LAM, dsc, iota, ang, mag, cs_, sn_], [dsc]
            step, lrs, th, den, cr, ci, t0_, t1_ = (sc(k) for k in range(8))
            p.act(step, ls, AF.Exp, [LAM], [dsc])
            p.tt("dve", lrs, lr, step, ALU.mult, [LAM, dsc], [dsc])
            p.tt("dve", th, li, step, ALU.mult, [LAM, dsc], [dsc])
            p.ts("dve", ang[:, :], iota[:, :], th, None, ALU.mult, None, [iota, dsc], [ang])
            p.ts("dve", cs_[:, :], ang[:, :], 1.5 * np.pi, None, ALU.add, None, [ang], [cs_])
            p.ts("dve", twopi[:, :], cs_[:, :], 1.0 / TWO_PI, None, ALU.mult, None, [cs_], [twopi])
            p.cp("dve", kint[:, :], twopi[:, :], [twopi], [kint])
            p.cp("dve", twopi[:, :], kint[:, :], [kint], [twopi])
            p.stt("dve", cs_[:, :], twopi[:, :], -TWO_PI, cs_[:, :], ALU.mult, ALU.add, [twopi, cs_], [cs_])
            p.ts("dve", twopi[:, :], cs_[:, :], 0.0, None, ALU.is_lt, None, [cs_], [twopi])
            p.stt("dve", cs_[:, :], twopi[:, :], TWO_PI, cs_[:, :], ALU.mult, ALU.add, [twopi, cs_], [cs_])
            p.ts("dve", sn_[:, :], ang[:, :], np.pi, None, ALU.add, None, [ang], [sn_])
            p.ts("dve", twopi[:, :], sn_[:, :], 1.0 / TWO_PI, None, ALU.mult, None, [sn_], [twopi])
            p.cp("dve", kint[:, :], twopi[:, :], [twopi], [kint])
            p.cp("dve", twopi[:, :], kint[:, :], [kint], [twopi])
            p.stt("dve", sn_[:, :], twopi[:, :], -TWO_PI, sn_[:, :], ALU.mult, ALU.add, [twopi, sn_], [sn_])
            p.ts("dve", twopi[:, :], sn_[:, :], 0.0, None, ALU.is_lt, None, [sn_], [twopi])
            p.stt("dve", sn_[:, :], twopi[:, :], TWO_PI, sn_[:, :], ALU.mult, ALU.add, [twopi, sn_], [sn_])
            p.act(cs_[:, :], cs_[:, :], AF.Sin, [cs_], [cs_], bias=-np.pi)
            p.act(sn_[:, :], sn_[:, :], AF.Sin, [sn_], [sn_], bias=-np.pi)
            p.act(mag[:, :], iota[:, :], AF.Exp, [iota, dsc], [mag], scale=lrs)
            ar, ai, a128r, a128i = (sc(k) for k in range(8, 12))
            p.tt("dve", ar, mag[:, 1:2], cs_[:, 1:2], ALU.mult, [mag, cs_], [dsc])
            p.tt("dve", ai, mag[:, 1:2], sn_[:, 1:2], ALU.mult, [mag, sn_], [dsc])
            p.tt("dve", a128r, mag[:, 128:129], cs_[:, 128:129], ALU.mult, [mag, cs_], [dsc])
            p.tt("dve", a128i, mag[:, 128:129], sn_[:, 128:129], ALU.mult, [mag, sn_], [dsc])
            p.tt("dve", den, lr, lr, ALU.mult, [LAM], [dsc])
            p.stt("dve", den, li, li, den, ALU.mult, ALU.add, [LAM, dsc], [dsc])
            p.op("dve", lambda g, den=den: g.reciprocal(den, den), [dsc], [dsc])
            p.ts("dve", t0_, ar, -1.0, None, ALU.add, None, [dsc], [dsc])
            p.tt("dve", t1_, ai, li, ALU.mult, [dsc, LAM], [dsc])
            p.stt("dve", cr, t0_, lr, t1_, ALU.mult, ALU.add, [dsc, LAM], [dsc])
            p.tt("dve", cr, cr, den, ALU.mult, [dsc], [dsc])
            p.tt("dve", t1_, t0_, li, ALU.mult, [dsc, LAM], [dsc])
            p.stt("dve", ci, ai, lr, t1_, ALU.mult, ALU.subtract, [dsc, LAM], [dsc])
            p.tt("dve", ci, ci, den, ALU.mult, [dsc], [dsc])
            p.tt("dve", EP[:, 0, 0:128], mag[:, 0:128], cs_[:, 0:128], ALU.mult, [mag, cs_], [EP])
            p.tt("dve", EP[:, 1, 0:128], mag[:, 0:128], sn_[:, 0:128], ALU.mult, [mag, sn_], [EP])
            p.op("dve", lambda g: g.reciprocal(mag[:, :], mag[:, :]), [mag], [mag])
            p.tt("dve", cs_[:, :], cs_[:, :], mag[:, :], ALU.mult, [cs_, mag], [cs_])
            p.tt("dve", sn_[:, :], sn_[:, :], mag[:, :], ALU.mult, [sn_, mag], [sn_])
            p.ts("dve", ang[:, :], sn_[:, :], ci, None, ALU.mult, None, [sn_, dsc], [ang])
            p.stt("dve", EN[:, 0, 0:128], cs_[:, 0:128], cr, ang[:, 0:128], ALU.mult, ALU.add, [cs_, dsc, ang], [EN])
            p.ts("dve", ang[:, :], sn_[:, :], cr, None, ALU.mult, None, [sn_, dsc], [ang])
            p.stt("dve", EN[:, 1, 0:128], cs_[:, 0:128], ci, ang[:, 0:128], ALU.mult, ALU.subtract, [cs_, dsc, ang], [EN])
            for rep in range(1, 4):
                p.cp("pool", EP[:, :, rep * 128:(rep + 1) * 128], EP[:, :, 0:128], [EP], [EP])
                p.cp("pool", EN[:, :, rep * 128:(rep + 1) * 128], EN[:, :, 0:128], [EN], [EN])
            p.dma("pool", blr[:, :], BLr[pr], (), [blr])
            p.dma("pool", bli[:, :], BLi[pr], (), [bli])
            p.dma("pool", clr[:, :], CLr[pr], (), [clr])
            p.dma("pool", cli[:, :], CLi[pr], (), [cli])
            p.ts("dve", cli[:, :], cli[:, :], -1.0, None, ALU.mult, None, [cli], [cli])
            p.dma("pool", dl[:, :], DL[pr], (), [dl])
            uc = pr // 4
            for b in range(nblk):
                t0 = b * 512
                n = min(512, nt * 128 - t0)
                Pr, Pi = ((B0, B1), (B3, B4))[b % 2]
                p.mm(Pr[:, 0:n], blr[:, :], UT[:, uc, t0:t0 + n], True, True, [blr, UT], [Pr])
                p.mm(Pi[:, 0:n], bli[:, :], UT[:, uc, t0:t0 + n], True, True, [bli, UT], [Pi])
                p.tt("dve", q1[:, 0:n], Pr[:, 0:n], EN[:, 0, 0:n], ALU.mult, [Pr, EN], [q1])
                p.tt("dve", q2[:, 0:n], Pi[:, 0:n], EN[:, 1, 0:n], ALU.mult, [Pi, EN], [q2])
                p.tt("dve", q1[:, 0:n], q1[:, 0:n], q2[:, 0:n], ALU.subtract, [q1, q2], [q1])
                p.op("dve", lambda g, t0=t0, n=n: g.tensor_tensor_scan(Z[:, 0, t0:t0 + n], m01[:, 0:n], q1[:, 0:n], 0.0,
                                                                        ALU.mult, ALU.add), [m01, q1], [Z])
                p.tt("dve", q2[:, 0:n], Pi[:, 0:n], EN[:, 0, 0:n], ALU.mult, [Pi, EN], [q2])
                p.tt("dve", q1[:, 0:n], Pr[:, 0:n], EN[:, 1, 0:n], ALU.mult, [Pr, EN], [q1])
                p.tt("dve", q1[:, 0:n], q1[:, 0:n], q2[:, 0:n], ALU.add, [q1, q2], [q1])
                p.op("dve", lambda g, t0=t0, n=n: g.tensor_tensor_scan(Z[:, 1, t0:t0 + n], m01[:, 0:n], q1[:, 0:n], 0.0,
                                                                        ALU.mult, ALU.add), [m01, q1], [Z])
            zend = lambda ri: Z[:, ri, 0:nt * 128].rearrange("p (c t) -> p c t", t=128)[:, :, 127]
            cur, nxt = XA, XB
            CH = [Z, dsc, XA, XB, tq66, pw]
            p.ts("dve", tq66[:, 0:nt], zend(1), a128i, None, ALU.mult, None, CH, [tq66])
            p.stt("dve", cur[:, 0, 0:nt], zend(0), a128r, tq66[:, 0:nt], ALU.mult, ALU.subtract, CH, [cur])
            p.ts("dve", tq66[:, 0:nt], zend(0), a128i, None, ALU.mult, None, CH, [tq66])
            p.stt("dve", cur[:, 1, 0:nt], zend(1), a128r, tq66[:, 0:nt], ALU.mult, ALU.add, CH, [cur])
            p.cp("dve", pw[:, 0, 0:1], a128r, [dsc], [pw])
            p.cp("dve", pw[:, 1, 0:1], a128i, [dsc], [pw])
            k = 0
            while (1 << k) < nt:
                sft = 1 << k
                n = nt - sft
                Ar, Ai = pw[:, 0, k:k + 1], pw[:, 1, k:k + 1]
                p.cp("dve", nxt[:, :, 0:sft], cur[:, :, 0:sft], [cur], [nxt])
                p.ts("dve", tq66[:, 0:n], cur[:, 1, 0:n], Ai, None, ALU.mult, None, [cur, pw], [tq66])
                p.stt("dve", nxt[:, 0, sft:nt], cur[:, 0, 0:n], Ar, tq66[:, 0:n], ALU.mult, ALU.subtract, [cur, pw, tq66], [nxt])
                p.tt("dve", nxt[:, 0, sft:nt], nxt[:, 0, sft:nt], cur[:, 0, sft:nt], ALU.add, [cur, nxt], [nxt])
                p.ts("dve", tq66[:, 0:n], cur[:, 0, 0:n], Ai, None, ALU.mult, None, [cur, pw], [tq66])
                p.stt("dve", nxt[:, 1, sft:nt], cur[:, 1, 0:n], Ar, tq66[:, 0:n], ALU.mult, ALU.add, [cur, pw, tq66], [nxt])
                p.tt("dve", nxt[:, 1, sft:nt], nxt[:, 1, sft:nt], cur[:, 1, sft:nt], ALU.add, [cur, nxt], [nxt])
                p.tt("dve", pw[:, 0, k + 1:k + 2], Ai, Ai, ALU.mult, [pw], [pw])
                p.stt("dve", pw[:, 0, k + 1:k + 2], Ar, Ar, pw[:, 0, k + 1:k + 2], ALU.mult, ALU.subtract, [pw], [pw])
                p.stt("dve", pw[:, 1, k + 1:k + 2], Ar, 2.0, Ai, ALU.mult, ALU.mult, [pw], [pw])
                cur, nxt = nxt, cur
                k += 1
            if nt > 1:
                for ri, eng in ((0, "dve"), (1, "pool")):
                    zv = Z[:, ri, 128:nt * 128].rearrange("p (c t) -> p c t", t=128)
                    gb = cur[:, ri, 0:nt - 1].rearrange("p (c o) -> p c o", o=1).broadcast_to([128, nt - 1, 128])
                    p.tt(eng, zv, zv, gb, ALU.add, [Z, cur], [Z])
            for b in range(nblk):
                t0 = b * 512
                n = min(512, nt * 128 - t0)
                wr_, wi_ = WR[b % 2], WI[b % 2]
                p.tt("dve", q1[:, 0:n], Z[:, 0, t0:t0 + n], EP[:, 0, 0:n], ALU.mult, [Z, EP], [q1])
                p.tt("dve", q2[:, 0:n], Z[:, 1, t0:t0 + n], EP[:, 1, 0:n], ALU.mult, [Z, EP], [q2])
                p.tt("dve", wr_[:, 0:n], q1[:, 0:n], q2[:, 0:n], ALU.subtract, [q1, q2], [wr_])
                p.tt("dve", q2[:, 0:n], Z[:, 1, t0:t0 + n], EP[:, 0, 0:n], ALU.mult, [Z, EP], [q2])
                p.tt("dve", q1[:, 0:n], Z[:, 0, t0:t0 + n], EP[:, 1, 0:n], ALU.mult, [Z, EP], [q1])
                p.tt("dve", wi_[:, 0:n], q1[:, 0:n], q2[:, 0:n], ALU.add, [q1, q2], [wi_])
                Py = (B2, B5)[b % 2]
                p.mm(Py[0:32, 0:n], clr[:, :], wr_[:, 0:n], True, False, [clr, wr_], [Py])
                p.mm(Py[0:32, 0:n], cli[:, :], wi_[:, 0:n], False, False, [cli, wi_], [Py])
                p.mm(Py[0:32, 0:n], dl[:, :], UT[:, uc, t0:t0 + n], False, True, [dl, UT], [Py])
                p.cp("act", yo[:, 0:n], Py[0:32, 0:n], [Py], [yo])
                p.dma("sp", ys5[pr * 32:(pr + 1) * 32, t0:t0 + n], yo[:, 0:n], [yo], ())
    p.emit()
    return nc


def ssm_inputs(inp, xseq, b, dr, hf, nt=TS):
    w = inp["ssm_w_in"][0]
    cols = np.concatenate([np.arange(1024 + 512 * hf, 1024 + 512 * hf + 512), np.arange(2048 + 128 * hf, 2048 + 128 * hf + 128),
                           np.arange(2304 + 128 * hf, 2304 + 128 * hf + 128), np.arange(2592 + 256 * hf, 2592 + 256 * hf + 256),
                           np.arange(2560 + 16 * dr + 8 * hf, 2560 + 16 * dr + 8 * hf + 8)])
    cch = np.concatenate([np.arange(512 * hf, 512 * hf + 512), np.arange(1024 + 128 * hf, 1024 + 128 * hf + 128),
                          np.arange(1280 + 128 * hf, 1280 + 128 * hf + 128)])
    cwf = inp["ssd_conv_w"][0][:, cch]
    if dr == 1:
        cwf = cwf[::-1]
    cw = np.ascontiguousarray(cwf.T.reshape(6, 128, 3).transpose(1, 0, 2).reshape(128, 18))
    cb = np.ascontiguousarray(inp["ssd_conv_b"][0][cch].reshape(6, 128).T)
    hs = slice(8 * hf, 8 * hf + 8)
    zero8 = np.zeros((1, 8), np.float32)
    gs = 16 * hf
    lam = np.zeros((128, 24), np.float32)
    BLr = np.zeros((8, 128, 128), np.float32)
    BLi = np.zeros((8, 128, 128), np.float32)
    CLr = np.zeros((8, 128, 32), np.float32)
    CLi = np.zeros((8, 128, 32), np.float32)
    DLm = np.zeros((8, 128, 32), np.float32)
    sd = inp["s5_d"][0]
    for pr in range(8):
        for k in range(2):
            g = gs + 2 * pr + k
            rows = slice(64 * k, 64 * k + 64)
            lam[rows, pr] = inp["s5_lambda_re"][0, dr, g]
            lam[rows, 8 + pr] = inp["s5_lambda_im"][0, dr, g]
            lam[rows, 16 + pr] = inp["s5_log_step"][0, dr, g]
            ur = 32 * (pr % 4) + 16 * k
            BLr[pr, ur:ur + 16, rows] = inp["s5_b_re"][0, dr, g].T
            BLi[pr, ur:ur + 16, rows] = inp["s5_b_im"][0, dr, g].T
            CLr[pr, rows, 16 * k:16 * k + 16] = inp["s5_c_re"][0, dr, g].T
            CLi[pr, rows, 16 * k:16 * k + 16] = inp["s5_c_im"][0, dr, g].T
            if dr == 0:
                for c in range(16):
                    DLm[pr, ur + c, 16 * k + c] = sd[g * 16 + c]
    m01 = np.ones((128, 512), np.float32)
    m01[:, ::128] = 0.0
    cin = np.stack([inp["c"][b].reshape(8, 128).T, inp["c_ctx"].reshape(8, 128).T], axis=1)
    return {
        "xs": np.ascontiguousarray(xseq, dtype=np.float32), "cin": np.ascontiguousarray(cin, dtype=np.float32),
        "modw": np.ascontiguousarray(inp["mod_w"][1][:, 0:2 * D]), "modb": np.ascontiguousarray(inp["mod_b"][1][None, 0:2 * D]),
        "nrm": np.ascontiguousarray(inp["norm_mix"][1][None, :]),
        "wsel": np.ascontiguousarray(np.concatenate([w[:, cols], np.zeros((D, WS - 1032), np.float32)], axis=1)), "cw": cw, "cbias": cb,
        "dtb": np.ascontiguousarray(inp["ssd_dt_bias"][0, dr, hs][None, :]),
        "alog": np.ascontiguousarray(inp["ssd_a_log"][0, dr, hs][None, :]),
        "dsk": np.ascontiguousarray(inp["ssd_d"][0, hs][None, :]) if dr == 0 else zero8,
        "identd": np.eye(128, dtype=np.float32), "triud": np.triu(np.ones((128, 128), np.float32)),
        "iotad": np.tile(np.arange(129, dtype=np.float32)[None, :], (128, 1)), "m01d": m01,
        "lam": lam, "BLr": BLr, "BLi": BLi, "CLr": CLr, "CLi": CLi, "DL": DLm,
    }


NTT = 16
GELU_C = 1.5957691216057308


def build_fin():
    nc = bass.Bass("TRN2", target_bir_lowering=False)
    dt_in = lambda n, s: nc.dram_tensor(n, s, F32, kind="ExternalInput").ap()
    x = dt_in("x", [NTT * 128, D])
    cin = dt_in("cin", [128, 2, 8])
    modw = dt_in("modw", [D, 3 * D])
    modb = dt_in("modb", [1, 3 * D])
    nrm = dt_in("nrm", [1, D])
    wz = dt_in("wz", [D, D])
    yf = dt_in("yf", [NTT * 128, D])
    yb = dt_in("yb", [NTT * 128, D])
    vf = dt_in("vf", [NTT * 128, 512])
    vb = dt_in("vb", [NTT * 128, 512])
    snorm = dt_in("snorm", [1, D])
    gluw = dt_in("gluw", [512, 512])
    glub = dt_in("glub", [1, 512])
    wout = dt_in("wout", [1536, D])
    identd = dt_in("identd", [128, 128])
    xo = nc.dram_tensor("xo", [NTT * 128, D], F32, kind="ExternalOutput").ap()

    p = Prog(nc)
    wzb = p.sb([128, 8, D], BF16, "wzb")
    woutb = p.sb([128, 12, D], BF16, "woutb")
    gwb = p.sb([128, 4, 512], BF16, "gwb")
    MB = [p.sb([128, D], F32, f"mb{v}") for v in range(3)]
    xt = p.sb([128, D], F32, "xt")
    hb = p.sb([128, D], BF16, "hb")
    hTi = p.sb([128, 8, 128], BF16, "hTi")
    stage = p.sb([128, 8, 128], F32, "stage")
    bstage = p.sb([128, 128], F32, "bstage")
    cbc = p.sb([128, 8, 128], F32, "cbc")
    ones = p.sb([128, 128], F32, "ones")
    identf = p.sb([128, 128], F32, "identf")
    ident = p.sb([128, 128], BF16, "ident")
    cin_t = p.sb([128, 2, 8], F32, "cin_t")
    small = {"ss": p.sb([128, 4], F32, "ss"), "rs": p.sb([128, 4], F32, "rs"), "junk": p.sb([128, D], F32, "junk")}
    sn_bc = p.sb([128, D], F32, "sn_bc")
    gb_bc = p.sb([128, 512], F32, "gb_bc")
    zs = p.sb([128, D], F32, "zs")
    ya = p.sb([128, D], F32, "ya")
    ybt = p.sb([128, D], F32, "ybt")
    va = p.sb([128, 512], F32, "va")
    vbt = p.sb([128, 512], F32, "vbt")
    v2 = p.sb([128, 512], F32, "v2")
    gvb = p.sb([128, 512], BF16, "gvb")
    cat = p.sb([128, 1536], BF16, "cat")
    gT = p.sb([128, 4, 128], BF16, "gT")
    cT = p.sb([128, 12, 128], BF16, "cT")
    st2 = p.sb([128, 4], F32, "st2")
    Z0, Z1, G, O0, O1, B0 = [p.ps([128, 512], F32, f"pb{i}") for i in range(6)]
    ptr = p.ps([128, 8, 128], BF16, "ptr")

    p.dma("sp", identf[:, :], identd[:, :], (), [identf])
    p.cp("dve", ident[:, :], identf[:, :], [identf], [ident])
    p.memset("pool", ones[:, :], 1.0, [ones])
    p.dma("pool", wzb[:, :, :], wz.rearrange("(kc p) n -> p kc n", p=128), (), [wzb])
    p.dma("pool", gwb[:, :, :], gluw.rearrange("(kc p) n -> p kc n", p=128), (), [gwb])
    p.dma("pool", woutb[:, :, :], wout.rearrange("(kc p) n -> p kc n", p=128), (), [woutb])
    p.dma("sp", cin_t[:, :, :], cin[:, :, :], (), [cin_t])
    p.act(cin_t[:, :, :], cin_t[:, :, :], AF.Silu, [cin_t], [cin_t])
    p.dma("sp", xt[:, :], bcast_rows(nrm[0:1, :]), (), [xt])
    p.dma("sp", sn_bc[:, :], bcast_rows(snorm[0:1, :]), (), [sn_bc])
    p.dma("sp", gb_bc[:, :], bcast_rows(glub[0:1, :]), (), [gb_bc])

    def outsel(j):
        v, c = divmod(j, 8)
        t = MB[v]
        return t, t[:, c * 128:(c + 1) * 128]
    emit_mod_rows(p, cin_t, modw, modb, 3 * D, [outsel, outsel], B0, stage, cbc, ones, bstage, whichs=(0,))
    A = MB[1]
    p.stt("dve", A[:, :], A[:, :], 1.0, xt[:, :], ALU.add, ALU.mult, [A, xt], [A])

    for i in range(NTT):
        rows = slice(i * 128, (i + 1) * 128)
        p.dma("sp", xt[:, :], x[rows, :], (), [xt])
        emit_adanorm_T(p, xt, MB[1], MB[0], hb, ptr, hTi[:, :, :], hTi, ident, small)
        for half, Zp in enumerate((Z0, Z1)):
            for kc in range(8):
                p.mm(Zp[:, :], hTi[:, kc, :], wzb[:, kc, half * 512:(half + 1) * 512], kc == 0, kc == 7, [hTi, wzb], [Zp])
            p.act(zs[:, half * 512:(half + 1) * 512], Zp[:, :], AF.Silu, [Zp], [zs])
        p.dma("sp", ya[:, :], yf[rows, :], (), [ya])
        p.dma("sp", ybt[:, :], yb[rows, :], (), [ybt])
        p.tt("pool", ya[:, :], ya[:, :], ybt[:, :], ALU.add, [ya, ybt], [ya])
        p.tt("dve", ya[:, :], ya[:, :], zs[:, :], ALU.mult, [ya, zs], [ya])
        j = small["junk"]
        p.memset("dve", st2[:, 0:1], 0.0, [st2])
        p.act(j[:, :], ya[:, :], AF.Square, [ya, st2], [j, st2], accum_out=st2[:, 0:1])
        p.ts("dve", st2[:, 1:2], st2[:, 0:1], 1.0 / D, EPS, ALU.mult, ALU.add, [st2], [st2])
        p.op("act", lambda g: g.sqrt(st2[:, 2:3], st2[:, 1:2]), [st2], [st2])
        p.op("dve", lambda g: g.reciprocal(st2[:, 3:4], st2[:, 2:3]), [st2], [st2])
        p.stt("dve", cat[:, 0:1024], ya[:, :], st2[:, 3:4], sn_bc[:, :], ALU.mult, ALU.mult, [ya, st2, sn_bc], [cat])
        p.dma("sp", va[:, :], vf[rows, :], (), [va])
        p.dma("sp", vbt[:, :], vb[rows, :], (), [vbt])
        p.tt("pool", va[:, :], va[:, :], vbt[:, :], ALU.add, [va, vbt], [va])
        p.tt("dve", v2[:, :], va[:, :], va[:, :], ALU.mult, [va], [v2])
        p.ts("dve", v2[:, :], v2[:, :], 0.044715, 1.0, ALU.mult, ALU.add, [v2], [v2])
        p.tt("dve", v2[:, :], v2[:, :], va[:, :], ALU.mult, [v2, va], [v2])
        p.act(v2[:, :], v2[:, :], AF.Sigmoid, [v2], [v2], scale=GELU_C)
        p.tt("dve", va[:, :], va[:, :], v2[:, :], ALU.mult, [va, v2], [va])
        p.cp("dve", gvb[:, :], va[:, :], [va], [gvb])
        for c in range(4):
            p.tr(ptr[:, c, :], gvb[:, c * 128:(c + 1) * 128], ident[:, :], [gvb, ident], [ptr])
        p.cp("act", gT[:, :, :], ptr[:, 0:4, :], [ptr], [gT])
        for c in range(4):
            p.mm(G[:, :], gT[:, c, :], gwb[:, c, :], c == 0, c == 3, [gT, gwb], [G])
        p.tt("dve", v2[:, :], G[:, :], gb_bc[:, :], ALU.add, [G, gb_bc], [v2])
        p.act(v2[:, :], v2[:, :], AF.Sigmoid, [v2], [v2])
        p.tt("dve", cat[:, 1024:1536], va[:, :], v2[:, :], ALU.mult, [va, v2], [cat])
        for c in range(8):
            p.tr(ptr[:, c, :], cat[:, c * 128:(c + 1) * 128], ident[:, :], [cat, ident], [ptr])
        p.cp("act", cT[:, 0:8, :], ptr[:, :, :], [ptr], [cT])
        for c in range(4):
            p.tr(ptr[:, c, :], cat[:, 1024 + c * 128:1024 + (c + 1) * 128], ident[:, :], [cat, ident], [ptr])
        p.cp("act", cT[:, 8:12, :], ptr[:, 0:4, :], [ptr], [cT])
        for half, Op in enumerate((O0, O1)):
            for c in range(12):
                p.mm(Op[:, :], cT[:, c, :], woutb[:, c, half * 512:(half + 1) * 512], c == 0, c == 11, [cT, woutb], [Op])
        for half, Op in enumerate((O0, O1)):
            sl = slice(half * 512, (half + 1) * 512)
            p.tt("dve", j[:, sl], Op[:, :], MB[2][:, sl], ALU.mult, [Op, MB[2]], [j])
        p.tt("dve", j[:, :], j[:, :], xt[:, :], ALU.add, [j, xt], [j])
        p.dma("sp", xo[rows, :], j[:, :], [j], ())
    p.emit()
    return nc


def fin_inputs(inp, b, xcore, yfc, ybc, vfc, vbc):
    cin = np.stack([inp["c"][b].reshape(8, 128).T, inp["c_ctx"].reshape(8, 128).T], axis=1)
    ca = lambda a: np.ascontiguousarray(a, dtype=np.float32)
    return {
        "x": ca(xcore), "cin": ca(cin), "modw": ca(inp["mod_w"][1][:, 0:3 * D]), "modb": ca(inp["mod_b"][1][None, 0:3 * D]),
        "nrm": ca(inp["norm_mix"][1][None, :]), "wz": ca(inp["ssm_w_in"][0][:, 0:1024]),
        "yf": ca(yfc), "yb": ca(ybc), "vf": ca(vfc), "vb": ca(vbc),
        "snorm": ca(inp["ssd_norm"][0][None, :]), "gluw": ca(inp["s5_glu_w"][0]), "glub": ca(inp["s5_glu_b"][0][None, :]),
        "wout": ca(inp["ssm_w_out"][0]), "identd": np.eye(128, dtype=np.float32),
    }


_CACHE = {}


def _prog(name, fn):
    if name not in _CACHE:
        _CACHE[name] = fn()
    return _CACHE[name]


def attn_inputs(inp, b, q, NSLAB=2):
    parts = [attn_inputs1(inp, b, NSLAB * q + k) for k in range(NSLAB)]
    out = dict(parts[0])
    out["xe"] = np.concatenate([pp["xe"] for pp in parts], axis=0)
    for k in ("ropec", "ropes", "wam"):
        out[k] = np.ascontiguousarray(np.concatenate([pp[k] for pp in parts], axis=1))
    out["nab"] = np.concatenate([pp["nab"] for pp in parts], axis=0)
    return out


def kernel(**inputs):
    inp = {k: np.asarray(v) for k, v in inputs.items()}
    C8 = list(range(8))
    xl = np.empty((2, 8192, D), np.float32)
    xc = np.empty((2, 256, D), np.float32)
    vcs = [(b, q) for b in range(2) for q in range(4)]
    res = run_bass_kernel_spmd(build_attn(), [attn_inputs(inp, b, q) for b, q in vcs], core_ids=C8)
    for (b, q), r in zip(vcs, res.results):
        for k in range(2):
            s_ = 2 * q + k
            xl[b, s_ * 1024:(s_ + 1) * 1024] = r["xo"][k * 1280:k * 1280 + 1024]
        if q == 0:
            xc[b] = r["xo"][1024:1280]

    def moe(L, xl_in, xc_in):
        cores = [(b, q) for b in range(2) for q in range(4)]
        ims = [moe_inputs(np.concatenate([xl_in[b, q * 2048:(q + 1) * 2048], xc_in[b]], axis=0) if xc_in is not None
                          else xl_in[b, q * 2048:(q + 1) * 2048], inp["c"][b], inp["c_ctx"],
                          inp["mod_w"][L], inp["mod_b"][L], inp["norm_ffn"][L], inp["moe_w_group"][L], inp["moe_b_group"][L],
                          inp["moe_w_expert"][L], inp["moe_b_expert"][L], inp["moe_w13"][L], inp["moe_w2"][L])
               for b, q in cores]
        res = run_bass_kernel_spmd(build_moe(NT=NT_MOE if xc_in is not None else NLAT_MOE), ims, core_ids=C8)
        xo = np.empty_like(xl_in)
        xco = np.empty_like(xc_in) if xc_in is not None else None
        for (b, q), r in zip(cores, res.results):
            xo[b, q * 2048:(q + 1) * 2048] = r["xo"][:2048]
            if q == 0 and xc_in is not None:
                xco[b] = r["xo"][2048:]
        return xo, xco

    xl, xc = moe(0, xl, xc)
    cores = [(b, dr, hf) for b in range(2) for dr in range(2) for hf in range(2)]
    ims = []
    for b, dr, hf in cores:
        seq = np.concatenate([xc[b], xl[b]], axis=0) if dr == 0 else np.concatenate([xc[b][::-1], xl[b][::-1]], axis=0)
        ims.append(ssm_inputs(inp, seq, b, dr, hf))
    res = run_bass_kernel_spmd(build_ssm(), ims, core_ids=C8)
    Y = np.empty((2, 2, 8192, D), np.float32)
    V = np.empty((2, 2, 8192, 512), np.float32)
    for (b, dr, hf), r in zip(cores, res.results):
        y = r["yssd"][256:]
        v = r["ys5"][:, 256:].T
        if dr == 1:
            y, v = y[::-1], v[::-1]
        Y[b, dr, :, hf * 512:(hf + 1) * 512] = y
        V[b, dr, :, hf * 256:(hf + 1) * 256] = v
    cores = [(b, q) for b in range(2) for q in range(4)]
    ims = []
    for b, q in cores:
        sl = slice(q * 2048, (q + 1) * 2048)
        ims.append(fin_inputs(inp, b, xl[b, sl], Y[b, 0, sl], Y[b, 1, sl], V[b, 0, sl], V[b, 1, sl]))
    res = run_bass_kernel_spmd(build_fin(), ims, core_ids=C8)
    for (b, q), r in zip(cores, res.results):
        xl[b, q * 2048:(q + 1) * 2048] = r["xo"]
    xl, _ = moe(1, xl, None)
    return xl
```

```python
import os
import numpy as np
import concourse.bass as bass
import concourse.mybir as mybir
from concourse.bass_utils import run_bass_kernel_spmd

F32 = mybir.dt.float32
BF16 = mybir.dt.bfloat16
AF = mybir.ActivationFunctionType
ALU = mybir.AluOpType
AX = mybir.AxisListType

EPOCH = 12000
D = 1024
EPS = 1e-6


class Res:
    __slots__ = ("name", "w", "readers", "dsem", "dcnt")

    def __init__(self, name):
        self.name = name
        self.w = None
        self.readers = {}
        self.dsem = None
        self.dcnt = 0


class T:
    def __init__(self, h, name):
        self.h = h
        self.r = Res(name)

    def __getitem__(self, k):
        return self.h[k]


class TV(T):
    def __init__(self, ap, name, src=None):
        self.h = ap
        self.r = Res(name)
        if src is not None:
            self.r.w = src.r.w
            self.r.readers = dict(src.r.readers)


class Prog:
    def __init__(self, nc):
        self.nc = nc
        self.eng = {"pe": nc.tensor, "dve": nc.vector, "act": nc.scalar, "pool": nc.gpsimd, "sp": nc.sync}
        self.ops = {k: [] for k in self.eng}
        self.cnt = {k: 0 for k in self.eng}
        self.esems = {k: [] for k in self.eng}
        self.known = {k: {} for k in self.eng}
        self.dres = []
        self.nsem = 0
        self.nt = 0

    def sem(self, name):
        self.nsem += 1
        return self.nc.alloc_semaphore(name)

    def sb(self, shape, dt=F32, name=None):
        self.nt += 1
        name = name or f"t{self.nt}"
        return T(self.nc.alloc_sbuf_tensor(name, list(shape), dt), name)

    def ps(self, shape, dt=F32, name=None):
        self.nt += 1
        name = name or f"p{self.nt}"
        return T(self.nc.alloc_psum_tensor(name, list(shape), dt), name)

    def _esem(self, e, ep):
        while len(self.esems[e]) <= ep:
            self.esems[e].append(self.sem(f"s_{e}_{len(self.esems[e])}"))
        return self.esems[e][ep]

    def _need(self, e, ev, waits):
        sem, val, src = ev
        if src == "pe" and e == "pe":
            return
        k = id(sem)
        if self.known[e].get(k, 0) >= val:
            return
        self.known[e][k] = val
        waits.append((sem, val))

    def _deps(self, e, reads, writes):
        waits = []
        for t in reads:
            if t.r.w is not None:
                self._need(e, t.r.w, waits)
        for t in writes:
            if t.r.w is not None:
                self._need(e, t.r.w, waits)
            for ev in t.r.readers.values():
                self._need(e, ev, waits)
        return waits

    def _mark(self, ev, reads, writes):
        for t in writes:
            t.r.w = ev
            t.r.readers = {}
        for t in reads:
            if t not in writes:
                old = t.r.readers.get(id(ev[0]))
                if old is None or old[1] < ev[1]:
                    t.r.readers[id(ev[0])] = ev

    def op(self, e, fn, reads=(), writes=()):
        waits = self._deps(e, reads, writes)
        idx = self.cnt[e]
        self.cnt[e] += 1
        sem = self._esem(e, idx // EPOCH)
        ev = (sem, idx % EPOCH + 1, e)
        self._mark(ev, reads, writes)
        self.ops[e].append((waits, fn, (sem, 1)))

    def dma(self, e, out, in_, reads=(), writes=(), sres=None):
        waits = self._deps(e, reads, writes)
        t = sres or (writes[0] if writes else reads[0])
        r = t.r
        kind = "sw" if e == "pool" else "hw"
        if r.dsem is None:
            r.dsem = {}
        ent = r.dsem.get(kind)
        if ent is None or ent[1] + 16 > 30000:
            ent = [self.sem(f"d_{r.name}_{kind}_{self.nsem}"), 0]
            r.dsem[kind] = ent
            self.dres.append(ent)
        ent[1] += 16
        ev = (ent[0], ent[1], "dma")
        self._mark(ev, reads, writes)
        self.ops[e].append((waits, lambda eng: eng.dma_start(out=out, in_=in_), (ent[0], 16)))

    def finish(self):
        finals = {}
        for ent in self.dres:
            finals[id(ent[0])] = (ent[0], max(finals.get(id(ent[0]), (None, 0))[1], ent[1]))
        waits = []
        for sem, val in finals.values():
            if self.known["sp"].get(id(sem), 0) < val:
                waits.append((sem, val))
        for e in ("pe", "dve", "act", "pool"):
            n = self.cnt[e]
            if n:
                waits.append((self._esem(e, (n - 1) // EPOCH), (n - 1) % EPOCH + 1))
        self.ops["sp"].append((waits, None, None))

    def emit(self):
        self.finish()
        with self.nc.Block() as block:
            decos = {"sp": block.sync, "pe": block.tensor, "dve": block.vector, "act": block.scalar,
                     "pool": block.gpsimd}
            for e in ("sp", "pe", "dve", "act", "pool"):
                def body(engine, e=e):
                    for waits, fn, inc in self.ops[e]:
                        for sem, val in waits:
                            engine.wait_ge(sem, val)
                        if fn is not None:
                            fn(engine).then_inc(inc[0], inc[1])
                decos[e](body)

    def mm(self, out, lhsT, rhs, start, stop, reads, writes):
        self.op("pe", lambda g: g.matmul(out, lhsT, rhs, start=start, stop=stop), reads, writes)

    def tr(self, out, in_, ident, reads, writes):
        self.op("pe", lambda g: g.transpose(out, in_, ident), reads, writes)

    def act(self, out, in_, func, reads, writes, bias=None, scale=None, accum_out=None, e="act"):
        kw = {}
        if bias is not None:
            kw["bias"] = bias
        if scale is not None:
            kw["scale"] = scale
        if accum_out is not None:
            kw["accum_out"] = accum_out
        self.op("act", lambda g: g.activation(out, in_, func, **kw), reads, writes)

    def ts(self, e, out, in0, s1, s2, op0, op1, reads, writes):
        if op1 is None:
            self.op(e, lambda g: g.tensor_scalar(out, in0, s1, None, op0), reads, writes)
        else:
            self.op(e, lambda g: g.tensor_scalar(out, in0, s1, s2, op0, op1), reads, writes)

    def tt(self, e, out, in0, in1, op, reads, writes):
        self.op(e, lambda g: g.tensor_tensor(out, in0, in1, op), reads, writes)

    def stt(self, e, out, in0, sc, in1, op0, op1, reads, writes):
        self.op(e, lambda g: g.scalar_tensor_tensor(out, in0, sc, in1, op0, op1), reads, writes)

    def cp(self, e, out, in_, reads, writes):
        if e == "act":
            self.op(e, lambda g: g.copy(out, in_), reads, writes)
        else:
            self.op(e, lambda g: g.tensor_copy(out, in_), reads, writes)

    def memset(self, e, ap, v, writes):
        self.op(e, lambda g: g.memset(ap, v), (), writes)


def bcast_rows(ap, n=128):
    return ap.partition_broadcast(n)


def emit_mod_rows(p, cin_t, modw, modb, ncols, outs, psums, stages, cbc, ones, bst, whichs=(0, 1)):
    nblk = ncols // 512
    n = 0
    for which in whichs:
        for kc in range(8):
            p.ts("dve", cbc[:, kc, :], ones[:, :], cin_t[:, which, kc:kc + 1], None, ALU.mult, None,
                 [ones, cin_t], [cbc])
        for j in range(nblk):
            st_t, st_v = stages[n % len(stages)]
            ps = psums[n % len(psums)]
            n += 1
            p.dma("sp", st_v, modw[:, j * 512:(j + 1) * 512].rearrange("(kc p) n -> p kc n", p=128), (), [st_t])
            p.dma("sp", bst[:, :], bcast_rows(modb[0:1, j * 512:(j + 1) * 512]), (), [bst])
            for kc in range(8):
                p.mm(ps[:, :], cbc[:, kc, :], st_v[:, kc, :], kc == 0, kc == 7, [cbc, st_t], [ps])
            tile, ap = outs[which](j)
            p.tt("dve", ap, ps[:, :], bst[:, :], ALU.add, [ps, bst], [tile])


def emit_adanorm_T(p, x_t, A_t, S_t, hb, ptr, hT_ap, hT_t, ident, small):
    ss, rs, junk = small["ss"], small["rs"], small["junk"]
    p.memset("dve", ss[:, 0:1], 0.0, [ss])
    p.act(junk[:, :], x_t[:, :], AF.Square, [x_t, ss], [junk, ss], accum_out=ss[:, 0:1])
    p.ts("dve", rs[:, 0:1], ss[:, 0:1], 1.0 / D, EPS, ALU.mult, ALU.add, [ss], [rs])
    p.op("act", lambda g: g.sqrt(rs[:, 1:2], rs[:, 0:1]), [rs], [rs])
    p.op("dve", lambda g: g.reciprocal(rs[:, 2:3], rs[:, 1:2]), [rs], [rs])
    p.stt("dve", junk[:, :], x_t[:, :], rs[:, 2:3], A_t[:, :], ALU.mult, ALU.mult, [x_t, rs, A_t], [junk])
    p.tt("dve", hb[:, :], junk[:, :], S_t[:, :], ALU.add, [junk, S_t], [hb])
    for kc in range(8):
        p.tr(ptr[:, kc, :], hb[:, kc * 128:(kc + 1) * 128], ident[:, :], [hb, ident], [ptr])
    p.cp("act", hT_ap, ptr[:, :, :], [ptr], [hT_t])


NT_MOE = 18
NLAT_MOE = 16


def build_moe(n_exp=32, NT=NT_MOE):
    nc = bass.Bass("TRN2", target_bir_lowering=False)
    x = nc.dram_tensor("x", [NT * 128, D], F32, kind="ExternalInput").ap()
    cin = nc.dram_tensor("cin", [128, 2, 8], F32, kind="ExternalInput").ap()
    modw = nc.dram_tensor("modw", [D, 3 * D], F32, kind="ExternalInput").ap()
    modb = nc.dram_tensor("modb", [1, 3 * D], F32, kind="ExternalInput").ap()
    nrm = nc.dram_tensor("nrm", [1, D], F32, kind="ExternalInput").ap()
    wr = nc.dram_tensor("wr", [D, 36], F32, kind="ExternalInput").ap()
    br = nc.dram_tensor("br", [1, 36], F32, kind="ExternalInput").ap()
    w13 = nc.dram_tensor("w13", [32, D, D], F32, kind="ExternalInput").ap()
    w2 = nc.dram_tensor("w2", [32, 512, D], F32, kind="ExternalInput").ap()
    identd = nc.dram_tensor("identd", [128, 128], F32, kind="ExternalInput").ap()
    xo = nc.dram_tensor("xo", [NT * 128, D], F32, kind="ExternalOutput").ap()

    p = Prog(nc)
    hT = p.sb([128, 8, NT * 128], BF16, "hT")
    acc = p.sb([128, NT, D], F32, "acc")
    w13b = [p.sb([128, 8, D], BF16, f"w13b{i}") for i in range(2)]
    w2b = [p.sb([128, 4, D], BF16, "w2b0")]
    MB = [[p.sb([128, D], F32, f"mb{w}{v}") for v in range(3)] for w in range(2)]
    xt = p.sb([128, D], F32, "xt")
    hb = p.sb([128, D], BF16, "hb")
    stage = p.sb([128, 8, 128], F32, "stage")
    bstage = p.sb([128, 128], F32, "bstage")
    actT = [p.sb([128, 4, 512], BF16, f"actT{i}") for i in range(2)]
    sa = p.sb([128, 512], F32, "sa")
    cbc = p.sb([128, 8, 128], F32, "cbc")
    CW = p.sb([128, NT, 32], F32, "CW")
    ones = p.sb([128, 128], F32, "ones")
    identf = p.sb([128, 128], F32, "identf")
    ident = p.sb([128, 128], BF16, "ident")
    cin_t = p.sb([128, 2, 8], F32, "cin_t")
    nrm_t = xt
    wrf = p.sb([128, 8, 36], F32, "wrf")
    wrb = p.sb([128, 8, 36], BF16, "wrb")
    brb = p.sb([128, 36], F32, "brb")
    small = {"ss": p.sb([128, 4], F32, "ss"), "rs": p.sb([128, 4], F32, "rs"), "junk": p.sb([128, D], F32, "junk")}
    rt = p.sb([128, 256], F32, "rt")
    pbank = [p.ps([128, 512], F32, f"pb{i}") for i in range(6)]
    ptr = p.ps([128, 8, 128], BF16, "ptr")

    p.dma("sp", identf[:, :], identd[:, :], (), [identf])
    p.cp("dve", ident[:, :], identf[:, :], [identf], [ident])
    p.memset("pool", ones[:, :], 1.0, [ones])
    p.memset("pool", acc[:, :, :], 0.0, [acc])
    p.dma("sp", cin_t[:, :, :], cin[:, :, :], (), [cin_t])
    p.act(cin_t[:, :, :], cin_t[:, :, :], AF.Silu, [cin_t], [cin_t])
    p.dma("sp", nrm_t[:, :], bcast_rows(nrm[0:1, :]), (), [nrm_t])
    p.dma("sp", wrf[:, :, :], wr.rearrange("(kc p) n -> p kc n", p=128), (), [wrf])
    p.cp("dve", wrb[:, :, :], wrf[:, :, :], [wrf], [wrb])
    p.dma("sp", brb[:, :], bcast_rows(br[0:1, :]), (), [brb])

    def outsel(which):
        def f(j):
            v, c = divmod(j, 2)
            t = MB[which][v]
            return t, t[:, c * 512:(c + 1) * 512]
        return f
    stg = w13b[1].h.bitcast(F32)[:, :, :]
    emit_mod_rows(p, cin_t, modw, modb, 3 * D, [outsel(0), outsel(1)], [pbank[0], pbank[1]], [(w13b[1], stg)], cbc, ones, sa,
                  whichs=(0, 1) if NT > NLAT_MOE else (0,))
    for w in range(2 if NT > NLAT_MOE else 1):
        A = MB[w][1]
        p.stt("dve", A[:, :], A[:, :], 1.0, nrm_t[:, :], ALU.add, ALU.mult, [A, nrm_t], [A])

    for i in range(NT):
        w = 0 if i < NLAT_MOE else 1
        p.dma("sp", xt[:, :], x[i * 128:(i + 1) * 128, :], (), [xt])
        emit_adanorm_T(p, xt, MB[w][1], MB[w][0], hb, ptr, hT[:, :, i * 128:(i + 1) * 128], hT, ident, small)
        lgp = pbank[1]
        for kc in range(8):
            p.mm(lgp[:, 0:36], hT[:, kc, i * 128:(i + 1) * 128], wrb[:, kc, :], kc == 0, kc == 7, [hT, wrb], [lgp])
        lg = rt[:, 0:36]
        p.tt("dve", lg, lgp[:, 0:36], brb[:, :], ALU.add, [lgp, brb], [rt])
        R, W_ = [rt], [rt]
        gmax, nb, se, m1, m2, den = (rt[:, 40 + k:41 + k] for k in range(6))
        oh = rt[:, 48:52]
        pen = rt[:, 52:56]
        m32 = rt[:, 64:96]
        e32 = rt[:, 96:128]
        m32b = rt[:, 128:160]
        sel = rt[:, 160:192]
        gex = rt[:, 192:196]
        p.op("dve", lambda g, gmax=gmax, lg=lg: g.reduce_max(gmax, lg[:, 0:4], AX.X), R, W_)
        p.ts("dve", oh, lg[:, 0:4], gmax, None, ALU.is_ge, None, R, W_)
        p.ts("dve", nb, gmax, -1.0, None, ALU.mult, None, R, W_)
        p.act(gex, lg[:, 0:4], AF.Exp, R, W_, bias=nb, accum_out=se)
        p.ts("dve", pen, oh, 1e9, -1e9, ALU.mult, ALU.add, R, W_)
        p.tt("dve", m32.rearrange("p (g e) -> p g e", g=4), lg[:, 4:36].rearrange("p (g e) -> p g e", g=4),
             pen.to_broadcast([128, 4, 8]) if False else rt[:, 52:56].rearrange("p (g o) -> p g o", o=1).broadcast_to([128, 4, 8]),
             ALU.add, R, W_)
        p.op("dve", lambda g, m1=m1, m32=m32: g.reduce_max(m1, m32, AX.X), R, W_)
        p.ts("dve", nb, m1, -1.0, None, ALU.mult, None, R, W_)
        p.act(e32, m32, AF.Exp, R, W_, bias=nb)
        p.ts("dve", m32b, m32, m1, -1e9, ALU.is_ge, ALU.mult, R, W_)
        p.tt("dve", m32b, m32b, m32, ALU.add, R, W_)
        p.op("dve", lambda g, m2=m2, m32b=m32b: g.reduce_max(m2, m32b, AX.X), R, W_)
        p.ts("dve", sel, m32, m2, None, ALU.is_ge, None, R, W_)
        p.tt("dve", sel, sel, e32, ALU.mult, R, W_)
        p.op("dve", lambda g, sel=sel, den=den: g.reduce_sum(den, sel, AX.X), R, W_)
        p.tt("dve", den, den, se, ALU.mult, R, W_)
        p.op("dve", lambda g, den=den: g.reciprocal(den, den), R, W_)
        p.ts("dve", CW[:, i, :], sel, den, None, ALU.mult, None, [rt], [CW])

    blocks = [(b * 4, 4) for b in range(NLAT_MOE // 4)] + ([(NLAT_MOE, NT - NLAT_MOE)] if NT > NLAT_MOE else [])
    pa = [pbank[0], pbank[1]]
    pbb = [pbank[2], pbank[3]]
    po = [pbank[4], pbank[5]]
    cnt_ab = 0
    cnt_o = 0
    cnt_act = 0
    for e in range(n_exp):
        wa = w13b[e % 2]
        p.dma("pool", wa[:, :, :], w13[e].rearrange("(kc p) n -> p kc n", p=128), (), [wa])
        if e % 2 == 0:
            wb = w2b[0]
            p.dma("pool", wb[:, :, :], w2[e].rearrange("(kc p) n -> p kc n", p=128), (), [wb])
            wbv = [(wb, wb[:, fc, :]) for fc in range(4)]
        else:
            wbv = []
            for hh, tl in enumerate((stage, cbc)):
                v = tl.h.bitcast(BF16)[:, :, :].rearrange("p a b -> p (a b)").rearrange("p (f n) -> p f n", f=2)
                p.dma("pool", v, w2[e][hh * 256:(hh + 1) * 256, :].rearrange("(kc p) n -> p kc n", p=128), (), [tl])
                wbv += [(tl, v[:, 0, :]), (tl, v[:, 1, :])]
        for (t0, nt) in blocks:
            ntok = nt * 128
            at = actT[cnt_act % 2]
            cnt_act += 1
            for fc in range(4):
                A_, B_ = pa[cnt_ab % 2], pbb[cnt_ab % 2]
                cnt_ab += 1
                for kc in range(8):
                    p.mm(A_[:, 0:ntok], wa[:, kc, fc * 128:(fc + 1) * 128], hT[:, kc, t0 * 128:t0 * 128 + ntok],
                         kc == 0, kc == 7, [wa, hT], [A_])
                for kc in range(8):
                    p.mm(B_[:, 0:ntok], wa[:, kc, 512 + fc * 128:512 + (fc + 1) * 128],
                         hT[:, kc, t0 * 128:t0 * 128 + ntok], kc == 0, kc == 7, [wa, hT], [B_])
                p.act(sa[:, 0:ntok], A_[:, 0:ntok], AF.Silu, [A_], [sa])
                p.tt("dve", at[:, fc, 0:ntok], sa[:, 0:ntok], B_[:, 0:ntok], ALU.mult, [sa, B_], [at])
            for tt_ in range(nt):
                ti = t0 + tt_
                for half in range(2):
                    O_ = po[cnt_o % 2]
                    cnt_o += 1
                    for fc in range(4):
                        wt_, wv_ = wbv[fc]
                        p.mm(O_[:, :], at[:, fc, tt_ * 128:(tt_ + 1) * 128], wv_[:, half * 512:(half + 1) * 512],
                             fc == 0, fc == 3, [at, wt_], [O_])
                    accs = acc[:, ti, half * 512:(half + 1) * 512]
                    p.stt("dve", accs, O_[:, :], CW[:, ti, e:e + 1], accs, ALU.mult, ALU.add, [O_, CW, acc], [acc])

    for i in range(NT):
        w = 0 if i < NLAT_MOE else 1
        p.dma("sp", xt[:, :], x[i * 128:(i + 1) * 128, :], (), [xt])
        j = small["junk"]
        p.tt("dve", j[:, :], acc[:, i, :], MB[w][2][:, :], ALU.mult, [acc, MB[w][2]], [j])
        p.tt("dve", j[:, :], j[:, :], xt[:, :], ALU.add, [j, xt], [j])
        p.dma("sp", xo[i * 128:(i + 1) * 128, :], j[:, :], [j], ())
    p.emit()
    return nc


def moe_inputs(x_core, c_b, c_ctx, mod_w_i, mod_b_i, norm_ffn_i, wg, bg, we, be, w13, w2):
    cin = np.stack([c_b.reshape(8, 128).T, c_ctx.reshape(8, 128).T], axis=1)
    return {
        "x": np.ascontiguousarray(x_core, dtype=np.float32),
        "cin": np.ascontiguousarray(cin, dtype=np.float32),
        "modw": np.ascontiguousarray(mod_w_i[:, 3 * D:6 * D]),
        "modb": np.ascontiguousarray(mod_b_i[None, 3 * D:6 * D]),
        "nrm": np.ascontiguousarray(norm_ffn_i[None, :]),
        "wr": np.ascontiguousarray(np.concatenate([wg, we], axis=1)),
        "br": np.ascontiguousarray(np.concatenate([bg, be])[None, :]),
        "w13": w13, "w2": w2,
        "identd": np.eye(128, dtype=np.float32),
    }


NTA = 14
NLOC = 8
WCOLS = 2432
NEG = -30000.0


def alias(p, t, name):
    a = T(t.h, name)
    a.r.w = t.r.w
    a.r.readers = dict(t.r.readers)
    return a


def build_attn(ph=9, dbg=False, NSLAB=2):
    nc = bass.Bass("TRN2", target_bir_lowering=False)
    dt_in = lambda n, s: nc.dram_tensor(n, s, F32, kind="ExternalInput").ap()
    xe = dt_in("xe", [NSLAB * NTA * 128, D])
    cin = dt_in("cin", [128, 2, 8])
    modw = dt_in("modw", [D, 3 * D])
    modb = dt_in("modb", [1, 3 * D])
    nrm = dt_in("nrm", [1, D])
    win = dt_in("win", [D, WCOLS])
    wout = dt_in("wout", [D, D])
    gains = dt_in("gains", [128, 4])
    ropec = dt_in("ropec", [128, NSLAB * 1536])
    ropes = dt_in("ropes", [128, NSLAB * 1536])
    pmd = dt_in("pmd", [128, 128])
    onesd = dt_in("onesd", [128, 128])
    identd = dt_in("identd", [128, 128])
    nab = dt_in("nab", [NSLAB * 8, 128, 27 * 128])
    wam = dt_in("wam", [128, NSLAB * 4 * 512])
    sink = dt_in("sink", [1, 8])
    xo = nc.dram_tensor("xo", [NSLAB * (NLOC + 2) * 128, D], F32, kind="ExternalOutput").ap()

    p = Prog(nc)
    NTOK = NTA * 128
    big = p.sb([128, 8 * NTOK], BF16, "big")
    hTv = big[:, :].rearrange("p (kc t) -> p kc t", kc=8)
    big2 = p.sb([128, 8 * WCOLS], BF16, "big2")
    winv = big2[:, :].rearrange("p (kc n) -> p kc n", kc=8)
    QT = p.sb([128, 14, NTOK], BF16, "QT")
    VA = p.sb([128, NTA, 8, 65], BF16, "VA")
    VB = p.sb([128, NTA, 2, 65], BF16, "VB")
    PT = [p.sb([128, 8, 128], BF16, f"PT{i}") for i in range(2)]
    PTWt = p.sb([128, 5, 512], BF16, "PTWs")
    PTW = PTWt.h
    MB = [[p.sb([128, D], F32, f"mb{w}{v}") for v in range(3)] for w in range(2)]
    xt = p.sb([128, D], F32, "xt")
    hb = p.sb([128, D], BF16, "hb")
    stage = p.sb([128, 8, 128], F32, "stage")
    bstage = p.sb([128, 128], F32, "bstage")
    cbc = p.sb([128, 8, 128], F32, "cbc")
    ones = p.sb([128, 128], F32, "ones")
    identf = p.sb([128, 128], F32, "identf")
    ident = p.sb([128, 128], BF16, "ident")
    onesb = p.sb([128, 128], BF16, "onesb")
    pm = p.sb([128, 128], BF16, "pm")
    cin_t = p.sb([128, 2, 8], F32, "cin_t")
    G = p.sb([128, 4], F32, "G")
    RC = p.sb([128, 1536], BF16, "RC")
    RS = p.sb([128, 1536], BF16, "RS")
    WM = p.sb([128, 4, 512], BF16, "WM")
    esink = p.sb([128, 8], F32, "esink")
    small = {"ss": p.sb([128, 4], F32, "ss"), "rs": p.sb([128, 4], F32, "rs"), "junk": p.sb([128, D], F32, "junk")}
    sq = p.sb([128, 512], BF16, "sq")
    rstd = p.sb([128, 512], F32, "rstd")
    qn = p.sb([128, 512], BF16, "qn")
    t1 = p.sb([128, 512], F32, "t1")
    rec = p.sb([128, 8], F32, "rec")
    oT = p.sb([128, 8, 128], BF16, "oT")
    pbank = [p.ps([128, 512], F32, f"pb{i}") for i in range(7)]
    ptr = p.ps([128, 8, 128], BF16, "ptr")

    p.dma("sp", identf[:, :], identd[:, :], (), [identf])
    p.cp("dve", ident[:, :], identf[:, :], [identf], [ident])
    p.dma("pool", onesb[:, :], onesd[:, :], (), [onesb])
    p.dma("pool", pm[:, :], pmd[:, :], (), [pm])
    p.memset("pool", ones[:, :], 1.0, [ones])
    p.memset("pool", VA[:, :, :, :], 1.0, [VA])
    p.memset("pool", VB[:, :, :, :], 1.0, [VB])
    p.dma("sp", cin_t[:, :, :], cin[:, :, :], (), [cin_t])
    p.act(cin_t[:, :, :], cin_t[:, :, :], AF.Silu, [cin_t], [cin_t])
    p.dma("sp", xt[:, :], bcast_rows(nrm[0:1, :]), (), [xt])
    p.dma("sp", G[:, :], gains[:, :], (), [G])
    p.ts("dve", G[:, 0:1], G[:, 0:1], 0.125, None, ALU.mult, None, [G], [G])
    p.ts("dve", G[:, 2:3], G[:, 2:3], 0.125, None, ALU.mult, None, [G], [G])
    p.dma("sp", esink[:, :], bcast_rows(sink[0:1, :]), (), [esink])
    p.act(esink[:, :], esink[:, :], AF.Exp, [esink], [esink])

    def outsel(which):
        def f(j):
            v, c = divmod(j, 2)
            t = MB[which][v]
            return t, t[:, c * 512:(c + 1) * 512]
        return f
    qf = QT.h.bitcast(F32)[:, :, :].rearrange("p c t -> p (c t)")
    stgs = [(QT, qf[:, k * 4096:(k + 1) * 4096].rearrange("p (kc n) -> p kc n", kc=8)) for k in range(2)]
    emit_mod_rows(p, cin_t, modw, modb, 3 * D, [outsel(0), outsel(1)], [pbank[0], pbank[1]], stgs, cbc, ones, rstd)
    for w in range(2):
        A = MB[w][1]
        p.stt("dve", A[:, :], A[:, :], 1.0, xt[:, :], ALU.add, ALU.mult, [A, xt], [A])

    prev_alias = []
    for slab in range(NSLAB):
        for base, al in prev_alias:
            for ev in ([al.r.w] if al.r.w is not None else []) + list(al.r.readers.values()):
                old = base.r.readers.get(id(ev[0]))
                if old is None or old[1] < ev[1]:
                    base.r.readers[id(ev[0])] = ev
        xe_s = xe[slab * NTA * 128:(slab + 1) * NTA * 128, :]
        p.dma("pool", RC[:, :], ropec[:, slab * 1536:(slab + 1) * 1536], (), [RC])
        p.dma("pool", RS[:, :], ropes[:, slab * 1536:(slab + 1) * 1536], (), [RS])
        p.dma("pool", WM[:, :, :], wam[:, slab * 2048:(slab + 1) * 2048].rearrange("p (s q) -> p s q", s=4), (), [WM])
        p.dma("pool", winv, win.rearrange("(kc p) n -> p kc n", p=128), (), [big2])
        for i in range(NTA):
            w = 0 if i < 12 else 1
            p.dma("sp", xt[:, :], xe_s[i * 128:(i + 1) * 128, :], (), [xt])
            emit_adanorm_T(p, xt, MB[w][1], MB[w][0], hb, ptr, hTv[:, :, i * 128:(i + 1) * 128], big, ident, small)

        blocks = [(0, 512), (512, 512), (1024, 512), (1536, 256)]
        cntp = 0
        for ch in range(14 if ph >= 2 else 0):
            gi = 0 if ch < 4 else 1 if ch < 8 else 2 if ch < 12 else 3
            rope = ch >= 8
            for (t0, n) in blocks:
                pq = pbank[cntp % 2]
                pmm = pbank[2 + cntp % 2]
                cntp += 1
                for kc in range(8):
                    p.mm(pq[:, 0:n], winv[:, kc, ch * 128:(ch + 1) * 128], hTv[:, kc, t0:t0 + n], kc == 0, kc == 7,
                         [big2, big], [pq])
                p.act(sq[:, 0:n], pq[:, 0:n], AF.Square, [pq], [sq])
                p.mm(pmm[:, 0:n], onesb[:, :], sq[:, 0:n], True, True, [onesb, sq], [pmm])
                p.ts("dve", rstd[:, 0:n], pmm[:, 0:n], 1.0 / 64, EPS, ALU.mult, ALU.add, [pmm], [rstd])
                p.op("act", lambda g, n=n: g.sqrt(rstd[:, 0:n], rstd[:, 0:n]), [rstd], [rstd])
                p.op("dve", lambda g, n=n: g.reciprocal(rstd[:, 0:n], rstd[:, 0:n]), [rstd], [rstd])
                if rope and t0 < 1536:
                    p.stt("dve", qn[:, 0:n], pq[:, 0:n], G[:, gi:gi + 1], rstd[:, 0:n], ALU.mult, ALU.mult,
                          [pq, G, rstd], [qn])
                    pr = pbank[4 + cntp % 2]
                    p.mm(pr[:, 0:n], pm[:, :], qn[:, 0:n], True, True, [pm, qn], [pr])
                    p.tt("pool", t1[:, 0:n], qn[:, 0:n], RC[:, t0:t0 + n], ALU.mult, [qn, RC], [t1])
                    p.tt("dve", rstd[:, 0:n], pr[:, 0:n], RS[:, t0:t0 + n], ALU.mult, [pr, RS], [rstd])
                    p.tt("dve", QT[:, ch, t0:t0 + n], t1[:, 0:n], rstd[:, 0:n], ALU.add, [t1, rstd], [QT])
                else:
                    p.stt("dve", QT[:, ch, t0:t0 + n], pq[:, 0:n], G[:, gi:gi + 1], rstd[:, 0:n], ALU.mult, ALU.mult,
                          [pq, G, rstd], [QT])

        for i in range(NTA if ph >= 3 else 0):
            pv, pv2 = pbank[cntp % 2], pbank[2 + cntp % 2]
            cntp += 1
            for kc in range(8):
                p.mm(pv[:, :], hTv[:, kc, i * 128:(i + 1) * 128], winv[:, kc, 1792:2304], kc == 0, kc == 7, [big, big2], [pv])
            for kc in range(8):
                p.mm(pv2[:, 0:128], hTv[:, kc, i * 128:(i + 1) * 128], winv[:, kc, 2304:2432], kc == 0, kc == 7,
                     [big, big2], [pv2])
            p.cp("act", VA[:, i, :, 0:64], pv[:, :].rearrange("p (h d) -> p h d", h=8), [pv], [VA])
            p.cp("dve", VB[:, i, :, 0:64], pv2[:, 0:128].rearrange("p (h d) -> p h d", h=2), [pv2], [VB])

        OAt = alias(p, big, "OA")
        OA = big[:, 0:(NLOC + 2) * 1024].rearrange("p (t d) -> p t d", d=1024)
        WO = alias(p, big2, "WO")
        wov = big2[:, 0:8192].rearrange("p (kc n) -> p kc n", kc=8)
        NABt = [alias(p, big2, f"NAB{i}") for i in range(2)]
        nabv = [big2[:, 8192 + i * 3456:8192 + (i + 1) * 3456].rearrange("p (s q) -> p s q", q=128) for i in range(2)]
        p.dma("pool", wov, wout.rearrange("(kc p) n -> p kc n", p=128), (), [WO])

        CT = [12, 13]

        def na_unit(h, qt, ktiles, slots, nabT, nabV, ot, u):
            half = slice((h % 2) * 64, (h % 2) * 64 + 64)
            qch, kch = h // 2, 4 + h // 2
            S = [pbank[(u % 2) * 2], pbank[(u % 2) * 2 + 1]]
            O = pbank[4 + u % 2]
            pt = PT[u % 2]
            allk = [(kt, sl) for kt, sl in zip(ktiles, slots)] + [(c, None) for c in CT]
            for c, (kt, sl) in enumerate(allk):
                bank = S[c // 4]
                o = bank[:, (c % 4) * 128:(c % 4 + 1) * 128]
                p.mm(o, QT[half, kch, kt * 128:(kt + 1) * 128], QT[half, qch, qt * 128:(qt + 1) * 128], True, sl is None,
                     [QT], [bank])
                if sl is not None:
                    p.mm(o, ident[:, :], nabV[:, sl, :], False, True, [ident, nabT], [bank])
            nck = len(allk)
            n0 = min(nck, 4)
            p.act(pt[:, 0:n0, :], S[0][:, 0:n0 * 128].rearrange("p (c q) -> p c q", q=128), AF.Exp, [S[0]], [pt])
            if nck > 4:
                p.act(pt[:, 4:nck, :], S[1][:, 0:(nck - 4) * 128].rearrange("p (c q) -> p c q", q=128), AF.Exp, [S[1]], [pt])
            for c, (kt, sl) in enumerate(allk):
                p.mm(O[:, 0:65], pt[:, c, :], VA[:, kt, h, :], c == 0, c == nck - 1, [pt, VA], [O])
            p.op("dve", lambda g, O=O, h=h: g.reciprocal(rec[:, h:h + 1], O[:, 64:65]), [O], [rec])
            p.ts("dve", OA[:, ot, h * 64:(h + 1) * 64], O[:, 0:64], rec[:, h:h + 1], None, ALU.mult, None, [O, rec], [OAt])

        u = 0
        for h in range(8 if ph >= 4 else 0):
            nT, nV = NABt[h % 2], nabv[h % 2]
            p.dma("pool", nV, nab[slab * 8 + h].rearrange("p (s q) -> p s q", q=128), (), [nT])
            for rp in range(NLOC):
                if rp == 0:
                    kts, sls = list(range(0, 6)), list(range(5, 11))
                elif rp == 1:
                    kts, sls = list(range(1, 6)), list(range(11, 16))
                elif rp == NLOC - 2:
                    kts, sls = list(range(rp, rp + 5)), list(range(16, 21))
                elif rp == NLOC - 1:
                    kts, sls = list(range(rp - 1, rp + 5)), list(range(21, 27))
                else:
                    kts, sls = list(range(rp, rp + 5)), list(range(0, 5))
                na_unit(h, rp + 2, kts, sls, nT, nV, rp, u)
                u += 1
            for ci, ct in enumerate(CT):
                na_unit(h, ct, [], [], nT, nV, NLOC + ci, u)
                u += 1

        def wa_unit(qt, kvh, ktiles, mslots, ot, u):
            kch = 12 + kvh
            allk = [(kt, ms) for kt, ms in zip(ktiles, mslots)] + [(c, None) for c in CT]
            nck = len(allk)
            for c, (kt, ms) in enumerate(allk):
                for par in range(2):
                    bank = pbank[((u * 5 + c) % 2) * 2 + par]
                    half = slice(par * 64, par * 64 + 64)
                    for jj in range(2):
                        j = 2 * jj + par
                        h = 4 * kvh + j
                        o = bank[:, jj * 128:(jj + 1) * 128]
                        p.mm(o, QT[half, kch, kt * 128:(kt + 1) * 128], QT[half, 8 + h // 2, qt * 128:(qt + 1) * 128],
                             True, ms is None, [QT], [bank])
                        if ms is not None:
                            p.mm(o, ident[:, :], WM[:, ms, 0:128], False, True, [ident, WM], [bank])
                    p.act(PTW[:, c, par * 256:(par + 1) * 256], bank[:, 0:256], AF.Exp, [bank], [PTWt])
            O = pbank[4 + u % 2]
            WSUB = int(os.environ.get('WSUB', '9'))
            if WSUB < 1:
                return
            for j in range(4):
                pos = (j % 2) * 2 + j // 2
                for c, (kt, ms) in enumerate(allk):
                    p.mm(O[:, j * 128:j * 128 + 65], PTW[:, c, pos * 128:(pos + 1) * 128], VB[:, kt, kvh, :], c == 0,
                         c == nck - 1, [PTWt, VB], [O])
            if WSUB < 2:
                return
            for j in range(4):
                h = 4 * kvh + j
                p.tt("dve", rec[:, h:h + 1], O[:, j * 128 + 64:j * 128 + 65], esink[:, h:h + 1], ALU.add, [O, esink], [rec])
                p.op("dve", lambda g, h=h: g.reciprocal(rec[:, h:h + 1], rec[:, h:h + 1]), [rec], [rec])
                p.ts("dve", OA[:, ot, 512 + h * 64:512 + (h + 1) * 64], O[:, j * 128:j * 128 + 64], rec[:, h:h + 1], None,
                     ALU.mult, None, [O, rec], [OAt])

        for n in range(int(os.environ.get('WN', NLOC)) if ph >= 5 else 0):
            for kvh in range(2):
                ms = [2 if n == 0 else 0, None, 3 if n == NLOC - 1 else 1]
                wa_unit(n + 2, kvh, [n + 1, n + 2, n + 3], ms, n, u)
                u += 1
        for ci, ct in enumerate(CT if ph >= 5 and int(os.environ.get('WC', 1)) else []):
            for kvh in range(2):
                wa_unit(ct, kvh, [], [], NLOC + ci, u)
                u += 1

        if dbg:
            dOA = nc.dram_tensor("dOA", [128, 10 * 1024], F32, kind="ExternalOutput").ap()
            dQT = nc.dram_tensor("dQT", [128, 14 * NTOK], F32, kind="ExternalOutput").ap()
            dVA = nc.dram_tensor("dVA", [128, NTA * 8 * 65], F32, kind="ExternalOutput").ap()
            p.dma("pool", dOA[:, :], big[:, 0:10 * 1024], [OAt], ())
            p.dma("pool", dQT[:, :], QT[:, :, :].rearrange("p c t -> p (c t)"), [QT], ())
            p.dma("pool", dVA[:, :], VA[:, :, :, :].rearrange("p t h d -> p (t h d)"), [VA], ())
        for o in range(NLOC + 2):
            w = 0 if o < NLOC else 1
            src = o + 2 if o < NLOC else 12 + (o - NLOC)
            for kc in range(8):
                p.tr(ptr[:, kc, :], OA[:, o, kc * 128:(kc + 1) * 128], ident[:, :], [OAt, ident], [ptr])
            p.cp("act", oT[:, :, :], ptr[:, :, :], [ptr], [oT])
            y0, y1 = pbank[(o % 2) * 2], pbank[(o % 2) * 2 + 1]
            for half, y in enumerate((y0, y1)):
                for kc in range(8):
                    p.mm(y[:, :], oT[:, kc, :], wov[:, kc, half * 512:(half + 1) * 512], kc == 0, kc == 7, [oT, WO], [y])
            p.dma("sp", xt[:, :], xe_s[src * 128:(src + 1) * 128, :], (), [xt])
            j = small["junk"]
            g1 = MB[w][2]
            for half, y in enumerate((y0, y1)):
                sl = slice(half * 512, (half + 1) * 512)
                p.tt("dve", j[:, sl], y[:, :], g1[:, sl], ALU.mult, [y, g1], [j])
            p.tt("dve", j[:, :], j[:, :], xt[:, :], ALU.add, [j, xt], [j])
            p.dma("sp", xo[(slab * (NLOC + 2) + o) * 128:(slab * (NLOC + 2) + o + 1) * 128, :], j[:, :], [j], ())
        prev_alias = [(big, OAt), (big2, WO), (big2, NABt[0]), (big2, NABt[1])]
    p.emit()
    return nc


def rope_tables(tok0):
    t = tok0 + np.arange(1536)
    row, col = (t // 64).astype(np.float32), (t % 64).astype(np.float32)
    inv = (10000.0 ** (-np.arange(16, dtype=np.float32) / 16)).astype(np.float32)
    C = np.zeros((64, 1536), np.float32)
    S = np.zeros((64, 1536), np.float32)
    for d in range(64):
        pos = row if d < 32 else col
        q = d % 32
        ang = (pos * inv[q % 16]).astype(np.float32)
        C[d] = np.cos(ang)
        S[d] = -np.sin(ang) if q < 16 else np.sin(ang)
    return np.concatenate([C, C], 0), np.concatenate([S, S], 0)


def perm_matrix():
    P = np.zeros((128, 128), np.float32)
    for m in range(128):
        blk, d = divmod(m, 64)
        q = d % 32
        partner = d + 16 if q < 16 else d - 16
        P[blk * 64 + partner, m] = 1.0
    return P


def na_bias_tables(rel_bias, R0):
    out = np.full((8, 128, 27, 128), NEG, np.float32)
    specs = []
    for c in range(5):
        specs.append((c, 2, 2 + c))
    for c in range(6):
        specs.append((5 + c, 0, c))
    for c in range(5):
        specs.append((11 + c, 1, 1 + c))
    for c in range(5):
        specs.append((16 + c, NLOC - 2, NLOC - 2 + c))
    for c in range(6):
        specs.append((21 + c, NLOC - 1, NLOC - 2 + c))
    kp = np.arange(128)
    qi = np.arange(128)
    for slot, rp, kt in specs:
        r = R0 + 2 * rp + qi // 64
        i = qi % 64
        kr = R0 - 4 + 2 * kt + kp // 64
        jc = kp % 64
        r0 = np.clip(r - 4, 0, 120)
        c0 = np.clip(i - 8, 0, 48)
        valid = ((kr[:, None] >= r0[None, :]) & (kr[:, None] < r0[None, :] + 8) & (kr[:, None] >= 0) & (kr[:, None] < 128)
                 & (jc[:, None] >= c0[None, :]) & (jc[:, None] < c0[None, :] + 16))
        dr = np.clip(kr[:, None] - r[None, :] + 7, 0, 14)
        dc = np.clip(jc[:, None] - i[None, :] + 15, 0, 30)
        vals = rel_bias[:, dr, dc]
        out[:, :, slot, :] = np.where(valid[None], vals, NEG)
    return out.reshape(8, 128, 27 * 128)


def wa_masks(gb0):
    kp = np.arange(128)[:, None]
    qi = np.arange(128)[None, :]
    prev = np.where(kp >= qi, 0.0, NEG).astype(np.float32)
    nxt = np.where(kp <= qi, 0.0, NEG).astype(np.float32)
    allneg = np.full((128, 128), NEG, np.float32)
    m = [prev, nxt, prev if gb0 > 0 else allneg, nxt if gb0 + NLOC < 64 else allneg]
    return np.concatenate([np.tile(x, (1, 4)) for x in m], axis=1)


def attn_inputs1(inp, b, s):
    R0 = 16 * s
    x = inp["x"][b]
    xe = np.zeros((NTA * 128, D), np.float32)
    g0 = (R0 - 4) * 64
    lo, hi = max(g0, 0), min(g0 + 1536, 8192)
    xe[lo - g0:hi - g0] = x[lo:hi]
    xe[1536:] = inp["ctx"][b]
    w = inp["att_w_in"][0]
    win = np.concatenate([w[:, 0:512], w[:, 512:1024], w[:, 1536:2048], w[:, 2048:2112], w[:, 2048:2112],
                          w[:, 2112:2176], w[:, 2112:2176], w[:, 1024:1536], w[:, 2176:2304]], axis=1)
    gv = [inp["na_q_norm"][0], inp["na_k_norm"][0], inp["wa_q_norm"][0], inp["wa_k_norm"][0]]
    gains = np.stack([np.concatenate([g, g]) for g in gv], axis=1)
    rc, rs = rope_tables(g0)
    cin = np.stack([inp["c"][b].reshape(8, 128).T, inp["c_ctx"].reshape(8, 128).T], axis=1)
    ob = np.zeros((128, 128), np.float32)
    ob[:64, :64] = 1.0
    ob[64:, 64:] = 1.0
    return {
        "xe": xe, "cin": np.ascontiguousarray(cin, dtype=np.float32),
        "modw": np.ascontiguousarray(inp["mod_w"][0][:, 0:3 * D]), "modb": np.ascontiguousarray(inp["mod_b"][0][None, 0:3 * D]),
        "nrm": np.ascontiguousarray(inp["norm_mix"][0][None, :]),
        "win": np.ascontiguousarray(win), "wout": np.ascontiguousarray(inp["att_w_out"][0]),
        "gains": np.ascontiguousarray(gains, dtype=np.float32), "ropec": rc, "ropes": rs, "pmd": perm_matrix(),
        "onesd": ob, "identd": np.eye(128, dtype=np.float32),
        "nab": na_bias_tables(inp["na_rel_bias"][0], R0), "wam": wa_masks(R0 // 2),
        "sink": np.ascontiguousarray(inp["wa_sink"][0][None, :]),
    }


TS = 66
NSEQ = TS * 128
WS = 1056
TWO_PI = 2.0 * np.pi


def build_ssm(nt=TS, do_s5=True):
    nc = bass.Bass("TRN2", target_bir_lowering=False)
    dt_in = lambda n, s: nc.dram_tensor(n, s, F32, kind="ExternalInput").ap()
    xs = dt_in("xs", [NSEQ, D])
    cin = dt_in("cin", [128, 2, 8])
    modw = dt_in("modw", [D, 2 * D])
    modb = dt_in("modb", [1, 2 * D])
    nrm = dt_in("nrm", [1, D])
    wsel = dt_in("wsel", [D, WS])
    cw = dt_in("cw", [128, 6 * 3])
    cbias = dt_in("cbias", [128, 6])
    dtb = dt_in("dtb", [1, 8])
    alog = dt_in("alog", [1, 8])
    dsk = dt_in("dsk", [1, 8])
    identd = dt_in("identd", [128, 128])
    triud = dt_in("triud", [128, 128])
    iotad = dt_in("iotad", [128, 129])
    m01d = dt_in("m01d", [128, 512])
    lam = dt_in("lam", [128, 3 * 8])
    BLr = dt_in("BLr", [8, 128, 128])
    BLi = dt_in("BLi", [8, 128, 128])
    CLr = dt_in("CLr", [8, 128, 32])
    CLi = dt_in("CLi", [8, 128, 32])
    DL = dt_in("DL", [8, 128, 32])
    yssd = nc.dram_tensor("yssd", [NSEQ, 512], F32, kind="ExternalOutput").ap()
    ys5 = nc.dram_tensor("ys5", [256, NSEQ], F32, kind="ExternalOutput").ap()

    p = Prog(nc)
    UT = p.sb([128, 2, NSEQ], BF16, "UT")
    Z = p.sb([128, 2, NSEQ], F32, "Z")
    MB = [[p.sb([128, D], F32, f"mb{w}{v}") for v in range(2)] for w in range(2)]
    wsb = p.sb([128, 8, WS], BF16, "wsb")
    xt = p.sb([128, D], F32, "xt")
    hb = p.sb([128, D], BF16, "hb")
    hTi = p.sb([128, 8, 128], BF16, "hTi")
    stage = p.sb([128, 8, 128], F32, "stage")
    bstage = p.sb([128, 128], F32, "bstage")
    cbc = p.sb([128, 8, 128], F32, "cbc")
    ones = p.sb([128, 128], F32, "ones")
    identf = p.sb([128, 128], F32, "identf")
    ident = p.sb([128, 128], BF16, "ident")
    triu = p.sb([128, 128], F32, "triu")
    cin_t = p.sb([128, 2, 8], F32, "cin_t")
    small = {"ss": p.sb([128, 4], F32, "ss"), "rs": p.sb([128, 4], F32, "rs"), "junk": p.sb([128, D], F32, "junk")}
    CW = p.sb([128, 18], F32, "CWc")
    CBs = p.sb([128, 6], F32, "CBs")
    dtb_t = p.sb([128, 8], F32, "dtb_t")
    A_t = p.sb([128, 8], F32, "A_t")
    dsk_t = p.sb([128, 8], F32, "dsk_t")
    RAW = [p.sb([128, 6, 128], BF16, f"raw{i}") for i in range(3)]
    DTs = [p.sb([128, 8], F32, f"dts{i}") for i in range(3)]
    CBUF = p.sb([128, 6, 130], BF16, "CBUF")
    cacc6 = p.sb([128, 6, 128], F32, "cacc6")
    tmp6 = p.sb([128, 6, 128], F32, "tmp6")
    XC = p.sb([128, 6, 128], BF16, "XC")
    XTOK = p.sb([128, 512], BF16, "XTOK")
    BTOK = p.sb([128, 128], BF16, "BTOK")
    sm = p.sb([128, 64], F32, "sm")
    CBT = p.sb([128, 128], F32, "CBT")
    WT4 = [p.sb([128, 512], BF16, f"WT4{i}") for i in range(2)]
    H = p.sb([128, 512], F32, "H")
    Hb = p.sb([128, 512], BF16, "Hb")
    XW = p.sb([128, 512], BF16, "XW")
    ysb = p.sb([128, 512], F32, "ysb")
    ytmp = p.sb([128, 512], F32, "ytmp")
    B0, B1, B2, B3, B4, B5, B6 = [p.ps([128, 512], F32, f"pb{i}") for i in range(7)]
    ptr = p.ps([128, 8, 128], BF16, "ptr")

    p.dma("sp", identf[:, :], identd[:, :], (), [identf])
    p.cp("dve", ident[:, :], identf[:, :], [identf], [ident])
    p.dma("sp", triu[:, :], triud[:, :], (), [triu])
    p.memset("pool", ones[:, :], 1.0, [ones])
    p.memset("pool", H[:, :], 0.0, [H])
    p.dma("pool", wsb[:, :, :], wsel.rearrange("(kc p) n -> p kc n", p=128), (), [wsb])
    p.dma("sp", cin_t[:, :, :], cin[:, :, :], (), [cin_t])
    p.act(cin_t[:, :, :], cin_t[:, :, :], AF.Silu, [cin_t], [cin_t])
    p.dma("sp", xt[:, :], bcast_rows(nrm[0:1, :]), (), [xt])
    p.dma("sp", CW[:, :], cw[:, :], (), [CW])
    p.dma("sp", CBs[:, :], cbias[:, :], (), [CBs])
    p.dma("sp", dtb_t[:, :], bcast_rows(dtb[0:1, :]), (), [dtb_t])
    p.dma("sp", A_t[:, :], bcast_rows(alog[0:1, :]), (), [A_t])
    p.act(A_t[:, :], A_t[:, :], AF.Exp, [A_t], [A_t])
    p.ts("dve", A_t[:, :], A_t[:, :], -1.0, None, ALU.mult, None, [A_t], [A_t])
    p.dma("sp", dsk_t[:, :], bcast_rows(dsk[0:1, :]), (), [dsk_t])

    def outsel(which):
        def f(j):
            v, c = divmod(j, 2)
            t = MB[which][v]
            return t, t[:, c * 512:(c + 1) * 512]
        return f
    stgs = [(Z, Z[:, k, 0:4096].rearrange("p (kc n) -> p kc n", kc=8)) for k in range(2)]
    emit_mod_rows(p, cin_t, modw, modb, 2 * D, [outsel(0), outsel(1)], [B0, B1], stgs, cbc, ones, ysb)
    for w in range(2):
        A = MB[w][1]
        p.stt("dve", A[:, :], A[:, :], 1.0, xt[:, :], ALU.add, ALU.mult, [A, xt], [A])

    PSUB = int(os.environ.get('PSUB', '9'))

    RMt = [alias(p, stage, f"RM{i}") for i in range(2)]
    RMv = [stage[:, 4 * i:4 * i + 4, :].rearrange("p c t -> p (c t)") for i in range(2)]
    LMt = [alias(p, cbc, f"LM{i}") for i in range(2)]
    LMv = [cbc[:, 4 * i:4 * i + 4, :].rearrange("p c t -> p (c t)") for i in range(2)]

    bufsets = [(xt, hb, hTi)]

    def project(i, xt=xt, hb=hb, hTi=hTi):
        if PSUB < 1:
            return
        w = 1 if i < 2 else 0
        if i == 2:
            v = MB[1][1].h.bitcast(BF16)
            bufsets.append((TV(MB[1][0].h[:, :], "xt2", MB[1][0]), TV(v[:, 0:1024], "hb2", MB[1][1]),
                            TV(v[:, 1024:2048].rearrange("p (kc t) -> p kc t", kc=8), "hTi2", MB[1][1])))
        xt, hb, hTi = bufsets[i % len(bufsets)]
        p.dma("sp", xt[:, :], xs[i * 128:(i + 1) * 128, :], (), [xt])
        emit_adanorm_T(p, xt, MB[w][1], MB[w][0], hb, ptr, hTi[:, :, :], hTi, ident, small)

    def projectB(i):
        if PSUB < 1:
            return
        xt, hb, hTi = bufsets[i % len(bufsets)] if i >= 2 else bufsets[0]
        raw, dts = RAW[i % 3], DTs[i % 3]
        if PSUB < 2:
            return
        PQ = int(os.environ.get('PQ', '9'))
        for grp in range(2):
            if grp == 1 and PQ < 3:
                break
            for c4 in range(4):
                ch = grp * 4 + c4
                for kc in range(8):
                    p.mm(B0[:, c4 * 128:(c4 + 1) * 128], wsb[:, kc, ch * 128:(ch + 1) * 128], hTi[:, kc, :], kc == 0,
                         kc == 7, [wsb, hTi], [B0])
            if grp == 0:
                if PQ >= 2:
                    p.cp("act", raw[:, 0:4, :], B0[:, :].rearrange("p (c t) -> p c t", c=4), [B0], [raw])
            else:
                if PQ >= 4:
                    p.cp("act", raw[:, 4:6, :], B0[:, 0:256].rearrange("p (c t) -> p c t", c=2), [B0], [raw])
                if PQ >= 5:
                    for c2 in range(2):
                        p.cp("act", UT[:, c2, i * 128:(i + 1) * 128], B0[:, 256 + c2 * 128:384 + c2 * 128], [B0], [UT])
        if PSUB < 3:
            return
        for kc in range(8):
            p.mm(B1[:, 0:8], hTi[:, kc, :], wsb[:, kc, 1024:1032], kc == 0, kc == 7, [hTi, wsb], [B1])
        p.tt("dve", dts[:, :], B1[:, 0:8], dtb_t[:, :], ALU.add, [B1, dtb_t], [dts])
        p.act(dts[:, :], dts[:, :], AF.Exp, [dts], [dts])
        p.act(dts[:, :], dts[:, :], AF.Ln, [dts], [dts], bias=1.0)

    a_, acs, tot, eacs, wend, dec = (sm[:, 8 * k:8 * k + 8] for k in range(6))

    SSUB = int(os.environ.get('SSUB', '9'))

    def ssd_chunk(j):
        raw, dts = RAW[j % 3], DTs[j % 3]
        if SSUB < 1:
            return
        first = j in (0, 2)
        last = j in (1, nt - 1)
        if first:
            p.memset("pool", CBUF[:, :, 0:1], 0.0, [CBUF])
        else:
            p.cp("pool", CBUF[:, :, 0:1], RAW[(j - 1) % 3][:, :, 127:128], [RAW[(j - 1) % 3]], [CBUF])
        p.cp("pool", CBUF[:, :, 1:129], raw[:, :, :], [raw], [CBUF])
        if last:
            p.memset("pool", CBUF[:, :, 129:130], 0.0, [CBUF])
        else:
            p.cp("pool", CBUF[:, :, 129:130], RAW[(j + 1) % 3][:, :, 0:1], [RAW[(j + 1) % 3]], [CBUF])
        cw3 = CW[:, :].rearrange("p (c k) -> p c k", k=3)
        wk = lambda k: cw3[:, :, k:k + 1].broadcast_to([128, 6, 128])
        p.tt("dve", cacc6[:, :, :], CBUF[:, :, 0:128], wk(0), ALU.mult, [CBUF, CW], [cacc6])
        p.tt("pool", tmp6[:, :, :], CBUF[:, :, 1:129], wk(1), ALU.mult, [CBUF, CW], [tmp6])
        p.tt("dve", cacc6[:, :, :], cacc6[:, :, :], tmp6[:, :, :], ALU.add, [cacc6, tmp6], [cacc6])
        p.tt("pool", tmp6[:, :, :], CBUF[:, :, 2:130], wk(2), ALU.mult, [CBUF, CW], [tmp6])
        p.tt("dve", cacc6[:, :, :], cacc6[:, :, :], tmp6[:, :, :], ALU.add, [cacc6, tmp6], [cacc6])
        p.tt("dve", cacc6[:, :, :], cacc6[:, :, :], CBs[:, :].rearrange("p (c o) -> p c o", o=1).broadcast_to([128, 6, 128]),
             ALU.add, [cacc6, CBs], [cacc6])
        p.act(XC[:, :, :], cacc6[:, :, :], AF.Silu, [cacc6], [XC])
        if SSUB < 2:
            return
        for c in range(5):
            p.tr(ptr[:, c, :], XC[:, c, :], ident[:, :], [XC, ident], [ptr])
        p.cp("act", XTOK[:, :], ptr[:, 0:4, :].rearrange("p c t -> p (c t)"), [ptr], [XTOK])
        p.cp("act", BTOK[:, :], ptr[:, 4, :], [ptr], [BTOK])
        p.tt("dve", a_, dts[:, :], A_t[:, :], ALU.mult, [dts, A_t], [sm])
        p.mm(B1[:, 0:8], triu[:, :], a_, True, True, [triu, sm], [B1])
        p.mm(B1[:, 128:136], ones[:, :], a_, True, True, [ones, sm], [B1])
        p.cp("dve", acs, B1[:, 0:8], [B1], [sm])
        p.cp("dve", tot, B1[:, 128:136], [B1], [sm])
        if SSUB < 3:
            return
        p.mm(B2[:, 0:128], XC[:, 4, :], XC[:, 5, :], True, True, [XC], [B2])
        p.tt("dve", CBT[:, :], B2[:, 0:128], triu[:, :], ALU.mult, [B2, triu], [CBT])
        p.cp("act", Hb[:, :], H[:, :], [H], [Hb])
        v4 = lambda ap: ap.rearrange("p (h t) -> p h t", h=4)
        b4 = lambda ap: ap.rearrange("p (h o) -> p h o", o=1).broadcast_to([128, 4, 128])
        o4 = lambda ap: ap.rearrange("p (o t) -> p o t", o=1).broadcast_to([128, 4, 128])
        for g4 in range(2):
            hs = slice(g4 * 4, g4 * 4 + 4)
            rt_, rv_, lt_, lv_, wt4, Pb = RMt[g4], RMv[g4], LMt[g4], LMv[g4], WT4[g4], (B3, B2)[g4]
            p.tt("dve", v4(rv_), o4(triu[:, :]), b4(a_[:, hs]), ALU.mult, [triu, sm], [rt_])
            p.mm(Pb[:, :], ones[:, :], rv_, True, True, [ones, rt_], [Pb])
            p.tt("dve", v4(lv_), v4(Pb[:, :]), b4(acs[:, hs]), ALU.subtract, [Pb, sm], [lt_])
            p.ts("dve", lv_, lv_, 0.0, None, ALU.min, None, [lt_], [lt_])
            p.act(lv_, lv_, AF.Exp, [lt_], [lt_])
            p.tt("dve", v4(lv_), v4(lv_), b4(dts[:, hs]), ALU.mult, [lt_, dts], [lt_])
            p.tt("dve", v4(wt4[:, :]), v4(lv_), o4(CBT[:, :]), ALU.mult, [lt_, CBT], [wt4])
            for hh in range(4):
                hd = g4 * 4 + hh
                p.mm(B4[:, hd * 64:(hd + 1) * 64], wt4[:, hh * 128:(hh + 1) * 128], XTOK[:, hd * 64:(hd + 1) * 64], True, True,
                     [wt4, XTOK], [B4])
        if SSUB < 4:
            return
        p.mm(B5[:, :], XC[:, 5, :], Hb[:, :], True, True, [XC, Hb], [B5])
        p.act(eacs, acs, AF.Exp, [sm], [sm])
        v3 = lambda ap: ap.rearrange("p (h d) -> p h d", h=8)
        bc = lambda ap: ap.rearrange("p (h o) -> p h o", o=1).broadcast_to([128, 8, 64])
        p.tt("dve", v3(ytmp[:, :]), v3(B5[:, :]), bc(eacs), ALU.mult, [B5, sm], [ytmp])
        p.tt("dve", ysb[:, :], ytmp[:, :], B4[:, :], ALU.add, [ytmp, B4], [ysb])
        p.tt("dve", v3(ytmp[:, :]), v3(XTOK[:, :]), bc(dsk_t[:, :]), ALU.mult, [XTOK, dsk_t], [ytmp])
        p.tt("dve", ysb[:, :], ysb[:, :], ytmp[:, :], ALU.add, [ysb, ytmp], [ysb])
        p.dma("pool", yssd[j * 128:(j + 1) * 128, :], ysb[:, :], [ysb], ())
        if SSUB < 5:
            return
        p.tt("dve", wend, tot, acs, ALU.subtract, [sm], [sm])
        p.act(wend, wend, AF.Exp, [sm], [sm])
        p.tt("dve", wend, wend, dts[:, :], ALU.mult, [sm, dts], [sm])
        p.act(dec, tot, AF.Exp, [sm], [sm])
        p.tt("dve", v3(XW[:, :]), v3(XTOK[:, :]), bc(wend), ALU.mult, [XTOK, sm], [XW])
        p.mm(B6[:, :], BTOK[:, :], XW[:, :], True, True, [BTOK, XW], [B6])
        p.tt("dve", v3(H[:, :]), v3(H[:, :]), bc(dec), ALU.mult, [H, sm], [H])
        p.tt("dve", H[:, :], H[:, :], B6[:, :], ALU.add, [H, B6], [H])

    done_a = set()

    def doA(i):
        if i < nt and i not in done_a:
            done_a.add(i)
            project(i)

    for i in range(nt + 1):
        doA(i)
        if i >= 2:
            doA(i + 1)
        if i < nt:
            projectB(i)
        if i >= 1:
            ssd_chunk(i - 1)

    if do_s5:
        LAM = p.sb([128, 24], F32, "LAM")
        dsc = p.sb([128, 16], F32, "dsc")
        iota = p.sb([128, 129], F32, "iota")
        ang = p.sb([128, 129], F32, "ang")
        mag = p.sb([128, 129], F32, "mag")
        cs_ = p.sb([128, 129], F32, "cs_")
        sn_ = p.sb([128, 129], F32, "sn_")
        EP = p.sb([128, 2, 512], F32, "EP")
        EN = p.sb([128, 2, 512], F32, "EN")
        m01 = p.sb([128, 512], F32, "m01")
        blr = p.sb([128, 128], BF16, "blr")
        bli = p.sb([128, 128], BF16, "bli")
        clr = p.sb([128, 32], BF16, "clr")
        cli = p.sb([128, 32], BF16, "cli")
        dl = p.sb([128, 32], BF16, "dl")
        q1 = p.sb([128, 512], F32, "q1")
        q2 = p.sb([128, 512], F32, "q2")
        WR = [p.sb([128, 512], BF16, f"wr_{i}") for i in range(2)]
        WI = [p.sb([128, 512], BF16, f"wi_{i}") for i in range(2)]
        XA = p.sb([128, 2, 66], F32, "XA")
        XB = p.sb([128, 2, 66], F32, "XB")
        tq66 = ang
        pw = p.sb([128, 2, 8], F32, "pw")
        yo = p.sb([32, 512], F32, "yo")
        twopi = p.sb([128, 129], F32, "twopi")
        kint = p.sb([128, 129], mybir.dt.int32, "kint")
        p.dma("sp", LAM[:, :], lam[:, :], (), [LAM])
        p.dma("sp", iota[:, :], iotad[:, :], (), [iota])
        p.dma("sp", m01[:, :], m01d[:, :], (), [m01])
        nblk = (nt * 128 + 511) // 512
        for pr in range(8):
            lr, li, ls = LAM[:, pr:pr + 1], LAM[:, 8 + pr:9 + pr], LAM[:, 16 + pr:17 + pr]
            sc = lambda k: dsc[:, k:k + 1]
            R, W_ = [LAM, dsc, iota, ang, mag, cs_, sn_], [dsc]
            step, lrs, th, den, cr, ci, t0_, t1_ = (sc(k) for k in range(8))
            p.act(step, ls, AF.Exp, [LAM], [dsc])
            p.tt("dve", lrs, lr, step, ALU.mult, [LAM, dsc], [dsc])
            p.tt("dve", th, li, step, ALU.mult, [LAM, dsc], [dsc])
            p.ts("dve", ang[:, :], iota[:, :], th, None, ALU.mult, None, [iota, dsc], [ang])
            p.ts("dve", cs_[:, :], ang[:, :], 1.5 * np.pi, None, ALU.add, None, [ang], [cs_])
            p.ts("dve", twopi[:, :], cs_[:, :], 1.0 / TWO_PI, None, ALU.mult, None, [cs_], [twopi])
            p.cp("dve", kint[:, :], twopi[:, :], [twopi], [kint])
            p.cp("dve", twopi[:, :], kint[:, :], [kint], [twopi])
            p.stt("dve", cs_[:, :], twopi[:, :], -TWO_PI, cs_[:, :], ALU.mult, ALU.add, [twopi, cs_], [cs_])
            p.ts("dve", twopi[:, :], cs_[:, :], 0.0, None, ALU.is_lt, None, [cs_], [twopi])
            p.stt("dve", cs_[:, :], twopi[:, :], TWO_PI, cs_[:, :], ALU.mult, ALU.add, [twopi, cs_], [cs_])
            p.ts("dve", sn_[:, :], ang[:, :], np.pi, None, ALU.add, None, [ang], [sn_])
            p.ts("dve", twopi[:, :], sn_[:, :], 1.0 / TWO_PI, None, ALU.mult, None, [sn_], [twopi])
            p.cp("dve", kint[:, :], twopi[:, :], [twopi], [kint])
            p.cp("dve", twopi[:, :], kint[:, :], [kint], [twopi])
            p.stt("dve", sn_[:, :], twopi[:, :], -TWO_PI, sn_[:, :], ALU.mult, ALU.add, [twopi, sn_], [sn_])
            p.ts("dve", twopi[:, :], sn_[:, :], 0.0, None, ALU.is_lt, None, [sn_], [twopi])
            p.stt("dve", sn_[:, :], twopi[:, :], TWO_PI, sn_[:, :], ALU.mult, ALU.add, [twopi, sn_], [sn_])
            p.act(cs_[:, :], cs_[:, :], AF.Sin, [cs_], [cs_], bias=-np.pi)
            p.act(sn_[:, :], sn_[:, :], AF.Sin, [sn_], [sn_], bias=-np.pi)
            p.act(mag[:, :], iota[:, :], AF.Exp, [iota, dsc], [mag], scale=lrs)
            ar, ai, a128r, a128i = (sc(k) for k in range(8, 12))
            p.tt("dve", ar, mag[:, 1:2], cs_[:, 1:2], ALU.mult, [mag, cs_], [dsc])
            p.tt("dve", ai, mag[:, 1:2], sn_[:, 1:2], ALU.mult, [mag, sn_], [dsc])
            p.tt("dve", a128r, mag[:, 128:129], cs_[:, 128:129], ALU.mult, [mag, cs_], [dsc])
            p.tt("dve", a128i, mag[:, 128:129], sn_[:, 128:129], ALU.mult, [mag, sn_], [dsc])
            p.tt("dve", den, lr, lr, ALU.mult, [LAM], [dsc])
            p.stt("dve", den, li, li, den, ALU.mult, ALU.add, [LAM, dsc], [dsc])
            p.op("dve", lambda g, den=den: g.reciprocal(den, den), [dsc], [dsc])
            p.ts("dve", t0_, ar, -1.0, None, ALU.add, None, [dsc], [dsc])
            p.tt("dve", t1_, ai, li, ALU.mult, [dsc, LAM], [dsc])
            p.stt("dve", cr, t0_, lr, t1_, ALU.mult, ALU.add, [dsc, LAM], [dsc])
            p.tt("dve", cr, cr, den, ALU.mult, [dsc], [dsc])
            p.tt("dve", t1_, t0_, li, ALU.mult, [dsc, LAM], [dsc])
            p.stt("dve", ci, ai, lr, t1_, ALU.mult, ALU.subtract, [dsc, LAM], [dsc])
            p.tt("dve", ci, ci, den, ALU.mult, [dsc], [dsc])
            p.tt("dve", EP[:, 0, 0:128], mag[:, 0:128], cs_[:, 0:128], ALU.mult, [mag, cs_], [EP])
            p.tt("dve", EP[:, 1, 0:128], mag[:, 0:128], sn_[:, 0:128], ALU.mult, [mag, sn_], [EP])
            p.op("dve", lambda g: g.reciprocal(mag[:, :], mag[:, :]), [mag], [mag])
            p.tt("dve", cs_[:, :], cs_[:, :], mag[:, :], ALU.mult, [cs_, mag], [cs_])
            p.tt("dve", sn_[:, :], sn_[:, :], mag[:, :], ALU.mult, [sn_, mag], [sn_])
            p.ts("dve", ang[:, :], sn_[:, :], ci, None, ALU.mult, None, [sn_, dsc], [ang])
            p.stt("dve", EN[:, 0, 0:128], cs_[:, 0:128], cr, ang[:, 0:128], ALU.mult, ALU.add, [cs_, dsc, ang], [EN])
            p.ts("dve", ang[:, :], sn_[:, :], cr, None, ALU.mult, None, [sn_, dsc], [ang])
            p.stt("dve", EN[:, 1, 0:128], cs_[:, 0:128], ci, ang[:, 0:128], ALU.mult, ALU.subtract, [cs_, dsc, ang], [EN])
            for rep in range(1, 4):
                p.cp("pool", EP[:, :, rep * 128:(rep + 1) * 128], EP[:, :, 0:128], [EP], [EP])
                p.cp("pool", EN[:, :, rep * 128:(rep + 1) * 128], EN[:, :, 0:128], [EN], [EN])
            p.dma("pool", blr[:, :], BLr[pr], (), [blr])
            p.dma("pool", bli[:, :], BLi[pr], (), [bli])
            p.dma("pool", clr[:, :], CLr[pr], (), [clr])
            p.dma("pool", cli[:, :], CLi[pr], (), [cli])
            p.ts("dve", cli[:, :], cli[:, :], -1.0, None, ALU.mult, None, [cli], [cli])
            p.dma("pool", dl[:, :], DL[pr], (), [dl])
            uc = pr // 4
            for b in range(nblk):
                t0 = b * 512
                n = min(512, nt * 128 - t0)
                Pr, Pi = ((B0, B1), (B3, B4))[b % 2]
                p.mm(Pr[:, 0:n], blr[:, :], UT[:, uc, t0:t0 + n], True, True, [blr, UT], [Pr])
                p.mm(Pi[:, 0:n], bli[:, :], UT[:, uc, t0:t0 + n], True, True, [bli, UT], [Pi])
                p.tt("dve", q1[:, 0:n], Pr[:, 0:n], EN[:, 0, 0:n], ALU.mult, [Pr, EN], [q1])
                p.tt("dve", q2[:, 0:n], Pi[:, 0:n], EN[:, 1, 0:n], ALU.mult, [Pi, EN], [q2])
                p.tt("dve", q1[:, 0:n], q1[:, 0:n], q2[:, 0:n], ALU.subtract, [q1, q2], [q1])
                p.op("dve", lambda g, t0=t0, n=n: g.tensor_tensor_scan(Z[:, 0, t0:t0 + n], m01[:, 0:n], q1[:, 0:n], 0.0,
                                                                        ALU.mult, ALU.add), [m01, q1], [Z])
                p.tt("dve", q2[:, 0:n], Pi[:, 0:n], EN[:, 0, 0:n], ALU.mult, [Pi, EN], [q2])
                p.tt("dve", q1[:, 0:n], Pr[:, 0:n], EN[:, 1, 0:n], ALU.mult, [Pr, EN], [q1])
                p.tt("dve", q1[:, 0:n], q1[:, 0:n], q2[:, 0:n], ALU.add, [q1, q2], [q1])
                p.op("dve", lambda g, t0=t0, n=n: g.tensor_tensor_scan(Z[:, 1, t0:t0 + n], m01[:, 0:n], q1[:, 0:n], 0.0,
                                                                        ALU.mult, ALU.add), [m01, q1], [Z])
            zend = lambda ri: Z[:, ri, 0:nt * 128].rearrange("p (c t) -> p c t", t=128)[:, :, 127]
            cur, nxt = XA, XB
            CH = [Z, dsc, XA, XB, tq66, pw]
            p.ts("dve", tq66[:, 0:nt], zend(1), a128i, None, ALU.mult, None, CH, [tq66])
            p.stt("dve", cur[:, 0, 0:nt], zend(0), a128r, tq66[:, 0:nt], ALU.mult, ALU.subtract, CH, [cur])
            p.ts("dve", tq66[:, 0:nt], zend(0), a128i, None, ALU.mult, None, CH, [tq66])
            p.stt("dve", cur[:, 1, 0:nt], zend(1), a128r, tq66[:, 0:nt], ALU.mult, ALU.add, CH, [cur])
            p.cp("dve", pw[:, 0, 0:1], a128r, [dsc], [pw])
            p.cp("dve", pw[:, 1, 0:1], a128i, [dsc], [pw])
            k = 0
            while (1 << k) < nt:
                sft = 1 << k
                n = nt - sft
                Ar, Ai = pw[:, 0, k:k + 1], pw[:, 1, k:k + 1]
                p.cp("dve", nxt[:, :, 0:sft], cur[:, :, 0:sft], [cur], [nxt])
                p.ts("dve", tq66[:, 0:n], cur[:, 1, 0:n], Ai, None, ALU.mult, None, [cur, pw], [tq66])
                p.stt("dve", nxt[:, 0, sft:nt], cur[:, 0, 0:n], Ar, tq66[:, 0:n], ALU.mult, ALU.subtract, [cur, pw, tq66], [nxt])
                p.tt("dve", nxt[:, 0, sft:nt], nxt[:, 0, sft:nt], cur[:, 0, sft:nt], ALU.add, [cur, nxt], [nxt])
                p.ts("dve", tq66[:, 0:n], cur[:, 0, 0:n], Ai, None, ALU.mult, None, [cur, pw], [tq66])
                p.stt("dve", nxt[:, 1, sft:nt], cur[:, 1, 0:n], Ar, tq66[:, 0:n], ALU.mult, ALU.add, [cur, pw, tq66], [nxt])
                p.tt("dve", nxt[:, 1, sft:nt], nxt[:, 1, sft:nt], cur[:, 1, sft:nt], ALU.add, [cur, nxt], [nxt])
                p.tt("dve", pw[:, 0, k + 1:k + 2], Ai, Ai, ALU.mult, [pw], [pw])
                p.stt("dve", pw[:, 0, k + 1:k + 2], Ar, Ar, pw[:, 0, k + 1:k + 2], ALU.mult, ALU.subtract, [pw], [pw])
                p.stt("dve", pw[:, 1, k + 1:k + 2], Ar, 2.0, Ai, ALU.mult, ALU.mult, [pw], [pw])
                cur, nxt = nxt, cur
                k += 1
            if nt > 1:
                for ri, eng in ((0, "dve"), (1, "pool")):
                    zv = Z[:, ri, 128:nt * 128].rearrange("p (c t) -> p c t", t=128)
                    gb = cur[:, ri, 0:nt - 1].rearrange("p (c o) -> p c o", o=1).broadcast_to([128, nt - 1, 128])
                    p.tt(eng, zv, zv, gb, ALU.add, [Z, cur], [Z])
            for b in range(nblk):
                t0 = b * 512
                n = min(512, nt * 128 - t0)
                wr_, wi_ = WR[b % 2], WI[b % 2]
                p.tt("dve", q1[:, 0:n], Z[:, 0, t0:t0 + n], EP[:, 0, 0:n], ALU.mult, [Z, EP], [q1])
                p.tt("dve", q2[:, 0:n], Z[:, 1, t0:t0 + n], EP[:, 1, 0:n], ALU.mult, [Z, EP], [q2])
                p.tt("dve", wr_[:, 0:n], q1[:, 0:n], q2[:, 0:n], ALU.subtract, [q1, q2], [wr_])
                p.tt("dve", q2[:, 0:n], Z[:, 1, t0:t0 + n], EP[:, 0, 0:n], ALU.mult, [Z, EP], [q2])
                p.tt("dve", q1[:, 0:n], Z[:, 0, t0:t0 + n], EP[:, 1, 0:n], ALU.mult, [Z, EP], [q1])
                p.tt("dve", wi_[:, 0:n], q1[:, 0:n], q2[:, 0:n], ALU.add, [q1, q2], [wi_])
                Py = (B2, B5)[b % 2]
                p.mm(Py[0:32, 0:n], clr[:, :], wr_[:, 0:n], True, False, [clr, wr_], [Py])
                p.mm(Py[0:32, 0:n], cli[:, :], wi_[:, 0:n], False, False, [cli, wi_], [Py])
                p.mm(Py[0:32, 0:n], dl[:, :], UT[:, uc, t0:t0 + n], False, True, [dl, UT], [Py])
                p.cp("act", yo[:, 0:n], Py[0:32, 0:n], [Py], [yo])
                p.dma("sp", ys5[pr * 32:(pr + 1) * 32, t0:t0 + n], yo[:, 0:n], [yo], ())
    p.emit()
    return nc


def ssm_inputs(inp, xseq, b, dr, hf, nt=TS):
    w = inp["ssm_w_in"][0]
    cols = np.concatenate([np.arange(1024 + 512 * hf, 1024 + 512 * hf + 512), np.arange(2048 + 128 * hf, 2048 + 128 * hf + 128),
                           np.arange(2304 + 128 * hf, 2304 + 128 * hf + 128), np.arange(2592 + 256 * hf, 2592 + 256 * hf + 256),
                           np.arange(2560 + 16 * dr + 8 * hf, 2560 + 16 * dr + 8 * hf + 8)])
    cch = np.concatenate([np.arange(512 * hf, 512 * hf + 512), np.arange(1024 + 128 * hf, 1024 + 128 * hf + 128),
                          np.arange(1280 + 128 * hf, 1280 + 128 * hf + 128)])
    cwf = inp["ssd_conv_w"][0][:, cch]
    if dr == 1:
        cwf = cwf[::-1]
    cw = np.ascontiguousarray(cwf.T.reshape(6, 128, 3).transpose(1, 0, 2).reshape(128, 18))
    cb = np.ascontiguousarray(inp["ssd_conv_b"][0][cch].reshape(6, 128).T)
    hs = slice(8 * hf, 8 * hf + 8)
    zero8 = np.zeros((1, 8), np.float32)
    gs = 16 * hf
    lam = np.zeros((128, 24), np.float32)
    BLr = np.zeros((8, 128, 128), np.float32)
    BLi = np.zeros((8, 128, 128), np.float32)
    CLr = np.zeros((8, 128, 32), np.float32)
    CLi = np.zeros((8, 128, 32), np.float32)
    DLm = np.zeros((8, 128, 32), np.float32)
    sd = inp["s5_d"][0]
    for pr in range(8):
        for k in range(2):
            g = gs + 2 * pr + k
            rows = slice(64 * k, 64 * k + 64)
            lam[rows, pr] = inp["s5_lambda_re"][0, dr, g]
            lam[rows, 8 + pr] = inp["s5_lambda_im"][0, dr, g]
            lam[rows, 16 + pr] = inp["s5_log_step"][0, dr, g]
            ur = 32 * (pr % 4) + 16 * k
            BLr[pr, ur:ur + 16, rows] = inp["s5_b_re"][0, dr, g].T
            BLi[pr, ur:ur + 16, rows] = inp["s5_b_im"][0, dr, g].T
            CLr[pr, rows, 16 * k:16 * k + 16] = inp["s5_c_re"][0, dr, g].T
            CLi[pr, rows, 16 * k:16 * k + 16] = inp["s5_c_im"][0, dr, g].T
            if dr == 0:
                for c in range(16):
                    DLm[pr, ur + c, 16 * k + c] = sd[g * 16 + c]
    m01 = np.ones((128, 512), np.float32)
    m01[:, ::128] = 0.0
    cin = np.stack([inp["c"][b].reshape(8, 128).T, inp["c_ctx"].reshape(8, 128).T], axis=1)
    return {
        "xs": np.ascontiguousarray(xseq, dtype=np.float32), "cin": np.ascontiguousarray(cin, dtype=np.float32),
        "modw": np.ascontiguousarray(inp["mod_w"][1][:, 0:2 * D]), "modb": np.ascontiguousarray(inp["mod_b"][1][None, 0:2 * D]),
        "nrm": np.ascontiguousarray(inp["norm_mix"][1][None, :]),
        "wsel": np.ascontiguousarray(np.concatenate([w[:, cols], np.zeros((D, WS - 1032), np.float32)], axis=1)), "cw": cw, "cbias": cb,
        "dtb": np.ascontiguousarray(inp["ssd_dt_bias"][0, dr, hs][None, :]),
        "alog": np.ascontiguousarray(inp["ssd_a_log"][0, dr, hs][None, :]),
        "dsk": np.ascontiguousarray(inp["ssd_d"][0, hs][None, :]) if dr == 0 else zero8,
        "identd": np.eye(128, dtype=np.float32), "triud": np.triu(np.ones((128, 128), np.float32)),
        "iotad": np.tile(np.arange(129, dtype=np.float32)[None, :], (128, 1)), "m01d": m01,
        "lam": lam, "BLr": BLr, "BLi": BLi, "CLr": CLr, "CLi": CLi, "DL": DLm,
    }


NTT = 16
GELU_C = 1.5957691216057308


def build_fin():
    nc = bass.Bass("TRN2", target_bir_lowering=False)
    dt_in = lambda n, s: nc.dram_tensor(n, s, F32, kind="ExternalInput").ap()
    x = dt_in("x", [NTT * 128, D])
    cin = dt_in("cin", [128, 2, 8])
    modw = dt_in("modw", [D, 3 * D])
    modb = dt_in("modb", [1, 3 * D])
    nrm = dt_in("nrm", [1, D])
    wz = dt_in("wz", [D, D])
    yf = dt_in("yf", [NTT * 128, D])
    yb = dt_in("yb", [NTT * 128, D])
    vf = dt_in("vf", [NTT * 128, 512])
    vb = dt_in("vb", [NTT * 128, 512])
    snorm = dt_in("snorm", [1, D])
    gluw = dt_in("gluw", [512, 512])
    glub = dt_in("glub", [1, 512])
    wout = dt_in("wout", [1536, D])
    identd = dt_in("identd", [128, 128])
    xo = nc.dram_tensor("xo", [NTT * 128, D], F32, kind="ExternalOutput").ap()

    p = Prog(nc)
    wzb = p.sb([128, 8, D], BF16, "wzb")
    woutb = p.sb([128, 12, D], BF16, "woutb")
    gwb = p.sb([128, 4, 512], BF16, "gwb")
    MB = [p.sb([128, D], F32, f"mb{v}") for v in range(3)]
    xt = p.sb([128, D], F32, "xt")
    hb = p.sb([128, D], BF16, "hb")
    hTi = p.sb([128, 8, 128], BF16, "hTi")
    stage = p.sb([128, 8, 128], F32, "stage")
    bstage = p.sb([128, 128], F32, "bstage")
    cbc = p.sb([128, 8, 128], F32, "cbc")
    ones = p.sb([128, 128], F32, "ones")
    identf = p.sb([128, 128], F32, "identf")
    ident = p.sb([128, 128], BF16, "ident")
    cin_t = p.sb([128, 2, 8], F32, "cin_t")
    small = {"ss": p.sb([128, 4], F32, "ss"), "rs": p.sb([128, 4], F32, "rs"), "junk": p.sb([128, D], F32, "junk")}
    sn_bc = p.sb([128, D], F32, "sn_bc")
    gb_bc = p.sb([128, 512], F32, "gb_bc")
    zs = p.sb([128, D], F32, "zs")
    zs_b = p.sb([128, 512], F32, "zs_b")
    ya = p.sb([128, D], F32, "ya")
    ybt = p.sb([128, D], F32, "ybt")
    va = p.sb([128, 512], F32, "va")
    vbt = p.sb([128, 512], F32, "vbt")
    v2 = p.sb([128, 512], F32, "v2")
    gvb = p.sb([128, 512], BF16, "gvb")
    cat = p.sb([128, 1536], BF16, "cat")
    gT = p.sb([128, 4, 128], BF16, "gT")
    cT = p.sb([128, 12, 128], BF16, "cT")
    st2 = p.sb([128, 4], F32, "st2")
    Z0, Z1, G, O0, O1, B0 = [p.ps([128, 512], F32, f"pb{i}") for i in range(6)]
    ptr = p.ps([128, 8, 128], BF16, "ptr")

    p.dma("sp", identf[:, :], identd[:, :], (), [identf])
    p.cp("dve", ident[:, :], identf[:, :], [identf], [ident])
    p.memset("pool", ones[:, :], 1.0, [ones])
    p.dma("pool", wzb[:, :, :], wz.rearrange("(kc p) n -> p kc n", p=128), (), [wzb])
    p.dma("pool", gwb[:, :, :], gluw.rearrange("(kc p) n -> p kc n", p=128), (), [gwb])
    p.dma("pool", woutb[:, :, :], wout.rearrange("(kc p) n -> p kc n", p=128), (), [woutb])
    p.dma("sp", cin_t[:, :, :], cin[:, :, :], (), [cin_t])
    p.act(cin_t[:, :, :], cin_t[:, :, :], AF.Silu, [cin_t], [cin_t])
    p.dma("sp", xt[:, :], bcast_rows(nrm[0:1, :]), (), [xt])
    p.dma("sp", sn_bc[:, :], bcast_rows(snorm[0:1, :]), (), [sn_bc])
    p.dma("sp", gb_bc[:, :], bcast_rows(glub[0:1, :]), (), [gb_bc])

    def outsel(j):
        v, c = divmod(j, 2)
        t = MB[v]
        return t, t[:, c * 512:(c + 1) * 512]
    stgb = [p.sb([128, 8, 512], F32, f"stgb{k}") for k in range(2)]
    emit_mod_rows(p, cin_t, modw, modb, 3 * D, [outsel, outsel], [B0, G], [(t_, t_[:, :, :]) for t_ in stgb], cbc, ones, zs_b,
                  whichs=(0,))
    A = MB[1]
    p.stt("dve", A[:, :], A[:, :], 1.0, xt[:, :], ALU.add, ALU.mult, [A, xt], [A])

    for i in range(NTT):
        rows = slice(i * 128, (i + 1) * 128)
        p.dma("sp", xt[:, :], x[rows, :], (), [xt])
        emit_adanorm_T(p, xt, MB[1], MB[0], hb, ptr, hTi[:, :, :], hTi, ident, small)
        for half, Zp in enumerate((Z0, Z1)):
            for kc in range(8):
                p.mm(Zp[:, :], hTi[:, kc, :], wzb[:, kc, half * 512:(half + 1) * 512], kc == 0, kc == 7, [hTi, wzb], [Zp])
            p.act(zs[:, half * 512:(half + 1) * 512], Zp[:, :], AF.Silu, [Zp], [zs])
        p.dma("sp", ya[:, :], yf[rows, :], (), [ya])
        p.dma("sp", ybt[:, :], yb[rows, :], (), [ybt])
        p.tt("pool", ya[:, :], ya[:, :], ybt[:, :], ALU.add, [ya, ybt], [ya])
        p.tt("dve", ya[:, :], ya[:, :], zs[:, :], ALU.mult, [ya, zs], [ya])
        j = small["junk"]
        p.memset("dve", st2[:, 0:1], 0.0, [st2])
        p.act(j[:, :], ya[:, :], AF.Square, [ya, st2], [j, st2], accum_out=st2[:, 0:1])
        p.ts("dve", st2[:, 1:2], st2[:, 0:1], 1.0 / D, EPS, ALU.mult, ALU.add, [st2], [st2])
        p.op("act", lambda g: g.sqrt(st2[:, 2:3], st2[:, 1:2]), [st2], [st2])
        p.op("dve", lambda g: g.reciprocal(st2[:, 3:4], st2[:, 2:3]), [st2], [st2])
        p.stt("dve", cat[:, 0:1024], ya[:, :], st2[:, 3:4], sn_bc[:, :], ALU.mult, ALU.mult, [ya, st2, sn_bc], [cat])
        p.dma("sp", va[:, :], vf[rows, :], (), [va])
        p.dma("sp", vbt[:, :], vb[rows, :], (), [vbt])
        p.tt("pool", va[:, :], va[:, :], vbt[:, :], ALU.add, [va, vbt], [va])
        p.tt("dve", v2[:, :], va[:, :], va[:, :], ALU.mult, [va], [v2])
        p.ts("dve", v2[:, :], v2[:, :], 0.044715, 1.0, ALU.mult, ALU.add, [v2], [v2])
        p.tt("dve", v2[:, :], v2[:, :], va[:, :], ALU.mult, [v2, va], [v2])
        p.act(v2[:, :], v2[:, :], AF.Sigmoid, [v2], [v2], scale=GELU_C)
        p.tt("dve", va[:, :], va[:, :], v2[:, :], ALU.mult, [va, v2], [va])
        p.cp("dve", gvb[:, :], va[:, :], [va], [gvb])
        for c in range(4):
            p.tr(ptr[:, c, :], gvb[:, c * 128:(c + 1) * 128], ident[:, :], [gvb, ident], [ptr])
        p.cp("act", gT[:, :, :], ptr[:, 0:4, :], [ptr], [gT])
        for c in range(4):
            p.mm(G[:, :], gT[:, c, :], gwb[:, c, :], c == 0, c == 3, [gT, gwb], [G])
        p.tt("dve", v2[:, :], G[:, :], gb_bc[:, :], ALU.add, [G, gb_bc], [v2])
        p.act(v2[:, :], v2[:, :], AF.Sigmoid, [v2], [v2])
        p.tt("dve", cat[:, 1024:1536], va[:, :], v2[:, :], ALU.mult, [va, v2], [cat])
        for c in range(8):
            p.tr(ptr[:, c, :], cat[:, c * 128:(c + 1) * 128], ident[:, :], [cat, ident], [ptr])
        p.cp("act", cT[:, 0:8, :], ptr[:, :, :], [ptr], [cT])
        for c in range(4):
            p.tr(ptr[:, c, :], cat[:, 1024 + c * 128:1024 + (c + 1) * 128], ident[:, :], [cat, ident], [ptr])
        p.cp("act", cT[:, 8:12, :], ptr[:, 0:4, :], [ptr], [cT])
        for half, Op in enumerate((O0, O1)):
            for c in range(12):
                p.mm(Op[:, :], cT[:, c, :], woutb[:, c, half * 512:(half + 1) * 512], c == 0, c == 11, [cT, woutb], [Op])
        for half, Op in enumerate((O0, O1)):
            sl = slice(half * 512, (half + 1) * 512)
            p.tt("dve", j[:, sl], Op[:, :], MB[2][:, sl], ALU.mult, [Op, MB[2]], [j])
        p.tt("dve", j[:, :], j[:, :], xt[:, :], ALU.add, [j, xt], [j])
        p.dma("sp", xo[rows, :], j[:, :], [j], ())
    p.emit()
    return nc


def fin_inputs(inp, b, xcore, yfc, ybc, vfc, vbc):
    cin = np.stack([inp["c"][b].reshape(8, 128).T, inp["c_ctx"].reshape(8, 128).T], axis=1)
    ca = lambda a: np.ascontiguousarray(a, dtype=np.float32)
    return {
        "x": ca(xcore), "cin": ca(cin), "modw": ca(inp["mod_w"][1][:, 0:3 * D]), "modb": ca(inp["mod_b"][1][None, 0:3 * D]),
        "nrm": ca(inp["norm_mix"][1][None, :]), "wz": ca(inp["ssm_w_in"][0][:, 0:1024]),
        "yf": ca(yfc), "yb": ca(ybc), "vf": ca(vfc), "vb": ca(vbc),
        "snorm": ca(inp["ssd_norm"][0][None, :]), "gluw": ca(inp["s5_glu_w"][0]), "glub": ca(inp["s5_glu_b"][0][None, :]),
        "wout": ca(inp["ssm_w_out"][0]), "identd": np.eye(128, dtype=np.float32),
    }


_CACHE = {}


def _prog(name, fn):
    if name not in _CACHE:
        _CACHE[name] = fn()
    return _CACHE[name]


def attn_inputs(inp, b, q, NSLAB=2):
    parts = [attn_inputs1(inp, b, NSLAB * q + k) for k in range(NSLAB)]
    out = dict(parts[0])
    out["xe"] = np.concatenate([pp["xe"] for pp in parts], axis=0)
    for k in ("ropec", "ropes", "wam"):
        out[k] = np.ascontiguousarray(np.concatenate([pp[k] for pp in parts], axis=1))
    out["nab"] = np.concatenate([pp["nab"] for pp in parts], axis=0)
    return out


def kernel(**inputs):
    inp = {k: np.asarray(v) for k, v in inputs.items()}
    C8 = list(range(8))
    xl = np.empty((2, 8192, D), np.float32)
    xc = np.empty((2, 256, D), np.float32)
    vcs = [(b, q) for b in range(2) for q in range(4)]
    res = run_bass_kernel_spmd(build_attn(), [attn_inputs(inp, b, q) for b, q in vcs], core_ids=C8)
    for (b, q), r in zip(vcs, res.results):
        for k in range(2):
            s_ = 2 * q + k
            xl[b, s_ * 1024:(s_ + 1) * 1024] = r["xo"][k * 1280:k * 1280 + 1024]
        if q == 0:
            xc[b] = r["xo"][1024:1280]

    def moe(L, xl_in, xc_in):
        cores = [(b, q) for b in range(2) for q in range(4)]
        ims = [moe_inputs(np.concatenate([xl_in[b, q * 2048:(q + 1) * 2048], xc_in[b]], axis=0) if xc_in is not None
                          else xl_in[b, q * 2048:(q + 1) * 2048], inp["c"][b], inp["c_ctx"],
                          inp["mod_w"][L], inp["mod_b"][L], inp["norm_ffn"][L], inp["moe_w_group"][L], inp["moe_b_group"][L],
                          inp["moe_w_expert"][L], inp["moe_b_expert"][L], inp["moe_w13"][L], inp["moe_w2"][L])
               for b, q in cores]
        res = run_bass_kernel_spmd(build_moe(NT=NT_MOE if xc_in is not None else NLAT_MOE), ims, core_ids=C8)
        xo = np.empty_like(xl_in)
        xco = np.empty_like(xc_in) if xc_in is not None else None
        for (b, q), r in zip(cores, res.results):
            xo[b, q * 2048:(q + 1) * 2048] = r["xo"][:2048]
            if q == 0 and xc_in is not None:
                xco[b] = r["xo"][2048:]
        return xo, xco

    xl, xc = moe(0, xl, xc)
    cores = [(b, dr, hf) for b in range(2) for dr in range(2) for hf in range(2)]
    ims = []
    for b, dr, hf in cores:
        seq = np.concatenate([xc[b], xl[b]], axis=0) if dr == 0 else np.concatenate([xc[b][::-1], xl[b][::-1]], axis=0)
        ims.append(ssm_inputs(inp, seq, b, dr, hf))
    res = run_bass_kernel_spmd(build_ssm(), ims, core_ids=C8)
    Y = np.empty((2, 2, 8192, D), np.float32)
    V = np.empty((2, 2, 8192, 512), np.float32)
    for (b, dr, hf), r in zip(cores, res.results):
        y = r["yssd"][256:]
        v = r["ys5"][:, 256:].T
        if dr == 1:
            y, v = y[::-1], v[::-1]
        Y[b, dr, :, hf * 512:(hf + 1) * 512] = y
        V[b, dr, :, hf * 256:(hf + 1) * 256] = v
    cores = [(b, q) for b in range(2) for q in range(4)]
    ims = []
    for b, q in cores:
        sl = slice(q * 2048, (q + 1) * 2048)
        ims.append(fin_inputs(inp, b, xl[b, sl], Y[b, 0, sl], Y[b, 1, sl], V[b, 0, sl], V[b, 1, sl]))
    res = run_bass_kernel_spmd(build_fin(), ims, core_ids=C8)
    for (b, q), r in zip(cores, res.results):
        xl[b, q * 2048:(q + 1) * 2048] = r["xo"]
    xl, _ = moe(1, xl, None)
    return xl
```
